# Optimizing a Trainium2 kernel written in Bass

```python
import jax, jax.numpy as jnp
from jax import lax
import numpy as np

D_MODEL = 1024
BATCH = 32
SEQ = 2048
DEPTH = 2

N_MIXERS = 4
GROUP_W = D_MODEL // N_MIXERS
HEAD_DIM = 64
N_HEADS = GROUP_W // HEAD_DIM
NORM_EPS = 1e-6
RWKV_W_RANK = 32
RWKV_A_RANK = 32
RWKV_G_RANK = 64
RWKV_GN_EPS = 64e-5
SB_BLOCK = 128
ML_CHUNK = 64
ML_CONV = 4
GATE_CAP = 15.0
DSA_BLOCK = 128
IDX_HEADS = 4
IDX_DIM = 32
TOPK_MAX = 256
ROPE_THETA = 10000.0
N_GROUPS = 4
EXP_PER_GROUP = 8
N_EXPERTS = N_GROUPS * EXP_PER_GROUP
EXPERT_FF = D_MODEL // 2
TOP_IN_GROUP = 2
MOE_BLOCK = 128

A_SIZES = (GROUP_W, GROUP_W, GROUP_W, RWKV_W_RANK, RWKV_A_RANK, RWKV_G_RANK)
B_SIZES = (GROUP_W, GROUP_W, GROUP_W)
C_SIZES = (GROUP_W, GROUP_W, GROUP_W, GROUP_W, N_HEADS, N_HEADS)
D_SIZES = (GROUP_W, HEAD_DIM, HEAD_DIM, IDX_HEADS * IDX_DIM, IDX_DIM, IDX_HEADS)
A_COLS = sum(A_SIZES)
B_COLS = sum(B_SIZES)
C_COLS = sum(C_SIZES)
D_COLS = sum(D_SIZES)
P_TOTAL = A_COLS + B_COLS + C_COLS + D_COLS

kernel_name = "hymba_style_rwkv7_stickbreak_mlstm_dsa_hmoe"

F32 = jnp.float32


def split_cols(t, sizes):
    return jnp.split(t, [int(i) for i in np.cumsum(sizes)[:-1]], axis=-1)


def rms_norm(x, g):
    xf = x.astype(F32)
    y = xf * lax.rsqrt(jnp.mean(xf * xf, -1, keepdims=True) + NORM_EPS)
    return (y * g.astype(F32)).astype(x.dtype)


def rope(x, pos):
    half = x.shape[-1] // 2
    inv = ROPE_THETA ** (-jnp.arange(half, dtype=F32) / half)
    ang = pos.astype(F32)[:, None] * inv[None, :]
    cos = jnp.cos(ang)[None, :, None, :]
    sin = jnp.sin(ang)[None, :, None, :]
    xf = x.astype(F32)
    x1, x2 = xf[..., :half], xf[..., half:]
    return jnp.concatenate([x1 * cos - x2 * sin, x2 * cos + x1 * sin], -1).astype(x.dtype)


def token_shift(p, mu):
    prev = jnp.pad(p, ((0, 0), (1, 0), (0, 0)))[:, :-1]
    return p + (prev - p) * mu


def causal_dwconv(x, w, b):
    ch = x.shape[-1]
    y = lax.conv_general_dilated(x, w[:, None, :].astype(x.dtype), window_strides=(1,),
                                 padding=((w.shape[0] - 1, 0),),
                                 dimension_numbers=('NWC', 'WIO', 'NWC'), feature_group_count=ch)
    return y + b


def rwkv7_time_mix(p, mu, w0, w2, a0, a2, g2, k_k, k_a, r_k, ln_g, ln_b):
    B, S, _ = p.shape
    H, d = N_HEADS, HEAD_DIM
    p = token_shift(p, mu)
    r, k, v, wd, ad, gd = split_cols(p, A_SIZES)
    w = -jax.nn.softplus(-(w0 + jnp.tanh(wd) @ w2)) - 0.5
    decay = jnp.exp(-jnp.exp(w.astype(F32)))
    a = jax.nn.sigmoid(a0 + ad @ a2)
    g = jax.nn.sigmoid(gd) @ g2
    kk = (k * k_k).astype(F32).reshape(B, S, H, d)
    kk = kk / jnp.maximum(jnp.sqrt(jnp.sum(kk * kk, -1, keepdims=True)), 1e-12)
    k = k * (1 + (a - 1) * k_a)
    heads = lambda t: t.astype(F32).reshape(B, S, H, d)
    r_h, k_h, v_h, w_h, a_h = heads(r), heads(k), heads(v), heads(decay), heads(a)

    def step(state, inp):
        rt, wt, kt, vt, kkt, at = inp
        sa = jnp.einsum('bhvk,bhk->bhv', state, -kkt)
        state = (state * wt[:, :, None, :] + sa[..., None] * (kkt * at)[:, :, None, :]
                 + vt[..., None] * kt[:, :, None, :])
        return state, jnp.einsum('bhvk,bhk->bhv', state, rt)

    xs = tuple(jnp.moveaxis(t, 1, 0) for t in (r_h, w_h, k_h, v_h, kk, a_h))
    _, y = lax.scan(step, jnp.zeros((B, H, d, d), F32), xs)
    y = jnp.moveaxis(y, 0, 1)
    mean = jnp.mean(y, -1, keepdims=True)
    var = jnp.mean(jnp.square(y - mean), -1, keepdims=True)
    y = (y - mean) * lax.rsqrt(var + RWKV_GN_EPS) * ln_g.astype(F32).reshape(H, d) + ln_b.astype(F32).reshape(H, d)
    bonus = jnp.sum(r_h * k_h * r_k.astype(F32).reshape(H, d), -1, keepdims=True) * v_h
    y = (y + bonus).reshape(B, S, GROUP_W) * g.astype(F32)
    return y.astype(p.dtype)


def stick_breaking_attn(q, k, v):
    B, S, H, d = q.shape
    scale = d ** -0.5
    outs = []
    for i in range(S // SB_BLOCK):
        q0 = i * SB_BLOCK
        kl = q0 + SB_BLOCK
        z = jnp.einsum('bqhd,bkhd->bhqk', q[:, q0:kl], k[:, :kl]).astype(F32) * scale
        t_idx = q0 + jnp.arange(SB_BLOCK)[:, None]
        s_idx = jnp.arange(kl)[None, :]
        mask = s_idx < t_idx
        log1m = jnp.where(mask, jax.nn.log_sigmoid(-z), 0.0)
        cs = jnp.cumsum(log1m, -1)
        log_a = jax.nn.log_sigmoid(z) + cs[..., -1:] - cs
        att = jnp.where(mask, jnp.exp(log_a), 0.0)
        outs.append(jnp.einsum('bhqk,bkhd->bqhd', att.astype(v.dtype), v[:, :kl]))
    return jnp.concatenate(outs, 1)


def mlstm_chunkwise(q, k, v, log_i, log_f):
    B, S, H, d = q.shape
    L = ML_CHUNK
    nc = S // L

    def to_chunks(t):
        t = t.reshape((B, nc, L, H) + t.shape[3:])
        return jnp.moveaxis(jnp.moveaxis(t, 3, 2), 1, 0)

    causal = jnp.tril(jnp.ones((L, L), bool))

    def chunk(carry, inp):
        c_st, n_st, m_st = carry
        qc, kc, vc, li, lf = inp
        b = jnp.cumsum(lf, -1)
        dmat = jnp.where(causal, b[..., :, None] - b[..., None, :] + li[..., None, :], -jnp.inf)
        g_inter = b + m_st[..., None]
        m_t = jnp.maximum(g_inter, jnp.max(dmat, -1))
        s_inter = jnp.exp(g_inter - m_t)
        sqk = jnp.einsum('bhtd,bhsd->bhts', qc, kc) * jnp.exp(dmat - m_t[..., None])
        num = s_inter[..., None] * jnp.einsum('bhvd,bhtd->bhtv', c_st, qc) + jnp.einsum('bhts,bhsv->bhtv', sqk, vc)
        den = s_inter * jnp.einsum('bhd,bhtd->bht', n_st, qc) + jnp.sum(sqk, -1)
        h = num / jnp.maximum(jnp.abs(den), jnp.exp(-m_t))[..., None]
        b_last = b[..., -1]
        dec = b_last[..., None] - b + li
        m_new = jnp.maximum(b_last + m_st, jnp.max(dec, -1))
        wk = jnp.exp(dec - m_new[..., None])
        s_old = jnp.exp(b_last + m_st - m_new)
        c_st = s_old[..., None, None] * c_st + jnp.einsum('bhs,bhsv,bhsd->bhvd', wk, vc, kc)
        n_st = s_old[..., None] * n_st + jnp.einsum('bhs,bhsd->bhd', wk, kc)
        return (c_st, n_st, m_new), h

    init = (jnp.zeros((B, H, d, d), F32), jnp.zeros((B, H, d), F32), jnp.zeros((B, H), F32))
    _, hs = lax.scan(chunk, init, tuple(to_chunks(t) for t in (q, k, v, log_i, log_f)))
    hs = jnp.moveaxis(jnp.moveaxis(hs, 0, 1), 2, 3)
    return hs.reshape(B, S, H, d)


def mlstm_mix(p, conv_w, conv_b, ig_b, fg_b, norm_g):
    B, S, _ = p.shape
    H, d = N_HEADS, HEAD_DIM
    qk = jax.nn.silu(causal_dwconv(p[..., :2 * GROUP_W], conv_w, conv_b))
    q, k = qk[..., :GROUP_W], qk[..., GROUP_W:]
    _, _, v, o, ig, fg = split_cols(p, C_SIZES)
    cap = lambda t: GATE_CAP * jnp.tanh(t / GATE_CAP)
    log_i = cap((ig + ig_b).astype(F32))
    log_f = jax.nn.log_sigmoid(cap((fg + fg_b).astype(F32)))
    heads = lambda t: t.astype(F32).reshape(B, S, H, d)
    h = mlstm_chunkwise(heads(q), heads(k) * d ** -0.5, heads(v), log_i, log_f)
    h = rms_norm(h, norm_g.reshape(H, d)).reshape(B, S, GROUP_W)
    return (jax.nn.sigmoid(o.astype(F32)) * h).astype(p.dtype)


def dsa_attn(q, k, v, qi, ki, wi):
    B, S, H, d = q.shape
    n_sel = min(TOPK_MAX, S // 4)
    gather = jax.vmap(lambda t, ix: t[ix])
    outs = []
    for i in range(S // DSA_BLOCK):
        q0 = i * DSA_BLOCK
        q1 = q0 + DSA_BLOCK
        kl = min(S, max(q1, n_sel))
        adm = jnp.arange(kl)[None, :] <= (q0 + jnp.arange(DSA_BLOCK))[:, None]
        sc = jnp.einsum('bqhe,bke->bqhk', qi[:, q0:q1], ki[:, :kl]).astype(F32)
        idx_score = jnp.einsum('bqh,bqhk->bqk', wi[:, q0:q1].astype(F32), jax.nn.relu(sc))
        idx_score = jnp.where(adm, idx_score, -jnp.inf)
        vals, sel = lax.top_k(idx_score, n_sel)
        valid = jnp.isfinite(vals)
        ks = gather(k[:, :kl], sel)
        vs = gather(v[:, :kl], sel)
        logits = jnp.einsum('bqhd,bqnd->bhqn', q[:, q0:q1], ks).astype(F32) * d ** -0.5
        logits = jnp.where(valid[:, None], logits, -jnp.inf)
        prob = jax.nn.softmax(logits, -1)
        outs.append(jnp.einsum('bhqn,bqnd->bqhd', prob.astype(v.dtype), vs))
    return jnp.concatenate(outs, 1)


def hier_moe(h, wg, bg, we, be, w1, w3, w2):
    B, S, D = h.shape
    N = B * S
    hf = h.reshape(N, D)
    g_prob = jax.nn.softmax((hf @ wg).astype(F32) + bg, -1)
    g_p, g_idx = lax.top_k(g_prob, 1)
    e_logits = ((hf @ we).astype(F32) + be).reshape(N, N_GROUPS, EXP_PER_GROUP)
    e_logits = jnp.take_along_axis(e_logits, g_idx[:, :, None], axis=1)[:, 0]
    e_p, e_idx = lax.top_k(jax.nn.softmax(e_logits, -1), TOP_IN_GROUP)
    gate = g_p * e_p / jnp.sum(e_p, -1, keepdims=True)
    expert = g_idx * EXP_PER_GROUP + e_idx
    n_asg = N * TOP_IN_GROUP
    flat_e = expert.reshape(n_asg)
    flat_tok = jnp.repeat(jnp.arange(N, dtype=jnp.int32), TOP_IN_GROUP)
    flat_w = gate.reshape(n_asg)
    order = jnp.argsort(flat_e)
    se = flat_e[order]
    counts = jnp.bincount(flat_e, length=N_EXPERTS)
    start = jnp.cumsum(counts) - counts
    pad_counts = (counts + MOE_BLOCK - 1) // MOE_BLOCK * MOE_BLOCK
    pad_end = jnp.cumsum(pad_counts)
    pad_start = pad_end - pad_counts
    slot = pad_start[se] + jnp.arange(n_asg) - start[se]
    n_blocks = -(-n_asg // MOE_BLOCK) + N_EXPERTS
    n_slots = n_blocks * MOE_BLOCK
    slot_tok = jnp.full((n_slots,), N, jnp.int32).at[slot].set(flat_tok[order])
    slot_w = jnp.zeros((n_slots,), F32).at[slot].set(flat_w[order])
    blk_start = jnp.arange(n_blocks) * MOE_BLOCK
    blk_e = jnp.minimum(jnp.sum(pad_end[None, :] <= blk_start[:, None], 1), N_EXPERTS - 1)
    h_pad = jnp.concatenate([hf, jnp.zeros((1, D), hf.dtype)], 0)

    def run_block(args):
        tok, e, wt = args
        xb = h_pad[tok]
        y = (jax.nn.silu(xb @ w1[e]) * (xb @ w3[e])) @ w2[e]
        return y * wt[:, None].astype(y.dtype)

    yb = lax.map(run_block, (slot_tok.reshape(n_blocks, MOE_BLOCK), blk_e,
                             slot_w.reshape(n_blocks, MOE_BLOCK)))
    out = jnp.zeros((N + 1, D), yb.dtype).at[slot_tok].add(yb.reshape(n_slots, D))[:N]
    return out.reshape(B, S, D)


def setup_inputs(seed: int = 0) -> dict:
    key = jax.random.key(seed)
    ks = iter(jax.random.split(key, 64))
    nrm = lambda shape, s: jax.random.normal(next(ks), shape, F32) * s
    L = DEPTH
    D = D_MODEL
    return {
        "x": nrm((BATCH, SEQ, D), 1.0),
        "c": nrm((BATCH, D), 1.0),
        "ada_w": nrm((L, D, 6 * D), 0.3 * D ** -0.5),
        "ada_b": nrm((L, 6 * D), 0.02),
        "norm1_g": 1.0 + nrm((L, D), 0.1),
        "norm2_g": 1.0 + nrm((L, D), 0.1),
        "w_in": nrm((L, D, P_TOTAL), D ** -0.5),
        "rk_mu": jax.random.uniform(next(ks), (L, A_COLS), F32),
        "rk_w0": nrm((L, GROUP_W), 0.5),
        "rk_w2": nrm((L, RWKV_W_RANK, GROUP_W), 0.5 * RWKV_W_RANK ** -0.5),
        "rk_a0": nrm((L, GROUP_W), 0.5),
        "rk_a2": nrm((L, RWKV_A_RANK, GROUP_W), 0.5 * RWKV_A_RANK ** -0.5),
        "rk_g2": nrm((L, RWKV_G_RANK, GROUP_W), RWKV_G_RANK ** -0.5),
        "rk_kk": 0.85 + nrm((L, GROUP_W), 0.1),
        "rk_ka": 1.0 + nrm((L, GROUP_W), 0.1),
        "rk_rk": nrm((L, GROUP_W), 0.1),
        "rk_ln_g": 1.0 + nrm((L, GROUP_W), 0.1),
        "rk_ln_b": nrm((L, GROUP_W), 0.02),
        "sb_norm_g": 1.0 + nrm((L, GROUP_W), 0.1),
        "ml_conv_w": nrm((L, ML_CONV, 2 * GROUP_W), ML_CONV ** -0.5),
        "ml_conv_b": nrm((L, 2 * GROUP_W), 0.02),
        "ml_ig_b": -1.0 + nrm((L, N_HEADS), 0.5),
        "ml_fg_b": 3.0 + nrm((L, N_HEADS), 0.5),
        "ml_norm_g": 1.0 + nrm((L, GROUP_W), 0.1),
        "ds_qn_g": 1.0 + nrm((L, HEAD_DIM), 0.1),
        "ds_kn_g": 1.0 + nrm((L, HEAD_DIM), 0.1),
        "ds_out_g": 1.0 + nrm((L, GROUP_W), 0.1),
        "w_out": nrm((L, N_MIXERS * GROUP_W, D), (N_MIXERS * GROUP_W) ** -0.5),
        "moe_wg": nrm((L, D, N_GROUPS), D ** -0.5),
        "moe_bg": nrm((L, N_GROUPS), 0.01),
        "moe_we": nrm((L, D, N_EXPERTS), D ** -0.5),
        "moe_be": nrm((L, N_EXPERTS), 0.01),
        "moe_w1": nrm((L, N_EXPERTS, D, EXPERT_FF), D ** -0.5),
        "moe_w3": nrm((L, N_EXPERTS, D, EXPERT_FF), D ** -0.5),
        "moe_w2": nrm((L, N_EXPERTS, EXPERT_FF, D), EXPERT_FF ** -0.5),
    }


def reference(x, c, ada_w, ada_b, norm1_g, norm2_g, w_in, rk_mu, rk_w0, rk_w2, rk_a0, rk_a2, rk_g2,
              rk_kk, rk_ka, rk_rk, rk_ln_g, rk_ln_b, sb_norm_g, ml_conv_w, ml_conv_b, ml_ig_b, ml_fg_b,
              ml_norm_g, ds_qn_g, ds_kn_g, ds_out_g, w_out, moe_wg, moe_bg, moe_we, moe_be,
              moe_w1, moe_w3, moe_w2):
    B, S, _ = x.shape
    H, d = N_HEADS, HEAD_DIM
    pos = jnp.arange(S)
    c_act = jax.nn.silu(c)
    for l in range(DEPTH):
        mod = (c_act @ ada_w[l] + ada_b[l])[:, None, :]
        sh1, sc1, gt1, sh2, sc2, gt2 = jnp.split(mod, 6, axis=-1)

        h = rms_norm(x, norm1_g[l]) * (1 + sc1) + sh1
        p = h @ w_in[l]
        pA, pB, pC, pD = split_cols(p, (A_COLS, B_COLS, C_COLS, D_COLS))

        yA = rwkv7_time_mix(pA, rk_mu[l], rk_w0[l], rk_w2[l], rk_a0[l], rk_a2[l], rk_g2[l],
                            rk_kk[l], rk_ka[l], rk_rk[l], rk_ln_g[l], rk_ln_b[l])

        qb, kb, vb = (t.reshape(B, S, H, d) for t in split_cols(pB, B_SIZES))
        yB = rms_norm(stick_breaking_attn(qb, kb, vb), sb_norm_g[l].reshape(H, d)).reshape(B, S, GROUP_W)

        yC = mlstm_mix(pC, ml_conv_w[l], ml_conv_b[l], ml_ig_b[l], ml_fg_b[l], ml_norm_g[l])

        qd, kd, vd, qi, ki, wi = split_cols(pD, D_SIZES)
        qd = rope(rms_norm(qd.reshape(B, S, H, d), ds_qn_g[l]), pos)
        kd = rope(rms_norm(kd[:, :, None, :], ds_kn_g[l]), pos)[:, :, 0]
        qi = rope(qi.reshape(B, S, IDX_HEADS, IDX_DIM), pos)
        ki = rope(ki[:, :, None, :], pos)[:, :, 0]
        wi = wi * (IDX_HEADS ** -0.5 * IDX_DIM ** -0.5)
        yD = rms_norm(dsa_attn(qd, kd, vd, qi, ki, wi), ds_out_g[l].reshape(H, d)).reshape(B, S, GROUP_W)

        y = jnp.concatenate([yA, yB, yC, yD], -1) @ w_out[l]
        x = x + gt1 * y

        h = rms_norm(x, norm2_g[l]) * (1 + sc2) + sh2
        x = x + gt2 * hier_moe(h, moe_wg[l], moe_bg[l], moe_we[l], moe_be[l], moe_w1[l], moe_w3[l], moe_w2[l])
    return x
```

```python
import numpy as np
import concourse.bass as bass
import concourse.mybir as mybir
from concourse.bass_utils import run_bass_kernel_spmd

F32 = mybir.dt.float32
BF16 = mybir.dt.bfloat16
AF = mybir.ActivationFunctionType
ALU = mybir.AluOpType
AX = mybir.AxisListType

COMPUTE = ("pe", "act", "dve", "pool")


class Sched:
    def __init__(self, nc, n_dma_sems=32):
        self.nc = nc
        self.n_dma = n_dma_sems
        self.sem = {}
        self.stack = None
        self.ops = []
        self.last_w = {}
        self.readers = {}
        self.count = {e: 0 for e in COMPUTE}
        self.dma_cnt = [0] * n_dma_sems
        self.dma_rr = 0
        self.known = {e: {} for e in COMPUTE + ("sp",)}
        self.phase_start = 0
        self.n_inst = 0

    def open(self, stack):
        nc = self.nc
        self.stack = stack
        self.n_sem_alloc = 0
        for e in COMPUTE:
            self.sem[e] = stack.enter_context(nc.semaphore("s_" + e))
        for i in range(self.n_dma):
            self.sem[("d", i)] = stack.enter_context(nc.semaphore("s_d%d" % i))

    @staticmethod
    def key(ap):
        return ap.name

    def add(self, eng, fn, reads, writes, dma=False):
        idx = len(self.ops)
        deps = set()
        wdeps = set()
        for k in reads:
            if k in self.last_w:
                deps.add(self.last_w[k])
        for k in writes:
            if k in self.last_w:
                deps.add(self.last_w[k])
            rd = self.readers.get(k)
            if rd:
                wdeps.update(rd.values())
        for k in writes:
            self.last_w[k] = idx
            self.readers[k] = {}
        for k in reads:
            self.readers.setdefault(k, {})[("dma", idx) if dma else eng] = idx
        deps.discard(idx)
        wdeps.discard(idx)
        wdeps -= deps
        self.ops.append(dict(eng=eng, fn=fn, deps=deps, wdeps=wdeps, dma=dma, sig=False, waits=None))
        return idx

    def flush(self, barrier=True):
        ops = self.ops
        lo = self.phase_start
        n = len(ops)
        def skip(o, od, d):
            if od["dma"] or o["dma"] or od["eng"] != o["eng"]:
                return False
            if o["eng"] == "pe":
                return True
            return d not in o["deps"]

        for i in range(lo, n):
            o = ops[i]
            for d in (o["deps"] | o["wdeps"]):
                od = ops[d]
                if d < lo or od["dma"] or skip(o, od, d):
                    continue
                od["sig"] = True
        last_of = {}
        for i in range(lo, n):
            last_of[ops[i]["eng"]] = i
        if barrier:
            for e, i in last_of.items():
                if e in COMPUTE:
                    ops[i]["sig"] = True
        for i in range(lo, n):
            o = ops[i]
            e = o["eng"]
            kn = self.known[e]
            waits = []
            for d in sorted(o["deps"] | o["wdeps"]):
                od = ops[d]
                if d < lo or skip(o, od, d):
                    continue
                s, v = od["done"]
                if kn.get(s, 0) < v:
                    waits.append((s, v))
                    for s2, v2 in od["clock"].items():
                        if kn.get(s2, 0) < v2:
                            kn[s2] = v2
            if o["dma"]:
                j = self.dma_rr
                self.dma_rr = (j + 1) % self.n_dma
                s = ("d", j)
                if kn.get(s, 0) < self.dma_cnt[j]:
                    waits.append((s, self.dma_cnt[j]))
                    kn[s] = self.dma_cnt[j]
                self.dma_cnt[j] += 16
                o["done"] = (s, self.dma_cnt[j])
                o["inc"] = (s, 16)
                clock = dict(kn)
                clock[s] = self.dma_cnt[j]
                o["clock"] = clock
            else:
                if o["sig"]:
                    self.count[e] += 1
                    o["done"] = (e, self.count[e])
                    o["inc"] = (e, 1)
                    clock = dict(kn)
                    clock[e] = self.count[e]
                    o["clock"] = clock
                else:
                    o["inc"] = None
            w = {}
            for s, v in waits:
                w[s] = max(w.get(s, 0), v)
            o["waits"] = list(w.items())
        final = {}
        if barrier:
            for e in COMPUTE:
                final[e] = self.count[e]
            for j in range(self.n_dma):
                final[("d", j)] = self.dma_cnt[j]
        nc = self.nc
        sem = self.sem
        by_eng = {}
        for i in range(lo, n):
            by_eng.setdefault(ops[i]["eng"], []).append(ops[i])

        def emit(ename):
            def body(e):
                for o in by_eng.get(ename, []):
                    for s, v in o["waits"]:
                        e.wait_ge(sem[s], v)
                        self.n_inst += 1
                    ins = o["fn"](e)
                    self.n_inst += 1
                    if o["inc"] is not None:
                        ins.then_inc(sem[o["inc"][0]], o["inc"][1])
                kn = self.known[ename]
                for s, v in final.items():
                    if kn.get(s, 0) < v:
                        e.wait_ge(sem[s], v)
                        kn[s] = v
            return body

        with nc.Block() as block:
            block.tensor(emit("pe"))
            block.scalar(emit("act"))
            block.vector(emit("dve"))
            block.gpsimd(emit("pool"))
            block.sync(emit("sp"))
        for i in range(lo, n):
            ops[i]["fn"] = None
            ops[i]["clock"] = None if i < n else None
        self.phase_start = n
        if barrier:
            self.last_w = {}
            self.readers = {}
            for e in COMPUTE:
                if self.count[e] > 20000:
                    self.n_sem_alloc += 1
                    self.sem[e] = self.stack.enter_context(nc.semaphore("s_%s_%d" % (e, self.n_sem_alloc)))
                    self.count[e] = 0
                    for kn in self.known.values():
                        kn.pop(e, None)
            for j in range(self.n_dma):
                if self.dma_cnt[j] > 20000:
                    self.n_sem_alloc += 1
                    self.sem[("d", j)] = self.stack.enter_context(nc.semaphore("s_d%d_%d" % (j, self.n_sem_alloc)))
                    self.dma_cnt[j] = 0
                    for kn in self.known.values():
                        kn.pop(("d", j), None)

    def _rw(self, outs, ins, rk, wk):
        r = list(rk) if rk is not None else [self.key(a) for a in ins if hasattr(a, "name") and a.space != "DRAM"]
        w = list(wk) if wk is not None else [self.key(a) for a in outs if a.space != "DRAM"]
        return r, w

    def mm(self, out, lhsT, rhs, start=True, stop=True, rk=None, wk=None):
        r, w = self._rw([out], [lhsT, rhs], rk, wk)
        return self.add("pe", lambda e: e.matmul(out, lhsT=lhsT, rhs=rhs, start=start, stop=stop), r, w)

    def transpose(self, out, in_, ident, rk=None, wk=None):
        r, w = self._rw([out], [in_, ident], rk, wk)
        return self.add("pe", lambda e: e.transpose(out, in_, ident), r, w)

    def act(self, out, in_, func, bias=None, scale=1.0, accum_out=None, rk=None, wk=None):
        ins = [in_] + ([bias] if hasattr(bias, "name") else []) + ([scale] if hasattr(scale, "name") else [])
        outs = [out] + ([accum_out] if accum_out is not None else [])
        r, w = self._rw(outs, ins, rk, wk)
        kw = {}
        if bias is not None:
            kw["bias"] = bias
        if accum_out is not None:
            kw["accum_out"] = accum_out
        return self.add("act", lambda e: e.activation(out, in_, func, scale=scale, **kw), r, w)

    def tt(self, out, in0, in1, op, eng="dve", rk=None, wk=None):
        r, w = self._rw([out], [in0, in1], rk, wk)
        return self.add(eng, lambda e: e.tensor_tensor(out, in0, in1, op), r, w)

    def ts(self, out, in0, s1, s2=None, op0=ALU.mult, op1=None, eng="dve", accum_out=None, rk=None, wk=None):
        ins = [in0] + [s for s in (s1, s2) if hasattr(s, "name")]
        outs = [out] + ([accum_out] if accum_out is not None else [])
        r, w = self._rw(outs, ins, rk, wk)
        kw = {}
        if op1 is not None:
            kw["op1"] = op1
        if accum_out is not None:
            kw["accum_out"] = accum_out
        return self.add(eng, lambda e: e.tensor_scalar(out, in0, s1, s2, op0, **kw), r, w)

    def stt(self, out, in0, scalar, in1, op0, op1, eng="dve", rk=None, wk=None):
        ins = [in0, in1] + ([scalar] if hasattr(scalar, "name") else [])
        r, w = self._rw([out], ins, rk, wk)
        return self.add(eng, lambda e: e.scalar_tensor_tensor(out, in0, scalar, in1, op0, op1), r, w)

    def copy(self, out, in_, eng="dve", rk=None, wk=None):
        r, w = self._rw([out], [in_], rk, wk)
        if eng == "act":
            return self.add("act", lambda e: e.activation(out, in_, AF.Copy), r, w)
        return self.add(eng, lambda e: e.tensor_copy(out, in_), r, w)

    def memset(self, out, val, eng="dve", wk=None):
        r, w = self._rw([out], [], None, wk)
        return self.add(eng, lambda e: e.memset(out, val), r, w)

    def reduce(self, out, in_, op, axis=AX.X, eng="dve", rk=None, wk=None):
        r, w = self._rw([out], [in_], rk, wk)
        return self.add(eng, lambda e: e.tensor_reduce(out, in_, axis, op), r, w)

    def recip(self, out, in_, rk=None, wk=None):
        r, w = self._rw([out], [in_], rk, wk)
        return self.add("dve", lambda e: e.reciprocal(out, in_), r, w)

    def scan(self, out, d0, d1, init, op0, op1, eng="dve", rk=None, wk=None):
        r, w = self._rw([out], [d0, d1], rk, wk)
        return self.add(eng, lambda e: e.tensor_tensor_scan(out, d0, d1, init, op0, op1), r, w)

    def max8(self, out, in_, rk=None, wk=None):
        r, w = self._rw([out], [in_], rk, wk)
        return self.add("dve", lambda e: e.max(out, in_), r, w)

    def match_replace(self, out, rep, vals, imm, rk=None, wk=None):
        r, w = self._rw([out], [rep, vals], rk, wk)
        return self.add("dve", lambda e: e.match_replace(out, rep, vals, imm), r, w)

    def dma(self, out, in_, rk=None, wk=None, **kw):
        r, w = self._rw([out], [in_], rk, wk)
        return self.add("sp", lambda e: e.dma_start(out, in_, **kw), r, w, dma=True)


from contextlib import ExitStack

P = 128
T = 2048
D = 1024
NSEQ = 4
NTM = 2084
NFM = 1416
TM_AR, TM_AK, TM_AV, TM_BV, TM_CV, TM_CO, TM_DQ, TM_DK, TM_DV, TM_DQI, TM_DKI, TM_DWI = (
    0, 256, 512, 768, 1024, 1280, 1536, 1792, 1856, 1920, 2048, 2080)
FM_AV, FM_AWD, FM_AAD, FM_AGD, FM_BQ, FM_BK, FM_CQ, FM_CK, FM_CIG, FM_CFG = (
    0, 256, 288, 320, 384, 640, 896, 1152, 1408, 1412)
NEG = -1.0e30
_uid = [0]


def uname(s):
    _uid[0] += 1
    return "%s_%d" % (s, _uid[0])


def sb(nc, st, name, shape, dt):
    return st.enter_context(nc.sbuf_tensor(uname(name), list(shape), dt))


def bc(ap, shape):
    return ap.to_broadcast(list(shape))


class G:
    pass


def host_consts():
    c = {}
    i = np.arange(128)
    c["ident"] = np.eye(128, dtype=np.float32)
    c["ones"] = np.ones((128, 128), np.float32)
    c["tri_lt"] = (i[:, None] < i[None, :]).astype(np.float32)
    c["tri_le"] = (i[:, None] <= i[None, :]).astype(np.float32)
    c["tri_gt"] = (i[:, None] > i[None, :]).astype(np.float32)
    c["cbias"] = np.where(i[None, :] <= i[:, None], 0.0, NEG).astype(np.float32)
    half = 32
    inv = 10000.0 ** (-np.arange(half, dtype=np.float32) / half)
    ang = np.arange(T, dtype=np.float32)[:, None] * inv[None, :]
    c["rope64"] = np.concatenate([np.cos(ang), np.sin(ang)], 1).astype(np.float32)
    half = 16
    inv = 10000.0 ** (-np.arange(half, dtype=np.float32) / half)
    ang = np.arange(T, dtype=np.float32)[:, None] * inv[None, :]
    c["rope32"] = np.concatenate([np.cos(ang), np.sin(ang)], 1).astype(np.float32)
    c["lebias"] = np.where(i[:, None] <= i[None, :], 0.0, NEG).astype(np.float32)
    selh = np.zeros((4, 4, 128), np.float32)
    for h in range(4):
        selh[h, h, :] = 1.0
    c["selh"] = selh.reshape(4, 512)
    return c


def load_const(S, nc, st, g, name, shape, dt=F32):
    t = sb(nc, st, "c_" + name, shape, F32)
    S.dma(t[:], g.dram[name])
    if dt == F32:
        return t
    tb = sb(nc, st, "cb_" + name, shape, dt)
    S.copy(tb[:], t[:])
    return tb


def ph_ada(S, nc, g, l):
    with ExitStack() as st:
        cT = sb(nc, st, "cT", [P, 8, 4], F32)
        S.dma(cT[:], g.dram["cT"].rearrange("(c p) b -> p c b", p=P))
        cact = sb(nc, st, "cact", [P, 8, 4], F32)
        S.act(cact[:], cT[:], AF.Silu)
        bias = sb(nc, st, "adab", [P, 48], F32)
        S.dma(bias[:], g.dram["ada_b_fm"][l])
        g1 = sb(nc, st, "g1", [P, 8], F32)
        g2 = sb(nc, st, "g2", [P, 8], F32)
        S.dma(g1[:], g.dram["norm1_g_fm"][l])
        S.dma(g2[:], g.dram["norm2_g_fm"][l])
        wts = [sb(nc, st, "adaw%d" % i, [P, 8, 768], F32) for i in range(2)]
        ps = g.ps[0]
        for cb in range(8):
            wt = wts[cb % 2]
            S.dma(wt[:], g.dram["ada_w"][l][:, cb * 768:(cb + 1) * 768].rearrange("(c p) n -> p c n", p=P))
            for cc in range(6):
                c = cb * 6 + cc
                for k in range(8):
                    S.mm(ps[:, 4 * c:4 * c + 4], wt[:, k, cc * 128:(cc + 1) * 128], cact[:, k, :],
                         start=(k == 0), stop=(k == 7))
        S.tt(g.modT[:], ps[:, 0:192].rearrange("p (c b) -> p c b", b=4),
             bc(bias[:].unsqueeze(2), [P, 48, 4]), ALU.add)
        for (A, gg, off) in ((g.A1, g1, 8), (g.A2, g2, 32)):
            S.ts(A[:], g.modT[:, off:off + 8, :], 1.0, None, ALU.add)
            S.tt(A[:], A[:], bc(gg[:].unsqueeze(2), [P, 8, 4]), ALU.mult)
        S.flush()


def emit_norm_mod(S, nc, g, xT_d, tok0, b, A, shift, hT, col0=1, route=None):
    with ExitStack() as st:
        xs = [sb(nc, st, "xs%d" % i, [P, 8, 512], F32) for i in range(2)]
        sq = sb(nc, st, "sq", [P, 8, 512], F32)
        tmp = sb(nc, st, "tmp", [P, 8, 512], F32)
        rstd = sb(nc, st, "rstd", [P, 512], F32)
        ps = g.ps[1]
        for sblk in range(4):
            x = xs[sblk % 2]
            S.dma(x[:], xT_d[:, tok0 + sblk * 512: tok0 + (sblk + 1) * 512].rearrange("(c p) n -> p c n", p=P))
            S.act(sq[:], x[:], AF.Square)
            for c in range(8):
                S.mm(ps[:, :], g.ones[:], sq[:, c, :], start=(c == 0), stop=(c == 7))
            S.act(rstd[:], ps[:, :], AF.Sqrt, scale=1.0 / D, bias=g.eps6[:, 0:1])
            S.recip(rstd[:], rstd[:])
            S.tt(tmp[:], x[:], bc(rstd[:].unsqueeze(1), [P, 8, 512]), ALU.mult)
            for c in range(8):
                S.act(hT[:, c, col0 + sblk * 512: col0 + (sblk + 1) * 512], tmp[:, c, :], AF.Identity,
                      scale=A[:, c, b:b + 1], bias=shift[:, c, b:b + 1])
            if route is not None:
                wge, lg = route
                for c in range(8):
                    S.act(sq[:, c, :], tmp[:, c, :], AF.Identity, scale=A[:, c, b:b + 1], bias=shift[:, c, b:b + 1])
                for tb4 in range(4):
                    pr = g.ps[2 + (tb4 % 2)]
                    for c in range(8):
                        S.mm(pr[:, 0:36], sq[:, c, tb4 * 128:(tb4 + 1) * 128], wge[:, c, :], start=(c == 0), stop=(c == 7))
                    S.copy(lg[:, sblk * 4 + tb4, :], pr[:, 0:36])
        S.flush()


def load_weights_bf16(S, nc, st, g, w_d, ncols, nshift, mu_d, Wb, W0b):
    stg = [sb(nc, st, "wstg%d" % i, [P, 8, 512], F32) for i in range(2)]
    mu_b = sb(nc, st, "mu_b", [P, max(nshift, 1)], F32)
    tmp = sb(nc, st, "wtmp", [P, 8, 512], F32)
    if nshift:
        S.dma(mu_b[:], mu_d.partition_broadcast(P))
    i = 0
    for c0 in range(0, ncols, 512):
        w = min(512, ncols - c0)
        s_ = stg[i % 2]
        i += 1
        S.dma(s_[:, :, :w], w_d[:, c0:c0 + w].rearrange("(c p) n -> p c n", p=P))
        if c0 < nshift:
            ws = min(w, nshift - c0)
            S.tt(tmp[:, :, :ws], s_[:, :, :ws], bc(mu_b[:, c0:c0 + ws].unsqueeze(1), [P, 8, ws]), ALU.mult, eng="pool")
            S.copy(W0b[:, :, c0:c0 + ws], tmp[:, :, :ws], eng="act")
            S.tt(Wb[:, :, c0:c0 + ws], s_[:, :, :ws], tmp[:, :, :ws], ALU.subtract)
            if ws < w:
                S.copy(Wb[:, :, c0 + ws:c0 + w], s_[:, :, ws:w], eng="act")
        else:
            S.copy(Wb[:, :, c0:c0 + w], s_[:, :, :w], eng=("act" if (i % 2) else "dve"))


def ph_inproj(S, nc, g, l, hT, ptm_d, pfm_d):
    with ExitStack() as st:
        Wb = sb(nc, st, "Wb", [P, 8, NTM], BF16)
        W0b = sb(nc, st, "W0b", [P, 8, 768], BF16)
        load_weights_bf16(S, nc, st, g, g.dram["w_tm"][l], NTM, 768, g.dram["mu_tm"][l], Wb, W0b)
        stg = [sb(nc, st, "ptm_stg%d" % i, [P, NTM], F32) for i in range(2)]
        ev = 0
        for tb in range(16):
            so = stg[tb % 2]
            for c0 in range(0, NTM, 512):
                w = min(512, NTM - c0)
                ps = g.ps[2 + (ev % 4)]
                shifted = c0 < 768
                for k in range(8):
                    S.mm(ps[:, :w], hT[:, k, 1 + tb * 128: 1 + (tb + 1) * 128], Wb[:, k, c0:c0 + w],
                         start=(k == 0), stop=(k == 7 and not shifted))
                if shifted:
                    ws = min(w, 768 - c0)
                    for k in range(8):
                        S.mm(ps[:, :ws], hT[:, k, tb * 128:(tb + 1) * 128], W0b[:, k, c0:c0 + ws],
                             start=False, stop=(k == 7))
                S.copy(so[:, c0:c0 + w], ps[:, :w], eng=("act" if ev % 2 else "dve"))
                ev += 1
            S.dma(ptm_d[tb * 128:(tb + 1) * 128, :], so[:])
        S.flush()
    with ExitStack() as st:
        Wb = sb(nc, st, "Wf", [P, 8, NFM], BF16)
        W0b = sb(nc, st, "W0f", [P, 8, 384], BF16)
        load_weights_bf16(S, nc, st, g, g.dram["w_fm"][l], NFM, 384, g.dram["mu_fm"][l], Wb, W0b)
        stg = [sb(nc, st, "pfm_stg%d" % i, [P, T], F32) for i in range(2)]
        ev = 0
        ci = 0
        for r0 in range(0, NFM, 128):
            m = min(128, NFM - r0)
            so = stg[ci % 2]
            ci += 1
            shifted = r0 < 384
            for sblk in range(4):
                ps = g.ps[2 + (ev % 4)]
                for k in range(8):
                    S.mm(ps[:m, :], Wb[:, k, r0:r0 + m], hT[:, k, 1 + sblk * 512: 1 + (sblk + 1) * 512],
                         start=(k == 0), stop=(k == 7 and not shifted))
                if shifted:
                    for k in range(8):
                        S.mm(ps[:m, :], W0b[:, k, r0:r0 + m], hT[:, k, sblk * 512:(sblk + 1) * 512],
                             start=False, stop=(k == 7))
                S.copy(so[:m, sblk * 512:(sblk + 1) * 512], ps[:m, :], eng=("act" if ev % 2 else "dve"))
                ev += 1
            S.dma(pfm_d[r0:r0 + m, :], so[:m, :])
        S.flush()


def _r(a, b):
    return list(range(a, b))


TM_COLS = (_r(0, 256) + _r(256, 512) + _r(512, 768) + _r(1408, 1664) + _r(2176, 2432) + _r(2432, 2688)
           + _r(2696, 2952) + _r(2952, 3016) + _r(3016, 3080) + _r(3080, 3208) + _r(3208, 3240) + _r(3240, 3244))
FM_COLS = (_r(512, 768) + _r(768, 800) + _r(800, 832) + _r(832, 896) + _r(896, 1152) + _r(1152, 1408)
           + _r(1664, 1920) + _r(1920, 2176) + _r(2688, 2692) + _r(2692, 2696))
assert len(TM_COLS) == NTM and len(FM_COLS) == NFM


def host_shared(inp):
    f = lambda a: np.ascontiguousarray(a, dtype=np.float32)
    L = inp["w_in"].shape[0]
    sh = dict(host_consts())
    sh["ada_w"] = f(inp["ada_w"])
    sh["ada_b_fm"] = f(inp["ada_b"].reshape(L, 48, 128).transpose(0, 2, 1))
    sh["norm1_g_fm"] = f(inp["norm1_g"].reshape(L, 8, 128).transpose(0, 2, 1))
    sh["norm2_g_fm"] = f(inp["norm2_g"].reshape(L, 8, 128).transpose(0, 2, 1))
    sh["w_tm"] = f(inp["w_in"][:, :, TM_COLS])
    sh["w_fm"] = f(inp["w_in"][:, :, FM_COLS])
    sh["mu_tm"] = f(inp["rk_mu"][:, TM_COLS[:768]])
    sh["mu_fm"] = f(inp["rk_mu"][:, FM_COLS[:384]])
    sh["conv_w_fm"] = f(inp["ml_conv_w"].reshape(L, 4, 4, 128).transpose(0, 3, 2, 1))
    sh["conv_b_fm"] = f(inp["ml_conv_b"].reshape(L, 4, 128).transpose(0, 2, 1))
    sh["moe_wge"] = f(np.concatenate([inp["moe_wg"], inp["moe_we"]], -1))
    sh["moe_bge"] = f(np.concatenate([inp["moe_bg"], inp["moe_be"]], -1))
    for k in ("moe_w1", "moe_w3", "moe_w2"):
        sh[k] = f(inp[k])
    for k in ("rk_w0", "rk_w2", "rk_a0", "rk_a2", "rk_g2", "rk_kk", "rk_ka", "rk_rk", "rk_ln_g", "rk_ln_b",
              "sb_norm_g", "ml_norm_g", "ds_qn_g", "ds_kn_g", "ds_out_g", "w_out", "ml_ig_b", "ml_fg_b"):
        sh[k] = f(inp[k])
    return sh


def host_core(inp, core, nseq=NSEQ):
    f = lambda a: np.ascontiguousarray(a, dtype=np.float32)
    x = inp["x"][core * nseq:(core + 1) * nseq]
    d = {}
    d["xT"] = f(x.reshape(nseq * T, D).T)
    cT = np.zeros((D, 4), np.float32)
    cT[:, :nseq] = inp["c"][core * nseq:(core + 1) * nseq].T
    d["cT"] = cT
    return d


def load_row_bcast(S, nc, st, name, row_ap, n):
    t = sb(nc, st, name, [P, n], F32)
    S.dma(t[:], row_ap.partition_broadcast(P))
    return t


def tm_head_rmsnorm(S, nc, st, g, y, nb, gain, eps, per_head_gain=True):
    nh = nb * 4
    yv = y[:].rearrange("p b (h d) -> p (b h) d", d=64)
    sq = sb(nc, st, "rn_sq", [P, nh, 64], F32)
    ss = sb(nc, st, "rn_ss", [P, nh], F32)
    S.tt(sq[:], yv, yv, ALU.mult)
    S.reduce(ss[:], sq[:], ALU.add, AX.X)
    S.act(ss[:], ss[:], AF.Sqrt, scale=1.0 / 64, bias=eps[:, 0:1])
    S.recip(ss[:], ss[:])
    S.tt(yv, yv, bc(ss[:].unsqueeze(2), [P, nh, 64]), ALU.mult)
    if per_head_gain:
        S.tt(y[:], y[:], bc(gain[:].unsqueeze(1), [P, nb, 256]), ALU.mult)
    else:
        S.tt(yv, yv, bc(gain[:, 0:64].unsqueeze(1), [P, nh, 64]), ALU.mult)


def ph_sb(S, nc, g, l, ptm_d, pfm_d, ycat_d):
    with ExitStack() as st:
        q16 = sb(nc, st, "sbq", [P, 2, T], BF16)
        k16 = sb(nc, st, "sbk", [P, 2, T], BF16)
        v16 = sb(nc, st, "sbv", [P, 16, 256], BF16)
        yraw = sb(nc, st, "sby", [P, 16, 256], F32)
        gain = load_row_bcast(S, nc, st, "sbg", g.dram["sb_norm_g"][l], 256)
        with ExitStack() as st2:
            qf = sb(nc, st2, "sbqf", [P, 2, T], F32)
            kf = sb(nc, st2, "sbkf", [P, 2, T], F32)
            vf = sb(nc, st2, "sbvf", [P, 16, 256], F32)
            S.dma(qf[:], pfm_d[FM_BQ:FM_BQ + 256, :].rearrange("(c p) t -> p c t", p=P))
            S.dma(kf[:], pfm_d[FM_BK:FM_BK + 256, :].rearrange("(c p) t -> p c t", p=P))
            S.dma(vf[:], ptm_d[:, TM_BV:TM_BV + 256].rearrange("(b p) n -> p b n", p=P))
            S.copy(q16[:], qf[:], eng="act")
            S.copy(k16[:], kf[:], eng="dve")
            S.copy(v16[:], vf[:], eng="pool")
            S.flush()
        e1 = [sb(nc, st, "sbe%d" % i, [P, 512], F32) for i in range(2)]
        lt = [sb(nc, st, "sbl%d" % i, [P, 512], F32) for i in range(2)]
        Lm = [sb(nc, st, "sbL%d" % i, [P, 512], F32) for i in range(2)]
        aa = [sb(nc, st, "sba%d" % i, [P, 512], F32) for i in range(2)]
        attA = [sb(nc, st, "sbt%d" % i, [P, 16, 512], BF16) for i in range(2)]
        TotB = sb(nc, st, "sbT", [P, 512], F32)
        it = 0
        for h in range(4):
            c = h // 2
            pb = (h % 2) * 64
            for I in range(4):
                po = g.ps[6 + (I % 2)]
                att_all = attA[(h * 4 + I) % 2]
                S.memset(TotB[:], 0.0, eng="pool")
                for j in range(4 * I + 3, -1, -1):
                    u = it % 2
                    it += 1
                    pz, pr, pt = g.ps[u], g.ps[2 + u], g.ps[4 + u]
                    d = j - 4 * I
                    dd = max(d, 0)
                    c0 = dd * 128
                    n = 512 - c0
                    q0 = I * 512 + c0
                    S.mm(pz[:, :n], k16[pb:pb + 64, c, j * 128:(j + 1) * 128], q16[pb:pb + 64, c, q0:q0 + n])
                    S.act(e1[u][:, :n], pz[:, :n], AF.Exp, scale=-0.125)
                    S.act(lt[u][:, :n], e1[u][:, :n], AF.Ln, bias=g.one1[:, 0:1])
                    S.stt(Lm[u][:, :n], pz[:, :n], -0.125, lt[u][:, :n], ALU.mult, ALU.subtract)
                    if d >= 0:
                        S.tt(Lm[u][:, 0:128], Lm[u][:, 0:128], g.tri_lt[:], ALU.mult)
                    S.mm(pr[:, :n], g.tri_gt[:], Lm[u][:, :n])
                    S.tt(aa[u][:, :n], pr[:, :n], TotB[:, c0:512], ALU.add)
                    S.tt(aa[u][:, :n], aa[u][:, :n], lt[u][:, :n], ALU.subtract, eng="pool")
                    S.act(att_all[:, j, c0:512], aa[u][:, :n], AF.Exp)
                    if d >= 0:
                        S.tt(att_all[:, j, c0:c0 + 128], att_all[:, j, c0:c0 + 128], g.tri_lt16[:], ALU.mult, eng="pool")
                    if j > 0:
                        S.mm(pt[:, :n], g.ones[:], Lm[u][:, :n])
                        S.tt(TotB[:, c0:512], TotB[:, c0:512], pt[:, :n], ALU.add)
                for qb in range(4):
                    for j in range(4 * I + qb, -1, -1):
                        S.mm(po[:, qb * 64:(qb + 1) * 64], att_all[:, j, qb * 128:(qb + 1) * 128],
                             v16[:, j, h * 64:(h + 1) * 64], start=(j == 4 * I + qb), stop=(j == 0))
                S.copy(yraw[:, 4 * I:4 * I + 4, h * 64:(h + 1) * 64],
                       po[:, 0:256].rearrange("p (b d) -> p b d", d=64), eng="act")
        if getattr(g, "debug", False):
            S.dma(ycat_d[:, 0:256].rearrange("(b p) n -> p b n", p=P), yraw[:])
        tm_head_rmsnorm(S, nc, st, g, yraw, 16, gain, g.eps6)
        S.dma(ycat_d[:, 256:512].rearrange("(b p) n -> p b n", p=P), yraw[:])
        S.flush()


def setup_consts(S, nc, st, g):
    def ld(name, shape, src=None):
        t = sb(nc, st, "k_" + name, shape, F32)
        S.dma(t[:], g.dram[src or name])
        return t
    g.ones = ld("ones", [P, P])
    g.ident = ld("ident", [P, P])
    g.tri_lt = ld("tri_lt", [P, P])
    g.tri_le = ld("tri_le", [P, P])
    g.tri_gt = ld("tri_gt", [P, P])
    g.cbias = ld("cbias", [P, P])
    g.lebias = ld("lebias", [P, P])
    g.selh = sb(nc, st, "k_selh", [4, 4, P], F32)
    S.dma(g.selh[:], g.dram["selh"].rearrange("k (h m) -> k h m", m=P))
    g.tri_lt16 = sb(nc, st, "k_tri_lt16", [P, P], BF16)
    g.tri_le16 = sb(nc, st, "k_tri_le16", [P, P], BF16)
    g.ident16 = sb(nc, st, "k_ident16", [P, P], BF16)
    g.ones16 = sb(nc, st, "k_ones16", [P, P], BF16)
    S.copy(g.tri_lt16[:], g.tri_lt[:])
    S.copy(g.tri_le16[:], g.tri_le[:])
    S.copy(g.ident16[:], g.ident[:])
    S.copy(g.ones16[:], g.ones[:])
    g.eps6 = sb(nc, st, "k_eps6", [P, 1], F32)
    S.memset(g.eps6[:], 1e-6)
    g.one1 = sb(nc, st, "k_one1", [P, 1], F32)
    S.memset(g.one1[:], 1.0)
    g.modT = sb(nc, st, "modT", [P, 48, 4], F32)
    g.A1 = sb(nc, st, "A1", [P, 8, 4], F32)
    g.A2 = sb(nc, st, "A2", [P, 8, 4], F32)
    S.flush()


def ph_ml(S, nc, g, l, ptm_d, pfm_d, ycat_d):
    LN8 = float(np.log(0.125))
    with ExitStack() as st:
        q16 = sb(nc, st, "mlq", [P, 2, T], BF16)
        k16 = sb(nc, st, "mlk", [P, 2, T], BF16)
        v16 = sb(nc, st, "mlv", [P, 16, 4, 65], BF16)
        osig = sb(nc, st, "mlo", [P, 16, 256], F32)
        BtB = [sb(nc, st, "mlB%d" % h, [P, T], F32) for h in range(4)]
        c_tm = sb(nc, st, "mlc", [P, 16, 4], F32)
        gain = load_row_bcast(S, nc, st, "mlg", g.dram["ml_norm_g"][l], 256)
        with ExitStack() as st2:
            xq = sb(nc, st2, "mlxq", [P, 2, T + 3], F32)
            xk = sb(nc, st2, "mlxk", [P, 2, T + 3], F32)
            S.memset(xq[:, :, 0:3], 0.0)
            S.memset(xk[:, :, 0:3], 0.0)
            S.dma(xq[:, :, 3:T + 3], pfm_d[FM_CQ:FM_CQ + 256, :].rearrange("(c p) t -> p c t", p=P))
            S.dma(xk[:, :, 3:T + 3], pfm_d[FM_CK:FM_CK + 256, :].rearrange("(c p) t -> p c t", p=P))
            cw = sb(nc, st2, "mlcw", [P, 4, 4], F32)
            cb = sb(nc, st2, "mlcb", [P, 4], F32)
            S.dma(cw[:], g.dram["conv_w_fm"][l])
            S.dma(cb[:], g.dram["conv_b_fm"][l])
            acc = [sb(nc, st2, "mlacc%d" % i, [P, T], F32) for i in range(2)]
            ai = 0
            for (x, dst, ci0) in ((xq, q16, 0), (xk, k16, 2)):
                for c in range(2):
                    a = acc[ai % 2]
                    eng = "dve"
                    ai += 1
                    S.ts(a[:], x[:, c, 0:T], cw[:, ci0 + c, 0:1], None, ALU.mult, eng=eng)
                    for tap in range(1, 4):
                        S.stt(a[:], x[:, c, tap:T + tap], cw[:, ci0 + c, tap:tap + 1], a[:], ALU.mult, ALU.add, eng=eng)
                    S.act(dst[:, c, :], a[:], AF.Silu, bias=cb[:, ci0 + c:ci0 + c + 1])
            ig = sb(nc, st2, "mlig", [4, T], F32)
            fg = sb(nc, st2, "mlfg", [4, T], F32)
            S.dma(ig[:], pfm_d[FM_CIG:FM_CIG + 4, :])
            S.dma(fg[:], pfm_d[FM_CFG:FM_CFG + 4, :])
            gb = sb(nc, st2, "mlgb", [4, 2], F32)
            S.dma(gb[:, 0:1], g.dram["ml_ig_b"][l].rearrange("(h o) -> h o", o=1))
            S.dma(gb[:, 1:2], g.dram["ml_fg_b"][l].rearrange("(h o) -> h o", o=1))
            S.ts(gb[:], gb[:], 1.0 / 15.0, None, ALU.mult)
            S.act(ig[:], ig[:], AF.Tanh, scale=1.0 / 15.0, bias=gb[:, 0:1])
            S.act(fg[:], fg[:], AF.Tanh, scale=1.0 / 15.0, bias=gb[:, 1:2])
            S.act(fg[:], fg[:], AF.Exp, scale=-15.0)
            S.act(fg[:], fg[:], AF.Ln, bias=g.one1[0:4, 0:1])
            ones4 = sb(nc, st2, "mlones", [4, T], F32)
            S.memset(ones4[:], 1.0)
            Bn = sb(nc, st2, "mlBn", [4, T], F32)
            S.scan(Bn[:], ones4[:], fg[:], 0.0, ALU.mult, ALU.add)
            cT = sb(nc, st2, "mlcT", [4, T], F32)
            S.stt(cT[:], ig[:], 15.0, Bn[:], ALU.mult, ALU.add)
            S.ts(cT[:], cT[:], LN8, None, ALU.add)
            BT = sb(nc, st2, "mlBT", [4, T], F32)
            S.ts(BT[:], Bn[:], -1.0, None, ALU.mult)
            ev = 0
            for h in range(4):
                for sblk in range(4):
                    ps = g.ps[ev % 4]
                    S.mm(ps[:, :], g.selh[0:4, h, :], BT[0:4, sblk * 512:(sblk + 1) * 512])
                    S.copy(BtB[h][:, sblk * 512:(sblk + 1) * 512], ps[:, :], eng=("act" if ev % 2 else "dve"))
                    ev += 1
            pc = g.ps[4]
            for b in range(16):
                S.mm(pc[:, b * 4:(b + 1) * 4], cT[0:4, b * 128:(b + 1) * 128], g.ident[0:4, 0:4])
            S.copy(c_tm[:], pc[:, 0:64].rearrange("p (b h) -> p b h", h=4))
            vf = sb(nc, st2, "mlvf", [P, 16, 256], F32)
            S.dma(vf[:], ptm_d[:, TM_CV:TM_CV + 256].rearrange("(b p) n -> p b n", p=P))
            S.copy(v16[:, :, :, 0:64], vf[:].rearrange("p b (h d) -> p b h d", d=64), eng="pool")
            S.memset(v16[:, :, :, 64:65], 1.0, eng="pool")
            S.dma(osig[:], ptm_d[:, TM_CO:TM_CO + 256].rearrange("(b p) n -> p b n", p=P))
            S.act(osig[:], osig[:], AF.Sigmoid)
            S.flush()
        attA = [sb(nc, st, "mlt%d" % i, [P, 16, 512], BF16) for i in range(2)]
        Dm = [sb(nc, st, "mlD%d" % i, [P, 512], F32) for i in range(2)]
        dtmp = [sb(nc, st, "mldt%d" % i, [P, 128], F32) for i in range(2)]
        nd = [sb(nc, st, "mlnd%d" % i, [P, 4, 65], F32) for i in range(2)]
        dn = [sb(nc, st, "mldn%d" % i, [P, 4], F32) for i in range(2)]
        hraw = sb(nc, st, "mlh", [P, 16, 256], F32)
        it = 0
        for h in range(4):
            c = h // 2
            pb = (h % 2) * 64
            for I in range(4):
                gi = h * 4 + I
                po = g.ps[6 + (gi % 2)]
                att_all = attA[gi % 2]
                for j in range(4 * I + 3, -1, -1):
                    u = it % 2
                    it += 1
                    pz = g.ps[u]
                    d = j - 4 * I
                    dd = max(d, 0)
                    c0 = dd * 128
                    n = 512 - c0
                    q0 = I * 512 + c0
                    S.mm(pz[:, :n], k16[pb:pb + 64, c, j * 128:(j + 1) * 128], q16[pb:pb + 64, c, q0:q0 + n])
                    if d >= 0:
                        S.tt(dtmp[u][:], BtB[h][:, q0:q0 + 128], g.lebias[:], ALU.add, eng="pool")
                        S.act(Dm[u][:, 0:128], dtmp[u][:], AF.Exp, bias=c_tm[:, j, h:h + 1])
                        if n > 128:
                            S.act(Dm[u][:, 128:n], BtB[h][:, q0 + 128:q0 + n], AF.Exp, bias=c_tm[:, j, h:h + 1])
                    else:
                        S.act(Dm[u][:, :n], BtB[h][:, q0:q0 + n], AF.Exp, bias=c_tm[:, j, h:h + 1])
                    S.tt(att_all[:, j, c0:512], pz[:, :n], Dm[u][:, :n], ALU.mult)
                for qb in range(4):
                    for j in range(4 * I + qb, -1, -1):
                        S.mm(po[:, qb * 65:(qb + 1) * 65], att_all[:, j, qb * 128:(qb + 1) * 128],
                             v16[:, j, h, :], start=(j == 4 * I + qb), stop=(j == 0))
                u2 = gi % 2
                S.copy(nd[u2][:], po[:, 0:260].rearrange("p (b d) -> p b d", d=65), eng="act")
                S.stt(dn[u2][:], nd[u2][:, :, 64], -1.0, nd[u2][:, :, 64], ALU.mult, ALU.max)
                S.ts(dn[u2][:], dn[u2][:], 1.0, None, ALU.max)
                S.recip(dn[u2][:], dn[u2][:])
                S.tt(hraw[:, 4 * I:4 * I + 4, h * 64:(h + 1) * 64], nd[u2][:, :, 0:64],
                     bc(dn[u2][:].unsqueeze(2), [P, 4, 64]), ALU.mult)
        tm_head_rmsnorm(S, nc, st, g, hraw, 16, gain, g.eps6)
        S.tt(hraw[:], hraw[:], osig[:], ALU.mult)
        S.dma(ycat_d[:, 512:768].rearrange("(b p) n -> p b n", p=P), hraw[:])
        S.flush()


def _rope_tm(S, out, x, cos, sin, t1, t2, half):
    x1, x2 = x[:, :, 0:half], x[:, :, half:2 * half]
    S.tt(t1, x1, cos, ALU.mult)
    S.tt(t2, x2, sin, ALU.mult)
    S.tt(out[:, :, 0:half], t1, t2, ALU.subtract)
    S.tt(t1, x2, cos, ALU.mult)
    S.tt(t2, x1, sin, ALU.mult)
    S.tt(out[:, :, half:2 * half], t1, t2, ALU.add)


def ph_dsa(S, nc, g, l, ptm_d, ycat_d):
    WI_SCALE = float(4 ** -0.5 * 32 ** -0.5)
    with ExitStack() as st:
        qT = sb(nc, st, "dqT", [P, 2, T], BF16)
        kT2 = sb(nc, st, "dkT", [P, T], BF16)
        qiT = sb(nc, st, "dqi", [P, T], F32)
        kiX = [sb(nc, st, "dki%d" % h, [P, T], F32) for h in range(4)]
        wi = sb(nc, st, "dwi", [P, 16, 4], F32)
        v16 = sb(nc, st, "dv", [P, 16, 65], BF16)
        gain = load_row_bcast(S, nc, st, "dg", g.dram["ds_out_g"][l], 256)
        with ExitStack() as st2:
            rope64 = sb(nc, st2, "rope64", [P, 16, 64], F32)
            rope32 = sb(nc, st2, "rope32", [P, 16, 32], F32)
            S.dma(rope64[:], g.dram["rope64"].rearrange("(b p) n -> p b n", p=P))
            S.dma(rope32[:], g.dram["rope32"].rearrange("(b p) n -> p b n", p=P))
            gq = load_row_bcast(S, nc, st2, "dgq", g.dram["ds_qn_g"][l], 64)
            gk = load_row_bcast(S, nc, st2, "dgk", g.dram["ds_kn_g"][l], 64)
            xs = [sb(nc, st2, "dx%d" % i, [P, 548], F32) for i in range(2)]
            sq = sb(nc, st2, "dsq", [P, 5, 64], F32)
            ss = sb(nc, st2, "dss", [P, 5], F32)
            qn = sb(nc, st2, "dqn", [P, 5, 64], F32)
            qr = [sb(nc, st2, "dqr%d" % i, [P, 6, 64], F32) for i in range(2)]
            qir = [sb(nc, st2, "dqir%d" % i, [P, 5, 32], F32) for i in range(2)]
            t1 = sb(nc, st2, "dt1", [P, 5, 32], F32)
            t2 = sb(nc, st2, "dt2", [P, 5, 32], F32)
            t3 = sb(nc, st2, "dt3", [P, 5, 16], F32)
            t4 = sb(nc, st2, "dt4", [P, 5, 16], F32)
            kiz = [[sb(nc, st2, "dkz%d_%d" % (i, h), [P, 128], F32) for h in range(4)] for i in range(2)]
            for i in range(2):
                for h in range(4):
                    S.memset(kiz[i][h][:], 0.0, eng="pool")
            S.dma(wi[:], ptm_d[:, TM_DWI:TM_DWI + 4].rearrange("(b p) n -> p b n", p=P))
            S.ts(wi[:], wi[:], WI_SCALE, None, ALU.mult)
            ev = 0
            cut = getattr(g, 'dsa_cut', 0)
            for b in range(16):
                x = xs[b % 2]
                S.dma(x[:], ptm_d[b * 128:(b + 1) * 128, TM_DQ:TM_DQ + 548])
                qk = x[:, 0:320].rearrange("p (h d) -> p h d", d=64)
                S.tt(sq[:], qk, qk, ALU.mult)
                S.reduce(ss[:], sq[:], ALU.add, AX.X)
                S.act(ss[:], ss[:], AF.Sqrt, scale=1.0 / 64, bias=g.eps6[:, 0:1])
                S.recip(ss[:], ss[:])
                S.tt(qn[:], qk, bc(ss[:].unsqueeze(2), [P, 5, 64]), ALU.mult)
                S.tt(qn[:, 0:4, :], qn[:, 0:4, :], bc(gq[:].unsqueeze(1), [P, 4, 64]), ALU.mult)
                S.tt(qn[:, 4:5, :], qn[:, 4:5, :], gk[:].unsqueeze(1), ALU.mult)
                r_ = qr[b % 2]
                cos = bc(rope64[:, b, 0:32].unsqueeze(1), [P, 5, 32])
                sin = bc(rope64[:, b, 32:64].unsqueeze(1), [P, 5, 32])
                _rope_tm(S, r_[:, 0:5, :], qn[:], cos, sin, t1[:], t2[:], 32)
                S.copy(r_[:, 5, :], r_[:, 4, :], eng="act")
                ri = qir[b % 2]
                xi = x[:, 384:544].rearrange("p (h d) -> p h d", d=32)
                cos = bc(rope32[:, b, 0:16].unsqueeze(1), [P, 5, 16])
                sin = bc(rope32[:, b, 16:32].unsqueeze(1), [P, 5, 16])
                _rope_tm(S, ri[:], xi, cos, sin, t3[:], t4[:], 16)
                S.copy(v16[:, b, 0:64], x[:, 320:384], eng="act")
                if cut == 1:
                    continue
                tb = slice(b * 128, (b + 1) * 128)
                for c in range(3):
                    ps = g.ps[ev % 4]
                    ev += 1
                    S.transpose(ps[:, 0:128], r_[:, 2 * c:2 * c + 2, :].rearrange("p h d -> p (h d)"), g.ident[:])
                    if c < 2:
                        S.copy(qT[:, c, tb], ps[:, 0:128], eng="act")
                    else:
                        S.copy(kT2[:, tb], ps[:, 0:128], eng="act")
                if cut == 2:
                    continue
                ps = g.ps[ev % 4]
                ev += 1
                S.transpose(ps[:, 0:128], ri[:, 0:4, :].rearrange("p h d -> p (h d)"), g.ident[:])
                S.copy(qiT[:, tb], ps[:, 0:128], eng="dve")
                kz = kiz[b % 2]
                for h in range(4):
                    S.copy(kz[h][:, h * 32:(h + 1) * 32], ri[:, 4, :], eng="pool")
                    ps = g.ps[ev % 4]
                    ev += 1
                    S.transpose(ps[:, 0:128], kz[h][:], g.ident[:])
                    S.copy(kiX[h][:, tb], ps[:, 0:128], eng=("act" if h % 2 else "dve"))
            S.memset(v16[:, :, 64:65], 1.0, eng="pool")
            S.flush()
        if getattr(g, "dsa_stop", 0) == 1:
            return
        sc = [sb(nc, st, "dsc%d" % i, [P, T], F32) for i in range(1)] * 2
        work = sb(nc, st, "dwork", [P, T], F32)
        mk = sb(nc, st, "dmk", [P, T], F32)
        eqm = sb(nc, st, "deq", [P, T], F32)
        cum = sb(nc, st, "dcum", [P, T], F32)
        onesT = sb(nc, st, "dones", [P, T], F32)
        S.memset(onesT[:], 1.0, eng="pool")
        rl = [sb(nc, st, "drl%d" % i, [P, 512], F32) for i in range(2)]
        m8 = sb(nc, st, "dm8", [P, 8], F32)
        ngt = sb(nc, st, "dngt", [P, 1], F32)
        maskT = [sb(nc, st, "dmT%d" % i, [P, 16, 128], F32) for i in range(2)]
        E = [sb(nc, st, "dE%d" % i, [P, 512], F32) for i in range(2)]
        Pall = [sb(nc, st, "dP%d" % i, [P, 16, 512], BF16) for i in range(2)]
        nd = [sb(nc, st, "dnd%d" % i, [P, 4, 65], F32) for i in range(2)]
        dn = [sb(nc, st, "ddn%d" % i, [P, 4], F32) for i in range(2)]
        yraw = sb(nc, st, "dy", [P, 16, 256], F32)
        cnt = {'ev': 0, 'it': 0}

        def part_a(i):
            ev = cnt['ev']
            kl = 128 * (i + 1)
            qb = slice(i * 128, (i + 1) * 128)
            mT = maskT[i % 2]
            if i >= 2:
                s_ = sc[i % 2]
                for kb in range(0, kl, 512):
                    w = min(512, kl - kb)
                    for h in range(4):
                        ps = g.ps[0]
                        r2 = rl[ev % 2]
                        ev += 1
                        S.mm(ps[:, :w], qiT[:, qb], kiX[h][:, kb:kb + w])
                        S.act(r2[:, :w], ps[:, :w], AF.Relu)
                        if h == 0:
                            S.ts(s_[:, kb:kb + w], r2[:, :w], wi[:, i, 0:1], None, ALU.mult)
                        else:
                            S.stt(s_[:, kb:kb + w], r2[:, :w], wi[:, i, h:h + 1], s_[:, kb:kb + w], ALU.mult, ALU.add)
                S.tt(s_[:, qb], s_[:, qb], g.cbias[:], ALU.add)
                for r in range(32):
                    S.max8(m8[:], (s_ if r == 0 else work)[:, :kl])
                    if r < 31:
                        S.match_replace(work[:, :kl], m8[:], (s_ if r == 0 else work)[:, :kl], NEG)
                S.ts(mk[:, :kl], s_[:, :kl], m8[:, 7:8], None, ALU.is_gt)
                S.reduce(ngt[:], mk[:, :kl], ALU.add, AX.X)
                S.ts(ngt[:], ngt[:], -1.0, 256.0, ALU.mult, ALU.add)
                S.ts(eqm[:, :kl], s_[:, :kl], m8[:, 7:8], None, ALU.is_equal)
                S.scan(cum[:, :kl], onesT[:, :kl], eqm[:, :kl], 0.0, ALU.mult, ALU.add)
                S.ts(cum[:, :kl], cum[:, :kl], ngt[:, 0:1], None, ALU.is_le)
                S.tt(eqm[:, :kl], eqm[:, :kl], cum[:, :kl], ALU.mult)
                S.tt(mk[:, :kl], mk[:, :kl], eqm[:, :kl], ALU.add)
                for j0 in range(0, i + 1, 4):
                    nj = min(4, i + 1 - j0)
                    ps = g.ps[1]
                    ev += 1
                    for jj in range(nj):
                        S.transpose(ps[:, jj * 128:(jj + 1) * 128], mk[:, (j0 + jj) * 128:(j0 + jj + 1) * 128], g.ident[:])
                    S.copy(mT[:, j0:j0 + nj, :], ps[:, 0:nj * 128].rearrange("p (j q) -> p j q", q=128), eng="act")
            else:
                for j in range(i):
                    S.copy(mT[:, j, :], g.ones[:], eng="dve")
                S.copy(mT[:, i, :], g.tri_le[:], eng="dve")
            cnt['ev'] = ev

        def part_b(i):
            it = cnt['it']
            qb = slice(i * 128, (i + 1) * 128)
            mT = maskT[i % 2]
            Pa = Pall[i % 2]
            for j in range(i + 1):
                u = it % 2
                it += 1
                pzA, pzB = g.ps[2 + 2 * u], g.ps[3 + 2 * u]
                for h in range(4):
                    pb = (h % 2) * 64
                    pz = pzB if (h % 2) else pzA
                    S.mm(pz[:, (h // 2) * 128:(h // 2 + 1) * 128], kT2[pb:pb + 64, j * 128:(j + 1) * 128], qT[pb:pb + 64, h // 2, qb])
                S.act(E[u][:, 0:256], pzA[:, 0:256], AF.Exp, scale=0.125)
                S.act(E[u][:, 256:512], pzB[:, 0:256], AF.Exp, scale=0.125)
                S.tt(Pa[:, j, :].rearrange("p (h q) -> p h q", q=128), E[u][:].rearrange("p (h q) -> p h q", q=128),
                     bc(mT[:, j, :].unsqueeze(1), [P, 4, 128]), ALU.mult, eng="pool")
            if getattr(g, "dsa_cut2", 0) >= 1:
                return
            po = g.ps[6 + (i % 2)]
            for h in range(4):
                for j in range(i + 1):
                    hr = (h % 2) * 2 + h // 2
                    S.mm(po[:, h * 65:(h + 1) * 65], Pa[:, j, hr * 128:(hr + 1) * 128], v16[:, j, :],
                         start=(j == 0), stop=(j == i))
            u2 = i % 2
            S.copy(nd[u2][:], po[:, 0:260].rearrange("p (h d) -> p h d", d=65), eng="act")
            S.recip(dn[u2][:], nd[u2][:, :, 64])
            S.tt(yraw[:, i, :].rearrange("p (h d) -> p h d", d=64), nd[u2][:, :, 0:64],
                 bc(dn[u2][:].unsqueeze(2), [P, 4, 64]), ALU.mult)
            cnt['it'] = it

        nblk = getattr(g, "dsa_nblk", 16)
        part_a(0)
        for i in range(nblk):
            if i + 1 < nblk:
                part_a(i + 1)
            part_b(i)
        tm_head_rmsnorm(S, nc, st, g, yraw, 16, gain, g.eps6)
        S.dma(ycat_d[:, 768:1024].rearrange("(b p) n -> p b n", p=P), yraw[:])
        S.flush()


def ph_rwkv_prep(S, nc, g, l, s, ptm_d, pfm_d, sops_d, sv_d, sbg_d):
    x_, sp = s // 2, s % 2
    with ExitStack() as st:
        twT = sb(nc, st, "rtw", [33, T], F32)
        adT = sb(nc, st, "rad", [33, T], F32)
        sgT = sb(nc, st, "rsg", [64, T], F32)
        S.memset(twT[:], 1.0)
        S.memset(adT[:], 1.0, eng="pool")
        S.dma(twT[0:32, :], pfm_d[FM_AWD:FM_AWD + 32, :])
        S.dma(adT[0:32, :], pfm_d[FM_AAD:FM_AAD + 32, :])
        S.dma(sgT[:], pfm_d[FM_AGD:FM_AGD + 64, :])
        S.act(twT[0:32, :], twT[0:32, :], AF.Tanh)
        S.act(sgT[:], sgT[:], AF.Sigmoid)
        w2a = sb(nc, st, "rw2", [33, 256], F32)
        a2a = sb(nc, st, "ra2", [33, 256], F32)
        g2 = sb(nc, st, "rg2", [64, 256], F32)
        S.dma(w2a[0:32, :], g.dram["rk_w2"][l])
        S.dma(w2a[32:33, :], g.dram["rk_w0"][l].rearrange("(o n) -> o n", o=1))
        S.dma(a2a[0:32, :], g.dram["rk_a2"][l])
        S.dma(a2a[32:33, :], g.dram["rk_a0"][l].rearrange("(o n) -> o n", o=1))
        S.dma(g2[:], g.dram["rk_g2"][l])
        kk_b = load_row_bcast(S, nc, st, "rkk", g.dram["rk_kk"][l], 256)
        ka_b = load_row_bcast(S, nc, st, "rka", g.dram["rk_ka"][l], 256)
        rk_b = load_row_bcast(S, nc, st, "rrk", g.dram["rk_rk"][l], 256)
        vfm = sb(nc, st, "rvfm", [P, 2, T], F32)
        S.dma(vfm[:], pfm_d[FM_AV:FM_AV + 256, :].rearrange("(c p) t -> p c t", p=P))
        S.dma(sv_d[s].rearrange("(c p) t -> p c t", p=P), vfm[:])
        xs = [sb(nc, st, "rx%d" % i, [P, 768], F32) for i in range(2)]
        F = lambda n: [sb(nc, st, "%s%d" % (n, i), [P, 256], F32) for i in range(2)]
        sig, dec, a_, kkn, k2, nkka, tmp = F("rsig"), F("rdec"), F("ra"), F("rkkn"), F("rk2"), F("rnk"), F("rtmp")
        bg = [sb(nc, st, "rbg%d" % i, [P, 512], F32) for i in range(2)]
        ss = sb(nc, st, "rss", [P, 4], F32)
        bco = sb(nc, st, "rbc", [P, 4], F32)
        hi = [[sb(nc, st, "rhi%d_%d" % (i, o), [P, 256], BF16) for o in range(5)] for i in range(2)]
        lo = [[sb(nc, st, "rlo%d_%d" % (i, o), [P, 256], BF16) for o in range(5)] for i in range(2)]
        h32 = [sb(nc, st, "rh32_%d" % i, [P, 256], F32) for i in range(2)]
        hv = lambda ap: ap.rearrange("p (h d) -> p h d", d=64)
        for tb in range(16):
            u = tb % 2
            x = xs[u]
            tsl = slice(tb * 128, (tb + 1) * 128)
            S.dma(x[:], ptm_d[tsl, 0:768])
            r, k, v = x[:, 0:256], x[:, 256:512], x[:, 512:768]
            pw, pa, pg = g.ps[0 + u], g.ps[2 + u], g.ps[4 + u]
            S.mm(pw[:, 0:256], twT[0:33, tsl], w2a[0:33, :])
            S.mm(pa[:, 0:256], adT[0:33, tsl], a2a[0:33, :])
            S.mm(pg[:, 0:256], sgT[0:64, tsl], g2[0:64, :])
            S.act(sig[u][:], pw[:, 0:256], AF.Sigmoid)
            S.act(a_[u][:], pa[:, 0:256], AF.Sigmoid)
            S.act(dec[u][:], sig[u][:], AF.Exp, scale=-0.6065306597126334)
            S.copy(bg[u][:, 256:512], pg[:, 0:256], eng="act")
            S.tt(kkn[u][:], k, kk_b[:], ALU.mult)
            S.tt(tmp[u][:], kkn[u][:], kkn[u][:], ALU.mult)
            S.reduce(ss[:], hv(tmp[u][:]), ALU.add, AX.X)
            S.act(ss[:], ss[:], AF.Sqrt)
            S.ts(ss[:], ss[:], 1e-12, None, ALU.max)
            S.recip(ss[:], ss[:])
            S.tt(hv(kkn[u][:]), hv(kkn[u][:]), bc(ss[:].unsqueeze(2), [P, 4, 64]), ALU.mult)
            S.stt(k2[u][:], a_[u][:], -1.0, ka_b[:], ALU.add, ALU.mult)
            S.stt(k2[u][:], k2[u][:], 1.0, k, ALU.add, ALU.mult)
            S.stt(nkka[u][:], kkn[u][:], -1.0, a_[u][:], ALU.mult, ALU.mult)
            S.tt(tmp[u][:], r, k2[u][:], ALU.mult)
            S.tt(tmp[u][:], tmp[u][:], rk_b[:], ALU.mult)
            S.reduce(bco[:], hv(tmp[u][:]), ALU.add, AX.X)
            S.tt(hv(bg[u][:, 0:256]), hv(v), bc(bco[:].unsqueeze(2), [P, 4, 64]), ALU.mult)
            S.dma(sbg_d[s, tsl, :], bg[u][:])
            for o, src in enumerate((kkn[u][:], dec[u][:], nkka[u][:], k2[u][:], r)):
                S.copy(hi[u][o][:], src, eng="act")
                S.copy(h32[o % 2][:], hi[u][o][:], eng="pool")
                S.tt(h32[o % 2][:], src, h32[o % 2][:], ALU.subtract, eng="pool")
                S.copy(lo[u][o][:], h32[o % 2][:], eng="act")
                S.dma(sops_d[o, 0, x_, tsl, sp * 256:(sp + 1) * 256], hi[u][o][:])
                S.dma(sops_d[o, 1, x_, tsl, sp * 256:(sp + 1) * 256], lo[u][o][:])
        S.flush()


def ph_rwkv_scan(S, nc, g, sops_d, sv_d, sy_d, nsteps=T):
    CH = 32
    with ExitStack() as st:
        id2 = sb(nc, st, "sid2", [P, 128], BF16)
        S.memset(id2[:], 0.0)
        S.tt(id2[:, 0:32], g.ident16[:, 0:32], g.ident16[:, 32:64], ALU.add)
        S.tt(id2[:, 64:96], g.ident16[:, 64:96], g.ident16[:, 96:128], ALU.add)
        sel = sb(nc, st, "ssel", [P, CH, 128], BF16)
        for xp in range(2):
            for tp in range(CH):
                col = xp * 64 + tp
                S.copy(sel[:, tp, xp * 64:(xp + 1) * 64], bc(id2[:, col:col + 1], [P, 64]),
                       eng=("dve" if tp % 2 else "pool"))
        Stt = [sb(nc, st, "sS%d" % i, [P, 512], F32) for i in range(2)]
        S.memset(Stt[0][:], 0.0)
        S.memset(Stt[1][:], 0.0)
        ytmp = sb(nc, st, "sytmp", [P, 512], F32)
        Sw = sb(nc, st, "sSw", [P, 512], F32)
        tmp = sb(nc, st, "stmp", [P, 512], F32)
        sa = sb(nc, st, "ssa", [P, 8], F32)
        ND = 3
        ringW = [sb(nc, st, "srgW%d" % d, [P, 512], F32) for d in range(ND)]
        ringK = [sb(nc, st, "srgK%d" % d, [P, 512], F32) for d in range(ND)]
        vk = [sb(nc, st, "svk%d" % d, [P, 512], F32) for d in range(ND)]
        opt = [[sb(nc, st, "sop%d_%d" % (b_, o), [P, 512], BF16) for o in range(5)] for b_ in range(3)]
        vS = [sb(nc, st, "svS%d" % i, [P, 8, 256], F32) for i in range(2)]
        yb = [sb(nc, st, "syb%d" % i, [P, 8, 256], F32) for i in range(2)]
        g3 = lambda ap: ap.rearrange("p (g k) -> p g k", k=64)
        step = 0
        nbig = (nsteps + 255) // 256
        for big in range(nbig):
            vs, y_ = vS[big % 2], yb[big % 2]
            bsl = slice(big * 256, (big + 1) * 256)
            for x in range(2):
                for sp in range(2):
                    for h in range(4):
                        S.dma(vs[x * 64:(x + 1) * 64, sp * 4 + h, :], sv_d[2 * x + sp, h * 64:(h + 1) * 64, bsl])
            for cc in range(256 // CH):
                c = big * (256 // CH) + cc
                if c * CH >= nsteps:
                    break
                ob = opt[c % 3]
                for o in range(5):
                    for x in range(2):
                        for hl in range(2):
                            p0 = x * 64 + hl * 32
                            S.dma(ob[o][p0:p0 + CH, :], sops_d[o, hl, x, c * CH:(c + 1) * CH, :])
                for tp in range(CH):
                    ti = cc * CH + tp
                    d = step % ND
                    par = step % 2
                    banks = (g.ps[0 + par], g.ps[6], g.ps[2 + par], g.ps[7], g.ps[4 + par])
                    for o in range(5):
                        S.mm(banks[o][:, :], sel[:, tp, :], ob[o][:])
                    S.copy(ringW[d][:], banks[1][:, :], eng="act")
                    S.copy(ringK[d][:], banks[3][:, :], eng="act")
                    KK, NK, R = banks[0], banks[2], banks[4]
                    W, KB = ringW[d], ringK[d]
                    Sp, Sn = Stt[step % 2], Stt[(step + 1) % 2]
                    S.tt(g3(vk[d][:]), g3(KB[:]), bc(vs[:, :, ti:ti + 1], [P, 8, 64]), ALU.mult, eng="pool")
                    S.tt(Sw[:], Sp[:], W[:], ALU.mult, eng="pool")
                    S.tt(Sw[:], Sw[:], vk[d][:], ALU.add, eng="pool")
                    S.tt(tmp[:], Sp[:], KK[:, :], ALU.mult)
                    S.reduce(sa[:], g3(tmp[:]), ALU.add, AX.X)
                    S.tt(g3(tmp[:]), g3(NK[:, :]), bc(sa[:].unsqueeze(2), [P, 8, 64]), ALU.mult)
                    S.tt(Sn[:], Sw[:], tmp[:], ALU.add)
                    S.tt(ytmp[:], Sn[:], R[:, :], ALU.mult)
                    S.reduce(y_[:, :, ti], g3(ytmp[:]), ALU.add, AX.X)
                    step += 1
            for x in range(2):
                for sp in range(2):
                    for h in range(4):
                        S.dma(sy_d[2 * x + sp, h * 64:(h + 1) * 64, bsl], y_[x * 64:(x + 1) * 64, sp * 4 + h, :])
        S.flush()


def ph_rwkv_post(S, nc, g, l, s, sy_d, sbg_d, ycat_d):
    with ExitStack() as st:
        yT = sb(nc, st, "pyT", [P, 2, T], F32)
        S.dma(yT[:], sy_d[s].rearrange("(c p) t -> p c t", p=P))
        bgt = sb(nc, st, "pbg", [P, 16, 512], F32)
        S.dma(bgt[:], sbg_d[s].rearrange("(b p) n -> p b n", p=P))
        lng = load_row_bcast(S, nc, st, "plng", g.dram["rk_ln_g"][l], 256)
        lnb = load_row_bcast(S, nc, st, "plnb", g.dram["rk_ln_b"][l], 256)
        y = sb(nc, st, "py", [P, 16, 256], F32)
        for tb in range(16):
            ps = g.ps[tb % 4]
            for c in range(2):
                S.transpose(ps[:, c * 128:(c + 1) * 128], yT[:, c, tb * 128:(tb + 1) * 128], g.ident[:])
            S.copy(y[:, tb, :], ps[:, 0:256], eng=("act" if tb % 2 else "dve"))
        yv = y[:].rearrange("p b (h d) -> p (b h) d", d=64)
        mean = sb(nc, st, "pmean", [P, 64], F32)
        sq = sb(nc, st, "psq", [P, 64, 64], F32)
        S.reduce(mean[:], yv, ALU.add, AX.X)
        S.ts(mean[:], mean[:], 1.0 / 64, None, ALU.mult)
        S.tt(yv, yv, bc(mean[:].unsqueeze(2), [P, 64, 64]), ALU.subtract)
        S.tt(sq[:], yv, yv, ALU.mult)
        S.reduce(mean[:], sq[:], ALU.add, AX.X)
        eps = sb(nc, st, "peps", [P, 1], F32)
        S.memset(eps[:], 64e-5)
        S.act(mean[:], mean[:], AF.Sqrt, scale=1.0 / 64, bias=eps[:, 0:1])
        S.recip(mean[:], mean[:])
        S.tt(yv, yv, bc(mean[:].unsqueeze(2), [P, 64, 64]), ALU.mult)
        S.tt(y[:], y[:], bc(lng[:].unsqueeze(1), [P, 16, 256]), ALU.mult)
        S.tt(y[:], y[:], bc(lnb[:].unsqueeze(1), [P, 16, 256]), ALU.add)
        S.tt(y[:], y[:], bgt[:, :, 0:256], ALU.add)
        S.tt(y[:], y[:], bgt[:, :, 256:512], ALU.mult)
        S.dma(ycat_d[:, 0:256].rearrange("(b p) n -> p b n", p=P), y[:])
        S.flush()


def ph_outproj(S, nc, g, l, b, ycat_d, xin_d, xout_d, tok0):
    with ExitStack() as st:
        Wb = sb(nc, st, "oW", [P, 8, 1024], BF16)
        ycT = sb(nc, st, "oyT", [P, 8, T], BF16)
        with ExitStack() as st2:
            stg = [sb(nc, st2, "ostg%d" % i, [P, 8, 512], F32) for i in range(2)]
            for hf in range(2):
                S.dma(stg[hf][:], g.dram["w_out"][l][:, hf * 512:(hf + 1) * 512].rearrange("(c p) n -> p c n", p=P))
                S.copy(Wb[:, :, hf * 512:(hf + 1) * 512], stg[hf][:], eng=("act" if hf else "pool"))
            yb = [sb(nc, st2, "oyb%d" % i, [P, 1024], F32) for i in range(2)]
            ev = 0
            for tb in range(16):
                y = yb[tb % 2]
                S.dma(y[:], ycat_d[tb * 128:(tb + 1) * 128, :])
                for c4 in range(2):
                    ps = g.ps[ev % 4]
                    ev += 1
                    for cc in range(4):
                        c = c4 * 4 + cc
                        S.transpose(ps[:, cc * 128:(cc + 1) * 128], y[:, c * 128:(c + 1) * 128], g.ident[:])
                    S.copy(ycT[:, c4 * 4:c4 * 4 + 4, tb * 128:(tb + 1) * 128],
                           ps[:, :].rearrange("p (c t) -> p c t", t=128), eng=("act" if ev % 2 else "dve"))
            S.flush()
        xo = [sb(nc, st, "oxo%d" % i, [P, 512], F32) for i in range(3)]
        ev = 0
        for oc in range(8):
            for sblk in range(4):
                ps = g.ps[4 + (ev % 4)]
                x = xo[ev % 3]
                ev += 1
                tsl = slice(tok0 + sblk * 512, tok0 + (sblk + 1) * 512)
                S.dma(x[:], xin_d[oc * 128:(oc + 1) * 128, tsl])
                for k in range(8):
                    S.mm(ps[:, :], Wb[:, k, oc * 128:(oc + 1) * 128], ycT[:, k, sblk * 512:(sblk + 1) * 512],
                         start=(k == 0), stop=(k == 7))
                S.stt(x[:], ps[:, :], g.modT[:, 16 + oc, b:b + 1], x[:], ALU.mult, ALU.add)
                S.dma(xout_d[oc * 128:(oc + 1) * 128, tsl], x[:])
        S.flush()


def ph_moe(S, nc, g, l, b, xin_d, xout_d, tok0, n_exp=32):
    with ExitStack() as st:
        hT = sb(nc, st, "mhT", [P, 8, T], BF16)
        gate = sb(nc, st, "mgate", [P, 16, 32], F32)
        with ExitStack() as st2:
            wge = sb(nc, st2, "mwge", [P, 8, 36], F32)
            S.dma(wge[:], g.dram["moe_wge"][l].rearrange("(c p) n -> p c n", p=P))
            bge = load_row_bcast(S, nc, st2, "mbge", g.dram["moe_bge"][l], 36)
            lg = sb(nc, st2, "mlg", [P, 16, 36], F32)
            emit_norm_mod(S, nc, g, xin_d, tok0, b, g.A2, g.modT[:, 24:32, :], hT, col0=0, route=(wge, lg))
            S.tt(lg[:], lg[:], bc(bge[:].unsqueeze(1), [P, 16, 36]), ALU.add)
            G4 = lg[:, :, 0:4]
            gmax = sb(nc, st2, "mgmax", [P, 16], F32)
            ge = sb(nc, st2, "mge", [P, 16, 4], F32)
            gsum = sb(nc, st2, "mgsum", [P, 16], F32)
            pen = sb(nc, st2, "mpen", [P, 16, 4], F32)
            S.reduce(gmax[:], G4, ALU.max, AX.X)
            S.tt(ge[:], G4, bc(gmax[:].unsqueeze(2), [P, 16, 4]), ALU.subtract)
            S.ts(pen[:], ge[:], 0.0, NEG, ALU.is_lt, ALU.mult)
            S.act(ge[:], ge[:], AF.Exp)
            S.reduce(gsum[:], ge[:], ALU.add, AX.X)
            S.recip(gsum[:], gsum[:])
            Em = sb(nc, st2, "mEm", [P, 16, 32], F32)
            S.tt(Em[:].rearrange("p b (q e) -> p b q e", e=8), lg[:, :, 4:36].rearrange("p b (q e) -> p b q e", e=8),
                 bc(pen[:].unsqueeze(3), [P, 16, 4, 8]), ALU.add)
            m1 = sb(nc, st2, "mm1", [P, 16], F32)
            m2 = sb(nc, st2, "mm2", [P, 16], F32)
            E2 = sb(nc, st2, "mE2", [P, 16, 32], F32)
            S.reduce(m1[:], Em[:], ALU.max, AX.X)
            S.tt(E2[:], Em[:], bc(m1[:].unsqueeze(2), [P, 16, 32]), ALU.is_ge)
            S.stt(E2[:], E2[:], NEG, Em[:], ALU.mult, ALU.add)
            S.reduce(m2[:], E2[:], ALU.max, AX.X)
            ex = sb(nc, st2, "mex", [P, 16, 32], F32)
            S.tt(ex[:], Em[:], bc(m1[:].unsqueeze(2), [P, 16, 32]), ALU.subtract)
            S.act(ex[:], ex[:], AF.Exp)
            S.tt(E2[:], Em[:], bc(m2[:].unsqueeze(2), [P, 16, 32]), ALU.is_ge)
            S.tt(ex[:], ex[:], E2[:], ALU.mult)
            den = sb(nc, st2, "mden", [P, 16], F32)
            S.tt(den[:], m2[:], m1[:], ALU.subtract)
            S.act(den[:], den[:], AF.Exp)
            S.ts(den[:], den[:], 1.0, None, ALU.add)
            S.recip(den[:], den[:])
            S.tt(den[:], den[:], gsum[:], ALU.mult)
            S.tt(gate[:], ex[:], bc(den[:].unsqueeze(2), [P, 16, 32]), ALU.mult)
            S.flush()
        acc = sb(nc, st, "macc", [P, 16, 1024], F32)
        stg = [sb(nc, st, "mstg%d" % i, [P, 8, 512], F32) for i in range(2)]
        W1b = [sb(nc, st, "mW1_%d" % i, [P, 8, 512], BF16) for i in range(2)]
        W3b = [sb(nc, st, "mW3_%d" % i, [P, 8, 512], BF16) for i in range(2)]
        W2b = [sb(nc, st, "mW2_%d" % i, [P, 4, 1024], BF16) for i in range(2)]
        aT = [sb(nc, st, "maT%d" % i, [P, 4, 512], BF16) for i in range(2)]
        su = [sb(nc, st, "msu%d" % i, [P, 512], F32) for i in range(2)]
        si = 0
        it = 0
        for e in range(n_exp):
            u = e % 2
            for (dst, src) in ((W1b[u], g.dram["moe_w1"][l, e]), (W3b[u], g.dram["moe_w3"][l, e])):
                sg = stg[si % 2]
                si += 1
                S.dma(sg[:], src.rearrange("(c p) n -> p c n", p=P))
                S.copy(dst[:], sg[:], eng=("act" if si % 2 else "pool"))
            sg = stg[si % 2]
            si += 1
            S.dma(sg[:].rearrange("p c n -> p (c n)").rearrange("p (c n) -> p c n", n=1024),
                  g.dram["moe_w2"][l, e].rearrange("(c p) n -> p c n", p=P))
            S.copy(W2b[u][:].rearrange("p c n -> p (c n)"), sg[:].rearrange("p c n -> p (c n)"), eng="pool")
            for sblk in range(4):
                a = aT[it % 2]
                it += 1
                for f in range(4):
                    pu, pg3 = g.ps[(f % 2) * 2], g.ps[(f % 2) * 2 + 1]
                    for k in range(8):
                        S.mm(pu[:, :], W1b[u][:, k, f * 128:(f + 1) * 128], hT[:, k, sblk * 512:(sblk + 1) * 512],
                             start=(k == 0), stop=(k == 7))
                    for k in range(8):
                        S.mm(pg3[:, :], W3b[u][:, k, f * 128:(f + 1) * 128], hT[:, k, sblk * 512:(sblk + 1) * 512],
                             start=(k == 0), stop=(k == 7))
                    s_ = su[f % 2]
                    S.act(s_[:], pu[:, :], AF.Silu)
                    S.tt(a[:, f, :], s_[:], pg3[:, :], ALU.mult)
                for tb4 in range(4):
                    tb = sblk * 4 + tb4
                    for hf in range(2):
                        py = g.ps[4 + ((tb4 * 2 + hf) % 4)]
                        for f in range(4):
                            S.mm(py[:, :], a[:, f, tb4 * 128:(tb4 + 1) * 128], W2b[u][:, f, hf * 512:(hf + 1) * 512],
                                 start=(f == 0), stop=(f == 3))
                        dst = acc[:, tb, hf * 512:(hf + 1) * 512]
                        if e == 0:
                            S.ts(dst, py[:, :], gate[:, tb, e:e + 1], None, ALU.mult)
                        else:
                            S.stt(dst, py[:, :], gate[:, tb, e:e + 1], dst, ALU.mult, ALU.add)
        xo = [sb(nc, st, "mxo%d" % i, [P, 512], F32) for i in range(2)]
        ev = 0
        for c in range(8):
            for sblk in range(4):
                ps = g.ps[ev % 4]
                x = xo[ev % 2]
                ev += 1
                tsl = slice(tok0 + sblk * 512, tok0 + (sblk + 1) * 512)
                S.dma(x[:], xin_d[c * 128:(c + 1) * 128, tsl])
                for tb4 in range(4):
                    S.transpose(ps[:, tb4 * 128:(tb4 + 1) * 128], acc[:, sblk * 4 + tb4, c * 128:(c + 1) * 128], g.ident[:])
                S.stt(x[:], ps[:, :], g.modT[:, 40 + c, b:b + 1], x[:], ALU.mult, ALU.add)
                S.dma(xout_d[c * 128:(c + 1) * 128, tsl], x[:])
        S.flush()


def build_full(shared_shapes, n_layers=2):
    nc = bass.Bass("TRN2", target_bir_lowering=False)
    g = G()
    g.dram = {}
    for k, shp in shared_shapes.items():
        g.dram[k] = nc.dram_tensor(k, list(shp), F32, kind="ExternalInput").ap()
    NT = NSEQ * T
    g.dram["xT"] = nc.dram_tensor("xT", [D, NT], F32, kind="ExternalInput").ap()
    g.dram["cT"] = nc.dram_tensor("cT", [D, 4], F32, kind="ExternalInput").ap()
    outT = nc.dram_tensor("outT", [D, NT], F32, kind="ExternalOutput").ap()
    X1 = nc.dram_tensor("X1", [D, NT], F32).ap()
    X2 = nc.dram_tensor("X2", [D, NT], F32).ap()
    ptm = nc.dram_tensor("ptm", [T, NTM], F32).ap()
    pfm = nc.dram_tensor("pfm", [NFM, T], F32).ap()
    ycat = nc.dram_tensor("ycat", [NSEQ, T, D], F32).ap()
    sops = nc.dram_tensor("sops", [5, 2, 2, T, 512], BF16).ap()
    sv = nc.dram_tensor("sv", [NSEQ, 256, T], F32).ap()
    sy = nc.dram_tensor("sy", [NSEQ, 256, T], F32).ap()
    sbg = nc.dram_tensor("sbg", [NSEQ, T, 512], F32).ap()
    with ExitStack() as st:
        S = Sched(nc)
        S.open(st)
        g.ps = [st.enter_context(nc.psum_tensor("ps%d" % i, [P, 512], F32)) for i in range(8)]
        setup_consts(S, nc, st, g)
        for l in range(n_layers):
            xin = g.dram["xT"] if l == 0 else X2
            xmid = X1
            xout = outT if l == n_layers - 1 else X2
            ph_ada(S, nc, g, l)
            for s in range(NSEQ):
                with ExitStack() as st2:
                    hT = sb(nc, st2, "hT", [P, 8, T + 4], BF16)
                    S.memset(hT[:, :, 0:1], 0.0)
                    emit_norm_mod(S, nc, g, xin, s * T, s, g.A1, g.modT[:, 0:8, :], hT)
                    ph_inproj(S, nc, g, l, hT, ptm, pfm)
                ph_sb(S, nc, g, l, ptm, pfm, ycat[s])
                ph_ml(S, nc, g, l, ptm, pfm, ycat[s])
                ph_dsa(S, nc, g, l, ptm, ycat[s])
                ph_rwkv_prep(S, nc, g, l, s, ptm, pfm, sops, sv, sbg)
            ph_rwkv_scan(S, nc, g, sops, sv, sy)
            for s in range(NSEQ):
                ph_rwkv_post(S, nc, g, l, s, sy, sbg, ycat[s])
                ph_outproj(S, nc, g, l, s, ycat[s], xin, xmid, s * T)
            for s in range(NSEQ):
                ph_moe(S, nc, g, l, s, xmid, xout, s * T)
        S.flush()
        g.n_inst = S.n_inst
    return nc, g


_CACHE = {}


def kernel(**inputs):
    inp = {k: np.asarray(v) for k, v in inputs.items()}
    sh = host_shared(inp)
    n_layers = inp["w_in"].shape[0]
    if "nc" not in _CACHE:
        _CACHE["nc"] = build_full({k: v.shape for k, v in sh.items()}, n_layers)
    nc, g = _CACHE["nc"]
    in_maps = []
    for core in range(8):
        d = dict(sh)
        d.update(host_core(inp, core))
        in_maps.append(d)
    res = run_bass_kernel_spmd(nc, in_maps, core_ids=list(range(8)))
    out = np.empty((32, T, D), np.float32)
    for core in range(8):
        oT = np.asarray(res.results[core]["outT"])
        out[core * NSEQ:(core + 1) * NSEQ] = oT.T.reshape(NSEQ, T, D)
    return out
```

```python
import numpy as np
import concourse.bass as bass
import concourse.mybir as mybir
from concourse.bass_utils import run_bass_kernel_spmd

F32 = mybir.dt.float32
BF16 = mybir.dt.bfloat16
AF = mybir.ActivationFunctionType
ALU = mybir.AluOpType
AX = mybir.AxisListType

COMPUTE = ("pe", "act", "dve", "pool")


class Sched:
    def __init__(self, nc, n_dma_sems=32):
        self.nc = nc
        self.n_dma = n_dma_sems
        self.sem = {}
        self.stack = None
        self.ops = []
        self.last_w = {}
        self.readers = {}
        self.count = {e: 0 for e in COMPUTE}
        self.dma_cnt = [0] * n_dma_sems
        self.dma_rr = 0
        self.known = {e: {} for e in COMPUTE + ("sp",)}
        self.phase_start = 0
        self.n_inst = 0

    def open(self, stack):
        nc = self.nc
        self.stack = stack
        self.n_sem_alloc = 0
        for e in COMPUTE:
            self.sem[e] = stack.enter_context(nc.semaphore("s_" + e))
        for i in range(self.n_dma):
            self.sem[("d", i)] = stack.enter_context(nc.semaphore("s_d%d" % i))

    @staticmethod
    def key(ap):
        return ap.name

    def add(self, eng, fn, reads, writes, dma=False):
        idx = len(self.ops)
        deps = set()
        wdeps = set()
        for k in reads:
            if k in self.last_w:
                deps.add(self.last_w[k])
        for k in writes:
            if k in self.last_w:
                deps.add(self.last_w[k])
            rd = self.readers.get(k)
            if rd:
                wdeps.update(rd.values())
        for k in writes:
            self.last_w[k] = idx
            self.readers[k] = {}
        for k in reads:
            self.readers.setdefault(k, {})[("dma", idx) if dma else eng] = idx
        deps.discard(idx)
        wdeps.discard(idx)
        wdeps -= deps
        self.ops.append(dict(eng=eng, fn=fn, deps=deps, wdeps=wdeps, dma=dma, sig=False, waits=None))
        return idx

    def flush(self, barrier=True):
        ops = self.ops
        lo = self.phase_start
        n = len(ops)
        def skip(o, od, d):
            if od["dma"] or o["dma"] or od["eng"] != o["eng"]:
                return False
            if o["eng"] == "pe":
                return True
            return d not in o["deps"]

        for i in range(lo, n):
            o = ops[i]
            for d in (o["deps"] | o["wdeps"]):
                od = ops[d]
                if d < lo or od["dma"] or skip(o, od, d):
                    continue
                od["sig"] = True
        last_of = {}
        for i in range(lo, n):
            last_of[ops[i]["eng"]] = i
        if barrier:
            for e, i in last_of.items():
                if e in COMPUTE:
                    ops[i]["sig"] = True
        for i in range(lo, n):
            o = ops[i]
            e = o["eng"]
            kn = self.known[e]
            waits = []
            for d in sorted(o["deps"] | o["wdeps"]):
                od = ops[d]
                if d < lo or skip(o, od, d):
                    continue
                s, v = od["done"]
                if kn.get(s, 0) < v:
                    waits.append((s, v))
                    for s2, v2 in od["clock"].items():
                        if kn.get(s2, 0) < v2:
                            kn[s2] = v2
            if o["dma"]:
                j = self.dma_rr
                self.dma_rr = (j + 1) % self.n_dma
                s = ("d", j)
                if kn.get(s, 0) < self.dma_cnt[j]:
                    waits.append((s, self.dma_cnt[j]))
                    kn[s] = self.dma_cnt[j]
                self.dma_cnt[j] += 16
                o["done"] = (s, self.dma_cnt[j])
                o["inc"] = (s, 16)
                clock = dict(kn)
                clock[s] = self.dma_cnt[j]
                o["clock"] = clock
            else:
                if o["sig"]:
                    self.count[e] += 1
                    o["done"] = (e, self.count[e])
                    o["inc"] = (e, 1)
                    clock = dict(kn)
                    clock[e] = self.count[e]
                    o["clock"] = clock
                else:
                    o["inc"] = None
            w = {}
            for s, v in waits:
                w[s] = max(w.get(s, 0), v)
            o["waits"] = list(w.items())
        final = {}
        if barrier:
            for e in COMPUTE:
                final[e] = self.count[e]
            for j in range(self.n_dma):
                final[("d", j)] = self.dma_cnt[j]
        nc = self.nc
        sem = self.sem
        by_eng = {}
        for i in range(lo, n):
            by_eng.setdefault(ops[i]["eng"], []).append(ops[i])

        def emit(ename):
            def body(e):
                for o in by_eng.get(ename, []):
                    for s, v in o["waits"]:
                        e.wait_ge(sem[s], v)
                        self.n_inst += 1
                    ins = o["fn"](e)
                    self.n_inst += 1
                    if o["inc"] is not None:
                        ins.then_inc(sem[o["inc"][0]], o["inc"][1])
                kn = self.known[ename]
                for s, v in final.items():
                    if kn.get(s, 0) < v:
                        e.wait_ge(sem[s], v)
                        kn[s] = v
            return body

        with nc.Block() as block:
            block.tensor(emit("pe"))
            block.scalar(emit("act"))
            block.vector(emit("dve"))
            block.gpsimd(emit("pool"))
            block.sync(emit("sp"))
        for i in range(lo, n):
            ops[i]["fn"] = None
            ops[i]["clock"] = None if i < n else None
        self.phase_start = n
        if barrier:
            self.last_w = {}
            self.readers = {}
            for e in COMPUTE:
                if self.count[e] > 20000:
                    self.n_sem_alloc += 1
                    self.sem[e] = self.stack.enter_context(nc.semaphore("s_%s_%d" % (e, self.n_sem_alloc)))
                    self.count[e] = 0
                    for kn in self.known.values():
                        kn.pop(e, None)
            for j in range(self.n_dma):
                if self.dma_cnt[j] > 20000:
                    self.n_sem_alloc += 1
                    self.sem[("d", j)] = self.stack.enter_context(nc.semaphore("s_d%d_%d" % (j, self.n_sem_alloc)))
                    self.dma_cnt[j] = 0
                    for kn in self.known.values():
                        kn.pop(("d", j), None)

    def _rw(self, outs, ins, rk, wk):
        r = list(rk) if rk is not None else [self.key(a) for a in ins if hasattr(a, "name") and a.space != "DRAM"]
        w = list(wk) if wk is not None else [self.key(a) for a in outs if a.space != "DRAM"]
        return r, w

    def mm(self, out, lhsT, rhs, start=True, stop=True, rk=None, wk=None):
        r, w = self._rw([out], [lhsT, rhs], rk, wk)
        return self.add("pe", lambda e: e.matmul(out, lhsT=lhsT, rhs=rhs, start=start, stop=stop), r, w)

    def transpose(self, out, in_, ident, rk=None, wk=None):
        r, w = self._rw([out], [in_, ident], rk, wk)
        return self.add("pe", lambda e: e.transpose(out, in_, ident), r, w)

    def act(self, out, in_, func, bias=None, scale=1.0, accum_out=None, rk=None, wk=None):
        ins = [in_] + ([bias] if hasattr(bias, "name") else []) + ([scale] if hasattr(scale, "name") else [])
        outs = [out] + ([accum_out] if accum_out is not None else [])
        r, w = self._rw(outs, ins, rk, wk)
        kw = {}
        if bias is not None:
            kw["bias"] = bias
        if accum_out is not None:
            kw["accum_out"] = accum_out
        return self.add("act", lambda e: e.activation(out, in_, func, scale=scale, **kw), r, w)

    def tt(self, out, in0, in1, op, eng="dve", rk=None, wk=None):
        r, w = self._rw([out], [in0, in1], rk, wk)
        return self.add(eng, lambda e: e.tensor_tensor(out, in0, in1, op), r, w)

    def ts(self, out, in0, s1, s2=None, op0=ALU.mult, op1=None, eng="dve", accum_out=None, rk=None, wk=None):
        ins = [in0] + [s for s in (s1, s2) if hasattr(s, "name")]
        outs = [out] + ([accum_out] if accum_out is not None else [])
        r, w = self._rw(outs, ins, rk, wk)
        kw = {}
        if op1 is not None:
            kw["op1"] = op1
        if accum_out is not None:
            kw["accum_out"] = accum_out
        return self.add(eng, lambda e: e.tensor_scalar(out, in0, s1, s2, op0, **kw), r, w)

    def stt(self, out, in0, scalar, in1, op0, op1, eng="dve", rk=None, wk=None):
        ins = [in0, in1] + ([scalar] if hasattr(scalar, "name") else [])
        r, w = self._rw([out], ins, rk, wk)
        return self.add(eng, lambda e: e.scalar_tensor_tensor(out, in0, scalar, in1, op0, op1), r, w)

    def copy(self, out, in_, eng="dve", rk=None, wk=None):
        r, w = self._rw([out], [in_], rk, wk)
        if eng == "act":
            return self.add("act", lambda e: e.activation(out, in_, AF.Copy), r, w)
        return self.add(eng, lambda e: e.tensor_copy(out, in_), r, w)

    def memset(self, out, val, eng="dve", wk=None):
        r, w = self._rw([out], [], None, wk)
        return self.add(eng, lambda e: e.memset(out, val), r, w)

    def reduce(self, out, in_, op, axis=AX.X, eng="dve", rk=None, wk=None):
        r, w = self._rw([out], [in_], rk, wk)
        return self.add(eng, lambda e: e.tensor_reduce(out, in_, axis, op), r, w)

    def recip(self, out, in_, rk=None, wk=None):
        r, w = self._rw([out], [in_], rk, wk)
        return self.add("dve", lambda e: e.reciprocal(out, in_), r, w)

    def scan(self, out, d0, d1, init, op0, op1, eng="dve", rk=None, wk=None):
        r, w = self._rw([out], [d0, d1], rk, wk)
        return self.add(eng, lambda e: e.tensor_tensor_scan(out, d0, d1, init, op0, op1), r, w)

    def max8(self, out, in_, rk=None, wk=None):
        r, w = self._rw([out], [in_], rk, wk)
        return self.add("dve", lambda e: e.max(out, in_), r, w)

    def match_replace(self, out, rep, vals, imm, rk=None, wk=None):
        r, w = self._rw([out], [rep, vals], rk, wk)
        return self.add("dve", lambda e: e.match_replace(out, rep, vals, imm), r, w)

    def dma(self, out, in_, rk=None, wk=None, **kw):
        r, w = self._rw([out], [in_], rk, wk)
        return self.add("sp", lambda e: e.dma_start(out, in_, **kw), r, w, dma=True)


from contextlib import ExitStack

P = 128
T = 2048
D = 1024
NSEQ = 4
NTM = 2084
NFM = 1416
TM_AR, TM_AK, TM_AV, TM_BV, TM_CV, TM_CO, TM_DQ, TM_DK, TM_DV, TM_DQI, TM_DKI, TM_DWI = (
    0, 256, 512, 768, 1024, 1280, 1536, 1792, 1856, 1920, 2048, 2080)
FM_AV, FM_AWD, FM_AAD, FM_AGD, FM_BQ, FM_BK, FM_CQ, FM_CK, FM_CIG, FM_CFG = (
    0, 256, 288, 320, 384, 640, 896, 1152, 1408, 1412)
NEG = -1.0e30
_uid = [0]


def uname(s):
    _uid[0] += 1
    return "%s_%d" % (s, _uid[0])


def sb(nc, st, name, shape, dt):
    return st.enter_context(nc.sbuf_tensor(uname(name), list(shape), dt))


def bc(ap, shape):
    return ap.to_broadcast(list(shape))


class G:
    pass


def host_consts():
    c = {}
    i = np.arange(128)
    c["ident"] = np.eye(128, dtype=np.float32)
    c["ones"] = np.ones((128, 128), np.float32)
    c["tri_lt"] = (i[:, None] < i[None, :]).astype(np.float32)
    c["tri_le"] = (i[:, None] <= i[None, :]).astype(np.float32)
    c["tri_gt"] = (i[:, None] > i[None, :]).astype(np.float32)
    c["cbias"] = np.where(i[None, :] <= i[:, None], 0.0, NEG).astype(np.float32)
    half = 32
    inv = 10000.0 ** (-np.arange(half, dtype=np.float32) / half)
    ang = np.arange(T, dtype=np.float32)[:, None] * inv[None, :]
    c["rope64"] = np.concatenate([np.cos(ang), np.sin(ang)], 1).astype(np.float32)
    half = 16
    inv = 10000.0 ** (-np.arange(half, dtype=np.float32) / half)
    ang = np.arange(T, dtype=np.float32)[:, None] * inv[None, :]
    c["rope32"] = np.concatenate([np.cos(ang), np.sin(ang)], 1).astype(np.float32)
    c["lebias"] = np.where(i[:, None] <= i[None, :], 0.0, NEG).astype(np.float32)
    selh = np.zeros((4, 4, 128), np.float32)
    for h in range(4):
        selh[h, h, :] = 1.0
    c["selh"] = selh.reshape(4, 512)
    return c


def load_const(S, nc, st, g, name, shape, dt=F32):
    t = sb(nc, st, "c_" + name, shape, F32)
    S.dma(t[:], g.dram[name])
    if dt == F32:
        return t
    tb = sb(nc, st, "cb_" + name, shape, dt)
    S.copy(tb[:], t[:])
    return tb


def ph_ada(S, nc, g, l):
    with ExitStack() as st:
        cT = sb(nc, st, "cT", [P, 8, 4], F32)
        S.dma(cT[:], g.dram["cT"].rearrange("(c p) b -> p c b", p=P))
        cact = sb(nc, st, "cact", [P, 8, 4], F32)
        S.act(cact[:], cT[:], AF.Silu)
        bias = sb(nc, st, "adab", [P, 48], F32)
        S.dma(bias[:], g.dram["ada_b_fm"][l])
        g1 = sb(nc, st, "g1", [P, 8], F32)
        g2 = sb(nc, st, "g2", [P, 8], F32)
        S.dma(g1[:], g.dram["norm1_g_fm"][l])
        S.dma(g2[:], g.dram["norm2_g_fm"][l])
        wts = [sb(nc, st, "adaw%d" % i, [P, 8, 768], F32) for i in range(2)]
        ps = g.ps[0]
        for cb in range(8):
            wt = wts[cb % 2]
            S.dma(wt[:], g.dram["ada_w"][l][:, cb * 768:(cb + 1) * 768].rearrange("(c p) n -> p c n", p=P))
            for cc in range(6):
                c = cb * 6 + cc
                for k in range(8):
                    S.mm(ps[:, 4 * c:4 * c + 4], wt[:, k, cc * 128:(cc + 1) * 128], cact[:, k, :],
                         start=(k == 0), stop=(k == 7))
        S.tt(g.modT[:], ps[:, 0:192].rearrange("p (c b) -> p c b", b=4),
             bc(bias[:].unsqueeze(2), [P, 48, 4]), ALU.add)
        for (A, gg, off) in ((g.A1, g1, 8), (g.A2, g2, 32)):
            S.ts(A[:], g.modT[:, off:off + 8, :], 1.0, None, ALU.add)
            S.tt(A[:], A[:], bc(gg[:].unsqueeze(2), [P, 8, 4]), ALU.mult)
        S.flush()


def emit_norm_mod(S, nc, g, xT_d, tok0, b, A, shift, hT, col0=1, route=None):
    with ExitStack() as st:
        xs = [sb(nc, st, "xs%d" % i, [P, 8, 512], F32) for i in range(2)]
        sq = sb(nc, st, "sq", [P, 8, 512], F32)
        tmp = sb(nc, st, "tmp", [P, 8, 512], F32)
        rstd = sb(nc, st, "rstd", [P, 512], F32)
        ps = g.ps[1]
        for sblk in range(4):
            x = xs[sblk % 2]
            S.dma(x[:], xT_d[:, tok0 + sblk * 512: tok0 + (sblk + 1) * 512].rearrange("(c p) n -> p c n", p=P))
            S.act(sq[:], x[:], AF.Square)
            for c in range(8):
                S.mm(ps[:, :], g.ones[:], sq[:, c, :], start=(c == 0), stop=(c == 7))
            S.act(rstd[:], ps[:, :], AF.Sqrt, scale=1.0 / D, bias=g.eps6[:, 0:1])
            S.recip(rstd[:], rstd[:])
            S.tt(tmp[:], x[:], bc(rstd[:].unsqueeze(1), [P, 8, 512]), ALU.mult)
            for c in range(8):
                S.act(hT[:, c, col0 + sblk * 512: col0 + (sblk + 1) * 512], tmp[:, c, :], AF.Identity,
                      scale=A[:, c, b:b + 1], bias=shift[:, c, b:b + 1])
            if route is not None:
                wge, lg = route
                for c in range(8):
                    S.act(sq[:, c, :], tmp[:, c, :], AF.Identity, scale=A[:, c, b:b + 1], bias=shift[:, c, b:b + 1])
                for tb4 in range(4):
                    pr = g.ps[2 + (tb4 % 2)]
                    for c in range(8):
                        S.mm(pr[:, 0:36], sq[:, c, tb4 * 128:(tb4 + 1) * 128], wge[:, c, :], start=(c == 0), stop=(c == 7))
                    S.copy(lg[:, sblk * 4 + tb4, :], pr[:, 0:36])
        S.flush()


def load_weights_bf16(S, nc, st, g, w_d, ncols, nshift, mu_d, Wb, W0b):
    stg = [sb(nc, st, "wstg%d" % i, [P, 8, 512], F32) for i in range(2)]
    mu_b = sb(nc, st, "mu_b", [P, max(nshift, 1)], F32)
    tmp = sb(nc, st, "wtmp", [P, 8, 512], F32)
    if nshift:
        S.dma(mu_b[:], mu_d.partition_broadcast(P))
    i = 0
    for c0 in range(0, ncols, 512):
        w = min(512, ncols - c0)
        s_ = stg[i % 2]
        i += 1
        S.dma(s_[:, :, :w], w_d[:, c0:c0 + w].rearrange("(c p) n -> p c n", p=P))
        if c0 < nshift:
            ws = min(w, nshift - c0)
            S.tt(tmp[:, :, :ws], s_[:, :, :ws], bc(mu_b[:, c0:c0 + ws].unsqueeze(1), [P, 8, ws]), ALU.mult, eng="pool")
            S.copy(W0b[:, :, c0:c0 + ws], tmp[:, :, :ws], eng="act")
            S.tt(Wb[:, :, c0:c0 + ws], s_[:, :, :ws], tmp[:, :, :ws], ALU.subtract)
            if ws < w:
                S.copy(Wb[:, :, c0 + ws:c0 + w], s_[:, :, ws:w], eng="act")
        else:
            S.copy(Wb[:, :, c0:c0 + w], s_[:, :, :w], eng=("act" if (i % 2) else "dve"))


def ph_inproj(S, nc, g, l, hT, ptm_d, pfm_d):
    with ExitStack() as st:
        Wb = sb(nc, st, "Wb", [P, 8, NTM], BF16)
        W0b = sb(nc, st, "W0b", [P, 8, 768], BF16)
        load_weights_bf16(S, nc, st, g, g.dram["w_tm"][l], NTM, 768, g.dram["mu_tm"][l], Wb, W0b)
        stg = [sb(nc, st, "ptm_stg%d" % i, [P, NTM], F32) for i in range(2)]
        ev = 0
        for tb in range(16):
            so = stg[tb % 2]
            for c0 in range(0, NTM, 512):
                w = min(512, NTM - c0)
                ps = g.ps[2 + (ev % 4)]
                shifted = c0 < 768
                for k in range(8):
                    S.mm(ps[:, :w], hT[:, k, 1 + tb * 128: 1 + (tb + 1) * 128], Wb[:, k, c0:c0 + w],
                         start=(k == 0), stop=(k == 7 and not shifted))
                if shifted:
                    ws = min(w, 768 - c0)
                    for k in range(8):
                        S.mm(ps[:, :ws], hT[:, k, tb * 128:(tb + 1) * 128], W0b[:, k, c0:c0 + ws],
                             start=False, stop=(k == 7))
                S.copy(so[:, c0:c0 + w], ps[:, :w], eng=("act" if ev % 2 else "dve"))
                ev += 1
            S.dma(ptm_d[tb * 128:(tb + 1) * 128, :], so[:])
        S.flush()
    with ExitStack() as st:
        Wb = sb(nc, st, "Wf", [P, 8, NFM], BF16)
        W0b = sb(nc, st, "W0f", [P, 8, 384], BF16)
        load_weights_bf16(S, nc, st, g, g.dram["w_fm"][l], NFM, 384, g.dram["mu_fm"][l], Wb, W0b)
        stg = [sb(nc, st, "pfm_stg%d" % i, [P, T], F32) for i in range(2)]
        ev = 0
        ci = 0
        for r0 in range(0, NFM, 128):
            m = min(128, NFM - r0)
            so = stg[ci % 2]
            ci += 1
            shifted = r0 < 384
            for sblk in range(4):
                ps = g.ps[2 + (ev % 4)]
                for k in range(8):
                    S.mm(ps[:m, :], Wb[:, k, r0:r0 + m], hT[:, k, 1 + sblk * 512: 1 + (sblk + 1) * 512],
                         start=(k == 0), stop=(k == 7 and not shifted))
                if shifted:
                    for k in range(8):
                        S.mm(ps[:m, :], W0b[:, k, r0:r0 + m], hT[:, k, sblk * 512:(sblk + 1) * 512],
                             start=False, stop=(k == 7))
                S.copy(so[:m, sblk * 512:(sblk + 1) * 512], ps[:m, :], eng=("act" if ev % 2 else "dve"))
                ev += 1
            S.dma(pfm_d[r0:r0 + m, :], so[:m, :])
        S.flush()


def _r(a, b):
    return list(range(a, b))


TM_COLS = (_r(0, 256) + _r(256, 512) + _r(512, 768) + _r(1408, 1664) + _r(2176, 2432) + _r(2432, 2688)
           + _r(2696, 2952) + _r(2952, 3016) + _r(3016, 3080) + _r(3080, 3208) + _r(3208, 3240) + _r(3240, 3244))
FM_COLS = (_r(512, 768) + _r(768, 800) + _r(800, 832) + _r(832, 896) + _r(896, 1152) + _r(1152, 1408)
           + _r(1664, 1920) + _r(1920, 2176) + _r(2688, 2692) + _r(2692, 2696))
assert len(TM_COLS) == NTM and len(FM_COLS) == NFM


def host_shared(inp):
    f = lambda a: np.ascontiguousarray(a, dtype=np.float32)
    L = inp["w_in"].shape[0]
    sh = dict(host_consts())
    sh["ada_w"] = f(inp["ada_w"])
    sh["ada_b_fm"] = f(inp["ada_b"].reshape(L, 48, 128).transpose(0, 2, 1))
    sh["norm1_g_fm"] = f(inp["norm1_g"].reshape(L, 8, 128).transpose(0, 2, 1))
    sh["norm2_g_fm"] = f(inp["norm2_g"].reshape(L, 8, 128).transpose(0, 2, 1))
    sh["w_tm"] = f(inp["w_in"][:, :, TM_COLS])
    sh["w_fm"] = f(inp["w_in"][:, :, FM_COLS])
    sh["mu_tm"] = f(inp["rk_mu"][:, TM_COLS[:768]])
    sh["mu_fm"] = f(inp["rk_mu"][:, FM_COLS[:384]])
    sh["conv_w_fm"] = f(inp["ml_conv_w"].reshape(L, 4, 4, 128).transpose(0, 3, 2, 1))
    sh["conv_b_fm"] = f(inp["ml_conv_b"].reshape(L, 4, 128).transpose(0, 2, 1))
    sh["moe_wge"] = f(np.concatenate([inp["moe_wg"], inp["moe_we"]], -1))
    sh["moe_bge"] = f(np.concatenate([inp["moe_bg"], inp["moe_be"]], -1))
    for k in ("moe_w1", "moe_w3", "moe_w2"):
        sh[k] = f(inp[k])
    for k in ("rk_w0", "rk_w2", "rk_a0", "rk_a2", "rk_g2", "rk_kk", "rk_ka", "rk_rk", "rk_ln_g", "rk_ln_b",
              "sb_norm_g", "ml_norm_g", "ds_qn_g", "ds_kn_g", "ds_out_g", "w_out", "ml_ig_b", "ml_fg_b"):
        sh[k] = f(inp[k])
    return sh


def host_core(inp, core, nseq=NSEQ):
    f = lambda a: np.ascontiguousarray(a, dtype=np.float32)
    x = inp["x"][core * nseq:(core + 1) * nseq]
    d = {}
    d["xT"] = f(x.reshape(nseq * T, D).T)
    cT = np.zeros((D, 4), np.float32)
    cT[:, :nseq] = inp["c"][core * nseq:(core + 1) * nseq].T
    d["cT"] = cT
    return d


def load_row_bcast(S, nc, st, name, row_ap, n):
    t = sb(nc, st, name, [P, n], F32)
    S.dma(t[:], row_ap.partition_broadcast(P))
    return t


def tm_head_rmsnorm(S, nc, st, g, y, nb, gain, eps, per_head_gain=True):
    nh = nb * 4
    yv = y[:].rearrange("p b (h d) -> p (b h) d", d=64)
    sq = sb(nc, st, "rn_sq", [P, nh, 64], F32)
    ss = sb(nc, st, "rn_ss", [P, nh], F32)
    S.tt(sq[:], yv, yv, ALU.mult)
    S.reduce(ss[:], sq[:], ALU.add, AX.X)
    S.act(ss[:], ss[:], AF.Sqrt, scale=1.0 / 64, bias=eps[:, 0:1])
    S.recip(ss[:], ss[:])
    S.tt(yv, yv, bc(ss[:].unsqueeze(2), [P, nh, 64]), ALU.mult)
    if per_head_gain:
        S.tt(y[:], y[:], bc(gain[:].unsqueeze(1), [P, nb, 256]), ALU.mult)
    else:
        S.tt(yv, yv, bc(gain[:, 0:64].unsqueeze(1), [P, nh, 64]), ALU.mult)


def ph_sb(S, nc, g, l, ptm_d, pfm_d, ycat_d):
    with ExitStack() as st:
        q16 = sb(nc, st, "sbq", [P, 2, T], BF16)
        k16 = sb(nc, st, "sbk", [P, 2, T], BF16)
        v16 = sb(nc, st, "sbv", [P, 16, 256], BF16)
        yraw = sb(nc, st, "sby", [P, 16, 256], F32)
        gain = load_row_bcast(S, nc, st, "sbg", g.dram["sb_norm_g"][l], 256)
        with ExitStack() as st2:
            qf = sb(nc, st2, "sbqf", [P, 2, T], F32)
            kf = sb(nc, st2, "sbkf", [P, 2, T], F32)
            vf = sb(nc, st2, "sbvf", [P, 16, 256], F32)
            S.dma(qf[:], pfm_d[FM_BQ:FM_BQ + 256, :].rearrange("(c p) t -> p c t", p=P))
            S.dma(kf[:], pfm_d[FM_BK:FM_BK + 256, :].rearrange("(c p) t -> p c t", p=P))
            S.dma(vf[:], ptm_d[:, TM_BV:TM_BV + 256].rearrange("(b p) n -> p b n", p=P))
            S.copy(q16[:], qf[:], eng="act")
            S.copy(k16[:], kf[:], eng="dve")
            S.copy(v16[:], vf[:], eng="pool")
            S.flush()
        e1 = [sb(nc, st, "sbe%d" % i, [P, 512], F32) for i in range(2)]
        lt = [sb(nc, st, "sbl%d" % i, [P, 512], F32) for i in range(2)]
        Lm = [sb(nc, st, "sbL%d" % i, [P, 512], F32) for i in range(2)]
        aa = [sb(nc, st, "sba%d" % i, [P, 512], F32) for i in range(2)]
        attA = [sb(nc, st, "sbt%d" % i, [P, 16, 512], BF16) for i in range(2)]
        TotB = sb(nc, st, "sbT", [P, 512], F32)
        it = 0
        for h in range(4):
            c = h // 2
            pb = (h % 2) * 64
            for I in range(4):
                po = g.ps[6 + (I % 2)]
                att_all = attA[(h * 4 + I) % 2]
                S.memset(TotB[:], 0.0, eng="pool")
                for j in range(4 * I + 3, -1, -1):
                    u = it % 2
                    it += 1
                    pz, pr, pt = g.ps[u], g.ps[2 + u], g.ps[4 + u]
                    d = j - 4 * I
                    dd = max(d, 0)
                    c0 = dd * 128
                    n = 512 - c0
                    q0 = I * 512 + c0
                    S.mm(pz[:, :n], k16[pb:pb + 64, c, j * 128:(j + 1) * 128], q16[pb:pb + 64, c, q0:q0 + n])
                    S.act(e1[u][:, :n], pz[:, :n], AF.Exp, scale=-0.125)
                    S.act(lt[u][:, :n], e1[u][:, :n], AF.Ln, bias=g.one1[:, 0:1])
                    S.stt(Lm[u][:, :n], pz[:, :n], -0.125, lt[u][:, :n], ALU.mult, ALU.subtract)
                    if d >= 0:
                        S.tt(Lm[u][:, 0:128], Lm[u][:, 0:128], g.tri_lt[:], ALU.mult)
                    S.mm(pr[:, :n], g.tri_gt[:], Lm[u][:, :n])
                    S.tt(aa[u][:, :n], pr[:, :n], TotB[:, c0:512], ALU.add)
                    S.tt(aa[u][:, :n], aa[u][:, :n], lt[u][:, :n], ALU.subtract, eng="pool")
                    S.act(att_all[:, j, c0:512], aa[u][:, :n], AF.Exp)
                    if d >= 0:
                        S.tt(att_all[:, j, c0:c0 + 128], att_all[:, j, c0:c0 + 128], g.tri_lt16[:], ALU.mult, eng="pool")
                    if j > 0:
                        S.mm(pt[:, :n], g.ones[:], Lm[u][:, :n])
                        S.tt(TotB[:, c0:512], TotB[:, c0:512], pt[:, :n], ALU.add)
                for qb in range(4):
                    for j in range(4 * I + qb, -1, -1):
                        S.mm(po[:, qb * 64:(qb + 1) * 64], att_all[:, j, qb * 128:(qb + 1) * 128],
                             v16[:, j, h * 64:(h + 1) * 64], start=(j == 4 * I + qb), stop=(j == 0))
                S.copy(yraw[:, 4 * I:4 * I + 4, h * 64:(h + 1) * 64],
                       po[:, 0:256].rearrange("p (b d) -> p b d", d=64), eng="act")
        if getattr(g, "debug", False):
            S.dma(ycat_d[:, 0:256].rearrange("(b p) n -> p b n", p=P), yraw[:])
        tm_head_rmsnorm(S, nc, st, g, yraw, 16, gain, g.eps6)
        S.dma(ycat_d[:, 256:512].rearrange("(b p) n -> p b n", p=P), yraw[:])
        S.flush()


def setup_consts(S, nc, st, g):
    def ld(name, shape, src=None):
        t = sb(nc, st, "k_" + name, shape, F32)
        S.dma(t[:], g.dram[src or name])
        return t
    g.ones = ld("ones", [P, P])
    g.ident = ld("ident", [P, P])
    g.tri_lt = ld("tri_lt", [P, P])
    g.tri_le = ld("tri_le", [P, P])
    g.tri_gt = ld("tri_gt", [P, P])
    g.cbias = ld("cbias", [P, P])
    g.lebias = ld("lebias", [P, P])
    g.selh = sb(nc, st, "k_selh", [4, 4, P], F32)
    S.dma(g.selh[:], g.dram["selh"].rearrange("k (h m) -> k h m", m=P))
    g.tri_lt16 = sb(nc, st, "k_tri_lt16", [P, P], BF16)
    g.tri_le16 = sb(nc, st, "k_tri_le16", [P, P], BF16)
    g.ident16 = sb(nc, st, "k_ident16", [P, P], BF16)
    g.ones16 = sb(nc, st, "k_ones16", [P, P], BF16)
    S.copy(g.tri_lt16[:], g.tri_lt[:])
    S.copy(g.tri_le16[:], g.tri_le[:])
    S.copy(g.ident16[:], g.ident[:])
    S.copy(g.ones16[:], g.ones[:])
    g.eps6 = sb(nc, st, "k_eps6", [P, 1], F32)
    S.memset(g.eps6[:], 1e-6)
    g.one1 = sb(nc, st, "k_one1", [P, 1], F32)
    S.memset(g.one1[:], 1.0)
    g.modT = sb(nc, st, "modT", [P, 48, 4], F32)
    g.A1 = sb(nc, st, "A1", [P, 8, 4], F32)
    g.A2 = sb(nc, st, "A2", [P, 8, 4], F32)
    S.flush()


def ph_ml(S, nc, g, l, ptm_d, pfm_d, ycat_d):
    LN8 = float(np.log(0.125))
    with ExitStack() as st:
        q16 = sb(nc, st, "mlq", [P, 2, T], BF16)
        k16 = sb(nc, st, "mlk", [P, 2, T], BF16)
        v16 = sb(nc, st, "mlv", [P, 16, 4, 65], BF16)
        osig = sb(nc, st, "mlo", [P, 16, 256], F32)
        BtB = [sb(nc, st, "mlB%d" % h, [P, T], F32) for h in range(4)]
        c_tm = sb(nc, st, "mlc", [P, 16, 4], F32)
        gain = load_row_bcast(S, nc, st, "mlg", g.dram["ml_norm_g"][l], 256)
        with ExitStack() as st2:
            xq = sb(nc, st2, "mlxq", [P, 2, T + 3], F32)
            xk = sb(nc, st2, "mlxk", [P, 2, T + 3], F32)
            S.memset(xq[:, :, 0:3], 0.0)
            S.memset(xk[:, :, 0:3], 0.0)
            S.dma(xq[:, :, 3:T + 3], pfm_d[FM_CQ:FM_CQ + 256, :].rearrange("(c p) t -> p c t", p=P))
            S.dma(xk[:, :, 3:T + 3], pfm_d[FM_CK:FM_CK + 256, :].rearrange("(c p) t -> p c t", p=P))
            cw = sb(nc, st2, "mlcw", [P, 4, 4], F32)
            cb = sb(nc, st2, "mlcb", [P, 4], F32)
            S.dma(cw[:], g.dram["conv_w_fm"][l])
            S.dma(cb[:], g.dram["conv_b_fm"][l])
            acc = [sb(nc, st2, "mlacc%d" % i, [P, T], F32) for i in range(2)]
            ai = 0
            for (x, dst, ci0) in ((xq, q16, 0), (xk, k16, 2)):
                for c in range(2):
                    a = acc[ai % 2]
                    eng = "dve"
                    ai += 1
                    S.ts(a[:], x[:, c, 0:T], cw[:, ci0 + c, 0:1], None, ALU.mult, eng=eng)
                    for tap in range(1, 4):
                        S.stt(a[:], x[:, c, tap:T + tap], cw[:, ci0 + c, tap:tap + 1], a[:], ALU.mult, ALU.add, eng=eng)
                    S.act(dst[:, c, :], a[:], AF.Silu, bias=cb[:, ci0 + c:ci0 + c + 1])
            ig = sb(nc, st2, "mlig", [4, T], F32)
            fg = sb(nc, st2, "mlfg", [4, T], F32)
            S.dma(ig[:], pfm_d[FM_CIG:FM_CIG + 4, :])
            S.dma(fg[:], pfm_d[FM_CFG:FM_CFG + 4, :])
            gb = sb(nc, st2, "mlgb", [4, 2], F32)
            S.dma(gb[:, 0:1], g.dram["ml_ig_b"][l].rearrange("(h o) -> h o", o=1))
            S.dma(gb[:, 1:2], g.dram["ml_fg_b"][l].rearrange("(h o) -> h o", o=1))
            S.ts(gb[:], gb[:], 1.0 / 15.0, None, ALU.mult)
            S.act(ig[:], ig[:], AF.Tanh, scale=1.0 / 15.0, bias=gb[:, 0:1])
            S.act(fg[:], fg[:], AF.Tanh, scale=1.0 / 15.0, bias=gb[:, 1:2])
            S.act(fg[:], fg[:], AF.Exp, scale=-15.0)
            S.act(fg[:], fg[:], AF.Ln, bias=g.one1[0:4, 0:1])
            ones4 = sb(nc, st2, "mlones", [4, T], F32)
            S.memset(ones4[:], 1.0)
            Bn = sb(nc, st2, "mlBn", [4, T], F32)
            S.scan(Bn[:], ones4[:], fg[:], 0.0, ALU.mult, ALU.add)
            cT = sb(nc, st2, "mlcT", [4, T], F32)
            S.stt(cT[:], ig[:], 15.0, Bn[:], ALU.mult, ALU.add)
            S.ts(cT[:], cT[:], LN8, None, ALU.add)
            BT = sb(nc, st2, "mlBT", [4, T], F32)
            S.ts(BT[:], Bn[:], -1.0, None, ALU.mult)
            ev = 0
            for h in range(4):
                for sblk in range(4):
                    ps = g.ps[ev % 4]
                    S.mm(ps[:, :], g.selh[0:4, h, :], BT[0:4, sblk * 512:(sblk + 1) * 512])
                    S.copy(BtB[h][:, sblk * 512:(sblk + 1) * 512], ps[:, :], eng=("act" if ev % 2 else "dve"))
                    ev += 1
            pc = g.ps[4]
            for b in range(16):
                S.mm(pc[:, b * 4:(b + 1) * 4], cT[0:4, b * 128:(b + 1) * 128], g.ident[0:4, 0:4])
            S.copy(c_tm[:], pc[:, 0:64].rearrange("p (b h) -> p b h", h=4))
            vf = sb(nc, st2, "mlvf", [P, 16, 256], F32)
            S.dma(vf[:], ptm_d[:, TM_CV:TM_CV + 256].rearrange("(b p) n -> p b n", p=P))
            S.copy(v16[:, :, :, 0:64], vf[:].rearrange("p b (h d) -> p b h d", d=64), eng="pool")
            S.memset(v16[:, :, :, 64:65], 1.0, eng="pool")
            S.dma(osig[:], ptm_d[:, TM_CO:TM_CO + 256].rearrange("(b p) n -> p b n", p=P))
            S.act(osig[:], osig[:], AF.Sigmoid)
            S.flush()
        attA = [sb(nc, st, "mlt%d" % i, [P, 16, 512], BF16) for i in range(2)]
        Dm = [sb(nc, st, "mlD%d" % i, [P, 512], F32) for i in range(2)]
        dtmp = [sb(nc, st, "mldt%d" % i, [P, 128], F32) for i in range(2)]
        nd = [sb(nc, st, "mlnd%d" % i, [P, 4, 65], F32) for i in range(2)]
        dn = [sb(nc, st, "mldn%d" % i, [P, 4], F32) for i in range(2)]
        hraw = sb(nc, st, "mlh", [P, 16, 256], F32)
        it = 0
        for h in range(4):
            c = h // 2
            pb = (h % 2) * 64
            for I in range(4):
                gi = h * 4 + I
                po = g.ps[6 + (gi % 2)]
                att_all = attA[gi % 2]
                for j in range(4 * I + 3, -1, -1):
                    u = it % 2
                    it += 1
                    pz = g.ps[u]
                    d = j - 4 * I
                    dd = max(d, 0)
                    c0 = dd * 128
                    n = 512 - c0
                    q0 = I * 512 + c0
                    S.mm(pz[:, :n], k16[pb:pb + 64, c, j * 128:(j + 1) * 128], q16[pb:pb + 64, c, q0:q0 + n])
                    if d >= 0:
                        S.tt(dtmp[u][:], BtB[h][:, q0:q0 + 128], g.lebias[:], ALU.add, eng="pool")
                        S.act(Dm[u][:, 0:128], dtmp[u][:], AF.Exp, bias=c_tm[:, j, h:h + 1])
                        if n > 128:
                            S.act(Dm[u][:, 128:n], BtB[h][:, q0 + 128:q0 + n], AF.Exp, bias=c_tm[:, j, h:h + 1])
                    else:
                        S.act(Dm[u][:, :n], BtB[h][:, q0:q0 + n], AF.Exp, bias=c_tm[:, j, h:h + 1])
                    S.tt(att_all[:, j, c0:512], pz[:, :n], Dm[u][:, :n], ALU.mult)
                for qb in range(4):
                    for j in range(4 * I + qb, -1, -1):
                        S.mm(po[:, qb * 65:(qb + 1) * 65], att_all[:, j, qb * 128:(qb + 1) * 128],
                             v16[:, j, h, :], start=(j == 4 * I + qb), stop=(j == 0))
                u2 = gi % 2
                S.copy(nd[u2][:], po[:, 0:260].rearrange("p (b d) -> p b d", d=65), eng="act")
                S.stt(dn[u2][:], nd[u2][:, :, 64], -1.0, nd[u2][:, :, 64], ALU.mult, ALU.max)
                S.ts(dn[u2][:], dn[u2][:], 1.0, None, ALU.max)
                S.recip(dn[u2][:], dn[u2][:])
                S.tt(hraw[:, 4 * I:4 * I + 4, h * 64:(h + 1) * 64], nd[u2][:, :, 0:64],
                     bc(dn[u2][:].unsqueeze(2), [P, 4, 64]), ALU.mult)
        tm_head_rmsnorm(S, nc, st, g, hraw, 16, gain, g.eps6)
        S.tt(hraw[:], hraw[:], osig[:], ALU.mult)
        S.dma(ycat_d[:, 512:768].rearrange("(b p) n -> p b n", p=P), hraw[:])
        S.flush()


def _rope_tm(S, out, x, cos, sin, t1, t2, half):
    x1, x2 = x[:, :, 0:half], x[:, :, half:2 * half]
    S.tt(t1, x1, cos, ALU.mult)
    S.tt(t2, x2, sin, ALU.mult)
    S.tt(out[:, :, 0:half], t1, t2, ALU.subtract)
    S.tt(t1, x2, cos, ALU.mult)
    S.tt(t2, x1, sin, ALU.mult)
    S.tt(out[:, :, half:2 * half], t1, t2, ALU.add)


def ph_dsa(S, nc, g, l, ptm_d, ycat_d):
    WI_SCALE = float(4 ** -0.5 * 32 ** -0.5)
    with ExitStack() as st:
        qT = sb(nc, st, "dqT", [P, 2, T], BF16)
        kT2 = sb(nc, st, "dkT", [P, T], BF16)
        qiT = sb(nc, st, "dqi", [P, T], F32)
        kiX = [sb(nc, st, "dki%d" % h, [P, T], F32) for h in range(4)]
        wi = sb(nc, st, "dwi", [P, 16, 4], F32)
        v16 = sb(nc, st, "dv", [P, 16, 65], BF16)
        gain = load_row_bcast(S, nc, st, "dg", g.dram["ds_out_g"][l], 256)
        with ExitStack() as st2:
            rope64 = sb(nc, st2, "rope64", [P, 16, 64], F32)
            rope32 = sb(nc, st2, "rope32", [P, 16, 32], F32)
            S.dma(rope64[:], g.dram["rope64"].rearrange("(b p) n -> p b n", p=P))
            S.dma(rope32[:], g.dram["rope32"].rearrange("(b p) n -> p b n", p=P))
            gq = load_row_bcast(S, nc, st2, "dgq", g.dram["ds_qn_g"][l], 64)
            gk = load_row_bcast(S, nc, st2, "dgk", g.dram["ds_kn_g"][l], 64)
            xs = [sb(nc, st2, "dx%d" % i, [P, 548], F32) for i in range(2)]
            sq = sb(nc, st2, "dsq", [P, 5, 64], F32)
            ss = sb(nc, st2, "dss", [P, 5], F32)
            qn = sb(nc, st2, "dqn", [P, 5, 64], F32)
            qr = [sb(nc, st2, "dqr%d" % i, [P, 6, 64], F32) for i in range(2)]
            qir = [sb(nc, st2, "dqir%d" % i, [P, 5, 32], F32) for i in range(2)]
            t1 = sb(nc, st2, "dt1", [P, 5, 32], F32)
            t2 = sb(nc, st2, "dt2", [P, 5, 32], F32)
            t3 = sb(nc, st2, "dt3", [P, 5, 16], F32)
            t4 = sb(nc, st2, "dt4", [P, 5, 16], F32)
            kiz = [[sb(nc, st2, "dkz%d_%d" % (i, h), [P, 128], F32) for h in range(4)] for i in range(2)]
            for i in range(2):
                for h in range(4):
                    S.memset(kiz[i][h][:], 0.0, eng="pool")
            S.dma(wi[:], ptm_d[:, TM_DWI:TM_DWI + 4].rearrange("(b p) n -> p b n", p=P))
            S.ts(wi[:], wi[:], WI_SCALE, None, ALU.mult)
            ev = 0
            cut = getattr(g, 'dsa_cut', 0)
            for b in range(16):
                x = xs[b % 2]
                S.dma(x[:], ptm_d[b * 128:(b + 1) * 128, TM_DQ:TM_DQ + 548])
                qk = x[:, 0:320].rearrange("p (h d) -> p h d", d=64)
                S.tt(sq[:], qk, qk, ALU.mult)
                S.reduce(ss[:], sq[:], ALU.add, AX.X)
                S.act(ss[:], ss[:], AF.Sqrt, scale=1.0 / 64, bias=g.eps6[:, 0:1])
                S.recip(ss[:], ss[:])
                S.tt(qn[:], qk, bc(ss[:].unsqueeze(2), [P, 5, 64]), ALU.mult)
                S.tt(qn[:, 0:4, :], qn[:, 0:4, :], bc(gq[:].unsqueeze(1), [P, 4, 64]), ALU.mult)
                S.tt(qn[:, 4:5, :], qn[:, 4:5, :], gk[:].unsqueeze(1), ALU.mult)
                r_ = qr[b % 2]
                cos = bc(rope64[:, b, 0:32].unsqueeze(1), [P, 5, 32])
                sin = bc(rope64[:, b, 32:64].unsqueeze(1), [P, 5, 32])
                _rope_tm(S, r_[:, 0:5, :], qn[:], cos, sin, t1[:], t2[:], 32)
                S.copy(r_[:, 5, :], r_[:, 4, :], eng="act")
                ri = qir[b % 2]
                xi = x[:, 384:544].rearrange("p (h d) -> p h d", d=32)
                cos = bc(rope32[:, b, 0:16].unsqueeze(1), [P, 5, 16])
                sin = bc(rope32[:, b, 16:32].unsqueeze(1), [P, 5, 16])
                _rope_tm(S, ri[:], xi, cos, sin, t3[:], t4[:], 16)
                S.copy(v16[:, b, 0:64], x[:, 320:384], eng="act")
                if cut == 1:
                    continue
                tb = slice(b * 128, (b + 1) * 128)
                for c in range(3):
                    ps = g.ps[ev % 4]
                    ev += 1
                    S.transpose(ps[:, 0:128], r_[:, 2 * c:2 * c + 2, :].rearrange("p h d -> p (h d)"), g.ident[:])
                    if c < 2:
                        S.copy(qT[:, c, tb], ps[:, 0:128], eng="act")
                    else:
                        S.copy(kT2[:, tb], ps[:, 0:128], eng="act")
                if cut == 2:
                    continue
                ps = g.ps[ev % 4]
                ev += 1
                S.transpose(ps[:, 0:128], ri[:, 0:4, :].rearrange("p h d -> p (h d)"), g.ident[:])
                S.copy(qiT[:, tb], ps[:, 0:128], eng="dve")
                kz = kiz[b % 2]
                for h in range(4):
                    S.copy(kz[h][:, h * 32:(h + 1) * 32], ri[:, 4, :], eng="pool")
                    ps = g.ps[ev % 4]
                    ev += 1
                    S.transpose(ps[:, 0:128], kz[h][:], g.ident[:])
                    S.copy(kiX[h][:, tb], ps[:, 0:128], eng=("act" if h % 2 else "dve"))
            S.memset(v16[:, :, 64:65], 1.0, eng="pool")
            S.flush()
        if getattr(g, "dsa_stop", 0) == 1:
            return
        sc = [sb(nc, st, "dsc%d" % i, [P, T], F32) for i in range(1)] * 2
        work = sb(nc, st, "dwork", [P, T], F32)
        mk = sb(nc, st, "dmk", [P, T], F32)
        eqm = sb(nc, st, "deq", [P, T], F32)
        cum = sb(nc, st, "dcum", [P, T], F32)
        onesT = sb(nc, st, "dones", [P, T], F32)
        S.memset(onesT[:], 1.0, eng="pool")
        rl = [sb(nc, st, "drl%d" % i, [P, 512], F32) for i in range(2)]
        m8 = sb(nc, st, "dm8", [P, 8], F32)
        ngt = sb(nc, st, "dngt", [P, 1], F32)
        maskT = [sb(nc, st, "dmT%d" % i, [P, 16, 128], F32) for i in range(2)]
        E = [sb(nc, st, "dE%d" % i, [P, 512], F32) for i in range(2)]
        Pall = [sb(nc, st, "dP%d" % i, [P, 16, 512], BF16) for i in range(2)]
        nd = [sb(nc, st, "dnd%d" % i, [P, 4, 65], F32) for i in range(2)]
        dn = [sb(nc, st, "ddn%d" % i, [P, 4], F32) for i in range(2)]
        yraw = sb(nc, st, "dy", [P, 16, 256], F32)
        cnt = {'ev': 0, 'it': 0}

        def part_a(i):
            ev = cnt['ev']
            kl = 128 * (i + 1)
            qb = slice(i * 128, (i + 1) * 128)
            mT = maskT[i % 2]
            if i >= 2:
                s_ = sc[i % 2]
                for kb in range(0, kl, 512):
                    w = min(512, kl - kb)
                    for h in range(4):
                        ps = g.ps[ev % 2]
                        r2 = rl[ev % 2]
                        ev += 1
                        S.mm(ps[:, :w], qiT[:, qb], kiX[h][:, kb:kb + w])
                        S.act(r2[:, :w], ps[:, :w], AF.Relu)
                        if h == 0:
                            S.ts(s_[:, kb:kb + w], r2[:, :w], wi[:, i, 0:1], None, ALU.mult)
                        else:
                            S.stt(s_[:, kb:kb + w], r2[:, :w], wi[:, i, h:h + 1], s_[:, kb:kb + w], ALU.mult, ALU.add)
                S.tt(s_[:, qb], s_[:, qb], g.cbias[:], ALU.add)
                for r in range(32):
                    S.max8(m8[:], (s_ if r == 0 else work)[:, :kl])
                    if r < 31:
                        S.match_replace(work[:, :kl], m8[:], (s_ if r == 0 else work)[:, :kl], NEG)
                S.ts(mk[:, :kl], s_[:, :kl], m8[:, 7:8], None, ALU.is_gt)
                S.reduce(ngt[:], mk[:, :kl], ALU.add, AX.X)
                S.ts(ngt[:], ngt[:], -1.0, 256.0, ALU.mult, ALU.add)
                S.ts(eqm[:, :kl], s_[:, :kl], m8[:, 7:8], None, ALU.is_equal)
                S.scan(cum[:, :kl], onesT[:, :kl], eqm[:, :kl], 0.0, ALU.mult, ALU.add)
                S.ts(cum[:, :kl], cum[:, :kl], ngt[:, 0:1], None, ALU.is_le)
                S.tt(eqm[:, :kl], eqm[:, :kl], cum[:, :kl], ALU.mult)
                S.tt(mk[:, :kl], mk[:, :kl], eqm[:, :kl], ALU.add)
                for j0 in range(0, i + 1, 4):
                    nj = min(4, i + 1 - j0)
                    ps = g.ps[1]
                    ev += 1
                    for jj in range(nj):
                        S.transpose(ps[:, jj * 128:(jj + 1) * 128], mk[:, (j0 + jj) * 128:(j0 + jj + 1) * 128], g.ident[:])
                    S.copy(mT[:, j0:j0 + nj, :], ps[:, 0:nj * 128].rearrange("p (j q) -> p j q", q=128), eng="act")
            else:
                for j in range(i):
                    S.copy(mT[:, j, :], g.ones[:], eng="dve")
                S.copy(mT[:, i, :], g.tri_le[:], eng="dve")
            cnt['ev'] = ev

        def part_b(i):
            it = cnt['it']
            qb = slice(i * 128, (i + 1) * 128)
            mT = maskT[i % 2]
            Pa = Pall[i % 2]
            for j in range(i + 1):
                u = it % 2
                it += 1
                pzA, pzB = g.ps[2 + 2 * u], g.ps[3 + 2 * u]
                for h in range(4):
                    pb = (h % 2) * 64
                    pz = pzB if (h % 2) else pzA
                    S.mm(pz[:, (h // 2) * 128:(h // 2 + 1) * 128], kT2[pb:pb + 64, j * 128:(j + 1) * 128], qT[pb:pb + 64, h // 2, qb])
                S.act(E[u][:, 0:256], pzA[:, 0:256], AF.Exp, scale=0.125)
                S.act(E[u][:, 256:512], pzB[:, 0:256], AF.Exp, scale=0.125)
                S.tt(Pa[:, j, :].rearrange("p (h q) -> p h q", q=128), E[u][:].rearrange("p (h q) -> p h q", q=128),
                     bc(mT[:, j, :].unsqueeze(1), [P, 4, 128]), ALU.mult, eng="pool")
            if getattr(g, "dsa_cut2", 0) >= 1:
                return
            po = g.ps[6 + (i % 2)]
            for h in range(4):
                for j in range(i + 1):
                    hr = (h % 2) * 2 + h // 2
                    S.mm(po[:, h * 65:(h + 1) * 65], Pa[:, j, hr * 128:(hr + 1) * 128], v16[:, j, :],
                         start=(j == 0), stop=(j == i))
            u2 = i % 2
            S.copy(nd[u2][:], po[:, 0:260].rearrange("p (h d) -> p h d", d=65), eng="act")
            S.recip(dn[u2][:], nd[u2][:, :, 64])
            S.tt(yraw[:, i, :].rearrange("p (h d) -> p h d", d=64), nd[u2][:, :, 0:64],
                 bc(dn[u2][:].unsqueeze(2), [P, 4, 64]), ALU.mult)
            cnt['it'] = it

        nblk = getattr(g, "dsa_nblk", 16)
        part_a(0)
        for i in range(nblk):
            if i + 1 < nblk:
                part_a(i + 1)
            part_b(i)
        tm_head_rmsnorm(S, nc, st, g, yraw, 16, gain, g.eps6)
        S.dma(ycat_d[:, 768:1024].rearrange("(b p) n -> p b n", p=P), yraw[:])
        S.flush()


def ph_rwkv_prep(S, nc, g, l, s, ptm_d, pfm_d, sops_d, sv_d, sbg_d):
    x_, sp = s // 2, s % 2
    with ExitStack() as st:
        twT = sb(nc, st, "rtw", [33, T], F32)
        adT = sb(nc, st, "rad", [33, T], F32)
        sgT = sb(nc, st, "rsg", [64, T], F32)
        S.memset(twT[:], 1.0)
        S.memset(adT[:], 1.0, eng="pool")
        S.dma(twT[0:32, :], pfm_d[FM_AWD:FM_AWD + 32, :])
        S.dma(adT[0:32, :], pfm_d[FM_AAD:FM_AAD + 32, :])
        S.dma(sgT[:], pfm_d[FM_AGD:FM_AGD + 64, :])
        S.act(twT[0:32, :], twT[0:32, :], AF.Tanh)
        S.act(sgT[:], sgT[:], AF.Sigmoid)
        w2a = sb(nc, st, "rw2", [33, 256], F32)
        a2a = sb(nc, st, "ra2", [33, 256], F32)
        g2 = sb(nc, st, "rg2", [64, 256], F32)
        S.dma(w2a[0:32, :], g.dram["rk_w2"][l])
        S.dma(w2a[32:33, :], g.dram["rk_w0"][l].rearrange("(o n) -> o n", o=1))
        S.dma(a2a[0:32, :], g.dram["rk_a2"][l])
        S.dma(a2a[32:33, :], g.dram["rk_a0"][l].rearrange("(o n) -> o n", o=1))
        S.dma(g2[:], g.dram["rk_g2"][l])
        kk_b = load_row_bcast(S, nc, st, "rkk", g.dram["rk_kk"][l], 256)
        ka_b = load_row_bcast(S, nc, st, "rka", g.dram["rk_ka"][l], 256)
        rk_b = load_row_bcast(S, nc, st, "rrk", g.dram["rk_rk"][l], 256)
        vfm = sb(nc, st, "rvfm", [P, 2, T], F32)
        S.dma(vfm[:], pfm_d[FM_AV:FM_AV + 256, :].rearrange("(c p) t -> p c t", p=P))
        S.dma(sv_d[s].rearrange("(c p) t -> p c t", p=P), vfm[:])
        xs = [sb(nc, st, "rx%d" % i, [P, 768], F32) for i in range(2)]
        F = lambda n: [sb(nc, st, "%s%d" % (n, i), [P, 256], F32) for i in range(2)]
        sig, dec, a_, kkn, k2, nkka, tmp = F("rsig"), F("rdec"), F("ra"), F("rkkn"), F("rk2"), F("rnk"), F("rtmp")
        bg = [sb(nc, st, "rbg%d" % i, [P, 512], F32) for i in range(2)]
        ss = sb(nc, st, "rss", [P, 4], F32)
        bco = sb(nc, st, "rbc", [P, 4], F32)
        hi = [[sb(nc, st, "rhi%d_%d" % (i, o), [P, 256], BF16) for o in range(5)] for i in range(2)]
        lo = [[sb(nc, st, "rlo%d_%d" % (i, o), [P, 256], BF16) for o in range(5)] for i in range(2)]
        h32 = [sb(nc, st, "rh32_%d" % i, [P, 256], F32) for i in range(2)]
        hv = lambda ap: ap.rearrange("p (h d) -> p h d", d=64)
        for tb in range(16):
            u = tb % 2
            x = xs[u]
            tsl = slice(tb * 128, (tb + 1) * 128)
            S.dma(x[:], ptm_d[tsl, 0:768])
            r, k, v = x[:, 0:256], x[:, 256:512], x[:, 512:768]
            pw, pa, pg = g.ps[0 + u], g.ps[2 + u], g.ps[4 + u]
            S.mm(pw[:, 0:256], twT[0:33, tsl], w2a[0:33, :])
            S.mm(pa[:, 0:256], adT[0:33, tsl], a2a[0:33, :])
            S.mm(pg[:, 0:256], sgT[0:64, tsl], g2[0:64, :])
            S.act(sig[u][:], pw[:, 0:256], AF.Sigmoid)
            S.act(a_[u][:], pa[:, 0:256], AF.Sigmoid)
            S.act(dec[u][:], sig[u][:], AF.Exp, scale=-0.6065306597126334)
            S.copy(bg[u][:, 256:512], pg[:, 0:256], eng="act")
            S.tt(kkn[u][:], k, kk_b[:], ALU.mult)
            S.tt(tmp[u][:], kkn[u][:], kkn[u][:], ALU.mult)
            S.reduce(ss[:], hv(tmp[u][:]), ALU.add, AX.X)
            S.act(ss[:], ss[:], AF.Sqrt)
            S.ts(ss[:], ss[:], 1e-12, None, ALU.max)
            S.recip(ss[:], ss[:])
            S.tt(hv(kkn[u][:]), hv(kkn[u][:]), bc(ss[:].unsqueeze(2), [P, 4, 64]), ALU.mult)
            S.stt(k2[u][:], a_[u][:], -1.0, ka_b[:], ALU.add, ALU.mult)
            S.stt(k2[u][:], k2[u][:], 1.0, k, ALU.add, ALU.mult)
            S.stt(nkka[u][:], kkn[u][:], -1.0, a_[u][:], ALU.mult, ALU.mult)
            S.tt(tmp[u][:], r, k2[u][:], ALU.mult)
            S.tt(tmp[u][:], tmp[u][:], rk_b[:], ALU.mult)
            S.reduce(bco[:], hv(tmp[u][:]), ALU.add, AX.X)
            S.tt(hv(bg[u][:, 0:256]), hv(v), bc(bco[:].unsqueeze(2), [P, 4, 64]), ALU.mult)
            S.dma(sbg_d[s, tsl, :], bg[u][:])
            for o, src in enumerate((kkn[u][:], dec[u][:], nkka[u][:], k2[u][:], r)):
                S.copy(hi[u][o][:], src, eng="act")
                S.copy(h32[o % 2][:], hi[u][o][:], eng="pool")
                S.tt(h32[o % 2][:], src, h32[o % 2][:], ALU.subtract, eng="pool")
                S.copy(lo[u][o][:], h32[o % 2][:], eng="act")
                S.dma(sops_d[o, 0, x_, tsl, sp * 256:(sp + 1) * 256], hi[u][o][:])
                S.dma(sops_d[o, 1, x_, tsl, sp * 256:(sp + 1) * 256], lo[u][o][:])
        S.flush()


SCAN_ACT_Y = False


def ph_rwkv_scan(S, nc, g, sops_d, sv_d, sy_d, nsteps=T):
    CH = 32
    with ExitStack() as st:
        id2 = sb(nc, st, "sid2", [P, 128], BF16)
        S.memset(id2[:], 0.0)
        S.tt(id2[:, 0:32], g.ident16[:, 0:32], g.ident16[:, 32:64], ALU.add)
        S.tt(id2[:, 64:96], g.ident16[:, 64:96], g.ident16[:, 96:128], ALU.add)
        sel = sb(nc, st, "ssel", [P, CH, 128], BF16)
        for xp in range(2):
            for tp in range(CH):
                col = xp * 64 + tp
                S.copy(sel[:, tp, xp * 64:(xp + 1) * 64], bc(id2[:, col:col + 1], [P, 64]),
                       eng=("dve" if tp % 2 else "pool"))
        Stt = [sb(nc, st, "sS%d" % i, [P, 512], F32) for i in range(2)]
        S.memset(Stt[0][:], 0.0)
        S.memset(Stt[1][:], 0.0)
        ytmps = [sb(nc, st, "sytmp%d" % i, [P, 512], F32) for i in range(2)]
        junk = sb(nc, st, "sjunk", [P, 512], F32)
        Sw = sb(nc, st, "sSw", [P, 512], F32)
        tmp = sb(nc, st, "stmp", [P, 512], F32)
        sa = sb(nc, st, "ssa", [P, 8], F32)
        ND = 3
        ringW = [sb(nc, st, "srgW%d" % d, [P, 512], F32) for d in range(ND)]
        ringK = [sb(nc, st, "srgK%d" % d, [P, 512], F32) for d in range(ND)]
        vk = [sb(nc, st, "svk%d" % d, [P, 512], F32) for d in range(ND)]
        opt = [[sb(nc, st, "sop%d_%d" % (b_, o), [P, 512], BF16) for o in range(5)] for b_ in range(3)]
        vS = [sb(nc, st, "svS%d" % i, [P, 8, 256], F32) for i in range(2)]
        yb = [sb(nc, st, "syb%d" % i, [P, 8, 256], F32) for i in range(2)]
        g3 = lambda ap: ap.rearrange("p (g k) -> p g k", k=64)
        step = 0
        pend = None
        nbig = (nsteps + 255) // 256
        for big in range(nbig):
            vs, y_ = vS[big % 2], yb[big % 2]
            bsl = slice(big * 256, (big + 1) * 256)
            for x in range(2):
                for sp in range(2):
                    for h in range(4):
                        S.dma(vs[x * 64:(x + 1) * 64, sp * 4 + h, :], sv_d[2 * x + sp, h * 64:(h + 1) * 64, bsl])
            for cc in range(256 // CH):
                c = big * (256 // CH) + cc
                if c * CH >= nsteps:
                    break
                ob = opt[c % 3]
                for o in range(5):
                    for x in range(2):
                        for hl in range(2):
                            p0 = x * 64 + hl * 32
                            S.dma(ob[o][p0:p0 + CH, :], sops_d[o, hl, x, c * CH:(c + 1) * CH, :])
                for tp in range(CH):
                    ti = cc * CH + tp
                    d = step % ND
                    par = step % 2
                    banks = (g.ps[0 + par], g.ps[6], g.ps[2 + par], g.ps[7], g.ps[4 + par])
                    for o in range(5):
                        S.mm(banks[o][:, :], sel[:, tp, :], ob[o][:])
                    S.copy(ringW[d][:], banks[1][:, :], eng="act")
                    S.copy(ringK[d][:], banks[3][:, :], eng="act")
                    KK, NK, R = banks[0], banks[2], banks[4]
                    W, KB = ringW[d], ringK[d]
                    Sp, Sn = Stt[step % 2], Stt[(step + 1) % 2]
                    S.tt(g3(vk[d][:]), g3(KB[:]), bc(vs[:, :, ti:ti + 1], [P, 8, 64]), ALU.mult, eng="pool")
                    S.tt(Sw[:], Sp[:], W[:], ALU.mult, eng="pool")
                    S.tt(Sw[:], Sw[:], vk[d][:], ALU.add, eng="pool")
                    S.tt(tmp[:], Sp[:], KK[:, :], ALU.mult)
                    if pend is not None:
                        S.tt(pend[0][:], pend[1][:], pend[2][:, :], ALU.mult)
                    S.reduce(sa[:], g3(tmp[:]), ALU.add, AX.X)
                    if pend is not None:
                        S.reduce(pend[3], g3(pend[0][:]), ALU.add, AX.X)
                        pend = None
                    S.tt(g3(tmp[:]), g3(NK[:, :]), bc(sa[:].unsqueeze(2), [P, 8, 64]), ALU.mult)
                    S.tt(Sn[:], Sw[:], tmp[:], ALU.add)
                    pend = (ytmps[step % 2], Sn, R, y_[:, :, ti])
                    step += 1
            if pend is not None:
                S.tt(pend[0][:], pend[1][:], pend[2][:, :], ALU.mult)
                S.reduce(pend[3], g3(pend[0][:]), ALU.add, AX.X)
                pend = None
            for x in range(2):
                for sp in range(2):
                    for h in range(4):
                        S.dma(sy_d[2 * x + sp, h * 64:(h + 1) * 64, bsl], y_[x * 64:(x + 1) * 64, sp * 4 + h, :])
        S.flush()


def ph_rwkv_post(S, nc, g, l, s, sy_d, sbg_d, ycat_d):
    with ExitStack() as st:
        yT = sb(nc, st, "pyT", [P, 2, T], F32)
        S.dma(yT[:], sy_d[s].rearrange("(c p) t -> p c t", p=P))
        bgt = sb(nc, st, "pbg", [P, 16, 512], F32)
        S.dma(bgt[:], sbg_d[s].rearrange("(b p) n -> p b n", p=P))
        lng = load_row_bcast(S, nc, st, "plng", g.dram["rk_ln_g"][l], 256)
        lnb = load_row_bcast(S, nc, st, "plnb", g.dram["rk_ln_b"][l], 256)
        y = sb(nc, st, "py", [P, 16, 256], F32)
        for tb in range(16):
            ps = g.ps[tb % 4]
            for c in range(2):
                S.transpose(ps[:, c * 128:(c + 1) * 128], yT[:, c, tb * 128:(tb + 1) * 128], g.ident[:])
            S.copy(y[:, tb, :], ps[:, 0:256], eng=("act" if tb % 2 else "dve"))
        yv = y[:].rearrange("p b (h d) -> p (b h) d", d=64)
        mean = sb(nc, st, "pmean", [P, 64], F32)
        sq = sb(nc, st, "psq", [P, 64, 64], F32)
        S.reduce(mean[:], yv, ALU.add, AX.X)
        S.ts(mean[:], mean[:], 1.0 / 64, None, ALU.mult)
        S.tt(yv, yv, bc(mean[:].unsqueeze(2), [P, 64, 64]), ALU.subtract)
        S.tt(sq[:], yv, yv, ALU.mult)
        S.reduce(mean[:], sq[:], ALU.add, AX.X)
        eps = sb(nc, st, "peps", [P, 1], F32)
        S.memset(eps[:], 64e-5)
        S.act(mean[:], mean[:], AF.Sqrt, scale=1.0 / 64, bias=eps[:, 0:1])
        S.recip(mean[:], mean[:])
        S.tt(yv, yv, bc(mean[:].unsqueeze(2), [P, 64, 64]), ALU.mult)
        S.tt(y[:], y[:], bc(lng[:].unsqueeze(1), [P, 16, 256]), ALU.mult)
        S.tt(y[:], y[:], bc(lnb[:].unsqueeze(1), [P, 16, 256]), ALU.add)
        S.tt(y[:], y[:], bgt[:, :, 0:256], ALU.add)
        S.tt(y[:], y[:], bgt[:, :, 256:512], ALU.mult)
        S.dma(ycat_d[:, 0:256].rearrange("(b p) n -> p b n", p=P), y[:])
        S.flush()


def ph_outproj(S, nc, g, l, b, ycat_d, xin_d, xout_d, tok0):
    with ExitStack() as st:
        Wb = sb(nc, st, "oW", [P, 8, 1024], BF16)
        ycT = sb(nc, st, "oyT", [P, 8, T], BF16)
        with ExitStack() as st2:
            stg = [sb(nc, st2, "ostg%d" % i, [P, 8, 512], F32) for i in range(2)]
            for hf in range(2):
                S.dma(stg[hf][:], g.dram["w_out"][l][:, hf * 512:(hf + 1) * 512].rearrange("(c p) n -> p c n", p=P))
                S.copy(Wb[:, :, hf * 512:(hf + 1) * 512], stg[hf][:], eng=("act" if hf else "pool"))
            yb = [sb(nc, st2, "oyb%d" % i, [P, 1024], F32) for i in range(2)]
            ev = 0
            for tb in range(16):
                y = yb[tb % 2]
                S.dma(y[:], ycat_d[tb * 128:(tb + 1) * 128, :])
                for c4 in range(2):
                    ps = g.ps[ev % 4]
                    ev += 1
                    for cc in range(4):
                        c = c4 * 4 + cc
                        S.transpose(ps[:, cc * 128:(cc + 1) * 128], y[:, c * 128:(c + 1) * 128], g.ident[:])
                    S.copy(ycT[:, c4 * 4:c4 * 4 + 4, tb * 128:(tb + 1) * 128],
                           ps[:, :].rearrange("p (c t) -> p c t", t=128), eng=("act" if ev % 2 else "dve"))
            S.flush()
        xo = [sb(nc, st, "oxo%d" % i, [P, 512], F32) for i in range(3)]
        ev = 0
        for oc in range(8):
            for sblk in range(4):
                ps = g.ps[4 + (ev % 4)]
                x = xo[ev % 3]
                ev += 1
                tsl = slice(tok0 + sblk * 512, tok0 + (sblk + 1) * 512)
                S.dma(x[:], xin_d[oc * 128:(oc + 1) * 128, tsl])
                for k in range(8):
                    S.mm(ps[:, :], Wb[:, k, oc * 128:(oc + 1) * 128], ycT[:, k, sblk * 512:(sblk + 1) * 512],
                         start=(k == 0), stop=(k == 7))
                S.stt(x[:], ps[:, :], g.modT[:, 16 + oc, b:b + 1], x[:], ALU.mult, ALU.add)
                S.dma(xout_d[oc * 128:(oc + 1) * 128, tsl], x[:])
        S.flush()


def ph_moe(S, nc, g, l, b, xin_d, xout_d, tok0, n_exp=32):
    with ExitStack() as st:
        hT = sb(nc, st, "mhT", [P, 8, T], BF16)
        gate = sb(nc, st, "mgate", [P, 16, 32], F32)
        with ExitStack() as st2:
            wge = sb(nc, st2, "mwge", [P, 8, 36], F32)
            S.dma(wge[:], g.dram["moe_wge"][l].rearrange("(c p) n -> p c n", p=P))
            bge = load_row_bcast(S, nc, st2, "mbge", g.dram["moe_bge"][l], 36)
            lg = sb(nc, st2, "mlg", [P, 16, 36], F32)
            emit_norm_mod(S, nc, g, xin_d, tok0, b, g.A2, g.modT[:, 24:32, :], hT, col0=0, route=(wge, lg))
            S.tt(lg[:], lg[:], bc(bge[:].unsqueeze(1), [P, 16, 36]), ALU.add)
            G4 = lg[:, :, 0:4]
            gmax = sb(nc, st2, "mgmax", [P, 16], F32)
            ge = sb(nc, st2, "mge", [P, 16, 4], F32)
            gsum = sb(nc, st2, "mgsum", [P, 16], F32)
            pen = sb(nc, st2, "mpen", [P, 16, 4], F32)
            S.reduce(gmax[:], G4, ALU.max, AX.X)
            S.tt(ge[:], G4, bc(gmax[:].unsqueeze(2), [P, 16, 4]), ALU.subtract)
            S.ts(pen[:], ge[:], 0.0, NEG, ALU.is_lt, ALU.mult)
            S.act(ge[:], ge[:], AF.Exp)
            S.reduce(gsum[:], ge[:], ALU.add, AX.X)
            S.recip(gsum[:], gsum[:])
            Em = sb(nc, st2, "mEm", [P, 16, 32], F32)
            S.tt(Em[:].rearrange("p b (q e) -> p b q e", e=8), lg[:, :, 4:36].rearrange("p b (q e) -> p b q e", e=8),
                 bc(pen[:].unsqueeze(3), [P, 16, 4, 8]), ALU.add)
            m1 = sb(nc, st2, "mm1", [P, 16], F32)
            m2 = sb(nc, st2, "mm2", [P, 16], F32)
            E2 = sb(nc, st2, "mE2", [P, 16, 32], F32)
            S.reduce(m1[:], Em[:], ALU.max, AX.X)
            S.tt(E2[:], Em[:], bc(m1[:].unsqueeze(2), [P, 16, 32]), ALU.is_ge)
            S.stt(E2[:], E2[:], NEG, Em[:], ALU.mult, ALU.add)
            S.reduce(m2[:], E2[:], ALU.max, AX.X)
            ex = sb(nc, st2, "mex", [P, 16, 32], F32)
            S.tt(ex[:], Em[:], bc(m1[:].unsqueeze(2), [P, 16, 32]), ALU.subtract)
            S.act(ex[:], ex[:], AF.Exp)
            S.tt(E2[:], Em[:], bc(m2[:].unsqueeze(2), [P, 16, 32]), ALU.is_ge)
            S.tt(ex[:], ex[:], E2[:], ALU.mult)
            den = sb(nc, st2, "mden", [P, 16], F32)
            S.tt(den[:], m2[:], m1[:], ALU.subtract)
            S.act(den[:], den[:], AF.Exp)
            S.ts(den[:], den[:], 1.0, None, ALU.add)
            S.recip(den[:], den[:])
            S.tt(den[:], den[:], gsum[:], ALU.mult)
            S.tt(gate[:], ex[:], bc(den[:].unsqueeze(2), [P, 16, 32]), ALU.mult)
            S.flush()
        acc = sb(nc, st, "macc", [P, 16, 1024], F32)
        stg = [sb(nc, st, "mstg%d" % i, [P, 8, 512], F32) for i in range(2)]
        W1b = [sb(nc, st, "mW1_%d" % i, [P, 8, 512], BF16) for i in range(2)]
        W3b = [sb(nc, st, "mW3_%d" % i, [P, 8, 512], BF16) for i in range(2)]
        W2b = [sb(nc, st, "mW2_%d" % i, [P, 4, 1024], BF16) for i in range(2)]
        aT = [sb(nc, st, "maT%d" % i, [P, 4, 512], BF16) for i in range(2)]
        su = [sb(nc, st, "msu%d" % i, [P, 512], F32) for i in range(2)]
        si = 0
        it = 0
        for e in range(n_exp):
            u = e % 2
            for (dst, src) in ((W1b[u], g.dram["moe_w1"][l, e]), (W3b[u], g.dram["moe_w3"][l, e])):
                sg = stg[si % 2]
                si += 1
                S.dma(sg[:], src.rearrange("(c p) n -> p c n", p=P))
                S.copy(dst[:], sg[:], eng=("act" if si % 2 else "pool"))
            sg = stg[si % 2]
            si += 1
            S.dma(sg[:].rearrange("p c n -> p (c n)").rearrange("p (c n) -> p c n", n=1024),
                  g.dram["moe_w2"][l, e].rearrange("(c p) n -> p c n", p=P))
            S.copy(W2b[u][:].rearrange("p c n -> p (c n)"), sg[:].rearrange("p c n -> p (c n)"), eng="pool")
            for sblk in range(4):
                a = aT[it % 2]
                it += 1
                for f in range(4):
                    pu, pg3 = g.ps[(f % 2) * 2], g.ps[(f % 2) * 2 + 1]
                    for k in range(8):
                        S.mm(pu[:, :], W1b[u][:, k, f * 128:(f + 1) * 128], hT[:, k, sblk * 512:(sblk + 1) * 512],
                             start=(k == 0), stop=(k == 7))
                    for k in range(8):
                        S.mm(pg3[:, :], W3b[u][:, k, f * 128:(f + 1) * 128], hT[:, k, sblk * 512:(sblk + 1) * 512],
                             start=(k == 0), stop=(k == 7))
                    s_ = su[f % 2]
                    S.act(s_[:], pu[:, :], AF.Silu)
                    S.tt(a[:, f, :], s_[:], pg3[:, :], ALU.mult)
                for tb4 in range(4):
                    tb = sblk * 4 + tb4
                    for hf in range(2):
                        py = g.ps[4 + ((tb4 * 2 + hf) % 4)]
                        for f in range(4):
                            S.mm(py[:, :], a[:, f, tb4 * 128:(tb4 + 1) * 128], W2b[u][:, f, hf * 512:(hf + 1) * 512],
                                 start=(f == 0), stop=(f == 3))
                        dst = acc[:, tb, hf * 512:(hf + 1) * 512]
                        if e == 0:
                            S.ts(dst, py[:, :], gate[:, tb, e:e + 1], None, ALU.mult)
                        else:
                            S.stt(dst, py[:, :], gate[:, tb, e:e + 1], dst, ALU.mult, ALU.add)
        xo = [sb(nc, st, "mxo%d" % i, [P, 512], F32) for i in range(2)]
        ev = 0
        for c in range(8):
            for sblk in range(4):
                ps = g.ps[ev % 4]
                x = xo[ev % 2]
                ev += 1
                tsl = slice(tok0 + sblk * 512, tok0 + (sblk + 1) * 512)
                S.dma(x[:], xin_d[c * 128:(c + 1) * 128, tsl])
                for tb4 in range(4):
                    S.transpose(ps[:, tb4 * 128:(tb4 + 1) * 128], acc[:, sblk * 4 + tb4, c * 128:(c + 1) * 128], g.ident[:])
                S.stt(x[:], ps[:, :], g.modT[:, 40 + c, b:b + 1], x[:], ALU.mult, ALU.add)
                S.dma(xout_d[c * 128:(c + 1) * 128, tsl], x[:])
        S.flush()


def build_full(shared_shapes, n_layers=2):
    nc = bass.Bass("TRN2", target_bir_lowering=False)
    g = G()
    g.dram = {}
    for k, shp in shared_shapes.items():
        g.dram[k] = nc.dram_tensor(k, list(shp), F32, kind="ExternalInput").ap()
    NT = NSEQ * T
    g.dram["xT"] = nc.dram_tensor("xT", [D, NT], F32, kind="ExternalInput").ap()
    g.dram["cT"] = nc.dram_tensor("cT", [D, 4], F32, kind="ExternalInput").ap()
    outT = nc.dram_tensor("outT", [D, NT], F32, kind="ExternalOutput").ap()
    X1 = nc.dram_tensor("X1", [D, NT], F32).ap()
    X2 = nc.dram_tensor("X2", [D, NT], F32).ap()
    ptm = nc.dram_tensor("ptm", [T, NTM], F32).ap()
    pfm = nc.dram_tensor("pfm", [NFM, T], F32).ap()
    ycat = nc.dram_tensor("ycat", [NSEQ, T, D], F32).ap()
    sops = nc.dram_tensor("sops", [5, 2, 2, T, 512], BF16).ap()
    sv = nc.dram_tensor("sv", [NSEQ, 256, T], F32).ap()
    sy = nc.dram_tensor("sy", [NSEQ, 256, T], F32).ap()
    sbg = nc.dram_tensor("sbg", [NSEQ, T, 512], F32).ap()
    with ExitStack() as st:
        S = Sched(nc)
        S.open(st)
        g.ps = [st.enter_context(nc.psum_tensor("ps%d" % i, [P, 512], F32)) for i in range(8)]
        setup_consts(S, nc, st, g)
        for l in range(n_layers):
            xin = g.dram["xT"] if l == 0 else X2
            xmid = X1
            xout = outT if l == n_layers - 1 else X2
            ph_ada(S, nc, g, l)
            for s in range(NSEQ):
                with ExitStack() as st2:
                    hT = sb(nc, st2, "hT", [P, 8, T + 4], BF16)
                    S.memset(hT[:, :, 0:1], 0.0)
                    emit_norm_mod(S, nc, g, xin, s * T, s, g.A1, g.modT[:, 0:8, :], hT)
                    ph_inproj(S, nc, g, l, hT, ptm, pfm)
                ph_sb(S, nc, g, l, ptm, pfm, ycat[s])
                ph_ml(S, nc, g, l, ptm, pfm, ycat[s])
                ph_dsa(S, nc, g, l, ptm, ycat[s])
                ph_rwkv_prep(S, nc, g, l, s, ptm, pfm, sops, sv, sbg)
            ph_rwkv_scan(S, nc, g, sops, sv, sy)
            for s in range(NSEQ):
                ph_rwkv_post(S, nc, g, l, s, sy, sbg, ycat[s])
                ph_outproj(S, nc, g, l, s, ycat[s], xin, xmid, s * T)
            for s in range(NSEQ):
                ph_moe(S, nc, g, l, s, xmid, xout, s * T)
        S.flush()
        g.n_inst = S.n_inst
    return nc, g


_CACHE = {}


def kernel(**inputs):
    inp = {k: np.asarray(v) for k, v in inputs.items()}
    sh = host_shared(inp)
    n_layers = inp["w_in"].shape[0]
    if "nc" not in _CACHE:
        _CACHE["nc"] = build_full({k: v.shape for k, v in sh.items()}, n_layers)
    nc, g = _CACHE["nc"]
    in_maps = []
    for core in range(8):
        d = dict(sh)
        d.update(host_core(inp, core))
        in_maps.append(d)
    res = run_bass_kernel_spmd(nc, in_maps, core_ids=list(range(8)))
    out = np.empty((32, T, D), np.float32)
    for core in range(8):
        oT = np.asarray(res.results[core]["outT"])
        out[core * NSEQ:(core + 1) * NSEQ] = oT.T.reshape(NSEQ, T, D)
    return out
```

```python
import numpy as np
import concourse.bass as bass
import concourse.mybir as mybir
from concourse.bass_utils import run_bass_kernel_spmd

F32 = mybir.dt.float32
BF16 = mybir.dt.bfloat16
AF = mybir.ActivationFunctionType
ALU = mybir.AluOpType
AX = mybir.AxisListType

COMPUTE = ("pe", "act", "dve", "pool")
SYNC_WAR = True


class Sched:
    def __init__(self, nc, n_dma_sems=32):
        self.nc = nc
        self.n_dma = n_dma_sems
        self.sem = {}
        self.stack = None
        self.ops = []
        self.last_w = {}
        self.readers = {}
        self.count = {e: 0 for e in COMPUTE}
        self.dma_cnt = [0] * n_dma_sems
        self.dma_rr = 0
        self.known = {e: {} for e in COMPUTE + ("sp",)}
        self.phase_start = 0
        self.n_inst = 0

    def open(self, stack):
        nc = self.nc
        self.stack = stack
        self.n_sem_alloc = 0
        for e in COMPUTE:
            self.sem[e] = stack.enter_context(nc.semaphore("s_" + e))
        for i in range(self.n_dma):
            self.sem[("d", i)] = stack.enter_context(nc.semaphore("s_d%d" % i))

    @staticmethod
    def key(ap):
        return ap.name

    def add(self, eng, fn, reads, writes, dma=False):
        idx = len(self.ops)
        deps = set()
        wdeps = set()
        for k in reads:
            if k in self.last_w:
                deps.add(self.last_w[k])
        for k in writes:
            if k in self.last_w:
                deps.add(self.last_w[k])
            rd = self.readers.get(k)
            if rd:
                wdeps.update(rd.values())
        for k in writes:
            self.last_w[k] = idx
            self.readers[k] = {}
        for k in reads:
            self.readers.setdefault(k, {})[("dma", idx) if dma else eng] = idx
        deps.discard(idx)
        wdeps.discard(idx)
        wdeps -= deps
        self.ops.append(dict(eng=eng, fn=fn, deps=deps, wdeps=wdeps, dma=dma, sig=False, waits=None))
        return idx

    def flush(self, barrier=True):
        ops = self.ops
        lo = self.phase_start
        n = len(ops)
        def skip(o, od, d):
            if od["dma"] or o["dma"] or od["eng"] != o["eng"]:
                return False
            if o["eng"] == "pe":
                return True
            return (not SYNC_WAR) and (d not in o["deps"])

        for i in range(lo, n):
            o = ops[i]
            for d in (o["deps"] | o["wdeps"]):
                od = ops[d]
                if d < lo or od["dma"] or skip(o, od, d):
                    continue
                od["sig"] = True
        last_of = {}
        for i in range(lo, n):
            last_of[ops[i]["eng"]] = i
        if barrier:
            for e, i in last_of.items():
                if e in COMPUTE:
                    ops[i]["sig"] = True
        for i in range(lo, n):
            o = ops[i]
            e = o["eng"]
            kn = self.known[e]
            waits = []
            for d in sorted(o["deps"] | o["wdeps"]):
                od = ops[d]
                if d < lo or skip(o, od, d):
                    continue
                s, v = od["done"]
                if kn.get(s, 0) < v:
                    waits.append((s, v))
                    for s2, v2 in od["clock"].items():
                        if kn.get(s2, 0) < v2:
                            kn[s2] = v2
            if o["dma"]:
                j = self.dma_rr
                self.dma_rr = (j + 1) % self.n_dma
                s = ("d", j)
                if kn.get(s, 0) < self.dma_cnt[j]:
                    waits.append((s, self.dma_cnt[j]))
                    kn[s] = self.dma_cnt[j]
                self.dma_cnt[j] += 16
                o["done"] = (s, self.dma_cnt[j])
                o["inc"] = (s, 16)
                clock = dict(kn)
                clock[s] = self.dma_cnt[j]
                o["clock"] = clock
            else:
                if o["sig"]:
                    self.count[e] += 1
                    o["done"] = (e, self.count[e])
                    o["inc"] = (e, 1)
                    clock = dict(kn)
                    clock[e] = self.count[e]
                    o["clock"] = clock
                else:
                    o["inc"] = None
            w = {}
            for s, v in waits:
                w[s] = max(w.get(s, 0), v)
            o["waits"] = list(w.items())
        final = {}
        if barrier:
            for e in COMPUTE:
                final[e] = self.count[e]
            for j in range(self.n_dma):
                final[("d", j)] = self.dma_cnt[j]
        nc = self.nc
        sem = self.sem
        by_eng = {}
        for i in range(lo, n):
            by_eng.setdefault(ops[i]["eng"], []).append(ops[i])

        def emit(ename):
            def body(e):
                for o in by_eng.get(ename, []):
                    for s, v in o["waits"]:
                        e.wait_ge(sem[s], v)
                        self.n_inst += 1
                    ins = o["fn"](e)
                    self.n_inst += 1
                    if o["inc"] is not None:
                        ins.then_inc(sem[o["inc"][0]], o["inc"][1])
                kn = self.known[ename]
                for s, v in final.items():
                    if kn.get(s, 0) < v:
                        e.wait_ge(sem[s], v)
                        kn[s] = v
            return body

        with nc.Block() as block:
            block.tensor(emit("pe"))
            block.scalar(emit("act"))
            block.vector(emit("dve"))
            block.gpsimd(emit("pool"))
            block.sync(emit("sp"))
        for i in range(lo, n):
            ops[i]["fn"] = None
            ops[i]["clock"] = None if i < n else None
        self.phase_start = n
        if barrier:
            self.last_w = {}
            self.readers = {}
            for e in COMPUTE:
                if self.count[e] > 20000:
                    self.n_sem_alloc += 1
                    self.sem[e] = self.stack.enter_context(nc.semaphore("s_%s_%d" % (e, self.n_sem_alloc)))
                    self.count[e] = 0
                    for kn in self.known.values():
                        kn.pop(e, None)
            for j in range(self.n_dma):
                if self.dma_cnt[j] > 20000:
                    self.n_sem_alloc += 1
                    self.sem[("d", j)] = self.stack.enter_context(nc.semaphore("s_d%d_%d" % (j, self.n_sem_alloc)))
                    self.dma_cnt[j] = 0
                    for kn in self.known.values():
                        kn.pop(("d", j), None)

    def _rw(self, outs, ins, rk, wk):
        r = list(rk) if rk is not None else [self.key(a) for a in ins if hasattr(a, "name") and a.space != "DRAM"]
        w = list(wk) if wk is not None else [self.key(a) for a in outs if a.space != "DRAM"]
        return r, w

    def mm(self, out, lhsT, rhs, start=True, stop=True, rk=None, wk=None):
        r, w = self._rw([out], [lhsT, rhs], rk, wk)
        return self.add("pe", lambda e: e.matmul(out, lhsT=lhsT, rhs=rhs, start=start, stop=stop), r, w)

    def transpose(self, out, in_, ident, rk=None, wk=None):
        r, w = self._rw([out], [in_, ident], rk, wk)
        return self.add("pe", lambda e: e.transpose(out, in_, ident), r, w)

    def act(self, out, in_, func, bias=None, scale=1.0, accum_out=None, rk=None, wk=None):
        ins = [in_] + ([bias] if hasattr(bias, "name") else []) + ([scale] if hasattr(scale, "name") else [])
        outs = [out] + ([accum_out] if accum_out is not None else [])
        r, w = self._rw(outs, ins, rk, wk)
        kw = {}
        if bias is not None:
            kw["bias"] = bias
        if accum_out is not None:
            kw["accum_out"] = accum_out
        return self.add("act", lambda e: e.activation(out, in_, func, scale=scale, **kw), r, w)

    def tt(self, out, in0, in1, op, eng="dve", rk=None, wk=None):
        r, w = self._rw([out], [in0, in1], rk, wk)
        return self.add(eng, lambda e: e.tensor_tensor(out, in0, in1, op), r, w)

    def ts(self, out, in0, s1, s2=None, op0=ALU.mult, op1=None, eng="dve", accum_out=None, rk=None, wk=None):
        ins = [in0] + [s for s in (s1, s2) if hasattr(s, "name")]
        outs = [out] + ([accum_out] if accum_out is not None else [])
        r, w = self._rw(outs, ins, rk, wk)
        kw = {}
        if op1 is not None:
            kw["op1"] = op1
        if accum_out is not None:
            kw["accum_out"] = accum_out
        return self.add(eng, lambda e: e.tensor_scalar(out, in0, s1, s2, op0, **kw), r, w)

    def stt(self, out, in0, scalar, in1, op0, op1, eng="dve", rk=None, wk=None):
        ins = [in0, in1] + ([scalar] if hasattr(scalar, "name") else [])
        r, w = self._rw([out], ins, rk, wk)
        return self.add(eng, lambda e: e.scalar_tensor_tensor(out, in0, scalar, in1, op0, op1), r, w)

    def copy(self, out, in_, eng="dve", rk=None, wk=None):
        r, w = self._rw([out], [in_], rk, wk)
        if eng == "act":
            return self.add("act", lambda e: e.activation(out, in_, AF.Copy), r, w)
        return self.add(eng, lambda e: e.tensor_copy(out, in_), r, w)

    def memset(self, out, val, eng="dve", wk=None):
        r, w = self._rw([out], [], None, wk)
        return self.add(eng, lambda e: e.memset(out, val), r, w)

    def reduce(self, out, in_, op, axis=AX.X, eng="dve", rk=None, wk=None):
        r, w = self._rw([out], [in_], rk, wk)
        return self.add(eng, lambda e: e.tensor_reduce(out, in_, axis, op), r, w)

    def recip(self, out, in_, rk=None, wk=None):
        r, w = self._rw([out], [in_], rk, wk)
        return self.add("dve", lambda e: e.reciprocal(out, in_), r, w)

    def scan(self, out, d0, d1, init, op0, op1, eng="dve", rk=None, wk=None):
        r, w = self._rw([out], [d0, d1], rk, wk)
        return self.add(eng, lambda e: e.tensor_tensor_scan(out, d0, d1, init, op0, op1), r, w)

    def max8(self, out, in_, rk=None, wk=None):
        r, w = self._rw([out], [in_], rk, wk)
        return self.add("dve", lambda e: e.max(out, in_), r, w)

    def match_replace(self, out, rep, vals, imm, rk=None, wk=None):
        r, w = self._rw([out], [rep, vals], rk, wk)
        return self.add("dve", lambda e: e.match_replace(out, rep, vals, imm), r, w)

    def dma(self, out, in_, rk=None, wk=None, **kw):
        r, w = self._rw([out], [in_], rk, wk)
        return self.add("sp", lambda e: e.dma_start(out, in_, **kw), r, w, dma=True)


from contextlib import ExitStack

P = 128
T = 2048
D = 1024
NSEQ = 4
NTM = 2084
NFM = 1416
TM_AR, TM_AK, TM_AV, TM_BV, TM_CV, TM_CO, TM_DQ, TM_DK, TM_DV, TM_DQI, TM_DKI, TM_DWI = (
    0, 256, 512, 768, 1024, 1280, 1536, 1792, 1856, 1920, 2048, 2080)
FM_AV, FM_AWD, FM_AAD, FM_AGD, FM_BQ, FM_BK, FM_CQ, FM_CK, FM_CIG, FM_CFG = (
    0, 256, 288, 320, 384, 640, 896, 1152, 1408, 1412)
NEG = -1.0e30
_uid = [0]


def uname(s):
    _uid[0] += 1
    return "%s_%d" % (s, _uid[0])


def sb(nc, st, name, shape, dt):
    return st.enter_context(nc.sbuf_tensor(uname(name), list(shape), dt))


def bc(ap, shape):
    return ap.to_broadcast(list(shape))


class G:
    pass


def host_consts():
    c = {}
    i = np.arange(128)
    c["ident"] = np.eye(128, dtype=np.float32)
    c["ones"] = np.ones((128, 128), np.float32)
    c["tri_lt"] = (i[:, None] < i[None, :]).astype(np.float32)
    c["tri_le"] = (i[:, None] <= i[None, :]).astype(np.float32)
    c["tri_gt"] = (i[:, None] > i[None, :]).astype(np.float32)
    c["cbias"] = np.where(i[None, :] <= i[:, None], 0.0, NEG).astype(np.float32)
    half = 32
    inv = 10000.0 ** (-np.arange(half, dtype=np.float32) / half)
    ang = np.arange(T, dtype=np.float32)[:, None] * inv[None, :]
    c["rope64"] = np.concatenate([np.cos(ang), np.sin(ang)], 1).astype(np.float32)
    half = 16
    inv = 10000.0 ** (-np.arange(half, dtype=np.float32) / half)
    ang = np.arange(T, dtype=np.float32)[:, None] * inv[None, :]
    c["rope32"] = np.concatenate([np.cos(ang), np.sin(ang)], 1).astype(np.float32)
    c["lebias"] = np.where(i[:, None] <= i[None, :], 0.0, NEG).astype(np.float32)
    selh = np.zeros((4, 4, 128), np.float32)
    for h in range(4):
        selh[h, h, :] = 1.0
    c["selh"] = selh.reshape(4, 512)
    return c


def load_const(S, nc, st, g, name, shape, dt=F32):
    t = sb(nc, st, "c_" + name, shape, F32)
    S.dma(t[:], g.dram[name])
    if dt == F32:
        return t
    tb = sb(nc, st, "cb_" + name, shape, dt)
    S.copy(tb[:], t[:])
    return tb


def ph_ada(S, nc, g, l):
    with ExitStack() as st:
        cT = sb(nc, st, "cT", [P, 8, 4], F32)
        S.dma(cT[:], g.dram["cT"].rearrange("(c p) b -> p c b", p=P))
        cact = sb(nc, st, "cact", [P, 8, 4], F32)
        S.act(cact[:], cT[:], AF.Silu)
        bias = sb(nc, st, "adab", [P, 48], F32)
        S.dma(bias[:], g.dram["ada_b_fm"][l])
        g1 = sb(nc, st, "g1", [P, 8], F32)
        g2 = sb(nc, st, "g2", [P, 8], F32)
        S.dma(g1[:], g.dram["norm1_g_fm"][l])
        S.dma(g2[:], g.dram["norm2_g_fm"][l])
        wts = [sb(nc, st, "adaw%d" % i, [P, 8, 768], F32) for i in range(2)]
        ps = g.ps[0]
        for cb in range(8):
            wt = wts[cb % 2]
            S.dma(wt[:], g.dram["ada_w"][l][:, cb * 768:(cb + 1) * 768].rearrange("(c p) n -> p c n", p=P))
            for cc in range(6):
                c = cb * 6 + cc
                for k in range(8):
                    S.mm(ps[:, 4 * c:4 * c + 4], wt[:, k, cc * 128:(cc + 1) * 128], cact[:, k, :],
                         start=(k == 0), stop=(k == 7))
        S.tt(g.modT[:], ps[:, 0:192].rearrange("p (c b) -> p c b", b=4),
             bc(bias[:].unsqueeze(2), [P, 48, 4]), ALU.add)
        for (A, gg, off) in ((g.A1, g1, 8), (g.A2, g2, 32)):
            S.ts(A[:], g.modT[:, off:off + 8, :], 1.0, None, ALU.add)
            S.tt(A[:], A[:], bc(gg[:].unsqueeze(2), [P, 8, 4]), ALU.mult)
        S.flush()


def emit_norm_mod(S, nc, g, xT_d, tok0, b, A, shift, hT, col0=1, route=None):
    with ExitStack() as st:
        xs = [sb(nc, st, "xs%d" % i, [P, 8, 512], F32) for i in range(2)]
        sq = sb(nc, st, "sq", [P, 8, 512], F32)
        tmp = sb(nc, st, "tmp", [P, 8, 512], F32)
        rstd = sb(nc, st, "rstd", [P, 512], F32)
        ps = g.ps[1]
        for sblk in range(4):
            x = xs[sblk % 2]
            S.dma(x[:], xT_d[:, tok0 + sblk * 512: tok0 + (sblk + 1) * 512].rearrange("(c p) n -> p c n", p=P))
            S.act(sq[:], x[:], AF.Square)
            for c in range(8):
                S.mm(ps[:, :], g.ones[:], sq[:, c, :], start=(c == 0), stop=(c == 7))
            S.act(rstd[:], ps[:, :], AF.Sqrt, scale=1.0 / D, bias=g.eps6[:, 0:1])
            S.recip(rstd[:], rstd[:])
            S.tt(tmp[:], x[:], bc(rstd[:].unsqueeze(1), [P, 8, 512]), ALU.mult)
            for c in range(8):
                S.act(hT[:, c, col0 + sblk * 512: col0 + (sblk + 1) * 512], tmp[:, c, :], AF.Identity,
                      scale=A[:, c, b:b + 1], bias=shift[:, c, b:b + 1])
            if route is not None:
                wge, lg = route
                for c in range(8):
                    S.act(sq[:, c, :], tmp[:, c, :], AF.Identity, scale=A[:, c, b:b + 1], bias=shift[:, c, b:b + 1])
                for tb4 in range(4):
                    pr = g.ps[2 + (tb4 % 2)]
                    for c in range(8):
                        S.mm(pr[:, 0:36], sq[:, c, tb4 * 128:(tb4 + 1) * 128], wge[:, c, :], start=(c == 0), stop=(c == 7))
                    S.copy(lg[:, sblk * 4 + tb4, :], pr[:, 0:36])
        S.flush()


def load_weights_bf16(S, nc, st, g, w_d, ncols, nshift, mu_d, Wb, W0b):
    stg = [sb(nc, st, "wstg%d" % i, [P, 8, 512], F32) for i in range(2)]
    mu_b = sb(nc, st, "mu_b", [P, max(nshift, 1)], F32)
    tmp = sb(nc, st, "wtmp", [P, 8, 512], F32)
    if nshift:
        S.dma(mu_b[:], mu_d.partition_broadcast(P))
    i = 0
    for c0 in range(0, ncols, 512):
        w = min(512, ncols - c0)
        s_ = stg[i % 2]
        i += 1
        S.dma(s_[:, :, :w], w_d[:, c0:c0 + w].rearrange("(c p) n -> p c n", p=P))
        if c0 < nshift:
            ws = min(w, nshift - c0)
            S.tt(tmp[:, :, :ws], s_[:, :, :ws], bc(mu_b[:, c0:c0 + ws].unsqueeze(1), [P, 8, ws]), ALU.mult, eng="pool")
            S.copy(W0b[:, :, c0:c0 + ws], tmp[:, :, :ws], eng="act")
            S.tt(Wb[:, :, c0:c0 + ws], s_[:, :, :ws], tmp[:, :, :ws], ALU.subtract)
            if ws < w:
                S.copy(Wb[:, :, c0 + ws:c0 + w], s_[:, :, ws:w], eng="act")
        else:
            S.copy(Wb[:, :, c0:c0 + w], s_[:, :, :w], eng=("act" if (i % 2) else "dve"))


def ph_inproj(S, nc, g, l, hT, ptm_d, pfm_d):
    with ExitStack() as st:
        Wb = sb(nc, st, "Wb", [P, 8, NTM], BF16)
        W0b = sb(nc, st, "W0b", [P, 8, 768], BF16)
        load_weights_bf16(S, nc, st, g, g.dram["w_tm"][l], NTM, 768, g.dram["mu_tm"][l], Wb, W0b)
        stg = [sb(nc, st, "ptm_stg%d" % i, [P, NTM], F32) for i in range(2)]
        ev = 0
        for tb in range(16):
            so = stg[tb % 2]
            for c0 in range(0, NTM, 512):
                w = min(512, NTM - c0)
                ps = g.ps[2 + (ev % 4)]
                shifted = c0 < 768
                for k in range(8):
                    S.mm(ps[:, :w], hT[:, k, 1 + tb * 128: 1 + (tb + 1) * 128], Wb[:, k, c0:c0 + w],
                         start=(k == 0), stop=(k == 7 and not shifted))
                if shifted:
                    ws = min(w, 768 - c0)
                    for k in range(8):
                        S.mm(ps[:, :ws], hT[:, k, tb * 128:(tb + 1) * 128], W0b[:, k, c0:c0 + ws],
                             start=False, stop=(k == 7))
                S.copy(so[:, c0:c0 + w], ps[:, :w], eng=("act" if ev % 2 else "dve"))
                ev += 1
            S.dma(ptm_d[tb * 128:(tb + 1) * 128, :], so[:])
        S.flush()
    with ExitStack() as st:
        Wb = sb(nc, st, "Wf", [P, 8, NFM], BF16)
        W0b = sb(nc, st, "W0f", [P, 8, 384], BF16)
        load_weights_bf16(S, nc, st, g, g.dram["w_fm"][l], NFM, 384, g.dram["mu_fm"][l], Wb, W0b)
        stg = [sb(nc, st, "pfm_stg%d" % i, [P, T], F32) for i in range(2)]
        ev = 0
        ci = 0
        for r0 in range(0, NFM, 128):
            m = min(128, NFM - r0)
            so = stg[ci % 2]
            ci += 1
            shifted = r0 < 384
            for sblk in range(4):
                ps = g.ps[2 + (ev % 4)]
                for k in range(8):
                    S.mm(ps[:m, :], Wb[:, k, r0:r0 + m], hT[:, k, 1 + sblk * 512: 1 + (sblk + 1) * 512],
                         start=(k == 0), stop=(k == 7 and not shifted))
                if shifted:
                    for k in range(8):
                        S.mm(ps[:m, :], W0b[:, k, r0:r0 + m], hT[:, k, sblk * 512:(sblk + 1) * 512],
                             start=False, stop=(k == 7))
                S.copy(so[:m, sblk * 512:(sblk + 1) * 512], ps[:m, :], eng=("act" if ev % 2 else "dve"))
                ev += 1
            S.dma(pfm_d[r0:r0 + m, :], so[:m, :])
        S.flush()


def _r(a, b):
    return list(range(a, b))


TM_COLS = (_r(0, 256) + _r(256, 512) + _r(512, 768) + _r(1408, 1664) + _r(2176, 2432) + _r(2432, 2688)
           + _r(2696, 2952) + _r(2952, 3016) + _r(3016, 3080) + _r(3080, 3208) + _r(3208, 3240) + _r(3240, 3244))
FM_COLS = (_r(512, 768) + _r(768, 800) + _r(800, 832) + _r(832, 896) + _r(896, 1152) + _r(1152, 1408)
           + _r(1664, 1920) + _r(1920, 2176) + _r(2688, 2692) + _r(2692, 2696))
assert len(TM_COLS) == NTM and len(FM_COLS) == NFM


def host_shared(inp):
    f = lambda a: np.ascontiguousarray(a, dtype=np.float32)
    L = inp["w_in"].shape[0]
    sh = dict(host_consts())
    sh["ada_w"] = f(inp["ada_w"])
    sh["ada_b_fm"] = f(inp["ada_b"].reshape(L, 48, 128).transpose(0, 2, 1))
    sh["norm1_g_fm"] = f(inp["norm1_g"].reshape(L, 8, 128).transpose(0, 2, 1))
    sh["norm2_g_fm"] = f(inp["norm2_g"].reshape(L, 8, 128).transpose(0, 2, 1))
    sh["w_tm"] = f(inp["w_in"][:, :, TM_COLS])
    sh["w_fm"] = f(inp["w_in"][:, :, FM_COLS])
    sh["mu_tm"] = f(inp["rk_mu"][:, TM_COLS[:768]])
    sh["mu_fm"] = f(inp["rk_mu"][:, FM_COLS[:384]])
    sh["conv_w_fm"] = f(inp["ml_conv_w"].reshape(L, 4, 4, 128).transpose(0, 3, 2, 1))
    sh["conv_b_fm"] = f(inp["ml_conv_b"].reshape(L, 4, 128).transpose(0, 2, 1))
    sh["moe_wge"] = f(np.concatenate([inp["moe_wg"], inp["moe_we"]], -1))
    sh["moe_bge"] = f(np.concatenate([inp["moe_bg"], inp["moe_be"]], -1))
    for k in ("moe_w1", "moe_w3", "moe_w2"):
        sh[k] = f(inp[k])
    for k in ("rk_w0", "rk_w2", "rk_a0", "rk_a2", "rk_g2", "rk_kk", "rk_ka", "rk_rk", "rk_ln_g", "rk_ln_b",
              "sb_norm_g", "ml_norm_g", "ds_qn_g", "ds_kn_g", "ds_out_g", "w_out", "ml_ig_b", "ml_fg_b"):
        sh[k] = f(inp[k])
    return sh


def host_core(inp, core, nseq=NSEQ):
    f = lambda a: np.ascontiguousarray(a, dtype=np.float32)
    x = inp["x"][core * nseq:(core + 1) * nseq]
    d = {}
    d["xT"] = f(x.reshape(nseq * T, D).T)
    cT = np.zeros((D, 4), np.float32)
    cT[:, :nseq] = inp["c"][core * nseq:(core + 1) * nseq].T
    d["cT"] = cT
    return d


def load_row_bcast(S, nc, st, name, row_ap, n):
    t = sb(nc, st, name, [P, n], F32)
    S.dma(t[:], row_ap.partition_broadcast(P))
    return t


def tm_head_rmsnorm(S, nc, st, g, y, nb, gain, eps, per_head_gain=True):
    nh = nb * 4
    yv = y[:].rearrange("p b (h d) -> p (b h) d", d=64)
    sq = sb(nc, st, "rn_sq", [P, nh, 64], F32)
    ss = sb(nc, st, "rn_ss", [P, nh], F32)
    S.tt(sq[:], yv, yv, ALU.mult)
    S.reduce(ss[:], sq[:], ALU.add, AX.X)
    S.act(ss[:], ss[:], AF.Sqrt, scale=1.0 / 64, bias=eps[:, 0:1])
    S.recip(ss[:], ss[:])
    S.tt(yv, yv, bc(ss[:].unsqueeze(2), [P, nh, 64]), ALU.mult)
    if per_head_gain:
        S.tt(y[:], y[:], bc(gain[:].unsqueeze(1), [P, nb, 256]), ALU.mult)
    else:
        S.tt(yv, yv, bc(gain[:, 0:64].unsqueeze(1), [P, nh, 64]), ALU.mult)


def ph_sb(S, nc, g, l, ptm_d, pfm_d, ycat_d):
    with ExitStack() as st:
        q16 = sb(nc, st, "sbq", [P, 2, T], BF16)
        k16 = sb(nc, st, "sbk", [P, 2, T], BF16)
        v16 = sb(nc, st, "sbv", [P, 16, 256], BF16)
        yraw = sb(nc, st, "sby", [P, 16, 256], F32)
        gain = load_row_bcast(S, nc, st, "sbg", g.dram["sb_norm_g"][l], 256)
        with ExitStack() as st2:
            qf = sb(nc, st2, "sbqf", [P, 2, T], F32)
            kf = sb(nc, st2, "sbkf", [P, 2, T], F32)
            vf = sb(nc, st2, "sbvf", [P, 16, 256], F32)
            S.dma(qf[:], pfm_d[FM_BQ:FM_BQ + 256, :].rearrange("(c p) t -> p c t", p=P))
            S.dma(kf[:], pfm_d[FM_BK:FM_BK + 256, :].rearrange("(c p) t -> p c t", p=P))
            S.dma(vf[:], ptm_d[:, TM_BV:TM_BV + 256].rearrange("(b p) n -> p b n", p=P))
            S.copy(q16[:], qf[:], eng="act")
            S.copy(k16[:], kf[:], eng="dve")
            S.copy(v16[:], vf[:], eng="pool")
            S.flush()
        e1 = [sb(nc, st, "sbe%d" % i, [P, 512], F32) for i in range(2)]
        lt = [sb(nc, st, "sbl%d" % i, [P, 512], F32) for i in range(2)]
        Lm = [sb(nc, st, "sbL%d" % i, [P, 512], F32) for i in range(2)]
        aa = [sb(nc, st, "sba%d" % i, [P, 512], F32) for i in range(2)]
        attA = [sb(nc, st, "sbt%d" % i, [P, 16, 512], BF16) for i in range(2)]
        TotB = sb(nc, st, "sbT", [P, 512], F32)
        iters = [(h, I, j) for h in range(4) for I in range(4) for j in range(4 * I + 3, -1, -1)]

        def geom(n):
            h, I, j = iters[n]
            d = j - 4 * I
            dd = max(d, 0)
            c0 = dd * 128
            return h, I, j, d, c0, 512 - c0, I * 512 + c0

        def s1(n):
            h, I, j, d, c0, nn, q0 = geom(n)
            u = n % 2
            c, pb = h // 2, (h % 2) * 64
            pz = g.ps[u]
            S.mm(pz[:, :nn], k16[pb:pb + 64, c, j * 128:(j + 1) * 128], q16[pb:pb + 64, c, q0:q0 + nn])
            S.act(e1[u][:, :nn], pz[:, :nn], AF.Exp, scale=-0.125)
            S.act(lt[u][:, :nn], e1[u][:, :nn], AF.Ln, bias=g.one1[:, 0:1])
            S.stt(Lm[u][:, :nn], pz[:, :nn], -0.125, lt[u][:, :nn], ALU.mult, ALU.subtract)
            if d >= 0:
                S.tt(Lm[u][:, 0:128], Lm[u][:, 0:128], g.tri_lt[:], ALU.mult)

        def s2(n):
            h, I, j, d, c0, nn, q0 = geom(n)
            u = n % 2
            pr, pt = g.ps[2 + u], g.ps[4 + u]
            att_all = attA[(h * 4 + I) % 2]
            if j == 4 * I + 3:
                S.memset(TotB[:], 0.0, eng="pool")
            S.mm(pr[:, :nn], g.tri_gt[:], Lm[u][:, :nn])
            S.tt(aa[u][:, :nn], pr[:, :nn], TotB[:, c0:512], ALU.add)
            S.tt(aa[u][:, :nn], aa[u][:, :nn], lt[u][:, :nn], ALU.subtract, eng="pool")
            S.act(att_all[:, j, c0:512], aa[u][:, :nn], AF.Exp)
            if d >= 0:
                S.tt(att_all[:, j, c0:c0 + 128], att_all[:, j, c0:c0 + 128], g.tri_lt16[:], ALU.mult, eng="pool")
            if j > 0:
                S.mm(pt[:, :nn], g.ones[:], Lm[u][:, :nn])
                S.tt(TotB[:, c0:512], TotB[:, c0:512], pt[:, :nn], ALU.add)
            else:
                po = g.ps[6 + (I % 2)]
                for qb in range(4):
                    for jj in range(4 * I + qb, -1, -1):
                        S.mm(po[:, qb * 64:(qb + 1) * 64], att_all[:, jj, qb * 128:(qb + 1) * 128],
                             v16[:, jj, h * 64:(h + 1) * 64], start=(jj == 4 * I + qb), stop=(jj == 0))
                S.copy(yraw[:, 4 * I:4 * I + 4, h * 64:(h + 1) * 64],
                       po[:, 0:256].rearrange("p (b d) -> p b d", d=64), eng="act")

        s1(0)
        for n in range(len(iters)):
            if n + 1 < len(iters):
                s1(n + 1)
            s2(n)
        if getattr(g, "debug", False):
            S.dma(ycat_d[:, 0:256].rearrange("(b p) n -> p b n", p=P), yraw[:])
        tm_head_rmsnorm(S, nc, st, g, yraw, 16, gain, g.eps6)
        S.dma(ycat_d[:, 256:512].rearrange("(b p) n -> p b n", p=P), yraw[:])
        S.flush()


def setup_consts(S, nc, st, g):
    def ld(name, shape, src=None):
        t = sb(nc, st, "k_" + name, shape, F32)
        S.dma(t[:], g.dram[src or name])
        return t
    g.ones = ld("ones", [P, P])
    g.ident = ld("ident", [P, P])
    g.tri_lt = ld("tri_lt", [P, P])
    g.tri_le = ld("tri_le", [P, P])
    g.tri_gt = ld("tri_gt", [P, P])
    g.cbias = ld("cbias", [P, P])
    g.lebias = ld("lebias", [P, P])
    g.selh = sb(nc, st, "k_selh", [4, 4, P], F32)
    S.dma(g.selh[:], g.dram["selh"].rearrange("k (h m) -> k h m", m=P))
    g.tri_lt16 = sb(nc, st, "k_tri_lt16", [P, P], BF16)
    g.tri_le16 = sb(nc, st, "k_tri_le16", [P, P], BF16)
    g.ident16 = sb(nc, st, "k_ident16", [P, P], BF16)
    g.ones16 = sb(nc, st, "k_ones16", [P, P], BF16)
    S.copy(g.tri_lt16[:], g.tri_lt[:])
    S.copy(g.tri_le16[:], g.tri_le[:])
    S.copy(g.ident16[:], g.ident[:])
    S.copy(g.ones16[:], g.ones[:])
    g.eps6 = sb(nc, st, "k_eps6", [P, 1], F32)
    S.memset(g.eps6[:], 1e-6)
    g.one1 = sb(nc, st, "k_one1", [P, 1], F32)
    S.memset(g.one1[:], 1.0)
    g.modT = sb(nc, st, "modT", [P, 48, 4], F32)
    g.A1 = sb(nc, st, "A1", [P, 8, 4], F32)
    g.A2 = sb(nc, st, "A2", [P, 8, 4], F32)
    S.flush()


def ph_ml(S, nc, g, l, ptm_d, pfm_d, ycat_d):
    LN8 = float(np.log(0.125))
    with ExitStack() as st:
        q16 = sb(nc, st, "mlq", [P, 2, T], BF16)
        k16 = sb(nc, st, "mlk", [P, 2, T], BF16)
        v16 = sb(nc, st, "mlv", [P, 16, 4, 65], BF16)
        osig = sb(nc, st, "mlo", [P, 16, 256], F32)
        BtB = [sb(nc, st, "mlB%d" % h, [P, T], F32) for h in range(4)]
        c_tm = sb(nc, st, "mlc", [P, 16, 4], F32)
        gain = load_row_bcast(S, nc, st, "mlg", g.dram["ml_norm_g"][l], 256)
        with ExitStack() as st2:
            xq = sb(nc, st2, "mlxq", [P, 2, T + 3], F32)
            xk = sb(nc, st2, "mlxk", [P, 2, T + 3], F32)
            S.memset(xq[:, :, 0:3], 0.0)
            S.memset(xk[:, :, 0:3], 0.0)
            S.dma(xq[:, :, 3:T + 3], pfm_d[FM_CQ:FM_CQ + 256, :].rearrange("(c p) t -> p c t", p=P))
            S.dma(xk[:, :, 3:T + 3], pfm_d[FM_CK:FM_CK + 256, :].rearrange("(c p) t -> p c t", p=P))
            cw = sb(nc, st2, "mlcw", [P, 4, 4], F32)
            cb = sb(nc, st2, "mlcb", [P, 4], F32)
            S.dma(cw[:], g.dram["conv_w_fm"][l])
            S.dma(cb[:], g.dram["conv_b_fm"][l])
            acc = [sb(nc, st2, "mlacc%d" % i, [P, T], F32) for i in range(2)]
            ai = 0
            for (x, dst, ci0) in ((xq, q16, 0), (xk, k16, 2)):
                for c in range(2):
                    a = acc[ai % 2]
                    eng = "dve"
                    ai += 1
                    S.ts(a[:], x[:, c, 0:T], cw[:, ci0 + c, 0:1], None, ALU.mult, eng=eng)
                    for tap in range(1, 4):
                        S.stt(a[:], x[:, c, tap:T + tap], cw[:, ci0 + c, tap:tap + 1], a[:], ALU.mult, ALU.add, eng=eng)
                    S.act(dst[:, c, :], a[:], AF.Silu, bias=cb[:, ci0 + c:ci0 + c + 1])
            ig = sb(nc, st2, "mlig", [4, T], F32)
            fg = sb(nc, st2, "mlfg", [4, T], F32)
            S.dma(ig[:], pfm_d[FM_CIG:FM_CIG + 4, :])
            S.dma(fg[:], pfm_d[FM_CFG:FM_CFG + 4, :])
            gb = sb(nc, st2, "mlgb", [4, 2], F32)
            S.dma(gb[:, 0:1], g.dram["ml_ig_b"][l].rearrange("(h o) -> h o", o=1))
            S.dma(gb[:, 1:2], g.dram["ml_fg_b"][l].rearrange("(h o) -> h o", o=1))
            S.ts(gb[:], gb[:], 1.0 / 15.0, None, ALU.mult)
            S.act(ig[:], ig[:], AF.Tanh, scale=1.0 / 15.0, bias=gb[:, 0:1])
            S.act(fg[:], fg[:], AF.Tanh, scale=1.0 / 15.0, bias=gb[:, 1:2])
            S.act(fg[:], fg[:], AF.Exp, scale=-15.0)
            S.act(fg[:], fg[:], AF.Ln, bias=g.one1[0:4, 0:1])
            ones4 = sb(nc, st2, "mlones", [4, T], F32)
            S.memset(ones4[:], 1.0)
            Bn = sb(nc, st2, "mlBn", [4, T], F32)
            S.scan(Bn[:], ones4[:], fg[:], 0.0, ALU.mult, ALU.add)
            cT = sb(nc, st2, "mlcT", [4, T], F32)
            S.stt(cT[:], ig[:], 15.0, Bn[:], ALU.mult, ALU.add)
            S.ts(cT[:], cT[:], LN8, None, ALU.add)
            BT = sb(nc, st2, "mlBT", [4, T], F32)
            S.ts(BT[:], Bn[:], -1.0, None, ALU.mult)
            ev = 0
            for h in range(4):
                for sblk in range(4):
                    ps = g.ps[ev % 4]
                    S.mm(ps[:, :], g.selh[0:4, h, :], BT[0:4, sblk * 512:(sblk + 1) * 512])
                    S.copy(BtB[h][:, sblk * 512:(sblk + 1) * 512], ps[:, :], eng=("act" if ev % 2 else "dve"))
                    ev += 1
            pc = g.ps[4]
            for b in range(16):
                S.mm(pc[:, b * 4:(b + 1) * 4], cT[0:4, b * 128:(b + 1) * 128], g.ident[0:4, 0:4])
            S.copy(c_tm[:], pc[:, 0:64].rearrange("p (b h) -> p b h", h=4))
            vf = sb(nc, st2, "mlvf", [P, 16, 256], F32)
            S.dma(vf[:], ptm_d[:, TM_CV:TM_CV + 256].rearrange("(b p) n -> p b n", p=P))
            S.copy(v16[:, :, :, 0:64], vf[:].rearrange("p b (h d) -> p b h d", d=64), eng="pool")
            S.memset(v16[:, :, :, 64:65], 1.0, eng="pool")
            S.dma(osig[:], ptm_d[:, TM_CO:TM_CO + 256].rearrange("(b p) n -> p b n", p=P))
            S.act(osig[:], osig[:], AF.Sigmoid)
            S.flush()
        attA = [sb(nc, st, "mlt%d" % i, [P, 16, 512], BF16) for i in range(2)]
        Dm = [sb(nc, st, "mlD%d" % i, [P, 512], F32) for i in range(2)]
        dtmp = [sb(nc, st, "mldt%d" % i, [P, 128], F32) for i in range(2)]
        nd = [sb(nc, st, "mlnd%d" % i, [P, 4, 65], F32) for i in range(2)]
        dn = [sb(nc, st, "mldn%d" % i, [P, 4], F32) for i in range(2)]
        hraw = sb(nc, st, "mlh", [P, 16, 256], F32)
        it = 0
        for h in range(4):
            c = h // 2
            pb = (h % 2) * 64
            for I in range(4):
                gi = h * 4 + I
                po = g.ps[6 + (gi % 2)]
                att_all = attA[gi % 2]
                for j in range(4 * I + 3, -1, -1):
                    u = it % 2
                    it += 1
                    pz = g.ps[u]
                    d = j - 4 * I
                    dd = max(d, 0)
                    c0 = dd * 128
                    n = 512 - c0
                    q0 = I * 512 + c0
                    S.mm(pz[:, :n], k16[pb:pb + 64, c, j * 128:(j + 1) * 128], q16[pb:pb + 64, c, q0:q0 + n])
                    if d >= 0:
                        S.tt(dtmp[u][:], BtB[h][:, q0:q0 + 128], g.lebias[:], ALU.add, eng="pool")
                        S.act(Dm[u][:, 0:128], dtmp[u][:], AF.Exp, bias=c_tm[:, j, h:h + 1])
                        if n > 128:
                            S.act(Dm[u][:, 128:n], BtB[h][:, q0 + 128:q0 + n], AF.Exp, bias=c_tm[:, j, h:h + 1])
                    else:
                        S.act(Dm[u][:, :n], BtB[h][:, q0:q0 + n], AF.Exp, bias=c_tm[:, j, h:h + 1])
                    S.tt(att_all[:, j, c0:512], pz[:, :n], Dm[u][:, :n], ALU.mult)
                for qb in range(4):
                    for j in range(4 * I + qb, -1, -1):
                        S.mm(po[:, qb * 65:(qb + 1) * 65], att_all[:, j, qb * 128:(qb + 1) * 128],
                             v16[:, j, h, :], start=(j == 4 * I + qb), stop=(j == 0))
                u2 = gi % 2
                S.copy(nd[u2][:], po[:, 0:260].rearrange("p (b d) -> p b d", d=65), eng="act")
                S.stt(dn[u2][:], nd[u2][:, :, 64], -1.0, nd[u2][:, :, 64], ALU.mult, ALU.max)
                S.ts(dn[u2][:], dn[u2][:], 1.0, None, ALU.max)
                S.recip(dn[u2][:], dn[u2][:])
                S.tt(hraw[:, 4 * I:4 * I + 4, h * 64:(h + 1) * 64], nd[u2][:, :, 0:64],
                     bc(dn[u2][:].unsqueeze(2), [P, 4, 64]), ALU.mult)
        tm_head_rmsnorm(S, nc, st, g, hraw, 16, gain, g.eps6)
        S.tt(hraw[:], hraw[:], osig[:], ALU.mult)
        S.dma(ycat_d[:, 512:768].rearrange("(b p) n -> p b n", p=P), hraw[:])
        S.flush()


def _rope_tm(S, out, x, cos, sin, t1, t2, half):
    x1, x2 = x[:, :, 0:half], x[:, :, half:2 * half]
    S.tt(t1, x1, cos, ALU.mult)
    S.tt(t2, x2, sin, ALU.mult)
    S.tt(out[:, :, 0:half], t1, t2, ALU.subtract)
    S.tt(t1, x2, cos, ALU.mult)
    S.tt(t2, x1, sin, ALU.mult)
    S.tt(out[:, :, half:2 * half], t1, t2, ALU.add)


def ph_dsa(S, nc, g, l, ptm_d, ycat_d):
    WI_SCALE = float(4 ** -0.5 * 32 ** -0.5)
    with ExitStack() as st:
        qT = sb(nc, st, "dqT", [P, 2, T], BF16)
        kT2 = sb(nc, st, "dkT", [P, T], BF16)
        qiT = sb(nc, st, "dqi", [P, T], F32)
        kiX = [sb(nc, st, "dki%d" % h, [P, T], F32) for h in range(4)]
        wi = sb(nc, st, "dwi", [P, 16, 4], F32)
        v16 = sb(nc, st, "dv", [P, 16, 65], BF16)
        gain = load_row_bcast(S, nc, st, "dg", g.dram["ds_out_g"][l], 256)
        with ExitStack() as st2:
            rope64 = sb(nc, st2, "rope64", [P, 16, 64], F32)
            rope32 = sb(nc, st2, "rope32", [P, 16, 32], F32)
            S.dma(rope64[:], g.dram["rope64"].rearrange("(b p) n -> p b n", p=P))
            S.dma(rope32[:], g.dram["rope32"].rearrange("(b p) n -> p b n", p=P))
            gq = load_row_bcast(S, nc, st2, "dgq", g.dram["ds_qn_g"][l], 64)
            gk = load_row_bcast(S, nc, st2, "dgk", g.dram["ds_kn_g"][l], 64)
            xs = [sb(nc, st2, "dx%d" % i, [P, 548], F32) for i in range(2)]
            sq = sb(nc, st2, "dsq", [P, 5, 64], F32)
            ss = sb(nc, st2, "dss", [P, 5], F32)
            qn = sb(nc, st2, "dqn", [P, 5, 64], F32)
            qr = [sb(nc, st2, "dqr%d" % i, [P, 6, 64], F32) for i in range(2)]
            qir = [sb(nc, st2, "dqir%d" % i, [P, 5, 32], F32) for i in range(2)]
            t1 = sb(nc, st2, "dt1", [P, 5, 32], F32)
            t2 = sb(nc, st2, "dt2", [P, 5, 32], F32)
            t3 = sb(nc, st2, "dt3", [P, 5, 16], F32)
            t4 = sb(nc, st2, "dt4", [P, 5, 16], F32)
            kiz = [[sb(nc, st2, "dkz%d_%d" % (i, h), [P, 128], F32) for h in range(4)] for i in range(2)]
            for i in range(2):
                for h in range(4):
                    S.memset(kiz[i][h][:], 0.0, eng="pool")
            S.dma(wi[:], ptm_d[:, TM_DWI:TM_DWI + 4].rearrange("(b p) n -> p b n", p=P))
            S.ts(wi[:], wi[:], WI_SCALE, None, ALU.mult)
            ev = 0
            cut = getattr(g, 'dsa_cut', 0)
            for b in range(16):
                x = xs[b % 2]
                S.dma(x[:], ptm_d[b * 128:(b + 1) * 128, TM_DQ:TM_DQ + 548])
                qk = x[:, 0:320].rearrange("p (h d) -> p h d", d=64)
                S.tt(sq[:], qk, qk, ALU.mult)
                S.reduce(ss[:], sq[:], ALU.add, AX.X)
                S.act(ss[:], ss[:], AF.Sqrt, scale=1.0 / 64, bias=g.eps6[:, 0:1])
                S.recip(ss[:], ss[:])
                S.tt(qn[:], qk, bc(ss[:].unsqueeze(2), [P, 5, 64]), ALU.mult)
                S.tt(qn[:, 0:4, :], qn[:, 0:4, :], bc(gq[:].unsqueeze(1), [P, 4, 64]), ALU.mult)
                S.tt(qn[:, 4:5, :], qn[:, 4:5, :], gk[:].unsqueeze(1), ALU.mult)
                r_ = qr[b % 2]
                cos = bc(rope64[:, b, 0:32].unsqueeze(1), [P, 5, 32])
                sin = bc(rope64[:, b, 32:64].unsqueeze(1), [P, 5, 32])
                _rope_tm(S, r_[:, 0:5, :], qn[:], cos, sin, t1[:], t2[:], 32)
                S.copy(r_[:, 5, :], r_[:, 4, :], eng="act")
                ri = qir[b % 2]
                xi = x[:, 384:544].rearrange("p (h d) -> p h d", d=32)
                cos = bc(rope32[:, b, 0:16].unsqueeze(1), [P, 5, 16])
                sin = bc(rope32[:, b, 16:32].unsqueeze(1), [P, 5, 16])
                _rope_tm(S, ri[:], xi, cos, sin, t3[:], t4[:], 16)
                S.copy(v16[:, b, 0:64], x[:, 320:384], eng="act")
                if cut == 1:
                    continue
                tb = slice(b * 128, (b + 1) * 128)
                for c in range(3):
                    ps = g.ps[ev % 4]
                    ev += 1
                    S.transpose(ps[:, 0:128], r_[:, 2 * c:2 * c + 2, :].rearrange("p h d -> p (h d)"), g.ident[:])
                    if c < 2:
                        S.copy(qT[:, c, tb], ps[:, 0:128], eng="act")
                    else:
                        S.copy(kT2[:, tb], ps[:, 0:128], eng="act")
                if cut == 2:
                    continue
                ps = g.ps[ev % 4]
                ev += 1
                S.transpose(ps[:, 0:128], ri[:, 0:4, :].rearrange("p h d -> p (h d)"), g.ident[:])
                S.copy(qiT[:, tb], ps[:, 0:128], eng="dve")
                kz = kiz[b % 2]
                for h in range(4):
                    S.copy(kz[h][:, h * 32:(h + 1) * 32], ri[:, 4, :], eng="pool")
                    ps = g.ps[ev % 4]
                    ev += 1
                    S.transpose(ps[:, 0:128], kz[h][:], g.ident[:])
                    S.copy(kiX[h][:, tb], ps[:, 0:128], eng=("act" if h % 2 else "dve"))
            S.memset(v16[:, :, 64:65], 1.0, eng="pool")
            S.flush()
        if getattr(g, "dsa_stop", 0) == 1:
            return
        sc = [sb(nc, st, "dsc%d" % i, [P, T], F32) for i in range(1)] * 2
        work = sb(nc, st, "dwork", [P, T], F32)
        mk = sb(nc, st, "dmk", [P, T], F32)
        eqm = sb(nc, st, "deq", [P, T], F32)
        cum = sb(nc, st, "dcum", [P, T], F32)
        onesT = sb(nc, st, "dones", [P, T], F32)
        S.memset(onesT[:], 1.0, eng="pool")
        rl = [sb(nc, st, "drl%d" % i, [P, 512], F32) for i in range(2)]
        m8 = sb(nc, st, "dm8", [P, 8], F32)
        ngt = sb(nc, st, "dngt", [P, 1], F32)
        maskT = [sb(nc, st, "dmT%d" % i, [P, 16, 128], F32) for i in range(2)]
        E = [sb(nc, st, "dE%d" % i, [P, 512], F32) for i in range(2)]
        Pall = [sb(nc, st, "dP%d" % i, [P, 16, 512], BF16) for i in range(2)]
        nd = [sb(nc, st, "dnd%d" % i, [P, 4, 65], F32) for i in range(2)]
        dn = [sb(nc, st, "ddn%d" % i, [P, 4], F32) for i in range(2)]
        yraw = sb(nc, st, "dy", [P, 16, 256], F32)
        cnt = {'ev': 0, 'it': 0}

        def part_a(i):
            ev = cnt['ev']
            kl = 128 * (i + 1)
            qb = slice(i * 128, (i + 1) * 128)
            mT = maskT[i % 2]
            if i >= 2:
                s_ = sc[i % 2]
                for kb in range(0, kl, 512):
                    w = min(512, kl - kb)
                    for h in range(4):
                        ps = g.ps[ev % 2]
                        r2 = rl[ev % 2]
                        ev += 1
                        S.mm(ps[:, :w], qiT[:, qb], kiX[h][:, kb:kb + w])
                        S.act(r2[:, :w], ps[:, :w], AF.Relu)
                        if h == 0:
                            S.ts(s_[:, kb:kb + w], r2[:, :w], wi[:, i, 0:1], None, ALU.mult)
                        else:
                            S.stt(s_[:, kb:kb + w], r2[:, :w], wi[:, i, h:h + 1], s_[:, kb:kb + w], ALU.mult, ALU.add)
                S.tt(s_[:, qb], s_[:, qb], g.cbias[:], ALU.add)
                for r in range(32):
                    S.max8(m8[:], (s_ if r == 0 else work)[:, :kl])
                    if r < 31:
                        S.match_replace(work[:, :kl], m8[:], (s_ if r == 0 else work)[:, :kl], NEG)
                S.ts(mk[:, :kl], s_[:, :kl], m8[:, 7:8], None, ALU.is_gt)
                S.reduce(ngt[:], mk[:, :kl], ALU.add, AX.X)
                S.ts(ngt[:], ngt[:], -1.0, 256.0, ALU.mult, ALU.add)
                S.ts(eqm[:, :kl], s_[:, :kl], m8[:, 7:8], None, ALU.is_equal)
                S.scan(cum[:, :kl], onesT[:, :kl], eqm[:, :kl], 0.0, ALU.mult, ALU.add)
                S.ts(cum[:, :kl], cum[:, :kl], ngt[:, 0:1], None, ALU.is_le)
                S.tt(eqm[:, :kl], eqm[:, :kl], cum[:, :kl], ALU.mult)
                S.tt(mk[:, :kl], mk[:, :kl], eqm[:, :kl], ALU.add)
                for j0 in range(0, i + 1, 4):
                    nj = min(4, i + 1 - j0)
                    ps = g.ps[1]
                    ev += 1
                    for jj in range(nj):
                        S.transpose(ps[:, jj * 128:(jj + 1) * 128], mk[:, (j0 + jj) * 128:(j0 + jj + 1) * 128], g.ident[:])
                    S.copy(mT[:, j0:j0 + nj, :], ps[:, 0:nj * 128].rearrange("p (j q) -> p j q", q=128), eng="act")
            else:
                for j in range(i):
                    S.copy(mT[:, j, :], g.ones[:], eng="dve")
                S.copy(mT[:, i, :], g.tri_le[:], eng="dve")
            cnt['ev'] = ev

        def part_b(i):
            it = cnt['it']
            qb = slice(i * 128, (i + 1) * 128)
            mT = maskT[i % 2]
            Pa = Pall[i % 2]
            for j in range(i + 1):
                u = it % 2
                it += 1
                pzA, pzB = g.ps[2 + 2 * u], g.ps[3 + 2 * u]
                for h in range(4):
                    pb = (h % 2) * 64
                    pz = pzB if (h % 2) else pzA
                    S.mm(pz[:, (h // 2) * 128:(h // 2 + 1) * 128], kT2[pb:pb + 64, j * 128:(j + 1) * 128], qT[pb:pb + 64, h // 2, qb])
                S.act(E[u][:, 0:256], pzA[:, 0:256], AF.Exp, scale=0.125)
                S.act(E[u][:, 256:512], pzB[:, 0:256], AF.Exp, scale=0.125)
                S.tt(Pa[:, j, :].rearrange("p (h q) -> p h q", q=128), E[u][:].rearrange("p (h q) -> p h q", q=128),
                     bc(mT[:, j, :].unsqueeze(1), [P, 4, 128]), ALU.mult, eng="pool")
            if getattr(g, "dsa_cut2", 0) >= 1:
                return
            po = g.ps[6 + (i % 2)]
            for h in range(4):
                for j in range(i + 1):
                    hr = (h % 2) * 2 + h // 2
                    S.mm(po[:, h * 65:(h + 1) * 65], Pa[:, j, hr * 128:(hr + 1) * 128], v16[:, j, :],
                         start=(j == 0), stop=(j == i))
            u2 = i % 2
            S.copy(nd[u2][:], po[:, 0:260].rearrange("p (h d) -> p h d", d=65), eng="act")
            S.recip(dn[u2][:], nd[u2][:, :, 64])
            S.tt(yraw[:, i, :].rearrange("p (h d) -> p h d", d=64), nd[u2][:, :, 0:64],
                 bc(dn[u2][:].unsqueeze(2), [P, 4, 64]), ALU.mult)
            cnt['it'] = it

        nblk = getattr(g, "dsa_nblk", 16)
        part_a(0)
        for i in range(nblk):
            if i + 1 < nblk:
                part_a(i + 1)
            part_b(i)
        tm_head_rmsnorm(S, nc, st, g, yraw, 16, gain, g.eps6)
        S.dma(ycat_d[:, 768:1024].rearrange("(b p) n -> p b n", p=P), yraw[:])
        S.flush()


def ph_rwkv_prep(S, nc, g, l, s, ptm_d, pfm_d, sops_d, sv_d, sbg_d):
    x_, sp = s // 2, s % 2
    with ExitStack() as st:
        twT = sb(nc, st, "rtw", [33, T], F32)
        adT = sb(nc, st, "rad", [33, T], F32)
        sgT = sb(nc, st, "rsg", [64, T], F32)
        S.memset(twT[:], 1.0)
        S.memset(adT[:], 1.0, eng="pool")
        S.dma(twT[0:32, :], pfm_d[FM_AWD:FM_AWD + 32, :])
        S.dma(adT[0:32, :], pfm_d[FM_AAD:FM_AAD + 32, :])
        S.dma(sgT[:], pfm_d[FM_AGD:FM_AGD + 64, :])
        S.act(twT[0:32, :], twT[0:32, :], AF.Tanh)
        S.act(sgT[:], sgT[:], AF.Sigmoid)
        w2a = sb(nc, st, "rw2", [33, 256], F32)
        a2a = sb(nc, st, "ra2", [33, 256], F32)
        g2 = sb(nc, st, "rg2", [64, 256], F32)
        S.dma(w2a[0:32, :], g.dram["rk_w2"][l])
        S.dma(w2a[32:33, :], g.dram["rk_w0"][l].rearrange("(o n) -> o n", o=1))
        S.dma(a2a[0:32, :], g.dram["rk_a2"][l])
        S.dma(a2a[32:33, :], g.dram["rk_a0"][l].rearrange("(o n) -> o n", o=1))
        S.dma(g2[:], g.dram["rk_g2"][l])
        kk_b = load_row_bcast(S, nc, st, "rkk", g.dram["rk_kk"][l], 256)
        ka_b = load_row_bcast(S, nc, st, "rka", g.dram["rk_ka"][l], 256)
        rk_b = load_row_bcast(S, nc, st, "rrk", g.dram["rk_rk"][l], 256)
        vfm = sb(nc, st, "rvfm", [P, 2, T], F32)
        S.dma(vfm[:], pfm_d[FM_AV:FM_AV + 256, :].rearrange("(c p) t -> p c t", p=P))
        S.dma(sv_d[s].rearrange("(c p) t -> p c t", p=P), vfm[:])
        xs = [sb(nc, st, "rx%d" % i, [P, 768], F32) for i in range(2)]
        F = lambda n: [sb(nc, st, "%s%d" % (n, i), [P, 256], F32) for i in range(2)]
        sig, dec, a_, kkn, k2, nkka, tmp = F("rsig"), F("rdec"), F("ra"), F("rkkn"), F("rk2"), F("rnk"), F("rtmp")
        bg = [sb(nc, st, "rbg%d" % i, [P, 512], F32) for i in range(2)]
        ss = sb(nc, st, "rss", [P, 4], F32)
        bco = sb(nc, st, "rbc", [P, 4], F32)
        hi = [[sb(nc, st, "rhi%d_%d" % (i, o), [P, 256], BF16) for o in range(5)] for i in range(2)]
        lo = [[sb(nc, st, "rlo%d_%d" % (i, o), [P, 256], BF16) for o in range(5)] for i in range(2)]
        h32 = [sb(nc, st, "rh32_%d" % i, [P, 256], F32) for i in range(2)]
        hv = lambda ap: ap.rearrange("p (h d) -> p h d", d=64)
        for tb in range(16):
            u = tb % 2
            x = xs[u]
            tsl = slice(tb * 128, (tb + 1) * 128)
            S.dma(x[:], ptm_d[tsl, 0:768])
            r, k, v = x[:, 0:256], x[:, 256:512], x[:, 512:768]
            pw, pa, pg = g.ps[0 + u], g.ps[2 + u], g.ps[4 + u]
            S.mm(pw[:, 0:256], twT[0:33, tsl], w2a[0:33, :])
            S.mm(pa[:, 0:256], adT[0:33, tsl], a2a[0:33, :])
            S.mm(pg[:, 0:256], sgT[0:64, tsl], g2[0:64, :])
            S.act(sig[u][:], pw[:, 0:256], AF.Sigmoid)
            S.act(a_[u][:], pa[:, 0:256], AF.Sigmoid)
            S.act(dec[u][:], sig[u][:], AF.Exp, scale=-0.6065306597126334)
            S.copy(bg[u][:, 256:512], pg[:, 0:256], eng="act")
            S.tt(kkn[u][:], k, kk_b[:], ALU.mult)
            S.tt(tmp[u][:], kkn[u][:], kkn[u][:], ALU.mult)
            S.reduce(ss[:], hv(tmp[u][:]), ALU.add, AX.X)
            S.act(ss[:], ss[:], AF.Sqrt)
            S.ts(ss[:], ss[:], 1e-12, None, ALU.max)
            S.recip(ss[:], ss[:])
            S.tt(hv(kkn[u][:]), hv(kkn[u][:]), bc(ss[:].unsqueeze(2), [P, 4, 64]), ALU.mult)
            S.stt(k2[u][:], a_[u][:], -1.0, ka_b[:], ALU.add, ALU.mult)
            S.stt(k2[u][:], k2[u][:], 1.0, k, ALU.add, ALU.mult)
            S.stt(nkka[u][:], kkn[u][:], -1.0, a_[u][:], ALU.mult, ALU.mult)
            S.tt(tmp[u][:], r, k2[u][:], ALU.mult)
            S.tt(tmp[u][:], tmp[u][:], rk_b[:], ALU.mult)
            S.reduce(bco[:], hv(tmp[u][:]), ALU.add, AX.X)
            S.tt(hv(bg[u][:, 0:256]), hv(v), bc(bco[:].unsqueeze(2), [P, 4, 64]), ALU.mult)
            S.dma(sbg_d[s, tsl, :], bg[u][:])
            for o, src in enumerate((kkn[u][:], dec[u][:], nkka[u][:], k2[u][:], r)):
                S.copy(hi[u][o][:], src, eng="act")
                S.copy(h32[o % 2][:], hi[u][o][:], eng="pool")
                S.tt(h32[o % 2][:], src, h32[o % 2][:], ALU.subtract, eng="pool")
                S.copy(lo[u][o][:], h32[o % 2][:], eng="act")
                S.dma(sops_d[o, 0, x_, tsl, sp * 256:(sp + 1) * 256], hi[u][o][:])
                S.dma(sops_d[o, 1, x_, tsl, sp * 256:(sp + 1) * 256], lo[u][o][:])
        S.flush()


SCAN_ACT_Y = False


def ph_rwkv_scan(S, nc, g, sops_d, sv_d, sy_d, nsteps=T):
    CH = 32
    with ExitStack() as st:
        id2 = sb(nc, st, "sid2", [P, 128], BF16)
        S.memset(id2[:], 0.0)
        S.tt(id2[:, 0:32], g.ident16[:, 0:32], g.ident16[:, 32:64], ALU.add)
        S.tt(id2[:, 64:96], g.ident16[:, 64:96], g.ident16[:, 96:128], ALU.add)
        sel = sb(nc, st, "ssel", [P, CH, 128], BF16)
        for xp in range(2):
            for tp in range(CH):
                col = xp * 64 + tp
                S.copy(sel[:, tp, xp * 64:(xp + 1) * 64], bc(id2[:, col:col + 1], [P, 64]),
                       eng=("dve" if tp % 2 else "pool"))
        Stt = [sb(nc, st, "sS%d" % i, [P, 512], F32) for i in range(2)]
        S.memset(Stt[0][:], 0.0)
        S.memset(Stt[1][:], 0.0)
        ytmps = [sb(nc, st, "sytmp%d" % i, [P, 512], F32) for i in range(2)]
        junk = sb(nc, st, "sjunk", [P, 512], F32)
        Sw = sb(nc, st, "sSw", [P, 512], F32)
        tmp = sb(nc, st, "stmp", [P, 512], F32)
        sa = sb(nc, st, "ssa", [P, 8], F32)
        ND = 3
        ringW = [sb(nc, st, "srgW%d" % d, [P, 512], F32) for d in range(ND)]
        ringK = [sb(nc, st, "srgK%d" % d, [P, 512], F32) for d in range(ND)]
        vk = [sb(nc, st, "svk%d" % d, [P, 512], F32) for d in range(ND)]
        opt = [[sb(nc, st, "sop%d_%d" % (b_, o), [P, 512], BF16) for o in range(5)] for b_ in range(3)]
        vS = [sb(nc, st, "svS%d" % i, [P, 8, 256], F32) for i in range(2)]
        yb = [sb(nc, st, "syb%d" % i, [P, 8, 256], F32) for i in range(2)]
        g3 = lambda ap: ap.rearrange("p (g k) -> p g k", k=64)
        step = 0
        pend = None
        nbig = (nsteps + 255) // 256
        for big in range(nbig):
            vs, y_ = vS[big % 2], yb[big % 2]
            bsl = slice(big * 256, (big + 1) * 256)
            for x in range(2):
                for sp in range(2):
                    for h in range(4):
                        S.dma(vs[x * 64:(x + 1) * 64, sp * 4 + h, :], sv_d[2 * x + sp, h * 64:(h + 1) * 64, bsl])
            for cc in range(256 // CH):
                c = big * (256 // CH) + cc
                if c * CH >= nsteps:
                    break
                ob = opt[c % 3]
                for o in range(5):
                    for x in range(2):
                        for hl in range(2):
                            p0 = x * 64 + hl * 32
                            S.dma(ob[o][p0:p0 + CH, :], sops_d[o, hl, x, c * CH:(c + 1) * CH, :])
                for tp in range(CH):
                    ti = cc * CH + tp
                    d = step % ND
                    par = step % 2
                    banks = (g.ps[0 + par], g.ps[6], g.ps[2 + par], g.ps[7], g.ps[4 + par])
                    for o in range(5):
                        S.mm(banks[o][:, :], sel[:, tp, :], ob[o][:])
                    S.copy(ringW[d][:], banks[1][:, :], eng="act")
                    S.copy(ringK[d][:], banks[3][:, :], eng="act")
                    KK, NK, R = banks[0], banks[2], banks[4]
                    W, KB = ringW[d], ringK[d]
                    Sp, Sn = Stt[step % 2], Stt[(step + 1) % 2]
                    S.tt(g3(vk[d][:]), g3(KB[:]), bc(vs[:, :, ti:ti + 1], [P, 8, 64]), ALU.mult, eng="pool")
                    S.tt(Sw[:], Sp[:], W[:], ALU.mult, eng="pool")
                    S.tt(Sw[:], Sw[:], vk[d][:], ALU.add, eng="pool")
                    S.tt(tmp[:], Sp[:], KK[:, :], ALU.mult)
                    if pend is not None:
                        S.tt(pend[0][:], pend[1][:], pend[2][:, :], ALU.mult)
                    S.reduce(sa[:], g3(tmp[:]), ALU.add, AX.X)
                    if pend is not None:
                        S.reduce(pend[3], g3(pend[0][:]), ALU.add, AX.X)
                        pend = None
                    S.tt(g3(tmp[:]), g3(NK[:, :]), bc(sa[:].unsqueeze(2), [P, 8, 64]), ALU.mult)
                    S.tt(Sn[:], Sw[:], tmp[:], ALU.add)
                    pend = (ytmps[step % 2], Sn, R, y_[:, :, ti])
                    step += 1
            if pend is not None:
                S.tt(pend[0][:], pend[1][:], pend[2][:, :], ALU.mult)
                S.reduce(pend[3], g3(pend[0][:]), ALU.add, AX.X)
                pend = None
            for x in range(2):
                for sp in range(2):
                    for h in range(4):
                        S.dma(sy_d[2 * x + sp, h * 64:(h + 1) * 64, bsl], y_[x * 64:(x + 1) * 64, sp * 4 + h, :])
        S.flush()


def ph_rwkv_post(S, nc, g, l, s, sy_d, sbg_d, ycat_d):
    with ExitStack() as st:
        yT = sb(nc, st, "pyT", [P, 2, T], F32)
        S.dma(yT[:], sy_d[s].rearrange("(c p) t -> p c t", p=P))
        bgt = sb(nc, st, "pbg", [P, 16, 512], F32)
        S.dma(bgt[:], sbg_d[s].rearrange("(b p) n -> p b n", p=P))
        lng = load_row_bcast(S, nc, st, "plng", g.dram["rk_ln_g"][l], 256)
        lnb = load_row_bcast(S, nc, st, "plnb", g.dram["rk_ln_b"][l], 256)
        y = sb(nc, st, "py", [P, 16, 256], F32)
        for tb in range(16):
            ps = g.ps[tb % 4]
            for c in range(2):
                S.transpose(ps[:, c * 128:(c + 1) * 128], yT[:, c, tb * 128:(tb + 1) * 128], g.ident[:])
            S.copy(y[:, tb, :], ps[:, 0:256], eng=("act" if tb % 2 else "dve"))
        yv = y[:].rearrange("p b (h d) -> p (b h) d", d=64)
        mean = sb(nc, st, "pmean", [P, 64], F32)
        sq = sb(nc, st, "psq", [P, 64, 64], F32)
        S.reduce(mean[:], yv, ALU.add, AX.X)
        S.ts(mean[:], mean[:], 1.0 / 64, None, ALU.mult)
        S.tt(yv, yv, bc(mean[:].unsqueeze(2), [P, 64, 64]), ALU.subtract)
        S.tt(sq[:], yv, yv, ALU.mult)
        S.reduce(mean[:], sq[:], ALU.add, AX.X)
        eps = sb(nc, st, "peps", [P, 1], F32)
        S.memset(eps[:], 64e-5)
        S.act(mean[:], mean[:], AF.Sqrt, scale=1.0 / 64, bias=eps[:, 0:1])
        S.recip(mean[:], mean[:])
        S.tt(yv, yv, bc(mean[:].unsqueeze(2), [P, 64, 64]), ALU.mult)
        S.tt(y[:], y[:], bc(lng[:].unsqueeze(1), [P, 16, 256]), ALU.mult)
        S.tt(y[:], y[:], bc(lnb[:].unsqueeze(1), [P, 16, 256]), ALU.add)
        S.tt(y[:], y[:], bgt[:, :, 0:256], ALU.add)
        S.tt(y[:], y[:], bgt[:, :, 256:512], ALU.mult)
        S.dma(ycat_d[:, 0:256].rearrange("(b p) n -> p b n", p=P), y[:])
        S.flush()


def ph_outproj(S, nc, g, l, b, ycat_d, xin_d, xout_d, tok0):
    with ExitStack() as st:
        Wb = sb(nc, st, "oW", [P, 8, 1024], BF16)
        ycT = sb(nc, st, "oyT", [P, 8, T], BF16)
        with ExitStack() as st2:
            stg = [sb(nc, st2, "ostg%d" % i, [P, 8, 512], F32) for i in range(2)]
            for hf in range(2):
                S.dma(stg[hf][:], g.dram["w_out"][l][:, hf * 512:(hf + 1) * 512].rearrange("(c p) n -> p c n", p=P))
                S.copy(Wb[:, :, hf * 512:(hf + 1) * 512], stg[hf][:], eng=("act" if hf else "pool"))
            yb = [sb(nc, st2, "oyb%d" % i, [P, 1024], F32) for i in range(2)]
            ev = 0
            for tb in range(16):
                y = yb[tb % 2]
                S.dma(y[:], ycat_d[tb * 128:(tb + 1) * 128, :])
                for c4 in range(2):
                    ps = g.ps[ev % 4]
                    ev += 1
                    for cc in range(4):
                        c = c4 * 4 + cc
                        S.transpose(ps[:, cc * 128:(cc + 1) * 128], y[:, c * 128:(c + 1) * 128], g.ident[:])
                    S.copy(ycT[:, c4 * 4:c4 * 4 + 4, tb * 128:(tb + 1) * 128],
                           ps[:, :].rearrange("p (c t) -> p c t", t=128), eng=("act" if ev % 2 else "dve"))
            S.flush()
        xo = [sb(nc, st, "oxo%d" % i, [P, 512], F32) for i in range(3)]
        ev = 0
        for oc in range(8):
            for sblk in range(4):
                ps = g.ps[4 + (ev % 4)]
                x = xo[ev % 3]
                ev += 1
                tsl = slice(tok0 + sblk * 512, tok0 + (sblk + 1) * 512)
                S.dma(x[:], xin_d[oc * 128:(oc + 1) * 128, tsl])
                for k in range(8):
                    S.mm(ps[:, :], Wb[:, k, oc * 128:(oc + 1) * 128], ycT[:, k, sblk * 512:(sblk + 1) * 512],
                         start=(k == 0), stop=(k == 7))
                S.stt(x[:], ps[:, :], g.modT[:, 16 + oc, b:b + 1], x[:], ALU.mult, ALU.add)
                S.dma(xout_d[oc * 128:(oc + 1) * 128, tsl], x[:])
        S.flush()


def ph_moe(S, nc, g, l, b, xin_d, xout_d, tok0, n_exp=32):
    with ExitStack() as st:
        hT = sb(nc, st, "mhT", [P, 8, T], BF16)
        gate = sb(nc, st, "mgate", [P, 16, 32], F32)
        with ExitStack() as st2:
            wge = sb(nc, st2, "mwge", [P, 8, 36], F32)
            S.dma(wge[:], g.dram["moe_wge"][l].rearrange("(c p) n -> p c n", p=P))
            bge = load_row_bcast(S, nc, st2, "mbge", g.dram["moe_bge"][l], 36)
            lg = sb(nc, st2, "mlg", [P, 16, 36], F32)
            emit_norm_mod(S, nc, g, xin_d, tok0, b, g.A2, g.modT[:, 24:32, :], hT, col0=0, route=(wge, lg))
            S.tt(lg[:], lg[:], bc(bge[:].unsqueeze(1), [P, 16, 36]), ALU.add)
            G4 = lg[:, :, 0:4]
            gmax = sb(nc, st2, "mgmax", [P, 16], F32)
            ge = sb(nc, st2, "mge", [P, 16, 4], F32)
            gsum = sb(nc, st2, "mgsum", [P, 16], F32)
            pen = sb(nc, st2, "mpen", [P, 16, 4], F32)
            S.reduce(gmax[:], G4, ALU.max, AX.X)
            S.tt(ge[:], G4, bc(gmax[:].unsqueeze(2), [P, 16, 4]), ALU.subtract)
            S.ts(pen[:], ge[:], 0.0, NEG, ALU.is_lt, ALU.mult)
            S.act(ge[:], ge[:], AF.Exp)
            S.reduce(gsum[:], ge[:], ALU.add, AX.X)
            S.recip(gsum[:], gsum[:])
            Em = sb(nc, st2, "mEm", [P, 16, 32], F32)
            S.tt(Em[:].rearrange("p b (q e) -> p b q e", e=8), lg[:, :, 4:36].rearrange("p b (q e) -> p b q e", e=8),
                 bc(pen[:].unsqueeze(3), [P, 16, 4, 8]), ALU.add)
            m1 = sb(nc, st2, "mm1", [P, 16], F32)
            m2 = sb(nc, st2, "mm2", [P, 16], F32)
            E2 = sb(nc, st2, "mE2", [P, 16, 32], F32)
            S.reduce(m1[:], Em[:], ALU.max, AX.X)
            S.tt(E2[:], Em[:], bc(m1[:].unsqueeze(2), [P, 16, 32]), ALU.is_ge)
            S.stt(E2[:], E2[:], NEG, Em[:], ALU.mult, ALU.add)
            S.reduce(m2[:], E2[:], ALU.max, AX.X)
            ex = sb(nc, st2, "mex", [P, 16, 32], F32)
            S.tt(ex[:], Em[:], bc(m1[:].unsqueeze(2), [P, 16, 32]), ALU.subtract)
            S.act(ex[:], ex[:], AF.Exp)
            S.tt(E2[:], Em[:], bc(m2[:].unsqueeze(2), [P, 16, 32]), ALU.is_ge)
            S.tt(ex[:], ex[:], E2[:], ALU.mult)
            den = sb(nc, st2, "mden", [P, 16], F32)
            S.tt(den[:], m2[:], m1[:], ALU.subtract)
            S.act(den[:], den[:], AF.Exp)
            S.ts(den[:], den[:], 1.0, None, ALU.add)
            S.recip(den[:], den[:])
            S.tt(den[:], den[:], gsum[:], ALU.mult)
            S.tt(gate[:], ex[:], bc(den[:].unsqueeze(2), [P, 16, 32]), ALU.mult)
            S.flush()
        acc = sb(nc, st, "macc", [P, 16, 1024], F32)
        stg = [sb(nc, st, "mstg%d" % i, [P, 8, 512], F32) for i in range(2)]
        W1b = [sb(nc, st, "mW1_%d" % i, [P, 8, 512], BF16) for i in range(2)]
        W3b = [sb(nc, st, "mW3_%d" % i, [P, 8, 512], BF16) for i in range(2)]
        W2b = [sb(nc, st, "mW2_%d" % i, [P, 4, 1024], BF16) for i in range(2)]
        aT = [sb(nc, st, "maT%d" % i, [P, 4, 512], BF16) for i in range(2)]
        su = [sb(nc, st, "msu%d" % i, [P, 512], F32) for i in range(2)]
        cnt = {"si": 0}

        def load_w(e):
            u = e % 2
            for (dst, src) in ((W1b[u], g.dram["moe_w1"][l, e]), (W3b[u], g.dram["moe_w3"][l, e])):
                sg = stg[cnt["si"] % 2]
                cnt["si"] += 1
                S.dma(sg[:], src.rearrange("(c p) n -> p c n", p=P))
                S.copy(dst[:], sg[:], eng=("act" if cnt["si"] % 2 else "pool"))
            sg = stg[cnt["si"] % 2]
            cnt["si"] += 1
            S.dma(sg[:].rearrange("p c n -> p (c n)").rearrange("p (c n) -> p c n", n=1024),
                  g.dram["moe_w2"][l, e].rearrange("(c p) n -> p c n", p=P))
            S.copy(W2b[u][:].rearrange("p c n -> p (c n)"), sg[:].rearrange("p c n -> p (c n)"), eng="pool")

        its = [(e, sblk) for e in range(n_exp) for sblk in range(4)]

        def st13(n):
            e, sblk = its[n]
            u = e % 2
            if sblk == 0:
                load_w(e)
            a = aT[n % 2]
            for f in range(4):
                pu, pg3 = g.ps[(f % 2) * 2], g.ps[(f % 2) * 2 + 1]
                for k in range(8):
                    S.mm(pu[:, :], W1b[u][:, k, f * 128:(f + 1) * 128], hT[:, k, sblk * 512:(sblk + 1) * 512],
                         start=(k == 0), stop=(k == 7))
                for k in range(8):
                    S.mm(pg3[:, :], W3b[u][:, k, f * 128:(f + 1) * 128], hT[:, k, sblk * 512:(sblk + 1) * 512],
                         start=(k == 0), stop=(k == 7))
                s_ = su[f % 2]
                S.act(s_[:], pu[:, :], AF.Silu)
                S.tt(a[:, f, :], s_[:], pg3[:, :], ALU.mult)

        def st2(n):
            e, sblk = its[n]
            u = e % 2
            a = aT[n % 2]
            for tb4 in range(4):
                tb = sblk * 4 + tb4
                for hf in range(2):
                    py = g.ps[4 + ((tb4 * 2 + hf) % 4)]
                    for f in range(4):
                        S.mm(py[:, :], a[:, f, tb4 * 128:(tb4 + 1) * 128], W2b[u][:, f, hf * 512:(hf + 1) * 512],
                             start=(f == 0), stop=(f == 3))
                    dst = acc[:, tb, hf * 512:(hf + 1) * 512]
                    if e == 0:
                        S.ts(dst, py[:, :], gate[:, tb, e:e + 1], None, ALU.mult)
                    else:
                        S.stt(dst, py[:, :], gate[:, tb, e:e + 1], dst, ALU.mult, ALU.add)

        st13(0)
        for n in range(len(its)):
            if n + 1 < len(its):
                st13(n + 1)
            st2(n)
        xo = [sb(nc, st, "mxo%d" % i, [P, 512], F32) for i in range(2)]
        ev = 0
        for c in range(8):
            for sblk in range(4):
                ps = g.ps[ev % 4]
                x = xo[ev % 2]
                ev += 1
                tsl = slice(tok0 + sblk * 512, tok0 + (sblk + 1) * 512)
                S.dma(x[:], xin_d[c * 128:(c + 1) * 128, tsl])
                for tb4 in range(4):
                    S.transpose(ps[:, tb4 * 128:(tb4 + 1) * 128], acc[:, sblk * 4 + tb4, c * 128:(c + 1) * 128], g.ident[:])
                S.stt(x[:], ps[:, :], g.modT[:, 40 + c, b:b + 1], x[:], ALU.mult, ALU.add)
                S.dma(xout_d[c * 128:(c + 1) * 128, tsl], x[:])
        S.flush()


def build_full(shared_shapes, n_layers=2):
    nc = bass.Bass("TRN2", target_bir_lowering=False)
    g = G()
    g.dram = {}
    for k, shp in shared_shapes.items():
        g.dram[k] = nc.dram_tensor(k, list(shp), F32, kind="ExternalInput").ap()
    NT = NSEQ * T
    g.dram["xT"] = nc.dram_tensor("xT", [D, NT], F32, kind="ExternalInput").ap()
    g.dram["cT"] = nc.dram_tensor("cT", [D, 4], F32, kind="ExternalInput").ap()
    outT = nc.dram_tensor("outT", [D, NT], F32, kind="ExternalOutput").ap()
    X1 = nc.dram_tensor("X1", [D, NT], F32).ap()
    X2 = nc.dram_tensor("X2", [D, NT], F32).ap()
    ptm = nc.dram_tensor("ptm", [T, NTM], F32).ap()
    pfm = nc.dram_tensor("pfm", [NFM, T], F32).ap()
    ycat = nc.dram_tensor("ycat", [NSEQ, T, D], F32).ap()
    sops = nc.dram_tensor("sops", [5, 2, 2, T, 512], BF16).ap()
    sv = nc.dram_tensor("sv", [NSEQ, 256, T], F32).ap()
    sy = nc.dram_tensor("sy", [NSEQ, 256, T], F32).ap()
    sbg = nc.dram_tensor("sbg", [NSEQ, T, 512], F32).ap()
    with ExitStack() as st:
        S = Sched(nc)
        S.open(st)
        g.ps = [st.enter_context(nc.psum_tensor("ps%d" % i, [P, 512], F32)) for i in range(8)]
        setup_consts(S, nc, st, g)
        for l in range(n_layers):
            xin = g.dram["xT"] if l == 0 else X2
            xmid = X1
            xout = outT if l == n_layers - 1 else X2
            ph_ada(S, nc, g, l)
            for s in range(NSEQ):
                with ExitStack() as st2:
                    hT = sb(nc, st2, "hT", [P, 8, T + 4], BF16)
                    S.memset(hT[:, :, 0:1], 0.0)
                    emit_norm_mod(S, nc, g, xin, s * T, s, g.A1, g.modT[:, 0:8, :], hT)
                    ph_inproj(S, nc, g, l, hT, ptm, pfm)
                ph_sb(S, nc, g, l, ptm, pfm, ycat[s])
                ph_ml(S, nc, g, l, ptm, pfm, ycat[s])
                ph_dsa(S, nc, g, l, ptm, ycat[s])
                ph_rwkv_prep(S, nc, g, l, s, ptm, pfm, sops, sv, sbg)
            ph_rwkv_scan(S, nc, g, sops, sv, sy)
            for s in range(NSEQ):
                ph_rwkv_post(S, nc, g, l, s, sy, sbg, ycat[s])
                ph_outproj(S, nc, g, l, s, ycat[s], xin, xmid, s * T)
            for s in range(NSEQ):
                ph_moe(S, nc, g, l, s, xmid, xout, s * T)
        S.flush()
        g.n_inst = S.n_inst
    return nc, g


_CACHE = {}


def kernel(**inputs):
    inp = {k: np.asarray(v) for k, v in inputs.items()}
    sh = host_shared(inp)
    n_layers = inp["w_in"].shape[0]
    if "nc" not in _CACHE:
        _CACHE["nc"] = build_full({k: v.shape for k, v in sh.items()}, n_layers)
    nc, g = _CACHE["nc"]
    in_maps = []
    for core in range(8):
        d = dict(sh)
        d.update(host_core(inp, core))
        in_maps.append(d)
    res = run_bass_kernel_spmd(nc, in_maps, core_ids=list(range(8)))
    out = np.empty((32, T, D), np.float32)
    for core in range(8):
        oT = np.asarray(res.results[core]["outT"])
        out[core * NSEQ:(core + 1) * NSEQ] = oT.T.reshape(NSEQ, T, D)
    return out
```

```python
import numpy as np
import concourse.bass as bass
import concourse.mybir as mybir
from concourse.bass_utils import run_bass_kernel_spmd

F32 = mybir.dt.float32
BF16 = mybir.dt.bfloat16
AF = mybir.ActivationFunctionType
ALU = mybir.AluOpType
AX = mybir.AxisListType

COMPUTE = ("pe", "act", "dve", "pool")
SYNC_WAR = True


class Sched:
    def __init__(self, nc, n_dma_sems=32):
        self.nc = nc
        self.n_dma = n_dma_sems
        self.sem = {}
        self.stack = None
        self.ops = []
        self.last_w = {}
        self.readers = {}
        self.count = {e: 0 for e in COMPUTE}
        self.dma_cnt = [0] * n_dma_sems
        self.dma_rr = 0
        self.known = {e: {} for e in COMPUTE + ("sp",)}
        self.phase_start = 0
        self.n_inst = 0

    def open(self, stack):
        nc = self.nc
        self.stack = stack
        self.n_sem_alloc = 0
        for e in COMPUTE:
            self.sem[e] = stack.enter_context(nc.semaphore("s_" + e))
        for i in range(self.n_dma):
            self.sem[("d", i)] = stack.enter_context(nc.semaphore("s_d%d" % i))

    @staticmethod
    def key(ap):
        return ap.name

    def add(self, eng, fn, reads, writes, dma=False):
        idx = len(self.ops)
        deps = set()
        wdeps = set()
        for k in reads:
            if k in self.last_w:
                deps.add(self.last_w[k])
        for k in writes:
            if k in self.last_w:
                deps.add(self.last_w[k])
            rd = self.readers.get(k)
            if rd:
                wdeps.update(rd.values())
        for k in writes:
            self.last_w[k] = idx
            self.readers[k] = {}
        for k in reads:
            self.readers.setdefault(k, {})[("dma", idx) if dma else eng] = idx
        deps.discard(idx)
        wdeps.discard(idx)
        wdeps -= deps
        self.ops.append(dict(eng=eng, fn=fn, deps=deps, wdeps=wdeps, dma=dma, sig=False, waits=None))
        return idx

    def flush(self, barrier=True):
        ops = self.ops
        lo = self.phase_start
        n = len(ops)
        def skip(o, od, d):
            if od["dma"] or o["dma"] or od["eng"] != o["eng"]:
                return False
            if o["eng"] == "pe":
                return True
            return (not SYNC_WAR) and (d not in o["deps"])

        for i in range(lo, n):
            o = ops[i]
            for d in (o["deps"] | o["wdeps"]):
                od = ops[d]
                if d < lo or od["dma"] or skip(o, od, d):
                    continue
                od["sig"] = True
        last_of = {}
        for i in range(lo, n):
            last_of[ops[i]["eng"]] = i
        if barrier:
            for e, i in last_of.items():
                if e in COMPUTE:
                    ops[i]["sig"] = True
        for i in range(lo, n):
            o = ops[i]
            e = o["eng"]
            kn = self.known[e]
            waits = []
            for d in sorted(o["deps"] | o["wdeps"]):
                od = ops[d]
                if d < lo or skip(o, od, d):
                    continue
                s, v = od["done"]
                if kn.get(s, 0) < v:
                    waits.append((s, v))
                    for s2, v2 in od["clock"].items():
                        if kn.get(s2, 0) < v2:
                            kn[s2] = v2
            if o["dma"]:
                j = self.dma_rr
                self.dma_rr = (j + 1) % self.n_dma
                s = ("d", j)
                if kn.get(s, 0) < self.dma_cnt[j]:
                    waits.append((s, self.dma_cnt[j]))
                    kn[s] = self.dma_cnt[j]
                self.dma_cnt[j] += 16
                o["done"] = (s, self.dma_cnt[j])
                o["inc"] = (s, 16)
                clock = dict(kn)
                clock[s] = self.dma_cnt[j]
                o["clock"] = clock
            else:
                if o["sig"]:
                    self.count[e] += 1
                    o["done"] = (e, self.count[e])
                    o["inc"] = (e, 1)
                    clock = dict(kn)
                    clock[e] = self.count[e]
                    o["clock"] = clock
                else:
                    o["inc"] = None
            w = {}
            for s, v in waits:
                w[s] = max(w.get(s, 0), v)
            o["waits"] = list(w.items())
        final = {}
        if barrier:
            for e in COMPUTE:
                final[e] = self.count[e]
            for j in range(self.n_dma):
                final[("d", j)] = self.dma_cnt[j]
        nc = self.nc
        sem = self.sem
        by_eng = {}
        for i in range(lo, n):
            by_eng.setdefault(ops[i]["eng"], []).append(ops[i])

        def emit(ename):
            def body(e):
                for o in by_eng.get(ename, []):
                    for s, v in o["waits"]:
                        e.wait_ge(sem[s], v)
                        self.n_inst += 1
                    ins = o["fn"](e)
                    self.n_inst += 1
                    if o["inc"] is not None:
                        ins.then_inc(sem[o["inc"][0]], o["inc"][1])
                kn = self.known[ename]
                for s, v in final.items():
                    if kn.get(s, 0) < v:
                        e.wait_ge(sem[s], v)
                        kn[s] = v
            return body

        with nc.Block() as block:
            block.tensor(emit("pe"))
            block.scalar(emit("act"))
            block.vector(emit("dve"))
            block.gpsimd(emit("pool"))
            block.sync(emit("sp"))
        for i in range(lo, n):
            ops[i]["fn"] = None
            ops[i]["clock"] = None if i < n else None
        self.phase_start = n
        if barrier:
            self.last_w = {}
            self.readers = {}
            for e in COMPUTE:
                if self.count[e] > 20000:
                    self.n_sem_alloc += 1
                    self.sem[e] = self.stack.enter_context(nc.semaphore("s_%s_%d" % (e, self.n_sem_alloc)))
                    self.count[e] = 0
                    for kn in self.known.values():
                        kn.pop(e, None)
            for j in range(self.n_dma):
                if self.dma_cnt[j] > 20000:
                    self.n_sem_alloc += 1
                    self.sem[("d", j)] = self.stack.enter_context(nc.semaphore("s_d%d_%d" % (j, self.n_sem_alloc)))
                    self.dma_cnt[j] = 0
                    for kn in self.known.values():
                        kn.pop(("d", j), None)

    def _rw(self, outs, ins, rk, wk):
        r = list(rk) if rk is not None else [self.key(a) for a in ins if hasattr(a, "name") and a.space != "DRAM"]
        w = list(wk) if wk is not None else [self.key(a) for a in outs if a.space != "DRAM"]
        return r, w

    def mm(self, out, lhsT, rhs, start=True, stop=True, rk=None, wk=None):
        r, w = self._rw([out], [lhsT, rhs], rk, wk)
        return self.add("pe", lambda e: e.matmul(out, lhsT=lhsT, rhs=rhs, start=start, stop=stop), r, w)

    def transpose(self, out, in_, ident, rk=None, wk=None):
        r, w = self._rw([out], [in_, ident], rk, wk)
        return self.add("pe", lambda e: e.transpose(out, in_, ident), r, w)

    def act(self, out, in_, func, bias=None, scale=1.0, accum_out=None, rk=None, wk=None):
        ins = [in_] + ([bias] if hasattr(bias, "name") else []) + ([scale] if hasattr(scale, "name") else [])
        outs = [out] + ([accum_out] if accum_out is not None else [])
        r, w = self._rw(outs, ins, rk, wk)
        kw = {}
        if bias is not None:
            kw["bias"] = bias
        if accum_out is not None:
            kw["accum_out"] = accum_out
        return self.add("act", lambda e: e.activation(out, in_, func, scale=scale, **kw), r, w)

    def tt(self, out, in0, in1, op, eng="dve", rk=None, wk=None):
        r, w = self._rw([out], [in0, in1], rk, wk)
        return self.add(eng, lambda e: e.tensor_tensor(out, in0, in1, op), r, w)

    def ts(self, out, in0, s1, s2=None, op0=ALU.mult, op1=None, eng="dve", accum_out=None, rk=None, wk=None):
        ins = [in0] + [s for s in (s1, s2) if hasattr(s, "name")]
        outs = [out] + ([accum_out] if accum_out is not None else [])
        r, w = self._rw(outs, ins, rk, wk)
        kw = {}
        if op1 is not None:
            kw["op1"] = op1
        if accum_out is not None:
            kw["accum_out"] = accum_out
        return self.add(eng, lambda e: e.tensor_scalar(out, in0, s1, s2, op0, **kw), r, w)

    def stt(self, out, in0, scalar, in1, op0, op1, eng="dve", rk=None, wk=None):
        ins = [in0, in1] + ([scalar] if hasattr(scalar, "name") else [])
        r, w = self._rw([out], ins, rk, wk)
        return self.add(eng, lambda e: e.scalar_tensor_tensor(out, in0, scalar, in1, op0, op1), r, w)

    def copy(self, out, in_, eng="dve", rk=None, wk=None):
        r, w = self._rw([out], [in_], rk, wk)
        if eng == "act":
            return self.add("act", lambda e: e.activation(out, in_, AF.Copy), r, w)
        return self.add(eng, lambda e: e.tensor_copy(out, in_), r, w)

    def memset(self, out, val, eng="dve", wk=None):
        r, w = self._rw([out], [], None, wk)
        return self.add(eng, lambda e: e.memset(out, val), r, w)

    def reduce(self, out, in_, op, axis=AX.X, eng="dve", rk=None, wk=None):
        r, w = self._rw([out], [in_], rk, wk)
        return self.add(eng, lambda e: e.tensor_reduce(out, in_, axis, op), r, w)

    def recip(self, out, in_, rk=None, wk=None):
        r, w = self._rw([out], [in_], rk, wk)
        return self.add("dve", lambda e: e.reciprocal(out, in_), r, w)

    def scan(self, out, d0, d1, init, op0, op1, eng="dve", rk=None, wk=None):
        r, w = self._rw([out], [d0, d1], rk, wk)
        return self.add(eng, lambda e: e.tensor_tensor_scan(out, d0, d1, init, op0, op1), r, w)

    def max8(self, out, in_, rk=None, wk=None):
        r, w = self._rw([out], [in_], rk, wk)
        return self.add("dve", lambda e: e.max(out, in_), r, w)

    def match_replace(self, out, rep, vals, imm, rk=None, wk=None):
        r, w = self._rw([out], [rep, vals], rk, wk)
        return self.add("dve", lambda e: e.match_replace(out, rep, vals, imm), r, w)

    def dma(self, out, in_, rk=None, wk=None, **kw):
        r, w = self._rw([out], [in_], rk, wk)
        return self.add("sp", lambda e: e.dma_start(out, in_, **kw), r, w, dma=True)


from contextlib import ExitStack

P = 128
T = 2048
D = 1024
NSEQ = 4
NTM = 2084
NFM = 1416
TM_AR, TM_AK, TM_AV, TM_BV, TM_CV, TM_CO, TM_DQ, TM_DK, TM_DV, TM_DQI, TM_DKI, TM_DWI = (
    0, 256, 512, 768, 1024, 1280, 1536, 1792, 1856, 1920, 2048, 2080)
FM_AV, FM_AWD, FM_AAD, FM_AGD, FM_BQ, FM_BK, FM_CQ, FM_CK, FM_CIG, FM_CFG = (
    0, 256, 288, 320, 384, 640, 896, 1152, 1408, 1412)
NEG = -1.0e30
_uid = [0]


def uname(s):
    _uid[0] += 1
    return "%s_%d" % (s, _uid[0])


def sb(nc, st, name, shape, dt):
    return st.enter_context(nc.sbuf_tensor(uname(name), list(shape), dt))


def bc(ap, shape):
    return ap.to_broadcast(list(shape))


class G:
    pass


def host_consts():
    c = {}
    i = np.arange(128)
    c["ident"] = np.eye(128, dtype=np.float32)
    c["ones"] = np.ones((128, 128), np.float32)
    c["tri_lt"] = (i[:, None] < i[None, :]).astype(np.float32)
    c["tri_le"] = (i[:, None] <= i[None, :]).astype(np.float32)
    c["tri_gt"] = (i[:, None] > i[None, :]).astype(np.float32)
    c["cbias"] = np.where(i[None, :] <= i[:, None], 0.0, NEG).astype(np.float32)
    half = 32
    inv = 10000.0 ** (-np.arange(half, dtype=np.float32) / half)
    ang = np.arange(T, dtype=np.float32)[:, None] * inv[None, :]
    c["rope64"] = np.concatenate([np.cos(ang), np.sin(ang)], 1).astype(np.float32)
    half = 16
    inv = 10000.0 ** (-np.arange(half, dtype=np.float32) / half)
    ang = np.arange(T, dtype=np.float32)[:, None] * inv[None, :]
    c["rope32"] = np.concatenate([np.cos(ang), np.sin(ang)], 1).astype(np.float32)
    c["lebias"] = np.where(i[:, None] <= i[None, :], 0.0, NEG).astype(np.float32)
    selh = np.zeros((4, 4, 128), np.float32)
    for h in range(4):
        selh[h, h, :] = 1.0
    c["selh"] = selh.reshape(4, 512)
    return c


def load_const(S, nc, st, g, name, shape, dt=F32):
    t = sb(nc, st, "c_" + name, shape, F32)
    S.dma(t[:], g.dram[name])
    if dt == F32:
        return t
    tb = sb(nc, st, "cb_" + name, shape, dt)
    S.copy(tb[:], t[:])
    return tb


def ph_ada(S, nc, g, l):
    with ExitStack() as st:
        cT = sb(nc, st, "cT", [P, 8, 4], F32)
        S.dma(cT[:], g.dram["cT"].rearrange("(c p) b -> p c b", p=P))
        cact = sb(nc, st, "cact", [P, 8, 4], F32)
        S.act(cact[:], cT[:], AF.Silu)
        bias = sb(nc, st, "adab", [P, 48], F32)
        S.dma(bias[:], g.dram["ada_b_fm"][l])
        g1 = sb(nc, st, "g1", [P, 8], F32)
        g2 = sb(nc, st, "g2", [P, 8], F32)
        S.dma(g1[:], g.dram["norm1_g_fm"][l])
        S.dma(g2[:], g.dram["norm2_g_fm"][l])
        wts = [sb(nc, st, "adaw%d" % i, [P, 8, 768], F32) for i in range(2)]
        ps = g.ps[0]
        for cb in range(8):
            wt = wts[cb % 2]
            S.dma(wt[:], g.dram["ada_w"][l][:, cb * 768:(cb + 1) * 768].rearrange("(c p) n -> p c n", p=P))
            for cc in range(6):
                c = cb * 6 + cc
                for k in range(8):
                    S.mm(ps[:, 4 * c:4 * c + 4], wt[:, k, cc * 128:(cc + 1) * 128], cact[:, k, :],
                         start=(k == 0), stop=(k == 7))
        S.tt(g.modT[:], ps[:, 0:192].rearrange("p (c b) -> p c b", b=4),
             bc(bias[:].unsqueeze(2), [P, 48, 4]), ALU.add)
        for (A, gg, off) in ((g.A1, g1, 8), (g.A2, g2, 32)):
            S.ts(A[:], g.modT[:, off:off + 8, :], 1.0, None, ALU.add)
            S.tt(A[:], A[:], bc(gg[:].unsqueeze(2), [P, 8, 4]), ALU.mult)
        S.flush()


def emit_norm_mod(S, nc, g, xT_d, tok0, b, A, shift, hT, col0=1, route=None):
    with ExitStack() as st:
        xs = [sb(nc, st, "xs%d" % i, [P, 8, 512], F32) for i in range(2)]
        sq = sb(nc, st, "sq", [P, 8, 512], F32)
        tmp = sb(nc, st, "tmp", [P, 8, 512], F32)
        rstd = sb(nc, st, "rstd", [P, 512], F32)
        ps = g.ps[1]
        for sblk in range(4):
            x = xs[sblk % 2]
            S.dma(x[:], xT_d[:, tok0 + sblk * 512: tok0 + (sblk + 1) * 512].rearrange("(c p) n -> p c n", p=P))
            S.act(sq[:], x[:], AF.Square)
            for c in range(8):
                S.mm(ps[:, :], g.ones[:], sq[:, c, :], start=(c == 0), stop=(c == 7))
            S.act(rstd[:], ps[:, :], AF.Sqrt, scale=1.0 / D, bias=g.eps6[:, 0:1])
            S.recip(rstd[:], rstd[:])
            S.tt(tmp[:], x[:], bc(rstd[:].unsqueeze(1), [P, 8, 512]), ALU.mult)
            for c in range(8):
                S.act(hT[:, c, col0 + sblk * 512: col0 + (sblk + 1) * 512], tmp[:, c, :], AF.Identity,
                      scale=A[:, c, b:b + 1], bias=shift[:, c, b:b + 1])
            if route is not None:
                wge, lg = route
                for c in range(8):
                    S.act(sq[:, c, :], tmp[:, c, :], AF.Identity, scale=A[:, c, b:b + 1], bias=shift[:, c, b:b + 1])
                for tb4 in range(4):
                    pr = g.ps[2 + (tb4 % 2)]
                    for c in range(8):
                        S.mm(pr[:, 0:36], sq[:, c, tb4 * 128:(tb4 + 1) * 128], wge[:, c, :], start=(c == 0), stop=(c == 7))
                    S.copy(lg[:, sblk * 4 + tb4, :], pr[:, 0:36])
        S.flush()


def load_weights_bf16(S, nc, st, g, w_d, ncols, nshift, mu_d, Wb, W0b):
    stg = [sb(nc, st, "wstg%d" % i, [P, 8, 512], F32) for i in range(2)]
    mu_b = sb(nc, st, "mu_b", [P, max(nshift, 1)], F32)
    tmp = sb(nc, st, "wtmp", [P, 8, 512], F32)
    if nshift:
        S.dma(mu_b[:], mu_d.partition_broadcast(P))
    i = 0
    for c0 in range(0, ncols, 512):
        w = min(512, ncols - c0)
        s_ = stg[i % 2]
        i += 1
        S.dma(s_[:, :, :w], w_d[:, c0:c0 + w].rearrange("(c p) n -> p c n", p=P))
        if c0 < nshift:
            ws = min(w, nshift - c0)
            S.tt(tmp[:, :, :ws], s_[:, :, :ws], bc(mu_b[:, c0:c0 + ws].unsqueeze(1), [P, 8, ws]), ALU.mult, eng="pool")
            S.copy(W0b[:, :, c0:c0 + ws], tmp[:, :, :ws], eng="act")
            S.tt(Wb[:, :, c0:c0 + ws], s_[:, :, :ws], tmp[:, :, :ws], ALU.subtract)
            if ws < w:
                S.copy(Wb[:, :, c0 + ws:c0 + w], s_[:, :, ws:w], eng="act")
        else:
            S.copy(Wb[:, :, c0:c0 + w], s_[:, :, :w], eng=("act" if (i % 2) else "dve"))


def ph_inproj(S, nc, g, l, hT, ptm_d, pfm_d):
    with ExitStack() as st:
        Wb = sb(nc, st, "Wb", [P, 8, NTM], BF16)
        W0b = sb(nc, st, "W0b", [P, 8, 768], BF16)
        load_weights_bf16(S, nc, st, g, g.dram["w_tm"][l], NTM, 768, g.dram["mu_tm"][l], Wb, W0b)
        stg = [sb(nc, st, "ptm_stg%d" % i, [P, NTM], F32) for i in range(2)]
        ev = 0
        for tb in range(16):
            so = stg[tb % 2]
            for c0 in range(0, NTM, 512):
                w = min(512, NTM - c0)
                ps = g.ps[2 + (ev % 4)]
                shifted = c0 < 768
                for k in range(8):
                    S.mm(ps[:, :w], hT[:, k, 1 + tb * 128: 1 + (tb + 1) * 128], Wb[:, k, c0:c0 + w],
                         start=(k == 0), stop=(k == 7 and not shifted))
                if shifted:
                    ws = min(w, 768 - c0)
                    for k in range(8):
                        S.mm(ps[:, :ws], hT[:, k, tb * 128:(tb + 1) * 128], W0b[:, k, c0:c0 + ws],
                             start=False, stop=(k == 7))
                S.copy(so[:, c0:c0 + w], ps[:, :w], eng=("act" if ev % 2 else "dve"))
                ev += 1
            S.dma(ptm_d[tb * 128:(tb + 1) * 128, :], so[:])
        S.flush()
    with ExitStack() as st:
        Wb = sb(nc, st, "Wf", [P, 8, NFM], BF16)
        W0b = sb(nc, st, "W0f", [P, 8, 384], BF16)
        load_weights_bf16(S, nc, st, g, g.dram["w_fm"][l], NFM, 384, g.dram["mu_fm"][l], Wb, W0b)
        stg = [sb(nc, st, "pfm_stg%d" % i, [P, T], F32) for i in range(2)]
        ev = 0
        ci = 0
        for r0 in range(0, NFM, 128):
            m = min(128, NFM - r0)
            so = stg[ci % 2]
            ci += 1
            shifted = r0 < 384
            for sblk in range(4):
                ps = g.ps[2 + (ev % 4)]
                for k in range(8):
                    S.mm(ps[:m, :], Wb[:, k, r0:r0 + m], hT[:, k, 1 + sblk * 512: 1 + (sblk + 1) * 512],
                         start=(k == 0), stop=(k == 7 and not shifted))
                if shifted:
                    for k in range(8):
                        S.mm(ps[:m, :], W0b[:, k, r0:r0 + m], hT[:, k, sblk * 512:(sblk + 1) * 512],
                             start=False, stop=(k == 7))
                S.copy(so[:m, sblk * 512:(sblk + 1) * 512], ps[:m, :], eng=("act" if ev % 2 else "dve"))
                ev += 1
            S.dma(pfm_d[r0:r0 + m, :], so[:m, :])
        S.flush()


def _r(a, b):
    return list(range(a, b))


TM_COLS = (_r(0, 256) + _r(256, 512) + _r(512, 768) + _r(1408, 1664) + _r(2176, 2432) + _r(2432, 2688)
           + _r(2696, 2952) + _r(2952, 3016) + _r(3016, 3080) + _r(3080, 3208) + _r(3208, 3240) + _r(3240, 3244))
FM_COLS = (_r(512, 768) + _r(768, 800) + _r(800, 832) + _r(832, 896) + _r(896, 1152) + _r(1152, 1408)
           + _r(1664, 1920) + _r(1920, 2176) + _r(2688, 2692) + _r(2692, 2696))
assert len(TM_COLS) == NTM and len(FM_COLS) == NFM


def host_shared(inp):
    f = lambda a: np.ascontiguousarray(a, dtype=np.float32)
    L = inp["w_in"].shape[0]
    sh = dict(host_consts())
    sh["ada_w"] = f(inp["ada_w"])
    sh["ada_b_fm"] = f(inp["ada_b"].reshape(L, 48, 128).transpose(0, 2, 1))
    sh["norm1_g_fm"] = f(inp["norm1_g"].reshape(L, 8, 128).transpose(0, 2, 1))
    sh["norm2_g_fm"] = f(inp["norm2_g"].reshape(L, 8, 128).transpose(0, 2, 1))
    sh["w_tm"] = f(inp["w_in"][:, :, TM_COLS])
    sh["w_fm"] = f(inp["w_in"][:, :, FM_COLS])
    sh["mu_tm"] = f(inp["rk_mu"][:, TM_COLS[:768]])
    sh["mu_fm"] = f(inp["rk_mu"][:, FM_COLS[:384]])
    sh["conv_w_fm"] = f(inp["ml_conv_w"].reshape(L, 4, 4, 128).transpose(0, 3, 2, 1))
    sh["conv_b_fm"] = f(inp["ml_conv_b"].reshape(L, 4, 128).transpose(0, 2, 1))
    sh["moe_wge"] = f(np.concatenate([inp["moe_wg"], inp["moe_we"]], -1))
    sh["moe_bge"] = f(np.concatenate([inp["moe_bg"], inp["moe_be"]], -1))
    for k in ("moe_w1", "moe_w3", "moe_w2"):
        sh[k] = f(inp[k])
    for k in ("rk_w0", "rk_w2", "rk_a0", "rk_a2", "rk_g2", "rk_kk", "rk_ka", "rk_rk", "rk_ln_g", "rk_ln_b",
              "sb_norm_g", "ml_norm_g", "ds_qn_g", "ds_kn_g", "ds_out_g", "w_out", "ml_ig_b", "ml_fg_b"):
        sh[k] = f(inp[k])
    return sh


def host_core(inp, core, nseq=NSEQ):
    f = lambda a: np.ascontiguousarray(a, dtype=np.float32)
    x = inp["x"][core * nseq:(core + 1) * nseq]
    d = {}
    d["xT"] = f(x.reshape(nseq * T, D).T)
    cT = np.zeros((D, 4), np.float32)
    cT[:, :nseq] = inp["c"][core * nseq:(core + 1) * nseq].T
    d["cT"] = cT
    return d


def load_row_bcast(S, nc, st, name, row_ap, n):
    t = sb(nc, st, name, [P, n], F32)
    S.dma(t[:], row_ap.partition_broadcast(P))
    return t


def tm_head_rmsnorm(S, nc, st, g, y, nb, gain, eps, per_head_gain=True):
    nh = nb * 4
    yv = y[:].rearrange("p b (h d) -> p (b h) d", d=64)
    sq = sb(nc, st, "rn_sq", [P, nh, 64], F32)
    ss = sb(nc, st, "rn_ss", [P, nh], F32)
    S.tt(sq[:], yv, yv, ALU.mult)
    S.reduce(ss[:], sq[:], ALU.add, AX.X)
    S.act(ss[:], ss[:], AF.Sqrt, scale=1.0 / 64, bias=eps[:, 0:1])
    S.recip(ss[:], ss[:])
    S.tt(yv, yv, bc(ss[:].unsqueeze(2), [P, nh, 64]), ALU.mult)
    if per_head_gain:
        S.tt(y[:], y[:], bc(gain[:].unsqueeze(1), [P, nb, 256]), ALU.mult)
    else:
        S.tt(yv, yv, bc(gain[:, 0:64].unsqueeze(1), [P, nh, 64]), ALU.mult)


def ph_sb(S, nc, g, l, ptm_d, pfm_d, ycat_d):
    with ExitStack() as st:
        q16 = sb(nc, st, "sbq", [P, 2, T], BF16)
        k16 = sb(nc, st, "sbk", [P, 2, T], BF16)
        v16 = sb(nc, st, "sbv", [P, 16, 256], BF16)
        yraw = sb(nc, st, "sby", [P, 16, 256], F32)
        gain = load_row_bcast(S, nc, st, "sbg", g.dram["sb_norm_g"][l], 256)
        with ExitStack() as st2:
            qf = sb(nc, st2, "sbqf", [P, 2, T], F32)
            kf = sb(nc, st2, "sbkf", [P, 2, T], F32)
            vf = sb(nc, st2, "sbvf", [P, 16, 256], F32)
            S.dma(qf[:], pfm_d[FM_BQ:FM_BQ + 256, :].rearrange("(c p) t -> p c t", p=P))
            S.dma(kf[:], pfm_d[FM_BK:FM_BK + 256, :].rearrange("(c p) t -> p c t", p=P))
            S.dma(vf[:], ptm_d[:, TM_BV:TM_BV + 256].rearrange("(b p) n -> p b n", p=P))
            S.copy(q16[:], qf[:], eng="act")
            S.copy(k16[:], kf[:], eng="dve")
            S.copy(v16[:], vf[:], eng="pool")
            S.flush()
        e1 = [sb(nc, st, "sbe%d" % i, [P, 512], F32) for i in range(2)]
        lt = [sb(nc, st, "sbl%d" % i, [P, 512], F32) for i in range(2)]
        Lm = [sb(nc, st, "sbL%d" % i, [P, 512], F32) for i in range(2)]
        aa = [sb(nc, st, "sba%d" % i, [P, 512], F32) for i in range(2)]
        attA = [sb(nc, st, "sbt%d" % i, [P, 16, 512], BF16) for i in range(2)]
        TotB = sb(nc, st, "sbT", [P, 512], F32)
        iters = [(h, I, j) for h in range(4) for I in range(4) for j in range(4 * I + 3, -1, -1)]

        def geom(n):
            h, I, j = iters[n]
            d = j - 4 * I
            dd = max(d, 0)
            c0 = dd * 128
            return h, I, j, d, c0, 512 - c0, I * 512 + c0

        def s1(n):
            h, I, j, d, c0, nn, q0 = geom(n)
            u = n % 2
            c, pb = h // 2, (h % 2) * 64
            pz = g.ps[u]
            S.mm(pz[:, :nn], k16[pb:pb + 64, c, j * 128:(j + 1) * 128], q16[pb:pb + 64, c, q0:q0 + nn])
            S.act(e1[u][:, :nn], pz[:, :nn], AF.Exp, scale=-0.125)
            S.act(lt[u][:, :nn], e1[u][:, :nn], AF.Ln, bias=g.one1[:, 0:1])
            S.stt(Lm[u][:, :nn], pz[:, :nn], -0.125, lt[u][:, :nn], ALU.mult, ALU.subtract)
            if d >= 0:
                S.tt(Lm[u][:, 0:128], Lm[u][:, 0:128], g.tri_lt[:], ALU.mult)

        def s2(n):
            h, I, j, d, c0, nn, q0 = geom(n)
            u = n % 2
            pr, pt = g.ps[2 + u], g.ps[4 + u]
            att_all = attA[(h * 4 + I) % 2]
            if j == 4 * I + 3:
                S.memset(TotB[:], 0.0, eng="pool")
            S.mm(pr[:, :nn], g.tri_gt[:], Lm[u][:, :nn])
            S.tt(aa[u][:, :nn], pr[:, :nn], TotB[:, c0:512], ALU.add)
            S.tt(aa[u][:, :nn], aa[u][:, :nn], lt[u][:, :nn], ALU.subtract, eng="pool")
            S.act(att_all[:, j, c0:512], aa[u][:, :nn], AF.Exp)
            if d >= 0:
                S.tt(att_all[:, j, c0:c0 + 128], att_all[:, j, c0:c0 + 128], g.tri_lt16[:], ALU.mult, eng="pool")
            if j > 0:
                S.mm(pt[:, :nn], g.ones[:], Lm[u][:, :nn])
                S.tt(TotB[:, c0:512], TotB[:, c0:512], pt[:, :nn], ALU.add)
            else:
                po = g.ps[6 + (I % 2)]
                for qb in range(4):
                    for jj in range(4 * I + qb, -1, -1):
                        S.mm(po[:, qb * 64:(qb + 1) * 64], att_all[:, jj, qb * 128:(qb + 1) * 128],
                             v16[:, jj, h * 64:(h + 1) * 64], start=(jj == 4 * I + qb), stop=(jj == 0))
                S.copy(yraw[:, 4 * I:4 * I + 4, h * 64:(h + 1) * 64],
                       po[:, 0:256].rearrange("p (b d) -> p b d", d=64), eng="act")

        s1(0)
        for n in range(len(iters)):
            if n + 1 < len(iters):
                s1(n + 1)
            s2(n)
        if getattr(g, "debug", False):
            S.dma(ycat_d[:, 0:256].rearrange("(b p) n -> p b n", p=P), yraw[:])
        tm_head_rmsnorm(S, nc, st, g, yraw, 16, gain, g.eps6)
        S.dma(ycat_d[:, 256:512].rearrange("(b p) n -> p b n", p=P), yraw[:])
        S.flush()


def setup_consts(S, nc, st, g):
    def ld(name, shape, src=None):
        t = sb(nc, st, "k_" + name, shape, F32)
        S.dma(t[:], g.dram[src or name])
        return t
    g.ones = ld("ones", [P, P])
    g.ident = ld("ident", [P, P])
    g.tri_lt = ld("tri_lt", [P, P])
    g.tri_le = ld("tri_le", [P, P])
    g.tri_gt = ld("tri_gt", [P, P])
    g.cbias = ld("cbias", [P, P])
    g.lebias = ld("lebias", [P, P])
    g.selh = sb(nc, st, "k_selh", [4, 4, P], F32)
    S.dma(g.selh[:], g.dram["selh"].rearrange("k (h m) -> k h m", m=P))
    g.tri_lt16 = sb(nc, st, "k_tri_lt16", [P, P], BF16)
    g.tri_le16 = sb(nc, st, "k_tri_le16", [P, P], BF16)
    g.ident16 = sb(nc, st, "k_ident16", [P, P], BF16)
    g.ones16 = sb(nc, st, "k_ones16", [P, P], BF16)
    S.copy(g.tri_lt16[:], g.tri_lt[:])
    S.copy(g.tri_le16[:], g.tri_le[:])
    S.copy(g.ident16[:], g.ident[:])
    S.copy(g.ones16[:], g.ones[:])
    g.eps6 = sb(nc, st, "k_eps6", [P, 1], F32)
    S.memset(g.eps6[:], 1e-6)
    g.one1 = sb(nc, st, "k_one1", [P, 1], F32)
    S.memset(g.one1[:], 1.0)
    g.modT = sb(nc, st, "modT", [P, 48, 4], F32)
    g.A1 = sb(nc, st, "A1", [P, 8, 4], F32)
    g.A2 = sb(nc, st, "A2", [P, 8, 4], F32)
    S.flush()


def ph_ml(S, nc, g, l, ptm_d, pfm_d, ycat_d):
    LN8 = float(np.log(0.125))
    with ExitStack() as st:
        q16 = sb(nc, st, "mlq", [P, 2, T], BF16)
        k16 = sb(nc, st, "mlk", [P, 2, T], BF16)
        v16 = sb(nc, st, "mlv", [P, 16, 4, 65], BF16)
        osig = sb(nc, st, "mlo", [P, 16, 256], F32)
        BtB = [sb(nc, st, "mlB%d" % h, [P, T], F32) for h in range(4)]
        c_tm = sb(nc, st, "mlc", [P, 16, 4], F32)
        gain = load_row_bcast(S, nc, st, "mlg", g.dram["ml_norm_g"][l], 256)
        with ExitStack() as st2:
            xq = sb(nc, st2, "mlxq", [P, 2, T + 3], F32)
            xk = sb(nc, st2, "mlxk", [P, 2, T + 3], F32)
            S.memset(xq[:, :, 0:3], 0.0)
            S.memset(xk[:, :, 0:3], 0.0)
            S.dma(xq[:, :, 3:T + 3], pfm_d[FM_CQ:FM_CQ + 256, :].rearrange("(c p) t -> p c t", p=P))
            S.dma(xk[:, :, 3:T + 3], pfm_d[FM_CK:FM_CK + 256, :].rearrange("(c p) t -> p c t", p=P))
            cw = sb(nc, st2, "mlcw", [P, 4, 4], F32)
            cb = sb(nc, st2, "mlcb", [P, 4], F32)
            S.dma(cw[:], g.dram["conv_w_fm"][l])
            S.dma(cb[:], g.dram["conv_b_fm"][l])
            acc = [sb(nc, st2, "mlacc%d" % i, [P, T], F32) for i in range(2)]
            ai = 0
            for (x, dst, ci0) in ((xq, q16, 0), (xk, k16, 2)):
                for c in range(2):
                    a = acc[ai % 2]
                    eng = "dve"
                    ai += 1
                    S.ts(a[:], x[:, c, 0:T], cw[:, ci0 + c, 0:1], None, ALU.mult, eng=eng)
                    for tap in range(1, 4):
                        S.stt(a[:], x[:, c, tap:T + tap], cw[:, ci0 + c, tap:tap + 1], a[:], ALU.mult, ALU.add, eng=eng)
                    S.act(dst[:, c, :], a[:], AF.Silu, bias=cb[:, ci0 + c:ci0 + c + 1])
            ig = sb(nc, st2, "mlig", [4, T], F32)
            fg = sb(nc, st2, "mlfg", [4, T], F32)
            S.dma(ig[:], pfm_d[FM_CIG:FM_CIG + 4, :])
            S.dma(fg[:], pfm_d[FM_CFG:FM_CFG + 4, :])
            gb = sb(nc, st2, "mlgb", [4, 2], F32)
            S.dma(gb[:, 0:1], g.dram["ml_ig_b"][l].rearrange("(h o) -> h o", o=1))
            S.dma(gb[:, 1:2], g.dram["ml_fg_b"][l].rearrange("(h o) -> h o", o=1))
            S.ts(gb[:], gb[:], 1.0 / 15.0, None, ALU.mult)
            S.act(ig[:], ig[:], AF.Tanh, scale=1.0 / 15.0, bias=gb[:, 0:1])
            S.act(fg[:], fg[:], AF.Tanh, scale=1.0 / 15.0, bias=gb[:, 1:2])
            S.act(fg[:], fg[:], AF.Exp, scale=-15.0)
            S.act(fg[:], fg[:], AF.Ln, bias=g.one1[0:4, 0:1])
            ones4 = sb(nc, st2, "mlones", [4, T], F32)
            S.memset(ones4[:], 1.0)
            Bn = sb(nc, st2, "mlBn", [4, T], F32)
            S.scan(Bn[:], ones4[:], fg[:], 0.0, ALU.mult, ALU.add)
            cT = sb(nc, st2, "mlcT", [4, T], F32)
            S.stt(cT[:], ig[:], 15.0, Bn[:], ALU.mult, ALU.add)
            S.ts(cT[:], cT[:], LN8, None, ALU.add)
            BT = sb(nc, st2, "mlBT", [4, T], F32)
            S.ts(BT[:], Bn[:], -1.0, None, ALU.mult)
            ev = 0
            for h in range(4):
                for sblk in range(4):
                    ps = g.ps[ev % 4]
                    S.mm(ps[:, :], g.selh[0:4, h, :], BT[0:4, sblk * 512:(sblk + 1) * 512])
                    S.copy(BtB[h][:, sblk * 512:(sblk + 1) * 512], ps[:, :], eng=("act" if ev % 2 else "dve"))
                    ev += 1
            pc = g.ps[4]
            for b in range(16):
                S.mm(pc[:, b * 4:(b + 1) * 4], cT[0:4, b * 128:(b + 1) * 128], g.ident[0:4, 0:4])
            S.copy(c_tm[:], pc[:, 0:64].rearrange("p (b h) -> p b h", h=4))
            vf = sb(nc, st2, "mlvf", [P, 16, 256], F32)
            S.dma(vf[:], ptm_d[:, TM_CV:TM_CV + 256].rearrange("(b p) n -> p b n", p=P))
            S.copy(v16[:, :, :, 0:64], vf[:].rearrange("p b (h d) -> p b h d", d=64), eng="pool")
            S.memset(v16[:, :, :, 64:65], 1.0, eng="pool")
            S.dma(osig[:], ptm_d[:, TM_CO:TM_CO + 256].rearrange("(b p) n -> p b n", p=P))
            S.act(osig[:], osig[:], AF.Sigmoid)
            S.flush()
        attA = [sb(nc, st, "mlt%d" % i, [P, 16, 512], BF16) for i in range(2)]
        Dm = [sb(nc, st, "mlD%d" % i, [P, 512], F32) for i in range(2)]
        dtmp = [sb(nc, st, "mldt%d" % i, [P, 128], F32) for i in range(2)]
        nd = [sb(nc, st, "mlnd%d" % i, [P, 4, 65], F32) for i in range(2)]
        dn = [sb(nc, st, "mldn%d" % i, [P, 4], F32) for i in range(2)]
        hraw = sb(nc, st, "mlh", [P, 16, 256], F32)
        it = 0
        for h in range(4):
            c = h // 2
            pb = (h % 2) * 64
            for I in range(4):
                gi = h * 4 + I
                po = g.ps[6 + (gi % 2)]
                att_all = attA[gi % 2]
                for j in range(4 * I + 3, -1, -1):
                    u = it % 2
                    it += 1
                    pz = g.ps[u]
                    d = j - 4 * I
                    dd = max(d, 0)
                    c0 = dd * 128
                    n = 512 - c0
                    q0 = I * 512 + c0
                    S.mm(pz[:, :n], k16[pb:pb + 64, c, j * 128:(j + 1) * 128], q16[pb:pb + 64, c, q0:q0 + n])
                    if d >= 0:
                        S.tt(dtmp[u][:], BtB[h][:, q0:q0 + 128], g.lebias[:], ALU.add, eng="pool")
                        S.act(Dm[u][:, 0:128], dtmp[u][:], AF.Exp, bias=c_tm[:, j, h:h + 1])
                        if n > 128:
                            S.act(Dm[u][:, 128:n], BtB[h][:, q0 + 128:q0 + n], AF.Exp, bias=c_tm[:, j, h:h + 1])
                    else:
                        S.act(Dm[u][:, :n], BtB[h][:, q0:q0 + n], AF.Exp, bias=c_tm[:, j, h:h + 1])
                    S.tt(att_all[:, j, c0:512], pz[:, :n], Dm[u][:, :n], ALU.mult)
                for qb in range(4):
                    for j in range(4 * I + qb, -1, -1):
                        S.mm(po[:, qb * 65:(qb + 1) * 65], att_all[:, j, qb * 128:(qb + 1) * 128],
                             v16[:, j, h, :], start=(j == 4 * I + qb), stop=(j == 0))
                u2 = gi % 2
                S.copy(nd[u2][:], po[:, 0:260].rearrange("p (b d) -> p b d", d=65), eng="act")
                S.stt(dn[u2][:], nd[u2][:, :, 64], -1.0, nd[u2][:, :, 64], ALU.mult, ALU.max)
                S.ts(dn[u2][:], dn[u2][:], 1.0, None, ALU.max)
                S.recip(dn[u2][:], dn[u2][:])
                S.tt(hraw[:, 4 * I:4 * I + 4, h * 64:(h + 1) * 64], nd[u2][:, :, 0:64],
                     bc(dn[u2][:].unsqueeze(2), [P, 4, 64]), ALU.mult)
        tm_head_rmsnorm(S, nc, st, g, hraw, 16, gain, g.eps6)
        S.tt(hraw[:], hraw[:], osig[:], ALU.mult)
        S.dma(ycat_d[:, 512:768].rearrange("(b p) n -> p b n", p=P), hraw[:])
        S.flush()


def _rope_tm(S, out, x, cos, sin, t1, t2, half):
    x1, x2 = x[:, :, 0:half], x[:, :, half:2 * half]
    S.tt(t1, x1, cos, ALU.mult)
    S.tt(t2, x2, sin, ALU.mult)
    S.tt(out[:, :, 0:half], t1, t2, ALU.subtract)
    S.tt(t1, x2, cos, ALU.mult)
    S.tt(t2, x1, sin, ALU.mult)
    S.tt(out[:, :, half:2 * half], t1, t2, ALU.add)


def ph_dsa(S, nc, g, l, ptm_d, ycat_d):
    WI_SCALE = float(4 ** -0.5 * 32 ** -0.5)
    with ExitStack() as st:
        qT = sb(nc, st, "dqT", [P, 2, T], BF16)
        kT2 = sb(nc, st, "dkT", [P, T], BF16)
        qiT = sb(nc, st, "dqi", [P, T], F32)
        kiX = [sb(nc, st, "dki%d" % h, [P, T], F32) for h in range(4)]
        wi = sb(nc, st, "dwi", [P, 16, 4], F32)
        v16 = sb(nc, st, "dv", [P, 16, 65], BF16)
        gain = load_row_bcast(S, nc, st, "dg", g.dram["ds_out_g"][l], 256)
        with ExitStack() as st2:
            rope64 = sb(nc, st2, "rope64", [P, 16, 64], F32)
            rope32 = sb(nc, st2, "rope32", [P, 16, 32], F32)
            S.dma(rope64[:], g.dram["rope64"].rearrange("(b p) n -> p b n", p=P))
            S.dma(rope32[:], g.dram["rope32"].rearrange("(b p) n -> p b n", p=P))
            gq = load_row_bcast(S, nc, st2, "dgq", g.dram["ds_qn_g"][l], 64)
            gk = load_row_bcast(S, nc, st2, "dgk", g.dram["ds_kn_g"][l], 64)
            xs = [sb(nc, st2, "dx%d" % i, [P, 548], F32) for i in range(2)]
            sq = sb(nc, st2, "dsq", [P, 5, 64], F32)
            ss = sb(nc, st2, "dss", [P, 5], F32)
            qn = sb(nc, st2, "dqn", [P, 5, 64], F32)
            qr = [sb(nc, st2, "dqr%d" % i, [P, 6, 64], F32) for i in range(2)]
            qir = [sb(nc, st2, "dqir%d" % i, [P, 5, 32], F32) for i in range(2)]
            t1 = sb(nc, st2, "dt1", [P, 5, 32], F32)
            t2 = sb(nc, st2, "dt2", [P, 5, 32], F32)
            t3 = sb(nc, st2, "dt3", [P, 5, 16], F32)
            t4 = sb(nc, st2, "dt4", [P, 5, 16], F32)
            kiz = [[sb(nc, st2, "dkz%d_%d" % (i, h), [P, 128], F32) for h in range(4)] for i in range(2)]
            for i in range(2):
                for h in range(4):
                    S.memset(kiz[i][h][:], 0.0, eng="pool")
            S.dma(wi[:], ptm_d[:, TM_DWI:TM_DWI + 4].rearrange("(b p) n -> p b n", p=P))
            S.ts(wi[:], wi[:], WI_SCALE, None, ALU.mult)
            ev = 0
            cut = getattr(g, 'dsa_cut', 0)
            for b in range(16):
                x = xs[b % 2]
                S.dma(x[:], ptm_d[b * 128:(b + 1) * 128, TM_DQ:TM_DQ + 548])
                qk = x[:, 0:320].rearrange("p (h d) -> p h d", d=64)
                S.tt(sq[:], qk, qk, ALU.mult)
                S.reduce(ss[:], sq[:], ALU.add, AX.X)
                S.act(ss[:], ss[:], AF.Sqrt, scale=1.0 / 64, bias=g.eps6[:, 0:1])
                S.recip(ss[:], ss[:])
                S.tt(qn[:], qk, bc(ss[:].unsqueeze(2), [P, 5, 64]), ALU.mult)
                S.tt(qn[:, 0:4, :], qn[:, 0:4, :], bc(gq[:].unsqueeze(1), [P, 4, 64]), ALU.mult)
                S.tt(qn[:, 4:5, :], qn[:, 4:5, :], gk[:].unsqueeze(1), ALU.mult)
                r_ = qr[b % 2]
                cos = bc(rope64[:, b, 0:32].unsqueeze(1), [P, 5, 32])
                sin = bc(rope64[:, b, 32:64].unsqueeze(1), [P, 5, 32])
                _rope_tm(S, r_[:, 0:5, :], qn[:], cos, sin, t1[:], t2[:], 32)
                S.copy(r_[:, 5, :], r_[:, 4, :], eng="act")
                ri = qir[b % 2]
                xi = x[:, 384:544].rearrange("p (h d) -> p h d", d=32)
                cos = bc(rope32[:, b, 0:16].unsqueeze(1), [P, 5, 16])
                sin = bc(rope32[:, b, 16:32].unsqueeze(1), [P, 5, 16])
                _rope_tm(S, ri[:], xi, cos, sin, t3[:], t4[:], 16)
                S.copy(v16[:, b, 0:64], x[:, 320:384], eng="act")
                if cut == 1:
                    continue
                tb = slice(b * 128, (b + 1) * 128)
                for c in range(3):
                    ps = g.ps[ev % 4]
                    ev += 1
                    S.transpose(ps[:, 0:128], r_[:, 2 * c:2 * c + 2, :].rearrange("p h d -> p (h d)"), g.ident[:])
                    if c < 2:
                        S.copy(qT[:, c, tb], ps[:, 0:128], eng="act")
                    else:
                        S.copy(kT2[:, tb], ps[:, 0:128], eng="act")
                if cut == 2:
                    continue
                ps = g.ps[ev % 4]
                ev += 1
                S.transpose(ps[:, 0:128], ri[:, 0:4, :].rearrange("p h d -> p (h d)"), g.ident[:])
                S.copy(qiT[:, tb], ps[:, 0:128], eng="dve")
                kz = kiz[b % 2]
                for h in range(4):
                    S.copy(kz[h][:, h * 32:(h + 1) * 32], ri[:, 4, :], eng="pool")
                    ps = g.ps[ev % 4]
                    ev += 1
                    S.transpose(ps[:, 0:128], kz[h][:], g.ident[:])
                    S.copy(kiX[h][:, tb], ps[:, 0:128], eng=("act" if h % 2 else "dve"))
            S.memset(v16[:, :, 64:65], 1.0, eng="pool")
            S.flush()
        if getattr(g, "dsa_stop", 0) == 1:
            return
        sc = [sb(nc, st, "dsc%d" % i, [P, T], F32) for i in range(1)] * 2
        work = sb(nc, st, "dwork", [P, T], F32)
        mk = sb(nc, st, "dmk", [P, T], F32)
        eqm = sb(nc, st, "deq", [P, T], F32)
        cum = sb(nc, st, "dcum", [P, T], F32)
        onesT = sb(nc, st, "dones", [P, T], F32)
        S.memset(onesT[:], 1.0, eng="pool")
        rl = [sb(nc, st, "drl%d" % i, [P, 512], F32) for i in range(2)]
        m8 = sb(nc, st, "dm8", [P, 8], F32)
        ngt = sb(nc, st, "dngt", [P, 1], F32)
        maskT = [sb(nc, st, "dmT%d" % i, [P, 16, 128], F32) for i in range(2)]
        E = [sb(nc, st, "dE%d" % i, [P, 512], F32) for i in range(2)]
        Pall = [sb(nc, st, "dP%d" % i, [P, 16, 512], BF16) for i in range(2)]
        nd = [sb(nc, st, "dnd%d" % i, [P, 4, 65], F32) for i in range(2)]
        dn = [sb(nc, st, "ddn%d" % i, [P, 4], F32) for i in range(2)]
        yraw = sb(nc, st, "dy", [P, 16, 256], F32)
        cnt = {'ev': 0, 'it': 0}

        def part_a(i):
            ev = cnt['ev']
            kl = 128 * (i + 1)
            qb = slice(i * 128, (i + 1) * 128)
            mT = maskT[i % 2]
            if i >= 2:
                s_ = sc[i % 2]
                for kb in range(0, kl, 512):
                    w = min(512, kl - kb)
                    for h in range(4):
                        ps = g.ps[ev % 2]
                        r2 = rl[ev % 2]
                        ev += 1
                        S.mm(ps[:, :w], qiT[:, qb], kiX[h][:, kb:kb + w])
                        S.act(r2[:, :w], ps[:, :w], AF.Relu)
                        if h == 0:
                            S.ts(s_[:, kb:kb + w], r2[:, :w], wi[:, i, 0:1], None, ALU.mult)
                        else:
                            S.stt(s_[:, kb:kb + w], r2[:, :w], wi[:, i, h:h + 1], s_[:, kb:kb + w], ALU.mult, ALU.add)
                S.tt(s_[:, qb], s_[:, qb], g.cbias[:], ALU.add)
                for r in range(32):
                    S.max8(m8[:], (s_ if r == 0 else work)[:, :kl])
                    if r < 31:
                        S.match_replace(work[:, :kl], m8[:], (s_ if r == 0 else work)[:, :kl], NEG)
                S.ts(mk[:, :kl], s_[:, :kl], m8[:, 7:8], None, ALU.is_gt)
                S.reduce(ngt[:], mk[:, :kl], ALU.add, AX.X)
                S.ts(ngt[:], ngt[:], -1.0, 256.0, ALU.mult, ALU.add)
                S.ts(eqm[:, :kl], s_[:, :kl], m8[:, 7:8], None, ALU.is_equal)
                S.scan(cum[:, :kl], onesT[:, :kl], eqm[:, :kl], 0.0, ALU.mult, ALU.add)
                S.ts(cum[:, :kl], cum[:, :kl], ngt[:, 0:1], None, ALU.is_le)
                S.tt(eqm[:, :kl], eqm[:, :kl], cum[:, :kl], ALU.mult)
                S.tt(mk[:, :kl], mk[:, :kl], eqm[:, :kl], ALU.add)
            else:
                for j in range(i):
                    S.copy(mT[:, j, :], g.ones[:], eng="dve")
                S.copy(mT[:, i, :], g.tri_le[:], eng="dve")
            cnt['ev'] = ev

        def part_a3(i):
            if i < 2:
                return
            mT = maskT[i % 2]
            for j0 in range(0, i + 1, 4):
                nj = min(4, i + 1 - j0)
                ps = g.ps[1]
                for jj in range(nj):
                    S.transpose(ps[:, jj * 128:(jj + 1) * 128], mk[:, (j0 + jj) * 128:(j0 + jj + 1) * 128], g.ident[:])
                S.copy(mT[:, j0:j0 + nj, :], ps[:, 0:nj * 128].rearrange("p (j q) -> p j q", q=128), eng="act")

        def part_b(i):
            it = cnt['it']
            qb = slice(i * 128, (i + 1) * 128)
            mT = maskT[i % 2]
            Pa = Pall[i % 2]
            for j in range(i + 1):
                u = it % 2
                it += 1
                pzA, pzB = g.ps[2 + 2 * u], g.ps[3 + 2 * u]
                for h in range(4):
                    pb = (h % 2) * 64
                    pz = pzB if (h % 2) else pzA
                    S.mm(pz[:, (h // 2) * 128:(h // 2 + 1) * 128], kT2[pb:pb + 64, j * 128:(j + 1) * 128], qT[pb:pb + 64, h // 2, qb])
                S.act(E[u][:, 0:256], pzA[:, 0:256], AF.Exp, scale=0.125)
                S.act(E[u][:, 256:512], pzB[:, 0:256], AF.Exp, scale=0.125)
                S.tt(Pa[:, j, :].rearrange("p (h q) -> p h q", q=128), E[u][:].rearrange("p (h q) -> p h q", q=128),
                     bc(mT[:, j, :].unsqueeze(1), [P, 4, 128]), ALU.mult, eng="pool")
            if getattr(g, "dsa_cut2", 0) >= 1:
                return
            po = g.ps[6 + (i % 2)]
            for h in range(4):
                for j in range(i + 1):
                    hr = (h % 2) * 2 + h // 2
                    S.mm(po[:, h * 65:(h + 1) * 65], Pa[:, j, hr * 128:(hr + 1) * 128], v16[:, j, :],
                         start=(j == 0), stop=(j == i))
            u2 = i % 2
            S.copy(nd[u2][:], po[:, 0:260].rearrange("p (h d) -> p h d", d=65), eng="act")
            S.recip(dn[u2][:], nd[u2][:, :, 64])
            S.tt(yraw[:, i, :].rearrange("p (h d) -> p h d", d=64), nd[u2][:, :, 0:64],
                 bc(dn[u2][:].unsqueeze(2), [P, 4, 64]), ALU.mult)
            cnt['it'] = it

        nblk = getattr(g, "dsa_nblk", 16)
        part_a(0)
        part_a3(0)
        for i in range(nblk):
            if i + 1 < nblk:
                part_a(i + 1)
            part_b(i)
            if i + 1 < nblk:
                part_a3(i + 1)
        tm_head_rmsnorm(S, nc, st, g, yraw, 16, gain, g.eps6)
        S.dma(ycat_d[:, 768:1024].rearrange("(b p) n -> p b n", p=P), yraw[:])
        S.flush()


def ph_rwkv_prep(S, nc, g, l, s, ptm_d, pfm_d, sops_d, sv_d, sbg_d):
    x_, sp = s // 2, s % 2
    with ExitStack() as st:
        twT = sb(nc, st, "rtw", [33, T], F32)
        adT = sb(nc, st, "rad", [33, T], F32)
        sgT = sb(nc, st, "rsg", [64, T], F32)
        S.memset(twT[:], 1.0)
        S.memset(adT[:], 1.0, eng="pool")
        S.dma(twT[0:32, :], pfm_d[FM_AWD:FM_AWD + 32, :])
        S.dma(adT[0:32, :], pfm_d[FM_AAD:FM_AAD + 32, :])
        S.dma(sgT[:], pfm_d[FM_AGD:FM_AGD + 64, :])
        S.act(twT[0:32, :], twT[0:32, :], AF.Tanh)
        S.act(sgT[:], sgT[:], AF.Sigmoid)
        w2a = sb(nc, st, "rw2", [33, 256], F32)
        a2a = sb(nc, st, "ra2", [33, 256], F32)
        g2 = sb(nc, st, "rg2", [64, 256], F32)
        S.dma(w2a[0:32, :], g.dram["rk_w2"][l])
        S.dma(w2a[32:33, :], g.dram["rk_w0"][l].rearrange("(o n) -> o n", o=1))
        S.dma(a2a[0:32, :], g.dram["rk_a2"][l])
        S.dma(a2a[32:33, :], g.dram["rk_a0"][l].rearrange("(o n) -> o n", o=1))
        S.dma(g2[:], g.dram["rk_g2"][l])
        kk_b = load_row_bcast(S, nc, st, "rkk", g.dram["rk_kk"][l], 256)
        ka_b = load_row_bcast(S, nc, st, "rka", g.dram["rk_ka"][l], 256)
        rk_b = load_row_bcast(S, nc, st, "rrk", g.dram["rk_rk"][l], 256)
        vfm = sb(nc, st, "rvfm", [P, 2, T], F32)
        S.dma(vfm[:], pfm_d[FM_AV:FM_AV + 256, :].rearrange("(c p) t -> p c t", p=P))
        S.dma(sv_d[s].rearrange("(c p) t -> p c t", p=P), vfm[:])
        xs = [sb(nc, st, "rx%d" % i, [P, 768], F32) for i in range(2)]
        F = lambda n: [sb(nc, st, "%s%d" % (n, i), [P, 256], F32) for i in range(2)]
        sig, dec, a_, kkn, k2, nkka, tmp = F("rsig"), F("rdec"), F("ra"), F("rkkn"), F("rk2"), F("rnk"), F("rtmp")
        bg = [sb(nc, st, "rbg%d" % i, [P, 512], F32) for i in range(2)]
        ss = sb(nc, st, "rss", [P, 4], F32)
        bco = sb(nc, st, "rbc", [P, 4], F32)
        hi = [[sb(nc, st, "rhi%d_%d" % (i, o), [P, 256], BF16) for o in range(5)] for i in range(2)]
        lo = [[sb(nc, st, "rlo%d_%d" % (i, o), [P, 256], BF16) for o in range(5)] for i in range(2)]
        h32 = [sb(nc, st, "rh32_%d" % i, [P, 256], F32) for i in range(2)]
        hv = lambda ap: ap.rearrange("p (h d) -> p h d", d=64)
        for tb in range(16):
            u = tb % 2
            x = xs[u]
            tsl = slice(tb * 128, (tb + 1) * 128)
            S.dma(x[:], ptm_d[tsl, 0:768])
            r, k, v = x[:, 0:256], x[:, 256:512], x[:, 512:768]
            pw, pa, pg = g.ps[0 + u], g.ps[2 + u], g.ps[4 + u]
            S.mm(pw[:, 0:256], twT[0:33, tsl], w2a[0:33, :])
            S.mm(pa[:, 0:256], adT[0:33, tsl], a2a[0:33, :])
            S.mm(pg[:, 0:256], sgT[0:64, tsl], g2[0:64, :])
            S.act(sig[u][:], pw[:, 0:256], AF.Sigmoid)
            S.act(a_[u][:], pa[:, 0:256], AF.Sigmoid)
            S.act(dec[u][:], sig[u][:], AF.Exp, scale=-0.6065306597126334)
            S.copy(bg[u][:, 256:512], pg[:, 0:256], eng="act")
            S.tt(kkn[u][:], k, kk_b[:], ALU.mult)
            S.tt(tmp[u][:], kkn[u][:], kkn[u][:], ALU.mult)
            S.reduce(ss[:], hv(tmp[u][:]), ALU.add, AX.X)
            S.act(ss[:], ss[:], AF.Sqrt)
            S.ts(ss[:], ss[:], 1e-12, None, ALU.max)
            S.recip(ss[:], ss[:])
            S.tt(hv(kkn[u][:]), hv(kkn[u][:]), bc(ss[:].unsqueeze(2), [P, 4, 64]), ALU.mult)
            S.stt(k2[u][:], a_[u][:], -1.0, ka_b[:], ALU.add, ALU.mult)
            S.stt(k2[u][:], k2[u][:], 1.0, k, ALU.add, ALU.mult)
            S.stt(nkka[u][:], kkn[u][:], -1.0, a_[u][:], ALU.mult, ALU.mult)
            S.tt(tmp[u][:], r, k2[u][:], ALU.mult)
            S.tt(tmp[u][:], tmp[u][:], rk_b[:], ALU.mult)
            S.reduce(bco[:], hv(tmp[u][:]), ALU.add, AX.X)
            S.tt(hv(bg[u][:, 0:256]), hv(v), bc(bco[:].unsqueeze(2), [P, 4, 64]), ALU.mult)
            S.dma(sbg_d[s, tsl, :], bg[u][:])
            for o, src in enumerate((kkn[u][:], dec[u][:], nkka[u][:], k2[u][:], r)):
                S.copy(hi[u][o][:], src, eng="act")
                S.copy(h32[o % 2][:], hi[u][o][:], eng="pool")
                S.tt(h32[o % 2][:], src, h32[o % 2][:], ALU.subtract, eng="pool")
                S.copy(lo[u][o][:], h32[o % 2][:], eng="act")
                S.dma(sops_d[o, 0, x_, tsl, sp * 256:(sp + 1) * 256], hi[u][o][:])
                S.dma(sops_d[o, 1, x_, tsl, sp * 256:(sp + 1) * 256], lo[u][o][:])
        S.flush()


SCAN_ACT_Y = False


def ph_rwkv_scan(S, nc, g, sops_d, sv_d, sy_d, nsteps=T):
    CH = 32
    with ExitStack() as st:
        id2 = sb(nc, st, "sid2", [P, 128], BF16)
        S.memset(id2[:], 0.0)
        S.tt(id2[:, 0:32], g.ident16[:, 0:32], g.ident16[:, 32:64], ALU.add)
        S.tt(id2[:, 64:96], g.ident16[:, 64:96], g.ident16[:, 96:128], ALU.add)
        sel = sb(nc, st, "ssel", [P, CH, 128], BF16)
        for xp in range(2):
            for tp in range(CH):
                col = xp * 64 + tp
                S.copy(sel[:, tp, xp * 64:(xp + 1) * 64], bc(id2[:, col:col + 1], [P, 64]),
                       eng=("dve" if tp % 2 else "pool"))
        Stt = [sb(nc, st, "sS%d" % i, [P, 512], F32) for i in range(2)]
        S.memset(Stt[0][:], 0.0)
        S.memset(Stt[1][:], 0.0)
        ytmps = [sb(nc, st, "sytmp%d" % i, [P, 512], F32) for i in range(2)]
        junk = sb(nc, st, "sjunk", [P, 512], F32)
        Sw = sb(nc, st, "sSw", [P, 512], F32)
        tmp = sb(nc, st, "stmp", [P, 512], F32)
        sa = sb(nc, st, "ssa", [P, 8], F32)
        ND = 3
        ringW = [sb(nc, st, "srgW%d" % d, [P, 512], F32) for d in range(ND)]
        ringK = [sb(nc, st, "srgK%d" % d, [P, 512], F32) for d in range(ND)]
        vk = [sb(nc, st, "svk%d" % d, [P, 512], F32) for d in range(ND)]
        opt = [[sb(nc, st, "sop%d_%d" % (b_, o), [P, 512], BF16) for o in range(5)] for b_ in range(3)]
        vS = [sb(nc, st, "svS%d" % i, [P, 8, 256], F32) for i in range(2)]
        yb = [sb(nc, st, "syb%d" % i, [P, 8, 256], F32) for i in range(2)]
        g3 = lambda ap: ap.rearrange("p (g k) -> p g k", k=64)
        step = 0
        pend = None
        nbig = (nsteps + 255) // 256
        for big in range(nbig):
            vs, y_ = vS[big % 2], yb[big % 2]
            bsl = slice(big * 256, (big + 1) * 256)
            for x in range(2):
                for sp in range(2):
                    for h in range(4):
                        S.dma(vs[x * 64:(x + 1) * 64, sp * 4 + h, :], sv_d[2 * x + sp, h * 64:(h + 1) * 64, bsl])
            for cc in range(256 // CH):
                c = big * (256 // CH) + cc
                if c * CH >= nsteps:
                    break
                ob = opt[c % 3]
                for o in range(5):
                    for x in range(2):
                        for hl in range(2):
                            p0 = x * 64 + hl * 32
                            S.dma(ob[o][p0:p0 + CH, :], sops_d[o, hl, x, c * CH:(c + 1) * CH, :])
                for tp in range(CH):
                    ti = cc * CH + tp
                    d = step % ND
                    par = step % 2
                    banks = (g.ps[0 + par], g.ps[6], g.ps[2 + par], g.ps[7], g.ps[4 + par])
                    for o in range(5):
                        S.mm(banks[o][:, :], sel[:, tp, :], ob[o][:])
                    S.copy(ringW[d][:], banks[1][:, :], eng="act")
                    S.copy(ringK[d][:], banks[3][:, :], eng="act")
                    KK, NK, R = banks[0], banks[2], banks[4]
                    W, KB = ringW[d], ringK[d]
                    Sp, Sn = Stt[step % 2], Stt[(step + 1) % 2]
                    S.tt(g3(vk[d][:]), g3(KB[:]), bc(vs[:, :, ti:ti + 1], [P, 8, 64]), ALU.mult, eng="pool")
                    S.tt(Sw[:], Sp[:], W[:], ALU.mult, eng="pool")
                    S.tt(Sw[:], Sw[:], vk[d][:], ALU.add, eng="pool")
                    S.tt(tmp[:], Sp[:], KK[:, :], ALU.mult)
                    if pend is not None:
                        S.tt(pend[0][:], pend[1][:], pend[2][:, :], ALU.mult)
                    S.reduce(sa[:], g3(tmp[:]), ALU.add, AX.X)
                    if pend is not None:
                        S.reduce(pend[3], g3(pend[0][:]), ALU.add, AX.X)
                        pend = None
                    S.tt(g3(tmp[:]), g3(NK[:, :]), bc(sa[:].unsqueeze(2), [P, 8, 64]), ALU.mult)
                    S.tt(Sn[:], Sw[:], tmp[:], ALU.add)
                    pend = (ytmps[step % 2], Sn, R, y_[:, :, ti])
                    step += 1
            if pend is not None:
                S.tt(pend[0][:], pend[1][:], pend[2][:, :], ALU.mult)
                S.reduce(pend[3], g3(pend[0][:]), ALU.add, AX.X)
                pend = None
            for x in range(2):
                for sp in range(2):
                    for h in range(4):
                        S.dma(sy_d[2 * x + sp, h * 64:(h + 1) * 64, bsl], y_[x * 64:(x + 1) * 64, sp * 4 + h, :])
        S.flush()


def ph_rwkv_post(S, nc, g, l, s, sy_d, sbg_d, ycat_d):
    with ExitStack() as st:
        yT = sb(nc, st, "pyT", [P, 2, T], F32)
        S.dma(yT[:], sy_d[s].rearrange("(c p) t -> p c t", p=P))
        bgt = sb(nc, st, "pbg", [P, 16, 512], F32)
        S.dma(bgt[:], sbg_d[s].rearrange("(b p) n -> p b n", p=P))
        lng = load_row_bcast(S, nc, st, "plng", g.dram["rk_ln_g"][l], 256)
        lnb = load_row_bcast(S, nc, st, "plnb", g.dram["rk_ln_b"][l], 256)
        y = sb(nc, st, "py", [P, 16, 256], F32)
        for tb in range(16):
            ps = g.ps[tb % 4]
            for c in range(2):
                S.transpose(ps[:, c * 128:(c + 1) * 128], yT[:, c, tb * 128:(tb + 1) * 128], g.ident[:])
            S.copy(y[:, tb, :], ps[:, 0:256], eng=("act" if tb % 2 else "dve"))
        yv = y[:].rearrange("p b (h d) -> p (b h) d", d=64)
        mean = sb(nc, st, "pmean", [P, 64], F32)
        sq = sb(nc, st, "psq", [P, 64, 64], F32)
        S.reduce(mean[:], yv, ALU.add, AX.X)
        S.ts(mean[:], mean[:], 1.0 / 64, None, ALU.mult)
        S.tt(yv, yv, bc(mean[:].unsqueeze(2), [P, 64, 64]), ALU.subtract)
        S.tt(sq[:], yv, yv, ALU.mult)
        S.reduce(mean[:], sq[:], ALU.add, AX.X)
        eps = sb(nc, st, "peps", [P, 1], F32)
        S.memset(eps[:], 64e-5)
        S.act(mean[:], mean[:], AF.Sqrt, scale=1.0 / 64, bias=eps[:, 0:1])
        S.recip(mean[:], mean[:])
        S.tt(yv, yv, bc(mean[:].unsqueeze(2), [P, 64, 64]), ALU.mult)
        S.tt(y[:], y[:], bc(lng[:].unsqueeze(1), [P, 16, 256]), ALU.mult)
        S.tt(y[:], y[:], bc(lnb[:].unsqueeze(1), [P, 16, 256]), ALU.add)
        S.tt(y[:], y[:], bgt[:, :, 0:256], ALU.add)
        S.tt(y[:], y[:], bgt[:, :, 256:512], ALU.mult)
        S.dma(ycat_d[:, 0:256].rearrange("(b p) n -> p b n", p=P), y[:])
        S.flush()


def ph_outproj(S, nc, g, l, b, ycat_d, xin_d, xout_d, tok0):
    with ExitStack() as st:
        Wb = sb(nc, st, "oW", [P, 8, 1024], BF16)
        ycT = sb(nc, st, "oyT", [P, 8, T], BF16)
        with ExitStack() as st2:
            stg = [sb(nc, st2, "ostg%d" % i, [P, 8, 512], F32) for i in range(2)]
            for hf in range(2):
                S.dma(stg[hf][:], g.dram["w_out"][l][:, hf * 512:(hf + 1) * 512].rearrange("(c p) n -> p c n", p=P))
                S.copy(Wb[:, :, hf * 512:(hf + 1) * 512], stg[hf][:], eng=("act" if hf else "pool"))
            yb = [sb(nc, st2, "oyb%d" % i, [P, 1024], F32) for i in range(2)]
            ev = 0
            for tb in range(16):
                y = yb[tb % 2]
                S.dma(y[:], ycat_d[tb * 128:(tb + 1) * 128, :])
                for c4 in range(2):
                    ps = g.ps[ev % 4]
                    ev += 1
                    for cc in range(4):
                        c = c4 * 4 + cc
                        S.transpose(ps[:, cc * 128:(cc + 1) * 128], y[:, c * 128:(c + 1) * 128], g.ident[:])
                    S.copy(ycT[:, c4 * 4:c4 * 4 + 4, tb * 128:(tb + 1) * 128],
                           ps[:, :].rearrange("p (c t) -> p c t", t=128), eng=("act" if ev % 2 else "dve"))
            S.flush()
        xo = [sb(nc, st, "oxo%d" % i, [P, 512], F32) for i in range(3)]
        ev = 0
        for oc in range(8):
            for sblk in range(4):
                ps = g.ps[4 + (ev % 4)]
                x = xo[ev % 3]
                ev += 1
                tsl = slice(tok0 + sblk * 512, tok0 + (sblk + 1) * 512)
                S.dma(x[:], xin_d[oc * 128:(oc + 1) * 128, tsl])
                for k in range(8):
                    S.mm(ps[:, :], Wb[:, k, oc * 128:(oc + 1) * 128], ycT[:, k, sblk * 512:(sblk + 1) * 512],
                         start=(k == 0), stop=(k == 7))
                S.stt(x[:], ps[:, :], g.modT[:, 16 + oc, b:b + 1], x[:], ALU.mult, ALU.add)
                S.dma(xout_d[oc * 128:(oc + 1) * 128, tsl], x[:])
        S.flush()


def ph_moe(S, nc, g, l, b, xin_d, xout_d, tok0, n_exp=32):
    with ExitStack() as st:
        hT = sb(nc, st, "mhT", [P, 8, T], BF16)
        gate = sb(nc, st, "mgate", [P, 16, 32], F32)
        with ExitStack() as st2:
            wge = sb(nc, st2, "mwge", [P, 8, 36], F32)
            S.dma(wge[:], g.dram["moe_wge"][l].rearrange("(c p) n -> p c n", p=P))
            bge = load_row_bcast(S, nc, st2, "mbge", g.dram["moe_bge"][l], 36)
            lg = sb(nc, st2, "mlg", [P, 16, 36], F32)
            emit_norm_mod(S, nc, g, xin_d, tok0, b, g.A2, g.modT[:, 24:32, :], hT, col0=0, route=(wge, lg))
            S.tt(lg[:], lg[:], bc(bge[:].unsqueeze(1), [P, 16, 36]), ALU.add)
            G4 = lg[:, :, 0:4]
            gmax = sb(nc, st2, "mgmax", [P, 16], F32)
            ge = sb(nc, st2, "mge", [P, 16, 4], F32)
            gsum = sb(nc, st2, "mgsum", [P, 16], F32)
            pen = sb(nc, st2, "mpen", [P, 16, 4], F32)
            S.reduce(gmax[:], G4, ALU.max, AX.X)
            S.tt(ge[:], G4, bc(gmax[:].unsqueeze(2), [P, 16, 4]), ALU.subtract)
            S.ts(pen[:], ge[:], 0.0, NEG, ALU.is_lt, ALU.mult)
            S.act(ge[:], ge[:], AF.Exp)
            S.reduce(gsum[:], ge[:], ALU.add, AX.X)
            S.recip(gsum[:], gsum[:])
            Em = sb(nc, st2, "mEm", [P, 16, 32], F32)
            S.tt(Em[:].rearrange("p b (q e) -> p b q e", e=8), lg[:, :, 4:36].rearrange("p b (q e) -> p b q e", e=8),
                 bc(pen[:].unsqueeze(3), [P, 16, 4, 8]), ALU.add)
            m1 = sb(nc, st2, "mm1", [P, 16], F32)
            m2 = sb(nc, st2, "mm2", [P, 16], F32)
            E2 = sb(nc, st2, "mE2", [P, 16, 32], F32)
            S.reduce(m1[:], Em[:], ALU.max, AX.X)
            S.tt(E2[:], Em[:], bc(m1[:].unsqueeze(2), [P, 16, 32]), ALU.is_ge)
            S.stt(E2[:], E2[:], NEG, Em[:], ALU.mult, ALU.add)
            S.reduce(m2[:], E2[:], ALU.max, AX.X)
            ex = sb(nc, st2, "mex", [P, 16, 32], F32)
            S.tt(ex[:], Em[:], bc(m1[:].unsqueeze(2), [P, 16, 32]), ALU.subtract)
            S.act(ex[:], ex[:], AF.Exp)
            S.tt(E2[:], Em[:], bc(m2[:].unsqueeze(2), [P, 16, 32]), ALU.is_ge)
            S.tt(ex[:], ex[:], E2[:], ALU.mult)
            den = sb(nc, st2, "mden", [P, 16], F32)
            S.tt(den[:], m2[:], m1[:], ALU.subtract)
            S.act(den[:], den[:], AF.Exp)
            S.ts(den[:], den[:], 1.0, None, ALU.add)
            S.recip(den[:], den[:])
            S.tt(den[:], den[:], gsum[:], ALU.mult)
            S.tt(gate[:], ex[:], bc(den[:].unsqueeze(2), [P, 16, 32]), ALU.mult)
            S.flush()
        acc = sb(nc, st, "macc", [P, 16, 1024], F32)
        stg = [sb(nc, st, "mstg%d" % i, [P, 8, 512], F32) for i in range(2)]
        W1b = [sb(nc, st, "mW1_%d" % i, [P, 8, 512], BF16) for i in range(2)]
        W3b = [sb(nc, st, "mW3_%d" % i, [P, 8, 512], BF16) for i in range(2)]
        W2b = [sb(nc, st, "mW2_%d" % i, [P, 4, 1024], BF16) for i in range(2)]
        aT = [sb(nc, st, "maT%d" % i, [P, 4, 512], BF16) for i in range(2)]
        su = [sb(nc, st, "msu%d" % i, [P, 512], F32) for i in range(2)]
        cnt = {"si": 0}

        def load_w(e):
            u = e % 2
            for (dst, src) in ((W1b[u], g.dram["moe_w1"][l, e]), (W3b[u], g.dram["moe_w3"][l, e])):
                sg = stg[cnt["si"] % 2]
                cnt["si"] += 1
                S.dma(sg[:], src.rearrange("(c p) n -> p c n", p=P))
                S.copy(dst[:], sg[:], eng=("act" if cnt["si"] % 2 else "pool"))
            sg = stg[cnt["si"] % 2]
            cnt["si"] += 1
            S.dma(sg[:].rearrange("p c n -> p (c n)").rearrange("p (c n) -> p c n", n=1024),
                  g.dram["moe_w2"][l, e].rearrange("(c p) n -> p c n", p=P))
            S.copy(W2b[u][:].rearrange("p c n -> p (c n)"), sg[:].rearrange("p c n -> p (c n)"), eng="pool")

        its = [(e, sblk) for e in range(n_exp) for sblk in range(4)]

        def st13(n):
            e, sblk = its[n]
            u = e % 2
            if sblk == 0:
                load_w(e)
            a = aT[n % 2]
            for f in range(4):
                pu, pg3 = g.ps[(f % 2) * 2], g.ps[(f % 2) * 2 + 1]
                for k in range(8):
                    S.mm(pu[:, :], W1b[u][:, k, f * 128:(f + 1) * 128], hT[:, k, sblk * 512:(sblk + 1) * 512],
                         start=(k == 0), stop=(k == 7))
                for k in range(8):
                    S.mm(pg3[:, :], W3b[u][:, k, f * 128:(f + 1) * 128], hT[:, k, sblk * 512:(sblk + 1) * 512],
                         start=(k == 0), stop=(k == 7))
                s_ = su[f % 2]
                S.act(s_[:], pu[:, :], AF.Silu)
                S.tt(a[:, f, :], s_[:], pg3[:, :], ALU.mult)

        def st2(n):
            e, sblk = its[n]
            u = e % 2
            a = aT[n % 2]
            for tb4 in range(4):
                tb = sblk * 4 + tb4
                for hf in range(2):
                    py = g.ps[4 + ((tb4 * 2 + hf) % 4)]
                    for f in range(4):
                        S.mm(py[:, :], a[:, f, tb4 * 128:(tb4 + 1) * 128], W2b[u][:, f, hf * 512:(hf + 1) * 512],
                             start=(f == 0), stop=(f == 3))
                    dst = acc[:, tb, hf * 512:(hf + 1) * 512]
                    if e == 0:
                        S.ts(dst, py[:, :], gate[:, tb, e:e + 1], None, ALU.mult)
                    else:
                        S.stt(dst, py[:, :], gate[:, tb, e:e + 1], dst, ALU.mult, ALU.add)

        st13(0)
        for n in range(len(its)):
            if n + 1 < len(its):
                st13(n + 1)
            st2(n)
        xo = [sb(nc, st, "mxo%d" % i, [P, 512], F32) for i in range(2)]
        ev = 0
        for c in range(8):
            for sblk in range(4):
                ps = g.ps[ev % 4]
                x = xo[ev % 2]
                ev += 1
                tsl = slice(tok0 + sblk * 512, tok0 + (sblk + 1) * 512)
                S.dma(x[:], xin_d[c * 128:(c + 1) * 128, tsl])
                for tb4 in range(4):
                    S.transpose(ps[:, tb4 * 128:(tb4 + 1) * 128], acc[:, sblk * 4 + tb4, c * 128:(c + 1) * 128], g.ident[:])
                S.stt(x[:], ps[:, :], g.modT[:, 40 + c, b:b + 1], x[:], ALU.mult, ALU.add)
                S.dma(xout_d[c * 128:(c + 1) * 128, tsl], x[:])
        S.flush()


def build_full(shared_shapes, n_layers=2):
    nc = bass.Bass("TRN2", target_bir_lowering=False)
    g = G()
    g.dram = {}
    for k, shp in shared_shapes.items():
        g.dram[k] = nc.dram_tensor(k, list(shp), F32, kind="ExternalInput").ap()
    NT = NSEQ * T
    g.dram["xT"] = nc.dram_tensor("xT", [D, NT], F32, kind="ExternalInput").ap()
    g.dram["cT"] = nc.dram_tensor("cT", [D, 4], F32, kind="ExternalInput").ap()
    outT = nc.dram_tensor("outT", [D, NT], F32, kind="ExternalOutput").ap()
    X1 = nc.dram_tensor("X1", [D, NT], F32).ap()
    X2 = nc.dram_tensor("X2", [D, NT], F32).ap()
    ptm = nc.dram_tensor("ptm", [T, NTM], F32).ap()
    pfm = nc.dram_tensor("pfm", [NFM, T], F32).ap()
    ycat = nc.dram_tensor("ycat", [NSEQ, T, D], F32).ap()
    sops = nc.dram_tensor("sops", [5, 2, 2, T, 512], BF16).ap()
    sv = nc.dram_tensor("sv", [NSEQ, 256, T], F32).ap()
    sy = nc.dram_tensor("sy", [NSEQ, 256, T], F32).ap()
    sbg = nc.dram_tensor("sbg", [NSEQ, T, 512], F32).ap()
    with ExitStack() as st:
        S = Sched(nc)
        S.open(st)
        g.ps = [st.enter_context(nc.psum_tensor("ps%d" % i, [P, 512], F32)) for i in range(8)]
        setup_consts(S, nc, st, g)
        for l in range(n_layers):
            xin = g.dram["xT"] if l == 0 else X2
            xmid = X1
            xout = outT if l == n_layers - 1 else X2
            ph_ada(S, nc, g, l)
            for s in range(NSEQ):
                with ExitStack() as st2:
                    hT = sb(nc, st2, "hT", [P, 8, T + 4], BF16)
                    S.memset(hT[:, :, 0:1], 0.0)
                    emit_norm_mod(S, nc, g, xin, s * T, s, g.A1, g.modT[:, 0:8, :], hT)
                    ph_inproj(S, nc, g, l, hT, ptm, pfm)
                ph_sb(S, nc, g, l, ptm, pfm, ycat[s])
                ph_ml(S, nc, g, l, ptm, pfm, ycat[s])
                ph_dsa(S, nc, g, l, ptm, ycat[s])
                ph_rwkv_prep(S, nc, g, l, s, ptm, pfm, sops, sv, sbg)
            ph_rwkv_scan(S, nc, g, sops, sv, sy)
            for s in range(NSEQ):
                ph_rwkv_post(S, nc, g, l, s, sy, sbg, ycat[s])
                ph_outproj(S, nc, g, l, s, ycat[s], xin, xmid, s * T)
            for s in range(NSEQ):
                ph_moe(S, nc, g, l, s, xmid, xout, s * T)
        S.flush()
        g.n_inst = S.n_inst
    return nc, g


_CACHE = {}


def kernel(**inputs):
    inp = {k: np.asarray(v) for k, v in inputs.items()}
    sh = host_shared(inp)
    n_layers = inp["w_in"].shape[0]
    if "nc" not in _CACHE:
        _CACHE["nc"] = build_full({k: v.shape for k, v in sh.items()}, n_layers)
    nc, g = _CACHE["nc"]
    in_maps = []
    for core in range(8):
        d = dict(sh)
        d.update(host_core(inp, core))
        in_maps.append(d)
    res = run_bass_kernel_spmd(nc, in_maps, core_ids=list(range(8)))
    out = np.empty((32, T, D), np.float32)
    for core in range(8):
        oT = np.asarray(res.results[core]["outT"])
        out[core * NSEQ:(core + 1) * NSEQ] = oT.T.reshape(NSEQ, T, D)
    return out
```

```python
import numpy as np
import concourse.bass as bass
import concourse.mybir as mybir
from concourse.bass_utils import run_bass_kernel_spmd

F32 = mybir.dt.float32
BF16 = mybir.dt.bfloat16
AF = mybir.ActivationFunctionType
ALU = mybir.AluOpType
AX = mybir.AxisListType

COMPUTE = ("pe", "act", "dve", "pool")
SYNC_WAR = True


class Sched:
    def __init__(self, nc, n_dma_sems=32):
        self.nc = nc
        self.n_dma = n_dma_sems
        self.sem = {}
        self.stack = None
        self.ops = []
        self.last_w = {}
        self.readers = {}
        self.count = {e: 0 for e in COMPUTE}
        self.dma_cnt = [0] * n_dma_sems
        self.dma_rr = 0
        self.known = {e: {} for e in COMPUTE + ("sp",)}
        self.phase_start = 0
        self.n_inst = 0

    def open(self, stack):
        nc = self.nc
        self.stack = stack
        self.n_sem_alloc = 0
        for e in COMPUTE:
            self.sem[e] = stack.enter_context(nc.semaphore("s_" + e))
        for i in range(self.n_dma):
            self.sem[("d", i)] = stack.enter_context(nc.semaphore("s_d%d" % i))

    @staticmethod
    def key(ap):
        return ap.name

    def add(self, eng, fn, reads, writes, dma=False):
        idx = len(self.ops)
        deps = set()
        wdeps = set()
        for k in reads:
            if k in self.last_w:
                deps.add(self.last_w[k])
        for k in writes:
            if k in self.last_w:
                deps.add(self.last_w[k])
            rd = self.readers.get(k)
            if rd:
                wdeps.update(rd.values())
        for k in writes:
            self.last_w[k] = idx
            self.readers[k] = {}
        for k in reads:
            self.readers.setdefault(k, {})[("dma", idx) if dma else eng] = idx
        deps.discard(idx)
        wdeps.discard(idx)
        wdeps -= deps
        self.ops.append(dict(eng=eng, fn=fn, deps=deps, wdeps=wdeps, dma=dma, sig=False, waits=None))
        return idx

    def flush(self, barrier=True):
        ops = self.ops
        lo = self.phase_start
        n = len(ops)
        def skip(o, od, d):
            if od["dma"] or o["dma"] or od["eng"] != o["eng"]:
                return False
            if o["eng"] == "pe":
                return True
            return (not SYNC_WAR) and (d not in o["deps"])

        for i in range(lo, n):
            o = ops[i]
            for d in (o["deps"] | o["wdeps"]):
                od = ops[d]
                if d < lo or od["dma"] or skip(o, od, d):
                    continue
                od["sig"] = True
        last_of = {}
        for i in range(lo, n):
            last_of[ops[i]["eng"]] = i
        if barrier:
            for e, i in last_of.items():
                if e in COMPUTE:
                    ops[i]["sig"] = True
        for i in range(lo, n):
            o = ops[i]
            e = o["eng"]
            kn = self.known[e]
            waits = []
            for d in sorted(o["deps"] | o["wdeps"]):
                od = ops[d]
                if d < lo or skip(o, od, d):
                    continue
                s, v = od["done"]
                if kn.get(s, 0) < v:
                    waits.append((s, v))
                    for s2, v2 in od["clock"].items():
                        if kn.get(s2, 0) < v2:
                            kn[s2] = v2
            if o["dma"]:
                j = self.dma_rr
                self.dma_rr = (j + 1) % self.n_dma
                s = ("d", j)
                if kn.get(s, 0) < self.dma_cnt[j]:
                    waits.append((s, self.dma_cnt[j]))
                    kn[s] = self.dma_cnt[j]
                self.dma_cnt[j] += 16
                o["done"] = (s, self.dma_cnt[j])
                o["inc"] = (s, 16)
                clock = dict(kn)
                clock[s] = self.dma_cnt[j]
                o["clock"] = clock
            else:
                if o["sig"]:
                    self.count[e] += 1
                    o["done"] = (e, self.count[e])
                    o["inc"] = (e, 1)
                    clock = dict(kn)
                    clock[e] = self.count[e]
                    o["clock"] = clock
                else:
                    o["inc"] = None
            w = {}
            for s, v in waits:
                w[s] = max(w.get(s, 0), v)
            o["waits"] = list(w.items())
        final = {}
        if barrier:
            for e in COMPUTE:
                final[e] = self.count[e]
            for j in range(self.n_dma):
                final[("d", j)] = self.dma_cnt[j]
        nc = self.nc
        sem = self.sem
        by_eng = {}
        for i in range(lo, n):
            by_eng.setdefault(ops[i]["eng"], []).append(ops[i])

        def emit(ename):
            def body(e):
                for o in by_eng.get(ename, []):
                    for s, v in o["waits"]:
                        e.wait_ge(sem[s], v)
                        self.n_inst += 1
                    ins = o["fn"](e)
                    self.n_inst += 1
                    if o["inc"] is not None:
                        ins.then_inc(sem[o["inc"][0]], o["inc"][1])
                kn = self.known[ename]
                for s, v in final.items():
                    if kn.get(s, 0) < v:
                        e.wait_ge(sem[s], v)
                        kn[s] = v
            return body

        with nc.Block() as block:
            block.tensor(emit("pe"))
            block.scalar(emit("act"))
            block.vector(emit("dve"))
            block.gpsimd(emit("pool"))
            block.sync(emit("sp"))
        for i in range(lo, n):
            ops[i]["fn"] = None
            ops[i]["clock"] = None if i < n else None
        self.phase_start = n
        if barrier:
            self.last_w = {}
            self.readers = {}
            for e in COMPUTE:
                if self.count[e] > 20000:
                    self.n_sem_alloc += 1
                    self.sem[e] = self.stack.enter_context(nc.semaphore("s_%s_%d" % (e, self.n_sem_alloc)))
                    self.count[e] = 0
                    for kn in self.known.values():
                        kn.pop(e, None)
            for j in range(self.n_dma):
                if self.dma_cnt[j] > 20000:
                    self.n_sem_alloc += 1
                    self.sem[("d", j)] = self.stack.enter_context(nc.semaphore("s_d%d_%d" % (j, self.n_sem_alloc)))
                    self.dma_cnt[j] = 0
                    for kn in self.known.values():
                        kn.pop(("d", j), None)

    def _rw(self, outs, ins, rk, wk):
        r = list(rk) if rk is not None else [self.key(a) for a in ins if hasattr(a, "name") and a.space != "DRAM"]
        w = list(wk) if wk is not None else [self.key(a) for a in outs if a.space != "DRAM"]
        return r, w

    def mm(self, out, lhsT, rhs, start=True, stop=True, rk=None, wk=None):
        r, w = self._rw([out], [lhsT, rhs], rk, wk)
        return self.add("pe", lambda e: e.matmul(out, lhsT=lhsT, rhs=rhs, start=start, stop=stop), r, w)

    def transpose(self, out, in_, ident, rk=None, wk=None):
        r, w = self._rw([out], [in_, ident], rk, wk)
        return self.add("pe", lambda e: e.transpose(out, in_, ident), r, w)

    def act(self, out, in_, func, bias=None, scale=1.0, accum_out=None, rk=None, wk=None):
        ins = [in_] + ([bias] if hasattr(bias, "name") else []) + ([scale] if hasattr(scale, "name") else [])
        outs = [out] + ([accum_out] if accum_out is not None else [])
        r, w = self._rw(outs, ins, rk, wk)
        kw = {}
        if bias is not None:
            kw["bias"] = bias
        if accum_out is not None:
            kw["accum_out"] = accum_out
        return self.add("act", lambda e: e.activation(out, in_, func, scale=scale, **kw), r, w)

    def tt(self, out, in0, in1, op, eng="dve", rk=None, wk=None):
        r, w = self._rw([out], [in0, in1], rk, wk)
        return self.add(eng, lambda e: e.tensor_tensor(out, in0, in1, op), r, w)

    def ts(self, out, in0, s1, s2=None, op0=ALU.mult, op1=None, eng="dve", accum_out=None, rk=None, wk=None):
        ins = [in0] + [s for s in (s1, s2) if hasattr(s, "name")]
        outs = [out] + ([accum_out] if accum_out is not None else [])
        r, w = self._rw(outs, ins, rk, wk)
        kw = {}
        if op1 is not None:
            kw["op1"] = op1
        if accum_out is not None:
            kw["accum_out"] = accum_out
        return self.add(eng, lambda e: e.tensor_scalar(out, in0, s1, s2, op0, **kw), r, w)

    def stt(self, out, in0, scalar, in1, op0, op1, eng="dve", rk=None, wk=None):
        ins = [in0, in1] + ([scalar] if hasattr(scalar, "name") else [])
        r, w = self._rw([out], ins, rk, wk)
        return self.add(eng, lambda e: e.scalar_tensor_tensor(out, in0, scalar, in1, op0, op1), r, w)

    def copy(self, out, in_, eng="dve", rk=None, wk=None):
        r, w = self._rw([out], [in_], rk, wk)
        if eng == "act":
            return self.add("act", lambda e: e.activation(out, in_, AF.Copy), r, w)
        return self.add(eng, lambda e: e.tensor_copy(out, in_), r, w)

    def memset(self, out, val, eng="dve", wk=None):
        r, w = self._rw([out], [], None, wk)
        return self.add(eng, lambda e: e.memset(out, val), r, w)

    def reduce(self, out, in_, op, axis=AX.X, eng="dve", rk=None, wk=None):
        r, w = self._rw([out], [in_], rk, wk)
        return self.add(eng, lambda e: e.tensor_reduce(out, in_, axis, op), r, w)

    def recip(self, out, in_, rk=None, wk=None):
        r, w = self._rw([out], [in_], rk, wk)
        return self.add("dve", lambda e: e.reciprocal(out, in_), r, w)

    def scan(self, out, d0, d1, init, op0, op1, eng="dve", rk=None, wk=None):
        r, w = self._rw([out], [d0, d1], rk, wk)
        return self.add(eng, lambda e: e.tensor_tensor_scan(out, d0, d1, init, op0, op1), r, w)

    def max8(self, out, in_, rk=None, wk=None):
        r, w = self._rw([out], [in_], rk, wk)
        return self.add("dve", lambda e: e.max(out, in_), r, w)

    def match_replace(self, out, rep, vals, imm, rk=None, wk=None):
        r, w = self._rw([out], [rep, vals], rk, wk)
        return self.add("dve", lambda e: e.match_replace(out, rep, vals, imm), r, w)

    def dma(self, out, in_, rk=None, wk=None, **kw):
        r, w = self._rw([out], [in_], rk, wk)
        return self.add("sp", lambda e: e.dma_start(out, in_, **kw), r, w, dma=True)


from contextlib import ExitStack

P = 128
T = 2048
D = 1024
NSEQ = 4
NTM = 2084
NFM = 1416
TM_AR, TM_AK, TM_AV, TM_BV, TM_CV, TM_CO, TM_DQ, TM_DK, TM_DV, TM_DQI, TM_DKI, TM_DWI = (
    0, 256, 512, 768, 1024, 1280, 1536, 1792, 1856, 1920, 2048, 2080)
FM_AV, FM_AWD, FM_AAD, FM_AGD, FM_BQ, FM_BK, FM_CQ, FM_CK, FM_CIG, FM_CFG = (
    0, 256, 288, 320, 384, 640, 896, 1152, 1408, 1412)
NEG = -1.0e30
_uid = [0]


def uname(s):
    _uid[0] += 1
    return "%s_%d" % (s, _uid[0])


def sb(nc, st, name, shape, dt):
    return st.enter_context(nc.sbuf_tensor(uname(name), list(shape), dt))


def bc(ap, shape):
    return ap.to_broadcast(list(shape))


class G:
    pass


def host_consts():
    c = {}
    i = np.arange(128)
    c["ident"] = np.eye(128, dtype=np.float32)
    c["ones"] = np.ones((128, 128), np.float32)
    c["tri_lt"] = (i[:, None] < i[None, :]).astype(np.float32)
    c["tri_le"] = (i[:, None] <= i[None, :]).astype(np.float32)
    c["tri_gt"] = (i[:, None] > i[None, :]).astype(np.float32)
    c["cbias"] = np.where(i[None, :] <= i[:, None], 0.0, NEG).astype(np.float32)
    half = 32
    inv = 10000.0 ** (-np.arange(half, dtype=np.float32) / half)
    ang = np.arange(T, dtype=np.float32)[:, None] * inv[None, :]
    c["rope64"] = np.concatenate([np.cos(ang), np.sin(ang)], 1).astype(np.float32)
    half = 16
    inv = 10000.0 ** (-np.arange(half, dtype=np.float32) / half)
    ang = np.arange(T, dtype=np.float32)[:, None] * inv[None, :]
    c["rope32"] = np.concatenate([np.cos(ang), np.sin(ang)], 1).astype(np.float32)
    c["lebias"] = np.where(i[:, None] <= i[None, :], 0.0, NEG).astype(np.float32)
    selh = np.zeros((4, 4, 128), np.float32)
    for h in range(4):
        selh[h, h, :] = 1.0
    c["selh"] = selh.reshape(4, 512)
    return c


def load_const(S, nc, st, g, name, shape, dt=F32):
    t = sb(nc, st, "c_" + name, shape, F32)
    S.dma(t[:], g.dram[name])
    if dt == F32:
        return t
    tb = sb(nc, st, "cb_" + name, shape, dt)
    S.copy(tb[:], t[:])
    return tb


def ph_ada(S, nc, g, l):
    with ExitStack() as st:
        cT = sb(nc, st, "cT", [P, 8, 4], F32)
        S.dma(cT[:], g.dram["cT"].rearrange("(c p) b -> p c b", p=P))
        cact = sb(nc, st, "cact", [P, 8, 4], F32)
        S.act(cact[:], cT[:], AF.Silu)
        bias = sb(nc, st, "adab", [P, 48], F32)
        S.dma(bias[:], g.dram["ada_b_fm"][l])
        g1 = sb(nc, st, "g1", [P, 8], F32)
        g2 = sb(nc, st, "g2", [P, 8], F32)
        S.dma(g1[:], g.dram["norm1_g_fm"][l])
        S.dma(g2[:], g.dram["norm2_g_fm"][l])
        wts = [sb(nc, st, "adaw%d" % i, [P, 8, 768], F32) for i in range(2)]
        ps = g.ps[0]
        for cb in range(8):
            wt = wts[cb % 2]
            S.dma(wt[:], g.dram["ada_w"][l][:, cb * 768:(cb + 1) * 768].rearrange("(c p) n -> p c n", p=P))
            for cc in range(6):
                c = cb * 6 + cc
                for k in range(8):
                    S.mm(ps[:, 4 * c:4 * c + 4], wt[:, k, cc * 128:(cc + 1) * 128], cact[:, k, :],
                         start=(k == 0), stop=(k == 7))
        S.tt(g.modT[:], ps[:, 0:192].rearrange("p (c b) -> p c b", b=4),
             bc(bias[:].unsqueeze(2), [P, 48, 4]), ALU.add)
        for (A, gg, off) in ((g.A1, g1, 8), (g.A2, g2, 32)):
            S.ts(A[:], g.modT[:, off:off + 8, :], 1.0, None, ALU.add)
            S.tt(A[:], A[:], bc(gg[:].unsqueeze(2), [P, 8, 4]), ALU.mult)
        S.flush()


def emit_norm_mod(S, nc, g, xT_d, tok0, b, A, shift, hT, col0=1, route=None):
    with ExitStack() as st:
        xs = [sb(nc, st, "xs%d" % i, [P, 8, 512], F32) for i in range(2)]
        sq = sb(nc, st, "sq", [P, 8, 512], F32)
        tmp = sb(nc, st, "tmp", [P, 8, 512], F32)
        rstd = sb(nc, st, "rstd", [P, 512], F32)
        ps = g.ps[1]
        for sblk in range(4):
            x = xs[sblk % 2]
            S.dma(x[:], xT_d[:, tok0 + sblk * 512: tok0 + (sblk + 1) * 512].rearrange("(c p) n -> p c n", p=P))
            S.act(sq[:], x[:], AF.Square)
            for c in range(8):
                S.mm(ps[:, :], g.ones[:], sq[:, c, :], start=(c == 0), stop=(c == 7))
            S.act(rstd[:], ps[:, :], AF.Sqrt, scale=1.0 / D, bias=g.eps6[:, 0:1])
            S.recip(rstd[:], rstd[:])
            S.tt(tmp[:], x[:], bc(rstd[:].unsqueeze(1), [P, 8, 512]), ALU.mult)
            if route is None:
                for c in range(8):
                    S.act(hT[:, c, col0 + sblk * 512: col0 + (sblk + 1) * 512], tmp[:, c, :], AF.Identity,
                          scale=A[:, c, b:b + 1], bias=shift[:, c, b:b + 1])
            else:
                wge, lg = route
                for c in range(8):
                    S.act(sq[:, c, :], tmp[:, c, :], AF.Identity, scale=A[:, c, b:b + 1], bias=shift[:, c, b:b + 1])
                S.copy(hT[:, :, col0 + sblk * 512: col0 + (sblk + 1) * 512], sq[:], eng="pool")
                for tb4 in range(4):
                    pr = g.ps[2 + (tb4 % 2)]
                    for c in range(8):
                        S.mm(pr[:, 0:36], sq[:, c, tb4 * 128:(tb4 + 1) * 128], wge[:, c, :], start=(c == 0), stop=(c == 7))
                    S.copy(lg[:, sblk * 4 + tb4, :], pr[:, 0:36])
        S.flush()


def load_weights_bf16(S, nc, st, g, w_d, ncols, nshift, mu_d, Wb, W0b):
    stg = [sb(nc, st, "wstg%d" % i, [P, 8, 512], F32) for i in range(2)]
    mu_b = sb(nc, st, "mu_b", [P, max(nshift, 1)], F32)
    tmp = sb(nc, st, "wtmp", [P, 8, 512], F32)
    if nshift:
        S.dma(mu_b[:], mu_d.partition_broadcast(P))
    i = 0
    for c0 in range(0, ncols, 512):
        w = min(512, ncols - c0)
        s_ = stg[i % 2]
        i += 1
        S.dma(s_[:, :, :w], w_d[:, c0:c0 + w].rearrange("(c p) n -> p c n", p=P))
        if c0 < nshift:
            ws = min(w, nshift - c0)
            S.tt(tmp[:, :, :ws], s_[:, :, :ws], bc(mu_b[:, c0:c0 + ws].unsqueeze(1), [P, 8, ws]), ALU.mult, eng="pool")
            S.copy(W0b[:, :, c0:c0 + ws], tmp[:, :, :ws], eng="act")
            S.tt(Wb[:, :, c0:c0 + ws], s_[:, :, :ws], tmp[:, :, :ws], ALU.subtract)
            if ws < w:
                S.copy(Wb[:, :, c0 + ws:c0 + w], s_[:, :, ws:w], eng="act")
        else:
            S.copy(Wb[:, :, c0:c0 + w], s_[:, :, :w], eng=("act" if (i % 2) else "dve"))


def ph_inproj(S, nc, g, l, hT, ptm_d, pfm_d):
    with ExitStack() as st:
        Wb = sb(nc, st, "Wb", [P, 8, NTM], BF16)
        W0b = sb(nc, st, "W0b", [P, 8, 768], BF16)
        load_weights_bf16(S, nc, st, g, g.dram["w_tm"][l], NTM, 768, g.dram["mu_tm"][l], Wb, W0b)
        stg = [sb(nc, st, "ptm_stg%d" % i, [P, NTM], F32) for i in range(2)]
        ev = 0
        for tb in range(16):
            so = stg[tb % 2]
            for c0 in range(0, NTM, 512):
                w = min(512, NTM - c0)
                ps = g.ps[2 + (ev % 4)]
                shifted = c0 < 768
                for k in range(8):
                    S.mm(ps[:, :w], hT[:, k, 1 + tb * 128: 1 + (tb + 1) * 128], Wb[:, k, c0:c0 + w],
                         start=(k == 0), stop=(k == 7 and not shifted))
                if shifted:
                    ws = min(w, 768 - c0)
                    for k in range(8):
                        S.mm(ps[:, :ws], hT[:, k, tb * 128:(tb + 1) * 128], W0b[:, k, c0:c0 + ws],
                             start=False, stop=(k == 7))
                S.copy(so[:, c0:c0 + w], ps[:, :w], eng=("act" if ev % 2 else "dve"))
                ev += 1
            S.dma(ptm_d[tb * 128:(tb + 1) * 128, :], so[:])
        S.flush()
    with ExitStack() as st:
        Wb = sb(nc, st, "Wf", [P, 8, NFM], BF16)
        W0b = sb(nc, st, "W0f", [P, 8, 384], BF16)
        load_weights_bf16(S, nc, st, g, g.dram["w_fm"][l], NFM, 384, g.dram["mu_fm"][l], Wb, W0b)
        stg = [sb(nc, st, "pfm_stg%d" % i, [P, T], F32) for i in range(2)]
        ev = 0
        ci = 0
        for r0 in range(0, NFM, 128):
            m = min(128, NFM - r0)
            so = stg[ci % 2]
            ci += 1
            shifted = r0 < 384
            for sblk in range(4):
                ps = g.ps[2 + (ev % 4)]
                for k in range(8):
                    S.mm(ps[:m, :], Wb[:, k, r0:r0 + m], hT[:, k, 1 + sblk * 512: 1 + (sblk + 1) * 512],
                         start=(k == 0), stop=(k == 7 and not shifted))
                if shifted:
                    for k in range(8):
                        S.mm(ps[:m, :], W0b[:, k, r0:r0 + m], hT[:, k, sblk * 512:(sblk + 1) * 512],
                             start=False, stop=(k == 7))
                S.copy(so[:m, sblk * 512:(sblk + 1) * 512], ps[:m, :], eng=("act" if ev % 2 else "dve"))
                ev += 1
            S.dma(pfm_d[r0:r0 + m, :], so[:m, :])
        S.flush()


def _r(a, b):
    return list(range(a, b))


TM_COLS = (_r(0, 256) + _r(256, 512) + _r(512, 768) + _r(1408, 1664) + _r(2176, 2432) + _r(2432, 2688)
           + _r(2696, 2952) + _r(2952, 3016) + _r(3016, 3080) + _r(3080, 3208) + _r(3208, 3240) + _r(3240, 3244))
FM_COLS = (_r(512, 768) + _r(768, 800) + _r(800, 832) + _r(832, 896) + _r(896, 1152) + _r(1152, 1408)
           + _r(1664, 1920) + _r(1920, 2176) + _r(2688, 2692) + _r(2692, 2696))
assert len(TM_COLS) == NTM and len(FM_COLS) == NFM


def host_shared(inp):
    f = lambda a: np.ascontiguousarray(a, dtype=np.float32)
    L = inp["w_in"].shape[0]
    sh = dict(host_consts())
    sh["ada_w"] = f(inp["ada_w"])
    sh["ada_b_fm"] = f(inp["ada_b"].reshape(L, 48, 128).transpose(0, 2, 1))
    sh["norm1_g_fm"] = f(inp["norm1_g"].reshape(L, 8, 128).transpose(0, 2, 1))
    sh["norm2_g_fm"] = f(inp["norm2_g"].reshape(L, 8, 128).transpose(0, 2, 1))
    sh["w_tm"] = f(inp["w_in"][:, :, TM_COLS])
    sh["w_fm"] = f(inp["w_in"][:, :, FM_COLS])
    sh["mu_tm"] = f(inp["rk_mu"][:, TM_COLS[:768]])
    sh["mu_fm"] = f(inp["rk_mu"][:, FM_COLS[:384]])
    sh["conv_w_fm"] = f(inp["ml_conv_w"].reshape(L, 4, 4, 128).transpose(0, 3, 2, 1))
    sh["conv_b_fm"] = f(inp["ml_conv_b"].reshape(L, 4, 128).transpose(0, 2, 1))
    sh["moe_wge"] = f(np.concatenate([inp["moe_wg"], inp["moe_we"]], -1))
    sh["moe_bge"] = f(np.concatenate([inp["moe_bg"], inp["moe_be"]], -1))
    for k in ("moe_w1", "moe_w3", "moe_w2"):
        sh[k] = f(inp[k])
    for k in ("rk_w0", "rk_w2", "rk_a0", "rk_a2", "rk_g2", "rk_kk", "rk_ka", "rk_rk", "rk_ln_g", "rk_ln_b",
              "sb_norm_g", "ml_norm_g", "ds_qn_g", "ds_kn_g", "ds_out_g", "w_out", "ml_ig_b", "ml_fg_b"):
        sh[k] = f(inp[k])
    return sh


def host_core(inp, core, nseq=NSEQ):
    f = lambda a: np.ascontiguousarray(a, dtype=np.float32)
    x = inp["x"][core * nseq:(core + 1) * nseq]
    d = {}
    d["xT"] = f(x.reshape(nseq * T, D).T)
    cT = np.zeros((D, 4), np.float32)
    cT[:, :nseq] = inp["c"][core * nseq:(core + 1) * nseq].T
    d["cT"] = cT
    return d


def load_row_bcast(S, nc, st, name, row_ap, n):
    t = sb(nc, st, name, [P, n], F32)
    S.dma(t[:], row_ap.partition_broadcast(P))
    return t


def tm_head_rmsnorm(S, nc, st, g, y, nb, gain, eps, per_head_gain=True):
    nh = nb * 4
    yv = y[:].rearrange("p b (h d) -> p (b h) d", d=64)
    sq = sb(nc, st, "rn_sq", [P, nh, 64], F32)
    ss = sb(nc, st, "rn_ss", [P, nh], F32)
    S.tt(sq[:], yv, yv, ALU.mult)
    S.reduce(ss[:], sq[:], ALU.add, AX.X)
    S.act(ss[:], ss[:], AF.Sqrt, scale=1.0 / 64, bias=eps[:, 0:1])
    S.recip(ss[:], ss[:])
    S.tt(yv, yv, bc(ss[:].unsqueeze(2), [P, nh, 64]), ALU.mult)
    if per_head_gain:
        S.tt(y[:], y[:], bc(gain[:].unsqueeze(1), [P, nb, 256]), ALU.mult)
    else:
        S.tt(yv, yv, bc(gain[:, 0:64].unsqueeze(1), [P, nh, 64]), ALU.mult)


def ph_sb(S, nc, g, l, ptm_d, pfm_d, ycat_d):
    with ExitStack() as st:
        q16 = sb(nc, st, "sbq", [P, 2, T], BF16)
        k16 = sb(nc, st, "sbk", [P, 2, T], BF16)
        v16 = sb(nc, st, "sbv", [P, 16, 256], BF16)
        yraw = sb(nc, st, "sby", [P, 16, 256], F32)
        gain = load_row_bcast(S, nc, st, "sbg", g.dram["sb_norm_g"][l], 256)
        with ExitStack() as st2:
            qf = sb(nc, st2, "sbqf", [P, 2, T], F32)
            kf = sb(nc, st2, "sbkf", [P, 2, T], F32)
            vf = sb(nc, st2, "sbvf", [P, 16, 256], F32)
            S.dma(qf[:], pfm_d[FM_BQ:FM_BQ + 256, :].rearrange("(c p) t -> p c t", p=P))
            S.dma(kf[:], pfm_d[FM_BK:FM_BK + 256, :].rearrange("(c p) t -> p c t", p=P))
            S.dma(vf[:], ptm_d[:, TM_BV:TM_BV + 256].rearrange("(b p) n -> p b n", p=P))
            S.copy(q16[:], qf[:], eng="act")
            S.copy(k16[:], kf[:], eng="dve")
            S.copy(v16[:], vf[:], eng="pool")
            S.flush()
        e1 = [sb(nc, st, "sbe%d" % i, [P, 512], F32) for i in range(2)]
        lt = [sb(nc, st, "sbl%d" % i, [P, 512], F32) for i in range(2)]
        Lm = [sb(nc, st, "sbL%d" % i, [P, 512], F32) for i in range(2)]
        aa = [sb(nc, st, "sba%d" % i, [P, 512], F32) for i in range(2)]
        attA = [sb(nc, st, "sbt%d" % i, [P, 16, 512], BF16) for i in range(2)]
        TotB = sb(nc, st, "sbT", [P, 512], F32)
        iters = [(h, I, j) for h in range(4) for I in range(4) for j in range(4 * I + 3, -1, -1)]

        def geom(n):
            h, I, j = iters[n]
            d = j - 4 * I
            dd = max(d, 0)
            c0 = dd * 128
            return h, I, j, d, c0, 512 - c0, I * 512 + c0

        def s1(n):
            h, I, j, d, c0, nn, q0 = geom(n)
            u = n % 2
            c, pb = h // 2, (h % 2) * 64
            pz = g.ps[u]
            S.mm(pz[:, :nn], k16[pb:pb + 64, c, j * 128:(j + 1) * 128], q16[pb:pb + 64, c, q0:q0 + nn])
            S.act(e1[u][:, :nn], pz[:, :nn], AF.Exp, scale=-0.125)
            S.act(lt[u][:, :nn], e1[u][:, :nn], AF.Ln, bias=g.one1[:, 0:1])
            S.stt(Lm[u][:, :nn], pz[:, :nn], -0.125, lt[u][:, :nn], ALU.mult, ALU.subtract)
            if d >= 0:
                S.tt(Lm[u][:, 0:128], Lm[u][:, 0:128], g.tri_lt[:], ALU.mult)

        def s2(n):
            h, I, j, d, c0, nn, q0 = geom(n)
            u = n % 2
            pr, pt = g.ps[2 + u], g.ps[4 + u]
            att_all = attA[(h * 4 + I) % 2]
            if j == 4 * I + 3:
                S.memset(TotB[:], 0.0, eng="pool")
            S.mm(pr[:, :nn], g.tri_gt[:], Lm[u][:, :nn])
            S.tt(aa[u][:, :nn], pr[:, :nn], TotB[:, c0:512], ALU.add)
            S.tt(aa[u][:, :nn], aa[u][:, :nn], lt[u][:, :nn], ALU.subtract, eng="pool")
            S.act(att_all[:, j, c0:512], aa[u][:, :nn], AF.Exp)
            if d >= 0:
                S.tt(att_all[:, j, c0:c0 + 128], att_all[:, j, c0:c0 + 128], g.tri_lt16[:], ALU.mult, eng="pool")
            if j > 0:
                S.mm(pt[:, :nn], g.ones[:], Lm[u][:, :nn])
                S.tt(TotB[:, c0:512], TotB[:, c0:512], pt[:, :nn], ALU.add)
            else:
                po = g.ps[6 + (I % 2)]
                for qb in range(4):
                    for jj in range(4 * I + qb, -1, -1):
                        S.mm(po[:, qb * 64:(qb + 1) * 64], att_all[:, jj, qb * 128:(qb + 1) * 128],
                             v16[:, jj, h * 64:(h + 1) * 64], start=(jj == 4 * I + qb), stop=(jj == 0))
                S.copy(yraw[:, 4 * I:4 * I + 4, h * 64:(h + 1) * 64],
                       po[:, 0:256].rearrange("p (b d) -> p b d", d=64), eng="act")

        s1(0)
        for n in range(len(iters)):
            if n + 1 < len(iters):
                s1(n + 1)
            s2(n)
        if getattr(g, "debug", False):
            S.dma(ycat_d[:, 0:256].rearrange("(b p) n -> p b n", p=P), yraw[:])
        tm_head_rmsnorm(S, nc, st, g, yraw, 16, gain, g.eps6)
        S.dma(ycat_d[:, 256:512].rearrange("(b p) n -> p b n", p=P), yraw[:])
        S.flush()


def setup_consts(S, nc, st, g):
    def ld(name, shape, src=None):
        t = sb(nc, st, "k_" + name, shape, F32)
        S.dma(t[:], g.dram[src or name])
        return t
    g.ones = ld("ones", [P, P])
    g.ident = ld("ident", [P, P])
    g.tri_lt = ld("tri_lt", [P, P])
    g.tri_le = ld("tri_le", [P, P])
    g.tri_gt = ld("tri_gt", [P, P])
    g.cbias = ld("cbias", [P, P])
    g.lebias = ld("lebias", [P, P])
    g.selh = sb(nc, st, "k_selh", [4, 4, P], F32)
    S.dma(g.selh[:], g.dram["selh"].rearrange("k (h m) -> k h m", m=P))
    g.tri_lt16 = sb(nc, st, "k_tri_lt16", [P, P], BF16)
    g.tri_le16 = sb(nc, st, "k_tri_le16", [P, P], BF16)
    g.ident16 = sb(nc, st, "k_ident16", [P, P], BF16)
    g.ones16 = sb(nc, st, "k_ones16", [P, P], BF16)
    S.copy(g.tri_lt16[:], g.tri_lt[:])
    S.copy(g.tri_le16[:], g.tri_le[:])
    S.copy(g.ident16[:], g.ident[:])
    S.copy(g.ones16[:], g.ones[:])
    g.eps6 = sb(nc, st, "k_eps6", [P, 1], F32)
    S.memset(g.eps6[:], 1e-6)
    g.one1 = sb(nc, st, "k_one1", [P, 1], F32)
    S.memset(g.one1[:], 1.0)
    g.modT = sb(nc, st, "modT", [P, 48, 4], F32)
    g.A1 = sb(nc, st, "A1", [P, 8, 4], F32)
    g.A2 = sb(nc, st, "A2", [P, 8, 4], F32)
    S.flush()


def ph_ml(S, nc, g, l, ptm_d, pfm_d, ycat_d):
    LN8 = float(np.log(0.125))
    with ExitStack() as st:
        q16 = sb(nc, st, "mlq", [P, 2, T], BF16)
        k16 = sb(nc, st, "mlk", [P, 2, T], BF16)
        v16 = sb(nc, st, "mlv", [P, 16, 4, 65], BF16)
        osig = sb(nc, st, "mlo", [P, 16, 256], F32)
        BtB = [sb(nc, st, "mlB%d" % h, [P, T], F32) for h in range(4)]
        c_tm = sb(nc, st, "mlc", [P, 16, 4], F32)
        gain = load_row_bcast(S, nc, st, "mlg", g.dram["ml_norm_g"][l], 256)
        with ExitStack() as st2:
            xq = sb(nc, st2, "mlxq", [P, 2, T + 3], F32)
            xk = sb(nc, st2, "mlxk", [P, 2, T + 3], F32)
            S.memset(xq[:, :, 0:3], 0.0)
            S.memset(xk[:, :, 0:3], 0.0)
            S.dma(xq[:, :, 3:T + 3], pfm_d[FM_CQ:FM_CQ + 256, :].rearrange("(c p) t -> p c t", p=P))
            S.dma(xk[:, :, 3:T + 3], pfm_d[FM_CK:FM_CK + 256, :].rearrange("(c p) t -> p c t", p=P))
            cw = sb(nc, st2, "mlcw", [P, 4, 4], F32)
            cb = sb(nc, st2, "mlcb", [P, 4], F32)
            S.dma(cw[:], g.dram["conv_w_fm"][l])
            S.dma(cb[:], g.dram["conv_b_fm"][l])
            acc = [sb(nc, st2, "mlacc%d" % i, [P, T], F32) for i in range(2)]
            ai = 0
            for (x, dst, ci0) in ((xq, q16, 0), (xk, k16, 2)):
                for c in range(2):
                    a = acc[ai % 2]
                    eng = "dve"
                    ai += 1
                    S.ts(a[:], x[:, c, 0:T], cw[:, ci0 + c, 0:1], None, ALU.mult, eng=eng)
                    for tap in range(1, 4):
                        S.stt(a[:], x[:, c, tap:T + tap], cw[:, ci0 + c, tap:tap + 1], a[:], ALU.mult, ALU.add, eng=eng)
                    S.act(dst[:, c, :], a[:], AF.Silu, bias=cb[:, ci0 + c:ci0 + c + 1])
            ig = sb(nc, st2, "mlig", [4, T], F32)
            fg = sb(nc, st2, "mlfg", [4, T], F32)
            S.dma(ig[:], pfm_d[FM_CIG:FM_CIG + 4, :])
            S.dma(fg[:], pfm_d[FM_CFG:FM_CFG + 4, :])
            gb = sb(nc, st2, "mlgb", [4, 2], F32)
            S.dma(gb[:, 0:1], g.dram["ml_ig_b"][l].rearrange("(h o) -> h o", o=1))
            S.dma(gb[:, 1:2], g.dram["ml_fg_b"][l].rearrange("(h o) -> h o", o=1))
            S.ts(gb[:], gb[:], 1.0 / 15.0, None, ALU.mult)
            S.act(ig[:], ig[:], AF.Tanh, scale=1.0 / 15.0, bias=gb[:, 0:1])
            S.act(fg[:], fg[:], AF.Tanh, scale=1.0 / 15.0, bias=gb[:, 1:2])
            S.act(fg[:], fg[:], AF.Exp, scale=-15.0)
            S.act(fg[:], fg[:], AF.Ln, bias=g.one1[0:4, 0:1])
            ones4 = sb(nc, st2, "mlones", [4, T], F32)
            S.memset(ones4[:], 1.0)
            Bn = sb(nc, st2, "mlBn", [4, T], F32)
            S.scan(Bn[:], ones4[:], fg[:], 0.0, ALU.mult, ALU.add)
            cT = sb(nc, st2, "mlcT", [4, T], F32)
            S.stt(cT[:], ig[:], 15.0, Bn[:], ALU.mult, ALU.add)
            S.ts(cT[:], cT[:], LN8, None, ALU.add)
            BT = sb(nc, st2, "mlBT", [4, T], F32)
            S.ts(BT[:], Bn[:], -1.0, None, ALU.mult)
            ev = 0
            for h in range(4):
                for sblk in range(4):
                    ps = g.ps[ev % 4]
                    S.mm(ps[:, :], g.selh[0:4, h, :], BT[0:4, sblk * 512:(sblk + 1) * 512])
                    S.copy(BtB[h][:, sblk * 512:(sblk + 1) * 512], ps[:, :], eng=("act" if ev % 2 else "dve"))
                    ev += 1
            pc = g.ps[4]
            for b in range(16):
                S.mm(pc[:, b * 4:(b + 1) * 4], cT[0:4, b * 128:(b + 1) * 128], g.ident[0:4, 0:4])
            S.copy(c_tm[:], pc[:, 0:64].rearrange("p (b h) -> p b h", h=4))
            vf = sb(nc, st2, "mlvf", [P, 16, 256], F32)
            S.dma(vf[:], ptm_d[:, TM_CV:TM_CV + 256].rearrange("(b p) n -> p b n", p=P))
            S.copy(v16[:, :, :, 0:64], vf[:].rearrange("p b (h d) -> p b h d", d=64), eng="pool")
            S.memset(v16[:, :, :, 64:65], 1.0, eng="pool")
            S.dma(osig[:], ptm_d[:, TM_CO:TM_CO + 256].rearrange("(b p) n -> p b n", p=P))
            S.act(osig[:], osig[:], AF.Sigmoid)
            S.flush()
        attA = [sb(nc, st, "mlt%d" % i, [P, 16, 512], BF16) for i in range(2)]
        Dm = [sb(nc, st, "mlD%d" % i, [P, 512], F32) for i in range(2)]
        dtmp = [sb(nc, st, "mldt%d" % i, [P, 128], F32) for i in range(2)]
        nd = [sb(nc, st, "mlnd%d" % i, [P, 4, 65], F32) for i in range(2)]
        dn = [sb(nc, st, "mldn%d" % i, [P, 4], F32) for i in range(2)]
        hraw = sb(nc, st, "mlh", [P, 16, 256], F32)
        it = 0
        for h in range(4):
            c = h // 2
            pb = (h % 2) * 64
            for I in range(4):
                gi = h * 4 + I
                po = g.ps[6 + (gi % 2)]
                att_all = attA[gi % 2]
                for j in range(4 * I + 3, -1, -1):
                    u = it % 2
                    it += 1
                    pz = g.ps[u]
                    d = j - 4 * I
                    dd = max(d, 0)
                    c0 = dd * 128
                    n = 512 - c0
                    q0 = I * 512 + c0
                    S.mm(pz[:, :n], k16[pb:pb + 64, c, j * 128:(j + 1) * 128], q16[pb:pb + 64, c, q0:q0 + n])
                    if d >= 0:
                        S.tt(dtmp[u][:], BtB[h][:, q0:q0 + 128], g.lebias[:], ALU.add, eng="pool")
                        S.act(Dm[u][:, 0:128], dtmp[u][:], AF.Exp, bias=c_tm[:, j, h:h + 1])
                        if n > 128:
                            S.act(Dm[u][:, 128:n], BtB[h][:, q0 + 128:q0 + n], AF.Exp, bias=c_tm[:, j, h:h + 1])
                    else:
                        S.act(Dm[u][:, :n], BtB[h][:, q0:q0 + n], AF.Exp, bias=c_tm[:, j, h:h + 1])
                    S.tt(att_all[:, j, c0:512], pz[:, :n], Dm[u][:, :n], ALU.mult)
                for qb in range(4):
                    for j in range(4 * I + qb, -1, -1):
                        S.mm(po[:, qb * 65:(qb + 1) * 65], att_all[:, j, qb * 128:(qb + 1) * 128],
                             v16[:, j, h, :], start=(j == 4 * I + qb), stop=(j == 0))
                u2 = gi % 2
                S.copy(nd[u2][:], po[:, 0:260].rearrange("p (b d) -> p b d", d=65), eng="act")
                S.stt(dn[u2][:], nd[u2][:, :, 64], -1.0, nd[u2][:, :, 64], ALU.mult, ALU.max)
                S.ts(dn[u2][:], dn[u2][:], 1.0, None, ALU.max)
                S.recip(dn[u2][:], dn[u2][:])
                S.tt(hraw[:, 4 * I:4 * I + 4, h * 64:(h + 1) * 64], nd[u2][:, :, 0:64],
                     bc(dn[u2][:].unsqueeze(2), [P, 4, 64]), ALU.mult)
        tm_head_rmsnorm(S, nc, st, g, hraw, 16, gain, g.eps6)
        S.tt(hraw[:], hraw[:], osig[:], ALU.mult)
        S.dma(ycat_d[:, 512:768].rearrange("(b p) n -> p b n", p=P), hraw[:])
        S.flush()


def _rope_tm(S, out, x, cos, sin, t1, t2, half):
    x1, x2 = x[:, :, 0:half], x[:, :, half:2 * half]
    S.tt(t1, x1, cos, ALU.mult)
    S.tt(t2, x2, sin, ALU.mult)
    S.tt(out[:, :, 0:half], t1, t2, ALU.subtract)
    S.tt(t1, x2, cos, ALU.mult)
    S.tt(t2, x1, sin, ALU.mult)
    S.tt(out[:, :, half:2 * half], t1, t2, ALU.add)


def ph_dsa(S, nc, g, l, ptm_d, ycat_d):
    WI_SCALE = float(4 ** -0.5 * 32 ** -0.5)
    with ExitStack() as st:
        qT = sb(nc, st, "dqT", [P, 2, T], BF16)
        kT2 = sb(nc, st, "dkT", [P, T], BF16)
        qiT = sb(nc, st, "dqi", [P, T], F32)
        kiX = [sb(nc, st, "dki%d" % h, [P, T], F32) for h in range(4)]
        wi = sb(nc, st, "dwi", [P, 16, 4], F32)
        v16 = sb(nc, st, "dv", [P, 16, 65], BF16)
        gain = load_row_bcast(S, nc, st, "dg", g.dram["ds_out_g"][l], 256)
        with ExitStack() as st2:
            rope64 = sb(nc, st2, "rope64", [P, 16, 64], F32)
            rope32 = sb(nc, st2, "rope32", [P, 16, 32], F32)
            S.dma(rope64[:], g.dram["rope64"].rearrange("(b p) n -> p b n", p=P))
            S.dma(rope32[:], g.dram["rope32"].rearrange("(b p) n -> p b n", p=P))
            gq = load_row_bcast(S, nc, st2, "dgq", g.dram["ds_qn_g"][l], 64)
            gk = load_row_bcast(S, nc, st2, "dgk", g.dram["ds_kn_g"][l], 64)
            xs = [sb(nc, st2, "dx%d" % i, [P, 548], F32) for i in range(2)]
            sq = sb(nc, st2, "dsq", [P, 5, 64], F32)
            ss = sb(nc, st2, "dss", [P, 5], F32)
            qn = sb(nc, st2, "dqn", [P, 5, 64], F32)
            qr = [sb(nc, st2, "dqr%d" % i, [P, 6, 64], F32) for i in range(2)]
            qir = [sb(nc, st2, "dqir%d" % i, [P, 5, 32], F32) for i in range(2)]
            t1 = sb(nc, st2, "dt1", [P, 5, 32], F32)
            t2 = sb(nc, st2, "dt2", [P, 5, 32], F32)
            t3 = sb(nc, st2, "dt3", [P, 5, 16], F32)
            t4 = sb(nc, st2, "dt4", [P, 5, 16], F32)
            kiz = [[sb(nc, st2, "dkz%d_%d" % (i, h), [P, 128], F32) for h in range(4)] for i in range(2)]
            for i in range(2):
                for h in range(4):
                    S.memset(kiz[i][h][:], 0.0, eng="pool")
            S.dma(wi[:], ptm_d[:, TM_DWI:TM_DWI + 4].rearrange("(b p) n -> p b n", p=P))
            S.ts(wi[:], wi[:], WI_SCALE, None, ALU.mult)
            ev = 0
            cut = getattr(g, 'dsa_cut', 0)
            for b in range(16):
                x = xs[b % 2]
                S.dma(x[:], ptm_d[b * 128:(b + 1) * 128, TM_DQ:TM_DQ + 548])
                qk = x[:, 0:320].rearrange("p (h d) -> p h d", d=64)
                S.tt(sq[:], qk, qk, ALU.mult)
                S.reduce(ss[:], sq[:], ALU.add, AX.X)
                S.act(ss[:], ss[:], AF.Sqrt, scale=1.0 / 64, bias=g.eps6[:, 0:1])
                S.recip(ss[:], ss[:])
                S.tt(qn[:], qk, bc(ss[:].unsqueeze(2), [P, 5, 64]), ALU.mult)
                S.tt(qn[:, 0:4, :], qn[:, 0:4, :], bc(gq[:].unsqueeze(1), [P, 4, 64]), ALU.mult)
                S.tt(qn[:, 4:5, :], qn[:, 4:5, :], gk[:].unsqueeze(1), ALU.mult)
                r_ = qr[b % 2]
                cos = bc(rope64[:, b, 0:32].unsqueeze(1), [P, 5, 32])
                sin = bc(rope64[:, b, 32:64].unsqueeze(1), [P, 5, 32])
                _rope_tm(S, r_[:, 0:5, :], qn[:], cos, sin, t1[:], t2[:], 32)
                S.copy(r_[:, 5, :], r_[:, 4, :], eng="act")
                ri = qir[b % 2]
                xi = x[:, 384:544].rearrange("p (h d) -> p h d", d=32)
                cos = bc(rope32[:, b, 0:16].unsqueeze(1), [P, 5, 16])
                sin = bc(rope32[:, b, 16:32].unsqueeze(1), [P, 5, 16])
                _rope_tm(S, ri[:], xi, cos, sin, t3[:], t4[:], 16)
                S.copy(v16[:, b, 0:64], x[:, 320:384], eng="act")
                if cut == 1:
                    continue
                tb = slice(b * 128, (b + 1) * 128)
                for c in range(3):
                    ps = g.ps[ev % 4]
                    ev += 1
                    S.transpose(ps[:, 0:128], r_[:, 2 * c:2 * c + 2, :].rearrange("p h d -> p (h d)"), g.ident[:])
                    if c < 2:
                        S.copy(qT[:, c, tb], ps[:, 0:128], eng="act")
                    else:
                        S.copy(kT2[:, tb], ps[:, 0:128], eng="act")
                if cut == 2:
                    continue
                ps = g.ps[ev % 4]
                ev += 1
                S.transpose(ps[:, 0:128], ri[:, 0:4, :].rearrange("p h d -> p (h d)"), g.ident[:])
                S.copy(qiT[:, tb], ps[:, 0:128], eng="dve")
                kz = kiz[b % 2]
                for h in range(4):
                    S.copy(kz[h][:, h * 32:(h + 1) * 32], ri[:, 4, :], eng="pool")
                    ps = g.ps[ev % 4]
                    ev += 1
                    S.transpose(ps[:, 0:128], kz[h][:], g.ident[:])
                    S.copy(kiX[h][:, tb], ps[:, 0:128], eng=("act" if h % 2 else "dve"))
            S.memset(v16[:, :, 64:65], 1.0, eng="pool")
            S.flush()
        if getattr(g, "dsa_stop", 0) == 1:
            return
        sc = [sb(nc, st, "dsc%d" % i, [P, T], F32) for i in range(1)] * 2
        work = sb(nc, st, "dwork", [P, T], F32)
        mk = sb(nc, st, "dmk", [P, T], F32)
        eqm = sb(nc, st, "deq", [P, T], F32)
        cum = sb(nc, st, "dcum", [P, T], F32)
        onesT = sb(nc, st, "dones", [P, T], F32)
        S.memset(onesT[:], 1.0, eng="pool")
        rl = [sb(nc, st, "drl%d" % i, [P, 512], F32) for i in range(2)]
        m8 = sb(nc, st, "dm8", [P, 8], F32)
        ngt = sb(nc, st, "dngt", [P, 1], F32)
        maskT = [sb(nc, st, "dmT%d" % i, [P, 16, 128], F32) for i in range(2)]
        E = [sb(nc, st, "dE%d" % i, [P, 512], F32) for i in range(2)]
        Pall = [sb(nc, st, "dP%d" % i, [P, 16, 512], BF16) for i in range(2)]
        nd = [sb(nc, st, "dnd%d" % i, [P, 4, 65], F32) for i in range(2)]
        dn = [sb(nc, st, "ddn%d" % i, [P, 4], F32) for i in range(2)]
        yraw = sb(nc, st, "dy", [P, 16, 256], F32)
        cnt = {'ev': 0, 'it': 0}

        def part_a(i):
            ev = cnt['ev']
            kl = 128 * (i + 1)
            qb = slice(i * 128, (i + 1) * 128)
            mT = maskT[i % 2]
            if i >= 2:
                s_ = sc[i % 2]
                for kb in range(0, kl, 512):
                    w = min(512, kl - kb)
                    for h in range(4):
                        ps = g.ps[ev % 2]
                        r2 = rl[ev % 2]
                        ev += 1
                        S.mm(ps[:, :w], qiT[:, qb], kiX[h][:, kb:kb + w])
                        S.act(r2[:, :w], ps[:, :w], AF.Relu)
                        if h == 0:
                            S.ts(s_[:, kb:kb + w], r2[:, :w], wi[:, i, 0:1], None, ALU.mult)
                        else:
                            S.stt(s_[:, kb:kb + w], r2[:, :w], wi[:, i, h:h + 1], s_[:, kb:kb + w], ALU.mult, ALU.add)
                S.tt(s_[:, qb], s_[:, qb], g.cbias[:], ALU.add)
                for r in range(32):
                    S.max8(m8[:], (s_ if r == 0 else work)[:, :kl])
                    if r < 31:
                        S.match_replace(work[:, :kl], m8[:], (s_ if r == 0 else work)[:, :kl], NEG)
                S.ts(mk[:, :kl], s_[:, :kl], m8[:, 7:8], None, ALU.is_gt)
                S.reduce(ngt[:], mk[:, :kl], ALU.add, AX.X)
                S.ts(ngt[:], ngt[:], -1.0, 256.0, ALU.mult, ALU.add)
                S.ts(eqm[:, :kl], s_[:, :kl], m8[:, 7:8], None, ALU.is_equal)
                S.scan(cum[:, :kl], onesT[:, :kl], eqm[:, :kl], 0.0, ALU.mult, ALU.add)
                S.ts(cum[:, :kl], cum[:, :kl], ngt[:, 0:1], None, ALU.is_le)
                S.tt(eqm[:, :kl], eqm[:, :kl], cum[:, :kl], ALU.mult)
                S.tt(mk[:, :kl], mk[:, :kl], eqm[:, :kl], ALU.add)
            else:
                for j in range(i):
                    S.copy(mT[:, j, :], g.ones[:], eng="dve")
                S.copy(mT[:, i, :], g.tri_le[:], eng="dve")
            cnt['ev'] = ev

        def part_a3(i):
            if i < 2:
                return
            mT = maskT[i % 2]
            for j0 in range(0, i + 1, 4):
                nj = min(4, i + 1 - j0)
                ps = g.ps[1]
                for jj in range(nj):
                    S.transpose(ps[:, jj * 128:(jj + 1) * 128], mk[:, (j0 + jj) * 128:(j0 + jj + 1) * 128], g.ident[:])
                S.copy(mT[:, j0:j0 + nj, :], ps[:, 0:nj * 128].rearrange("p (j q) -> p j q", q=128), eng="act")

        def part_b(i):
            it = cnt['it']
            qb = slice(i * 128, (i + 1) * 128)
            mT = maskT[i % 2]
            Pa = Pall[i % 2]
            for j in range(i + 1):
                u = it % 2
                it += 1
                pzA, pzB = g.ps[2 + 2 * u], g.ps[3 + 2 * u]
                for h in range(4):
                    pb = (h % 2) * 64
                    pz = pzB if (h % 2) else pzA
                    S.mm(pz[:, (h // 2) * 128:(h // 2 + 1) * 128], kT2[pb:pb + 64, j * 128:(j + 1) * 128], qT[pb:pb + 64, h // 2, qb])
                S.act(E[u][:, 0:256], pzA[:, 0:256], AF.Exp, scale=0.125)
                S.act(E[u][:, 256:512], pzB[:, 0:256], AF.Exp, scale=0.125)
                S.tt(Pa[:, j, :].rearrange("p (h q) -> p h q", q=128), E[u][:].rearrange("p (h q) -> p h q", q=128),
                     bc(mT[:, j, :].unsqueeze(1), [P, 4, 128]), ALU.mult, eng="pool")
            if getattr(g, "dsa_cut2", 0) >= 1:
                return
            po = g.ps[6 + (i % 2)]
            for h in range(4):
                for j in range(i + 1):
                    hr = (h % 2) * 2 + h // 2
                    S.mm(po[:, h * 65:(h + 1) * 65], Pa[:, j, hr * 128:(hr + 1) * 128], v16[:, j, :],
                         start=(j == 0), stop=(j == i))
            u2 = i % 2
            S.copy(nd[u2][:], po[:, 0:260].rearrange("p (h d) -> p h d", d=65), eng="act")
            S.recip(dn[u2][:], nd[u2][:, :, 64])
            S.tt(yraw[:, i, :].rearrange("p (h d) -> p h d", d=64), nd[u2][:, :, 0:64],
                 bc(dn[u2][:].unsqueeze(2), [P, 4, 64]), ALU.mult)
            cnt['it'] = it

        nblk = getattr(g, "dsa_nblk", 16)
        part_a(0)
        part_a3(0)
        for i in range(nblk):
            if i + 1 < nblk:
                part_a(i + 1)
            part_b(i)
            if i + 1 < nblk:
                part_a3(i + 1)
        tm_head_rmsnorm(S, nc, st, g, yraw, 16, gain, g.eps6)
        S.dma(ycat_d[:, 768:1024].rearrange("(b p) n -> p b n", p=P), yraw[:])
        S.flush()


def ph_rwkv_prep(S, nc, g, l, s, ptm_d, pfm_d, sops_d, sv_d, sbg_d):
    x_, sp = s // 2, s % 2
    with ExitStack() as st:
        twT = sb(nc, st, "rtw", [33, T], F32)
        adT = sb(nc, st, "rad", [33, T], F32)
        sgT = sb(nc, st, "rsg", [64, T], F32)
        S.memset(twT[:], 1.0)
        S.memset(adT[:], 1.0, eng="pool")
        S.dma(twT[0:32, :], pfm_d[FM_AWD:FM_AWD + 32, :])
        S.dma(adT[0:32, :], pfm_d[FM_AAD:FM_AAD + 32, :])
        S.dma(sgT[:], pfm_d[FM_AGD:FM_AGD + 64, :])
        S.act(twT[0:32, :], twT[0:32, :], AF.Tanh)
        S.act(sgT[:], sgT[:], AF.Sigmoid)
        w2a = sb(nc, st, "rw2", [33, 256], F32)
        a2a = sb(nc, st, "ra2", [33, 256], F32)
        g2 = sb(nc, st, "rg2", [64, 256], F32)
        S.dma(w2a[0:32, :], g.dram["rk_w2"][l])
        S.dma(w2a[32:33, :], g.dram["rk_w0"][l].rearrange("(o n) -> o n", o=1))
        S.dma(a2a[0:32, :], g.dram["rk_a2"][l])
        S.dma(a2a[32:33, :], g.dram["rk_a0"][l].rearrange("(o n) -> o n", o=1))
        S.dma(g2[:], g.dram["rk_g2"][l])
        kk_b = load_row_bcast(S, nc, st, "rkk", g.dram["rk_kk"][l], 256)
        ka_b = load_row_bcast(S, nc, st, "rka", g.dram["rk_ka"][l], 256)
        rk_b = load_row_bcast(S, nc, st, "rrk", g.dram["rk_rk"][l], 256)
        vfm = sb(nc, st, "rvfm", [P, 2, T], F32)
        S.dma(vfm[:], pfm_d[FM_AV:FM_AV + 256, :].rearrange("(c p) t -> p c t", p=P))
        S.dma(sv_d[s].rearrange("(c p) t -> p c t", p=P), vfm[:])
        xs = [sb(nc, st, "rx%d" % i, [P, 768], F32) for i in range(2)]
        F = lambda n: [sb(nc, st, "%s%d" % (n, i), [P, 256], F32) for i in range(2)]
        sig, dec, a_, kkn, k2, nkka, tmp = F("rsig"), F("rdec"), F("ra"), F("rkkn"), F("rk2"), F("rnk"), F("rtmp")
        bg = [sb(nc, st, "rbg%d" % i, [P, 512], F32) for i in range(2)]
        ss = sb(nc, st, "rss", [P, 4], F32)
        bco = sb(nc, st, "rbc", [P, 4], F32)
        hi = [[sb(nc, st, "rhi%d_%d" % (i, o), [P, 256], BF16) for o in range(5)] for i in range(2)]
        lo = [[sb(nc, st, "rlo%d_%d" % (i, o), [P, 256], BF16) for o in range(5)] for i in range(2)]
        h32 = [sb(nc, st, "rh32_%d" % i, [P, 256], F32) for i in range(2)]
        hv = lambda ap: ap.rearrange("p (h d) -> p h d", d=64)
        for tb in range(16):
            u = tb % 2
            x = xs[u]
            tsl = slice(tb * 128, (tb + 1) * 128)
            S.dma(x[:], ptm_d[tsl, 0:768])
            r, k, v = x[:, 0:256], x[:, 256:512], x[:, 512:768]
            pw, pa, pg = g.ps[0 + u], g.ps[2 + u], g.ps[4 + u]
            S.mm(pw[:, 0:256], twT[0:33, tsl], w2a[0:33, :])
            S.mm(pa[:, 0:256], adT[0:33, tsl], a2a[0:33, :])
            S.mm(pg[:, 0:256], sgT[0:64, tsl], g2[0:64, :])
            S.act(sig[u][:], pw[:, 0:256], AF.Sigmoid)
            S.act(a_[u][:], pa[:, 0:256], AF.Sigmoid)
            S.act(dec[u][:], sig[u][:], AF.Exp, scale=-0.6065306597126334)
            S.copy(bg[u][:, 256:512], pg[:, 0:256], eng="act")
            S.tt(kkn[u][:], k, kk_b[:], ALU.mult)
            S.tt(tmp[u][:], kkn[u][:], kkn[u][:], ALU.mult)
            S.reduce(ss[:], hv(tmp[u][:]), ALU.add, AX.X)
            S.act(ss[:], ss[:], AF.Sqrt)
            S.ts(ss[:], ss[:], 1e-12, None, ALU.max)
            S.recip(ss[:], ss[:])
            S.tt(hv(kkn[u][:]), hv(kkn[u][:]), bc(ss[:].unsqueeze(2), [P, 4, 64]), ALU.mult)
            S.stt(k2[u][:], a_[u][:], -1.0, ka_b[:], ALU.add, ALU.mult)
            S.stt(k2[u][:], k2[u][:], 1.0, k, ALU.add, ALU.mult)
            S.stt(nkka[u][:], kkn[u][:], -1.0, a_[u][:], ALU.mult, ALU.mult)
            S.tt(tmp[u][:], r, k2[u][:], ALU.mult)
            S.tt(tmp[u][:], tmp[u][:], rk_b[:], ALU.mult)
            S.reduce(bco[:], hv(tmp[u][:]), ALU.add, AX.X)
            S.tt(hv(bg[u][:, 0:256]), hv(v), bc(bco[:].unsqueeze(2), [P, 4, 64]), ALU.mult)
            S.dma(sbg_d[s, tsl, :], bg[u][:])
            for o, src in enumerate((kkn[u][:], dec[u][:], nkka[u][:], k2[u][:], r)):
                S.copy(hi[u][o][:], src, eng="act")
                S.copy(h32[o % 2][:], hi[u][o][:], eng="pool")
                S.tt(h32[o % 2][:], src, h32[o % 2][:], ALU.subtract, eng="pool")
                S.copy(lo[u][o][:], h32[o % 2][:], eng="act")
                S.dma(sops_d[o, 0, x_, tsl, sp * 256:(sp + 1) * 256], hi[u][o][:])
                S.dma(sops_d[o, 1, x_, tsl, sp * 256:(sp + 1) * 256], lo[u][o][:])
        S.flush()


SCAN_ACT_Y = False


def ph_rwkv_scan(S, nc, g, sops_d, sv_d, sy_d, nsteps=T):
    CH = 32
    with ExitStack() as st:
        id2 = sb(nc, st, "sid2", [P, 128], BF16)
        S.memset(id2[:], 0.0)
        S.tt(id2[:, 0:32], g.ident16[:, 0:32], g.ident16[:, 32:64], ALU.add)
        S.tt(id2[:, 64:96], g.ident16[:, 64:96], g.ident16[:, 96:128], ALU.add)
        sel = sb(nc, st, "ssel", [P, CH, 128], BF16)
        for xp in range(2):
            for tp in range(CH):
                col = xp * 64 + tp
                S.copy(sel[:, tp, xp * 64:(xp + 1) * 64], bc(id2[:, col:col + 1], [P, 64]),
                       eng=("dve" if tp % 2 else "pool"))
        Stt = [sb(nc, st, "sS%d" % i, [P, 512], F32) for i in range(2)]
        S.memset(Stt[0][:], 0.0)
        S.memset(Stt[1][:], 0.0)
        ytmps = [sb(nc, st, "sytmp%d" % i, [P, 512], F32) for i in range(2)]
        junk = sb(nc, st, "sjunk", [P, 512], F32)
        Sw = sb(nc, st, "sSw", [P, 512], F32)
        tmp = sb(nc, st, "stmp", [P, 512], F32)
        sa = sb(nc, st, "ssa", [P, 8], F32)
        ND = 3
        ringW = [sb(nc, st, "srgW%d" % d, [P, 512], F32) for d in range(ND)]
        ringK = [sb(nc, st, "srgK%d" % d, [P, 512], F32) for d in range(ND)]
        vk = [sb(nc, st, "svk%d" % d, [P, 512], F32) for d in range(ND)]
        opt = [[sb(nc, st, "sop%d_%d" % (b_, o), [P, 512], BF16) for o in range(5)] for b_ in range(3)]
        vS = [sb(nc, st, "svS%d" % i, [P, 8, 256], F32) for i in range(2)]
        yb = [sb(nc, st, "syb%d" % i, [P, 8, 256], F32) for i in range(2)]
        g3 = lambda ap: ap.rearrange("p (g k) -> p g k", k=64)
        step = 0
        pend = None
        nbig = (nsteps + 255) // 256
        for big in range(nbig):
            vs, y_ = vS[big % 2], yb[big % 2]
            bsl = slice(big * 256, (big + 1) * 256)
            for x in range(2):
                for sp in range(2):
                    for h in range(4):
                        S.dma(vs[x * 64:(x + 1) * 64, sp * 4 + h, :], sv_d[2 * x + sp, h * 64:(h + 1) * 64, bsl])
            for cc in range(256 // CH):
                c = big * (256 // CH) + cc
                if c * CH >= nsteps:
                    break
                ob = opt[c % 3]
                for o in range(5):
                    for x in range(2):
                        for hl in range(2):
                            p0 = x * 64 + hl * 32
                            S.dma(ob[o][p0:p0 + CH, :], sops_d[o, hl, x, c * CH:(c + 1) * CH, :])
                for tp in range(CH):
                    ti = cc * CH + tp
                    d = step % ND
                    par = step % 2
                    banks = (g.ps[0 + par], g.ps[6], g.ps[2 + par], g.ps[7], g.ps[4 + par])
                    for o in range(5):
                        S.mm(banks[o][:, :], sel[:, tp, :], ob[o][:])
                    S.copy(ringW[d][:], banks[1][:, :], eng="act")
                    S.copy(ringK[d][:], banks[3][:, :], eng="act")
                    KK, NK, R = banks[0], banks[2], banks[4]
                    W, KB = ringW[d], ringK[d]
                    Sp, Sn = Stt[step % 2], Stt[(step + 1) % 2]
                    S.tt(g3(vk[d][:]), g3(KB[:]), bc(vs[:, :, ti:ti + 1], [P, 8, 64]), ALU.mult, eng="pool")
                    S.tt(Sw[:], Sp[:], W[:], ALU.mult, eng="pool")
                    S.tt(Sw[:], Sw[:], vk[d][:], ALU.add, eng="pool")
                    S.tt(tmp[:], Sp[:], KK[:, :], ALU.mult)
                    if pend is not None:
                        S.tt(pend[0][:], pend[1][:], pend[2][:, :], ALU.mult)
                    S.reduce(sa[:], g3(tmp[:]), ALU.add, AX.X)
                    if pend is not None:
                        S.reduce(pend[3], g3(pend[0][:]), ALU.add, AX.X)
                        pend = None
                    S.tt(g3(tmp[:]), g3(NK[:, :]), bc(sa[:].unsqueeze(2), [P, 8, 64]), ALU.mult)
                    S.tt(Sn[:], Sw[:], tmp[:], ALU.add)
                    pend = (ytmps[step % 2], Sn, R, y_[:, :, ti])
                    step += 1
            if pend is not None:
                S.tt(pend[0][:], pend[1][:], pend[2][:, :], ALU.mult)
                S.reduce(pend[3], g3(pend[0][:]), ALU.add, AX.X)
                pend = None
            for x in range(2):
                for sp in range(2):
                    for h in range(4):
                        S.dma(sy_d[2 * x + sp, h * 64:(h + 1) * 64, bsl], y_[x * 64:(x + 1) * 64, sp * 4 + h, :])
        S.flush()


def ph_rwkv_post(S, nc, g, l, s, sy_d, sbg_d, ycat_d):
    with ExitStack() as st:
        yT = sb(nc, st, "pyT", [P, 2, T], F32)
        S.dma(yT[:], sy_d[s].rearrange("(c p) t -> p c t", p=P))
        bgt = sb(nc, st, "pbg", [P, 16, 512], F32)
        S.dma(bgt[:], sbg_d[s].rearrange("(b p) n -> p b n", p=P))
        lng = load_row_bcast(S, nc, st, "plng", g.dram["rk_ln_g"][l], 256)
        lnb = load_row_bcast(S, nc, st, "plnb", g.dram["rk_ln_b"][l], 256)
        y = sb(nc, st, "py", [P, 16, 256], F32)
        for tb in range(16):
            ps = g.ps[tb % 4]
            for c in range(2):
                S.transpose(ps[:, c * 128:(c + 1) * 128], yT[:, c, tb * 128:(tb + 1) * 128], g.ident[:])
            S.copy(y[:, tb, :], ps[:, 0:256], eng=("act" if tb % 2 else "dve"))
        yv = y[:].rearrange("p b (h d) -> p (b h) d", d=64)
        mean = sb(nc, st, "pmean", [P, 64], F32)
        sq = sb(nc, st, "psq", [P, 64, 64], F32)
        S.reduce(mean[:], yv, ALU.add, AX.X)
        S.ts(mean[:], mean[:], 1.0 / 64, None, ALU.mult)
        S.tt(yv, yv, bc(mean[:].unsqueeze(2), [P, 64, 64]), ALU.subtract)
        S.tt(sq[:], yv, yv, ALU.mult)
        S.reduce(mean[:], sq[:], ALU.add, AX.X)
        eps = sb(nc, st, "peps", [P, 1], F32)
        S.memset(eps[:], 64e-5)
        S.act(mean[:], mean[:], AF.Sqrt, scale=1.0 / 64, bias=eps[:, 0:1])
        S.recip(mean[:], mean[:])
        S.tt(yv, yv, bc(mean[:].unsqueeze(2), [P, 64, 64]), ALU.mult)
        S.tt(y[:], y[:], bc(lng[:].unsqueeze(1), [P, 16, 256]), ALU.mult)
        S.tt(y[:], y[:], bc(lnb[:].unsqueeze(1), [P, 16, 256]), ALU.add)
        S.tt(y[:], y[:], bgt[:, :, 0:256], ALU.add)
        S.tt(y[:], y[:], bgt[:, :, 256:512], ALU.mult)
        S.dma(ycat_d[:, 0:256].rearrange("(b p) n -> p b n", p=P), y[:])
        S.flush()


def ph_outproj(S, nc, g, l, b, ycat_d, xin_d, xout_d, tok0):
    with ExitStack() as st:
        Wb = sb(nc, st, "oW", [P, 8, 1024], BF16)
        ycT = sb(nc, st, "oyT", [P, 8, T], BF16)
        with ExitStack() as st2:
            stg = [sb(nc, st2, "ostg%d" % i, [P, 8, 512], F32) for i in range(2)]
            for hf in range(2):
                S.dma(stg[hf][:], g.dram["w_out"][l][:, hf * 512:(hf + 1) * 512].rearrange("(c p) n -> p c n", p=P))
                S.copy(Wb[:, :, hf * 512:(hf + 1) * 512], stg[hf][:], eng=("act" if hf else "pool"))
            yb = [sb(nc, st2, "oyb%d" % i, [P, 1024], F32) for i in range(2)]
            ev = 0
            for tb in range(16):
                y = yb[tb % 2]
                S.dma(y[:], ycat_d[tb * 128:(tb + 1) * 128, :])
                for c4 in range(2):
                    ps = g.ps[ev % 4]
                    ev += 1
                    for cc in range(4):
                        c = c4 * 4 + cc
                        S.transpose(ps[:, cc * 128:(cc + 1) * 128], y[:, c * 128:(c + 1) * 128], g.ident[:])
                    S.copy(ycT[:, c4 * 4:c4 * 4 + 4, tb * 128:(tb + 1) * 128],
                           ps[:, :].rearrange("p (c t) -> p c t", t=128), eng=("act" if ev % 2 else "dve"))
            S.flush()
        xo = [sb(nc, st, "oxo%d" % i, [P, 512], F32) for i in range(3)]
        ev = 0
        for oc in range(8):
            for sblk in range(4):
                ps = g.ps[4 + (ev % 4)]
                x = xo[ev % 3]
                ev += 1
                tsl = slice(tok0 + sblk * 512, tok0 + (sblk + 1) * 512)
                S.dma(x[:], xin_d[oc * 128:(oc + 1) * 128, tsl])
                for k in range(8):
                    S.mm(ps[:, :], Wb[:, k, oc * 128:(oc + 1) * 128], ycT[:, k, sblk * 512:(sblk + 1) * 512],
                         start=(k == 0), stop=(k == 7))
                S.stt(x[:], ps[:, :], g.modT[:, 16 + oc, b:b + 1], x[:], ALU.mult, ALU.add)
                S.dma(xout_d[oc * 128:(oc + 1) * 128, tsl], x[:])
        S.flush()


def ph_moe(S, nc, g, l, b, xin_d, xout_d, tok0, n_exp=32):
    with ExitStack() as st:
        hT = sb(nc, st, "mhT", [P, 8, T], BF16)
        gate = sb(nc, st, "mgate", [P, 16, 32], F32)
        with ExitStack() as st2:
            wge = sb(nc, st2, "mwge", [P, 8, 36], F32)
            S.dma(wge[:], g.dram["moe_wge"][l].rearrange("(c p) n -> p c n", p=P))
            bge = load_row_bcast(S, nc, st2, "mbge", g.dram["moe_bge"][l], 36)
            lg = sb(nc, st2, "mlg", [P, 16, 36], F32)
            emit_norm_mod(S, nc, g, xin_d, tok0, b, g.A2, g.modT[:, 24:32, :], hT, col0=0, route=(wge, lg))
            S.tt(lg[:], lg[:], bc(bge[:].unsqueeze(1), [P, 16, 36]), ALU.add)
            G4 = lg[:, :, 0:4]
            gmax = sb(nc, st2, "mgmax", [P, 16], F32)
            ge = sb(nc, st2, "mge", [P, 16, 4], F32)
            gsum = sb(nc, st2, "mgsum", [P, 16], F32)
            pen = sb(nc, st2, "mpen", [P, 16, 4], F32)
            S.reduce(gmax[:], G4, ALU.max, AX.X)
            S.tt(ge[:], G4, bc(gmax[:].unsqueeze(2), [P, 16, 4]), ALU.subtract)
            S.ts(pen[:], ge[:], 0.0, NEG, ALU.is_lt, ALU.mult)
            S.act(ge[:], ge[:], AF.Exp)
            S.reduce(gsum[:], ge[:], ALU.add, AX.X)
            S.recip(gsum[:], gsum[:])
            Em = sb(nc, st2, "mEm", [P, 16, 32], F32)
            S.tt(Em[:].rearrange("p b (q e) -> p b q e", e=8), lg[:, :, 4:36].rearrange("p b (q e) -> p b q e", e=8),
                 bc(pen[:].unsqueeze(3), [P, 16, 4, 8]), ALU.add)
            m1 = sb(nc, st2, "mm1", [P, 16], F32)
            m2 = sb(nc, st2, "mm2", [P, 16], F32)
            E2 = sb(nc, st2, "mE2", [P, 16, 32], F32)
            S.reduce(m1[:], Em[:], ALU.max, AX.X)
            S.tt(E2[:], Em[:], bc(m1[:].unsqueeze(2), [P, 16, 32]), ALU.is_ge)
            S.stt(E2[:], E2[:], NEG, Em[:], ALU.mult, ALU.add)
            S.reduce(m2[:], E2[:], ALU.max, AX.X)
            ex = sb(nc, st2, "mex", [P, 16, 32], F32)
            S.tt(ex[:], Em[:], bc(m1[:].unsqueeze(2), [P, 16, 32]), ALU.subtract)
            S.act(ex[:], ex[:], AF.Exp)
            S.tt(E2[:], Em[:], bc(m2[:].unsqueeze(2), [P, 16, 32]), ALU.is_ge)
            S.tt(ex[:], ex[:], E2[:], ALU.mult)
            den = sb(nc, st2, "mden", [P, 16], F32)
            S.tt(den[:], m2[:], m1[:], ALU.subtract)
            S.act(den[:], den[:], AF.Exp)
            S.ts(den[:], den[:], 1.0, None, ALU.add)
            S.recip(den[:], den[:])
            S.tt(den[:], den[:], gsum[:], ALU.mult)
            S.tt(gate[:], ex[:], bc(den[:].unsqueeze(2), [P, 16, 32]), ALU.mult)
            S.flush()
        acc = sb(nc, st, "macc", [P, 16, 1024], F32)
        stg = [sb(nc, st, "mstg%d" % i, [P, 8, 512], F32) for i in range(2)]
        W1b = [sb(nc, st, "mW1_%d" % i, [P, 8, 512], BF16) for i in range(2)]
        W3b = [sb(nc, st, "mW3_%d" % i, [P, 8, 512], BF16) for i in range(2)]
        W2b = [sb(nc, st, "mW2_%d" % i, [P, 4, 1024], BF16) for i in range(2)]
        aT = [sb(nc, st, "maT%d" % i, [P, 4, 512], BF16) for i in range(2)]
        su = [sb(nc, st, "msu%d" % i, [P, 512], F32) for i in range(2)]
        cnt = {"si": 0}

        def load_w(e):
            u = e % 2
            for (dst, src) in ((W1b[u], g.dram["moe_w1"][l, e]), (W3b[u], g.dram["moe_w3"][l, e])):
                sg = stg[cnt["si"] % 2]
                cnt["si"] += 1
                S.dma(sg[:], src.rearrange("(c p) n -> p c n", p=P))
                S.copy(dst[:], sg[:], eng=("act" if cnt["si"] % 2 else "pool"))
            sg = stg[cnt["si"] % 2]
            cnt["si"] += 1
            S.dma(sg[:].rearrange("p c n -> p (c n)").rearrange("p (c n) -> p c n", n=1024),
                  g.dram["moe_w2"][l, e].rearrange("(c p) n -> p c n", p=P))
            S.copy(W2b[u][:].rearrange("p c n -> p (c n)"), sg[:].rearrange("p c n -> p (c n)"), eng="pool")

        its = [(e, sblk) for e in range(n_exp) for sblk in range(4)]

        def st13(n):
            e, sblk = its[n]
            u = e % 2
            if sblk == 0:
                load_w(e)
            a = aT[n % 2]
            for f in range(4):
                pu, pg3 = g.ps[(f % 2) * 2], g.ps[(f % 2) * 2 + 1]
                for k in range(8):
                    S.mm(pu[:, :], W1b[u][:, k, f * 128:(f + 1) * 128], hT[:, k, sblk * 512:(sblk + 1) * 512],
                         start=(k == 0), stop=(k == 7))
                for k in range(8):
                    S.mm(pg3[:, :], W3b[u][:, k, f * 128:(f + 1) * 128], hT[:, k, sblk * 512:(sblk + 1) * 512],
                         start=(k == 0), stop=(k == 7))
                s_ = su[f % 2]
                S.act(s_[:], pu[:, :], AF.Silu)
                S.tt(a[:, f, :], s_[:], pg3[:, :], ALU.mult)

        def st2(n):
            e, sblk = its[n]
            u = e % 2
            a = aT[n % 2]
            for tb4 in range(4):
                tb = sblk * 4 + tb4
                for hf in range(2):
                    py = g.ps[4 + ((tb4 * 2 + hf) % 4)]
                    for f in range(4):
                        S.mm(py[:, :], a[:, f, tb4 * 128:(tb4 + 1) * 128], W2b[u][:, f, hf * 512:(hf + 1) * 512],
                             start=(f == 0), stop=(f == 3))
                    dst = acc[:, tb, hf * 512:(hf + 1) * 512]
                    if e == 0:
                        S.ts(dst, py[:, :], gate[:, tb, e:e + 1], None, ALU.mult)
                    else:
                        S.stt(dst, py[:, :], gate[:, tb, e:e + 1], dst, ALU.mult, ALU.add)

        st13(0)
        for n in range(len(its)):
            if n + 1 < len(its):
                st13(n + 1)
            st2(n)
        xo = [sb(nc, st, "mxo%d" % i, [P, 512], F32) for i in range(2)]
        ev = 0
        for c in range(8):
            for sblk in range(4):
                ps = g.ps[ev % 4]
                x = xo[ev % 2]
                ev += 1
                tsl = slice(tok0 + sblk * 512, tok0 + (sblk + 1) * 512)
                S.dma(x[:], xin_d[c * 128:(c + 1) * 128, tsl])
                for tb4 in range(4):
                    S.transpose(ps[:, tb4 * 128:(tb4 + 1) * 128], acc[:, sblk * 4 + tb4, c * 128:(c + 1) * 128], g.ident[:])
                S.stt(x[:], ps[:, :], g.modT[:, 40 + c, b:b + 1], x[:], ALU.mult, ALU.add)
                S.dma(xout_d[c * 128:(c + 1) * 128, tsl], x[:])
        S.flush()


def build_full(shared_shapes, n_layers=2):
    nc = bass.Bass("TRN2", target_bir_lowering=False)
    g = G()
    g.dram = {}
    for k, shp in shared_shapes.items():
        g.dram[k] = nc.dram_tensor(k, list(shp), F32, kind="ExternalInput").ap()
    NT = NSEQ * T
    g.dram["xT"] = nc.dram_tensor("xT", [D, NT], F32, kind="ExternalInput").ap()
    g.dram["cT"] = nc.dram_tensor("cT", [D, 4], F32, kind="ExternalInput").ap()
    outT = nc.dram_tensor("outT", [D, NT], F32, kind="ExternalOutput").ap()
    X1 = nc.dram_tensor("X1", [D, NT], F32).ap()
    X2 = nc.dram_tensor("X2", [D, NT], F32).ap()
    ptm = nc.dram_tensor("ptm", [T, NTM], F32).ap()
    pfm = nc.dram_tensor("pfm", [NFM, T], F32).ap()
    ycat = nc.dram_tensor("ycat", [NSEQ, T, D], F32).ap()
    sops = nc.dram_tensor("sops", [5, 2, 2, T, 512], BF16).ap()
    sv = nc.dram_tensor("sv", [NSEQ, 256, T], F32).ap()
    sy = nc.dram_tensor("sy", [NSEQ, 256, T], F32).ap()
    sbg = nc.dram_tensor("sbg", [NSEQ, T, 512], F32).ap()
    with ExitStack() as st:
        S = Sched(nc)
        S.open(st)
        g.ps = [st.enter_context(nc.psum_tensor("ps%d" % i, [P, 512], F32)) for i in range(8)]
        setup_consts(S, nc, st, g)
        for l in range(n_layers):
            xin = g.dram["xT"] if l == 0 else X2
            xmid = X1
            xout = outT if l == n_layers - 1 else X2
            ph_ada(S, nc, g, l)
            for s in range(NSEQ):
                with ExitStack() as st2:
                    hT = sb(nc, st2, "hT", [P, 8, T + 4], BF16)
                    S.memset(hT[:, :, 0:1], 0.0)
                    emit_norm_mod(S, nc, g, xin, s * T, s, g.A1, g.modT[:, 0:8, :], hT)
                    ph_inproj(S, nc, g, l, hT, ptm, pfm)
                ph_sb(S, nc, g, l, ptm, pfm, ycat[s])
                ph_ml(S, nc, g, l, ptm, pfm, ycat[s])
                ph_dsa(S, nc, g, l, ptm, ycat[s])
                ph_rwkv_prep(S, nc, g, l, s, ptm, pfm, sops, sv, sbg)
            ph_rwkv_scan(S, nc, g, sops, sv, sy)
            for s in range(NSEQ):
                ph_rwkv_post(S, nc, g, l, s, sy, sbg, ycat[s])
                ph_outproj(S, nc, g, l, s, ycat[s], xin, xmid, s * T)
            for s in range(NSEQ):
                ph_moe(S, nc, g, l, s, xmid, xout, s * T)
        S.flush()
        g.n_inst = S.n_inst
    return nc, g


_CACHE = {}


def kernel(**inputs):
    inp = {k: np.asarray(v) for k, v in inputs.items()}
    sh = host_shared(inp)
    n_layers = inp["w_in"].shape[0]
    if "nc" not in _CACHE:
        _CACHE["nc"] = build_full({k: v.shape for k, v in sh.items()}, n_layers)
    nc, g = _CACHE["nc"]
    in_maps = []
    for core in range(8):
        d = dict(sh)
        d.update(host_core(inp, core))
        in_maps.append(d)
    res = run_bass_kernel_spmd(nc, in_maps, core_ids=list(range(8)))
    out = np.empty((32, T, D), np.float32)
    for core in range(8):
        oT = np.asarray(res.results[core]["outT"])
        out[core * NSEQ:(core + 1) * NSEQ] = oT.T.reshape(NSEQ, T, D)
    return out
```
